# Optimizing a Trainium2 kernel written in Bass

```python
import jax
import jax.numpy as jnp
from jax import lax
import numpy as np

D_MODEL = 2048
BATCH = 2
SEQ = 4096
DEPTH = 2

GRID_W = 64
CTX_LEN = 256
HEAD_DIM = 128
N_Q_HEADS = 8
N_KV_HEADS = 2
Q_PER_KV = N_Q_HEADS // N_KV_HEADS
ATTN_WIDTH = N_Q_HEADS * HEAD_DIM
KV_WIDTH = N_KV_HEADS * HEAD_DIM
ROPE_AXIS_DIM = HEAD_DIM // 2
ROPE_THETA = 10000.0
Q_BLOCK = 128
FOURIER_GROUPS = 8
FOURIER_GROUP_DIM = 128
FOURIER_WIDTH = FOURIER_GROUPS * FOURIER_GROUP_DIM
AB_IN_WIDTH = ATTN_WIDTH + 2 * KV_WIDTH + FOURIER_WIDTH
AB_OUT_WIDTH = ATTN_WIDTH + FOURIER_WIDTH
CONV_WIDTH = D_MODEL
CONV_WINDOW = 31
N_EXPERTS = 32
TOP_K = 4
EXPERT_FF = D_MODEL // 2
SWIGLU_LIMIT = 7.0
SWIGLU_ALPHA = 1.702
MOE_BLOCK = 128
N_MOD = 6
EPS = 1e-6
N_AB_LAYERS = (DEPTH + 1) // 2
N_CONV_LAYERS = DEPTH // 2

kernel_name = 'hybrid_dit_attn_fnet_conformer_moe'


def rms_norm(x, g):
    xf = x.astype(jnp.float32)
    y = xf * lax.rsqrt(jnp.mean(xf * xf, axis=-1, keepdims=True) + EPS)
    return (y * g.astype(jnp.float32)).astype(x.dtype)


def layer_norm(x, g, b):
    xf = x.astype(jnp.float32)
    mu = jnp.mean(xf, axis=-1, keepdims=True)
    var = jnp.mean(jnp.square(xf - mu), axis=-1, keepdims=True)
    y = (xf - mu) * lax.rsqrt(var + EPS)
    return (y * g.astype(jnp.float32) + b.astype(jnp.float32)).astype(x.dtype)


def adaln_params(cond, w_mod, b_mod):
    m = jax.nn.silu(cond) @ w_mod + b_mod
    return jnp.split(m, N_MOD, axis=-1)


def modulate(h, shift, scale):
    return h * (1 + scale) + shift


def axial_rope_tables(n_tokens):
    rows = n_tokens // GRID_W
    row = jnp.repeat(jnp.arange(rows, dtype=jnp.float32), GRID_W)
    col = jnp.tile(jnp.arange(GRID_W, dtype=jnp.float32), rows)
    inv_freq = ROPE_THETA ** (-jnp.arange(0, ROPE_AXIS_DIM, 2, dtype=jnp.float32) / ROPE_AXIS_DIM)
    ang_r = row[:, None] * inv_freq
    ang_c = col[:, None] * inv_freq
    return (jnp.cos(ang_r), jnp.sin(ang_r), jnp.cos(ang_c), jnp.sin(ang_c))


def rotate_axis(x, cos, sin):
    x1, x2 = jnp.split(x, 2, axis=-1)
    cos = cos[:, None, :]
    sin = sin[:, None, :]
    return jnp.concatenate([x1 * cos - x2 * sin, x1 * sin + x2 * cos], axis=-1)


def apply_axial_rope(x, tables):
    cos_r, sin_r, cos_c, sin_c = tables
    xf = x.astype(jnp.float32)
    out = jnp.concatenate([rotate_axis(xf[..., :ROPE_AXIS_DIM], cos_r, sin_r),
                           rotate_axis(xf[..., ROPE_AXIS_DIM:], cos_c, sin_c)], axis=-1)
    return out.astype(x.dtype)


def block_attention(q, k, v):
    b, lq = q.shape[0], q.shape[1]
    nb = lq // Q_BLOCK
    qb = jnp.moveaxis(q.reshape(b, nb, Q_BLOCK, N_KV_HEADS, Q_PER_KV, HEAD_DIM), 1, 0)
    scale = HEAD_DIM ** -0.5

    def one_block(qi):
        s = jnp.einsum('bqhgd,bkhd->bhgqk', qi, k, preferred_element_type=jnp.float32) * scale
        p = jax.nn.softmax(s, axis=-1).astype(v.dtype)
        return jnp.einsum('bhgqk,bkhd->bqhgd', p, v)

    o = lax.map(one_block, qb)
    return jnp.moveaxis(o, 0, 1).reshape(b, lq, ATTN_WIDTH)


def fourier_mix(u):
    b, l, _ = u.shape
    ug = u.reshape(b, l, FOURIER_GROUPS, FOURIER_GROUP_DIM).astype(jnp.float32)
    y = jnp.fft.fft2(ug, axes=(1, 3), norm='ortho').real
    return y.reshape(b, l, FOURIER_WIDTH).astype(u.dtype)


def ab_mixer(h_lat, h_ctx, w_in, q_gain, k_gain, w_out, tables, ctx_live):
    b, l, _ = h_lat.shape
    cl = h_ctx.shape[1]
    cuts = [ATTN_WIDTH, ATTN_WIDTH + KV_WIDTH, ATTN_WIDTH + 2 * KV_WIDTH]
    q, k, v, f = jnp.split(h_lat @ w_in, cuts, axis=-1)
    q = apply_axial_rope(rms_norm(q.reshape(b, l, N_Q_HEADS, HEAD_DIM), q_gain), tables)
    k = apply_axial_rope(rms_norm(k.reshape(b, l, N_KV_HEADS, HEAD_DIM), k_gain), tables)
    v = v.reshape(b, l, N_KV_HEADS, HEAD_DIM)
    if ctx_live:
        qc, kc, vc, fc = jnp.split(h_ctx @ w_in, cuts, axis=-1)
    else:
        kc, vc = jnp.split(h_ctx @ w_in[:, ATTN_WIDTH:ATTN_WIDTH + 2 * KV_WIDTH], 2, axis=-1)
    kc = rms_norm(kc.reshape(b, cl, N_KV_HEADS, HEAD_DIM), k_gain)
    vc = vc.reshape(b, cl, N_KV_HEADS, HEAD_DIM)
    q = q.reshape(b, l, N_KV_HEADS, Q_PER_KV, HEAD_DIM)
    attn = block_attention(q, jnp.concatenate([k, kc], axis=1), jnp.concatenate([v, vc], axis=1))
    lat_out = jnp.concatenate([attn, fourier_mix(f)], axis=-1) @ w_out
    if not ctx_live:
        return lat_out, None
    qc = rms_norm(qc.reshape(b, cl, N_Q_HEADS, HEAD_DIM), q_gain).reshape(b, cl, N_KV_HEADS, Q_PER_KV, HEAD_DIM)
    attn_c = block_attention(qc, kc, vc)
    ctx_out = jnp.concatenate([attn_c, fourier_mix(fc)], axis=-1) @ w_out
    return lat_out, ctx_out


def conformer_conv(h, w_in, b_in, w_dw, b_dw, ln_g, ln_b, w_out, b_out):
    a, gate = jnp.split(h @ w_in + b_in, 2, axis=-1)
    u = a * jax.nn.sigmoid(gate)
    pad = CONV_WINDOW // 2
    u = lax.conv_general_dilated(u, w_dw[:, None, :].astype(u.dtype), window_strides=(1,),
                                 padding=[(pad, pad)], dimension_numbers=('NWC', 'WIO', 'NWC'),
                                 feature_group_count=CONV_WIDTH) + b_dw
    u = jax.nn.silu(layer_norm(u, ln_g, ln_b))
    return u @ w_out + b_out


def moe(x, w_router, b_router, w1, b1, w2, b2):
    n, d = x.shape
    logits = (x @ w_router + b_router).astype(jnp.float32)
    top_val, top_idx = lax.top_k(logits, TOP_K)
    gates = jax.nn.softmax(top_val, axis=-1)
    flat_e = top_idx.reshape(-1)
    flat_tok = jnp.repeat(jnp.arange(n, dtype=jnp.int32), TOP_K)
    flat_w = gates.reshape(-1)
    order = jnp.argsort(flat_e)
    sorted_e = flat_e[order]
    counts = jnp.bincount(flat_e, length=N_EXPERTS)
    padded = (counts + MOE_BLOCK - 1) // MOE_BLOCK * MOE_BLOCK
    pad_end = jnp.cumsum(padded)
    pad_start = pad_end - padded
    start = jnp.cumsum(counts) - counts
    rank = jnp.arange(n * TOP_K) - start[sorted_e]
    dest = pad_start[sorted_e] + rank
    n_rows = (n * TOP_K + MOE_BLOCK - 1) // MOE_BLOCK * MOE_BLOCK + N_EXPERTS * MOE_BLOCK
    n_blocks = n_rows // MOE_BLOCK
    row_tok = jnp.full((n_rows,), n, jnp.int32).at[dest].set(flat_tok[order])
    row_w = jnp.zeros((n_rows,), jnp.float32).at[dest].set(flat_w[order])
    block_e = jnp.minimum(jnp.searchsorted(pad_end, jnp.arange(n_blocks) * MOE_BLOCK, side='right'),
                          N_EXPERTS - 1)
    x_pad = jnp.concatenate([x, jnp.zeros((1, d), x.dtype)], axis=0)
    xb = x_pad[row_tok].reshape(n_blocks, MOE_BLOCK, d)

    def expert_block(args):
        xi, e = args
        h = xi @ w1[e] + b1[e]
        g = jnp.minimum(h[..., ::2], SWIGLU_LIMIT)
        lin = jnp.clip(h[..., 1::2], -SWIGLU_LIMIT, SWIGLU_LIMIT)
        act = g * jax.nn.sigmoid(SWIGLU_ALPHA * g) * (lin + 1)
        return act @ w2[e] + b2[e]

    yb = lax.map(expert_block, (xb, block_e))
    y = yb.reshape(n_rows, d) * row_w[:, None].astype(yb.dtype)
    return jax.ops.segment_sum(y, row_tok, num_segments=n + 1)[:n]


def setup_inputs(seed: int = 0) -> dict:
    key = jax.random.key(seed)
    ks = iter(jax.random.split(key, 40))

    def nrm(shape, scale):
        return jax.random.normal(next(ks), shape, jnp.float32) * scale

    def gain(shape):
        return 1.0 + nrm(shape, 0.02)

    d = D_MODEL
    return {
        'x': nrm((BATCH, SEQ, d), 1.0),
        'c': nrm((BATCH, d), 1.0),
        'ctx': nrm((BATCH, CTX_LEN, d), 1.0),
        'c_ctx': nrm((d,), 1.0),
        'w_mod': nrm((DEPTH, d, N_MOD * d), 0.5 * d ** -0.5),
        'b_mod': nrm((DEPTH, N_MOD * d), 0.02),
        'g_mix': gain((DEPTH, d)),
        'g_ffn': gain((DEPTH, d)),
        'ab_w_in': nrm((N_AB_LAYERS, d, AB_IN_WIDTH), d ** -0.5),
        'ab_q_gain': gain((N_AB_LAYERS, HEAD_DIM)),
        'ab_k_gain': gain((N_AB_LAYERS, HEAD_DIM)),
        'ab_w_out': nrm((N_AB_LAYERS, AB_OUT_WIDTH, d), AB_OUT_WIDTH ** -0.5),
        'cv_w_in': nrm((N_CONV_LAYERS, d, 2 * CONV_WIDTH), d ** -0.5),
        'cv_b_in': nrm((N_CONV_LAYERS, 2 * CONV_WIDTH), 0.02),
        'cv_w_dw': nrm((N_CONV_LAYERS, CONV_WINDOW, CONV_WIDTH), CONV_WINDOW ** -0.5),
        'cv_b_dw': nrm((N_CONV_LAYERS, CONV_WIDTH), 0.02),
        'cv_ln_g': gain((N_CONV_LAYERS, CONV_WIDTH)),
        'cv_ln_b': nrm((N_CONV_LAYERS, CONV_WIDTH), 0.02),
        'cv_w_out': nrm((N_CONV_LAYERS, CONV_WIDTH, d), CONV_WIDTH ** -0.5),
        'cv_b_out': nrm((N_CONV_LAYERS, d), 0.02),
        'moe_w_router': nrm((DEPTH, d, N_EXPERTS), d ** -0.5),
        'moe_b_router': nrm((DEPTH, N_EXPERTS), 0.01),
        'moe_w1': nrm((DEPTH, N_EXPERTS, d, 2 * EXPERT_FF), d ** -0.5),
        'moe_b1': nrm((DEPTH, N_EXPERTS, 2 * EXPERT_FF), 0.02),
        'moe_w2': nrm((DEPTH, N_EXPERTS, EXPERT_FF, d), EXPERT_FF ** -0.5),
        'moe_b2': nrm((DEPTH, N_EXPERTS, d), 0.02),
        'g_final': gain((d,)),
    }


def reference(x, c, ctx, c_ctx, w_mod, b_mod, g_mix, g_ffn, ab_w_in, ab_q_gain, ab_k_gain, ab_w_out,
              cv_w_in, cv_b_in, cv_w_dw, cv_b_dw, cv_ln_g, cv_ln_b, cv_w_out, cv_b_out,
              moe_w_router, moe_b_router, moe_w1, moe_b1, moe_w2, moe_b2, g_final):
    b, l, d = x.shape
    tables = axial_rope_tables(l)
    z = ctx
    for i in range(DEPTH):
        is_ab = i % 2 == 0
        j = i // 2
        ctx_live = any(k % 2 == 0 for k in range(i + 1, DEPTH))
        sh1, sc1, gt1, sh2, sc2, gt2 = adaln_params(c, w_mod[i], b_mod[i])
        h_lat = modulate(rms_norm(x, g_mix[i]), sh1[:, None], sc1[:, None])
        if is_ab or ctx_live:
            cm = adaln_params(c_ctx, w_mod[i], b_mod[i])
            h_ctx = modulate(rms_norm(z, g_mix[i]), cm[0], cm[1])
        if is_ab:
            mix_lat, mix_ctx = ab_mixer(h_lat, h_ctx, ab_w_in[j], ab_q_gain[j], ab_k_gain[j],
                                        ab_w_out[j], tables, ctx_live)
        else:
            conv_args = (cv_w_in[j], cv_b_in[j], cv_w_dw[j], cv_b_dw[j], cv_ln_g[j], cv_ln_b[j],
                         cv_w_out[j], cv_b_out[j])
            mix_lat = conformer_conv(h_lat, *conv_args)
            mix_ctx = conformer_conv(h_ctx, *conv_args) if ctx_live else None
        x = x + gt1[:, None] * mix_lat
        tokens = modulate(rms_norm(x, g_ffn[i]), sh2[:, None], sc2[:, None]).reshape(b * l, d)
        if ctx_live:
            z = z + cm[2] * mix_ctx
            f_ctx = modulate(rms_norm(z, g_ffn[i]), cm[3], cm[4]).reshape(-1, d)
            tokens = jnp.concatenate([tokens, f_ctx], axis=0)
        y = moe(tokens, moe_w_router[i], moe_b_router[i], moe_w1[i], moe_b1[i], moe_w2[i], moe_b2[i])
        x = x + gt2[:, None] * y[:b * l].reshape(b, l, d)
        if ctx_live:
            z = z + cm[5] * y[b * l:].reshape(b, -1, d)
    return rms_norm(x, g_final)
```

```python
import numpy as np
import ml_dtypes
import concourse.bass as bass
import concourse.mybir as mybir
from concourse.bass_utils import run_bass_kernel_spmd

F32 = mybir.dt.float32
BF16 = mybir.dt.bfloat16
AF = mybir.ActivationFunctionType
ALU = mybir.AluOpType
AX = mybir.AxisListType
NPBF = ml_dtypes.bfloat16

D = 2048
KC = 16
EPS = 1e-6
NCORES = 8


class Buf:
    def __init__(self, t, name):
        self.t = t
        self.name = name
        self.w = None
        self.r = []
        self.dsem = None
        self.dcnt = 0

    def __getitem__(self, idx):
        return self.t[idx]


class P:
    def __init__(self, nc):
        self.nc = nc
        self.eng = {'pe': nc.tensor, 'act': nc.scalar, 'dve': nc.vector, 'pool': nc.gpsimd, 'sp': nc.sync}
        self.sems = {}
        self.cnt = {}
        for k in self.eng:
            self.sems[k] = nc.alloc_semaphore('s_' + k)
            self.cnt[k] = 0
        self.seen = {k: {} for k in self.eng}
        self.acc_i = 0
        self.tp_i = 0
        self.dma_cnt = {}

    def sb(self, name, shape, dt):
        return Buf(self.nc.alloc_sbuf_tensor(name, list(shape), dt), name)

    def ps(self, name, shape, dt=F32):
        return Buf(self.nc.alloc_psum_tensor(name, list(shape), dt), name)

    def dram(self, name, shape, dt, kind="Internal"):
        return Buf(self.nc.dram_tensor(name, list(shape), dt, kind=kind), name)

    def _wait(self, e, ev):
        key, val = ev
        if self.seen[e].get(key, 0) >= val:
            return
        self.seen[e][key] = val
        self.eng[e].wait_ge(self.sems[key], val)

    def _deps(self, e, reads, writes):
        for b in reads:
            if b.w is not None:
                self._wait(e, b.w)
        for b in writes:
            if b.w is not None:
                self._wait(e, b.w)
            for ev in b.r:
                self._wait(e, ev)

    def _commit(self, ev, reads, writes):
        for b in reads:
            b.r.append(ev)
            if len(b.r) > 48:
                best = {}
                for k, v in b.r:
                    best[k] = max(best.get(k, 0), v)
                b.r = list(best.items())
        for b in writes:
            b.w = ev
            b.r = []

    def op(self, e, fn, reads=(), writes=()):
        self._deps(e, reads, writes)
        inst = fn()
        self.cnt[e] += 1
        inst.then_inc(self.sems[e], 1)
        self._commit((e, self.cnt[e]), reads, writes)
        return inst

    def dma(self, q, out_ap, in_ap, reads=(), writes=(), **kw):
        self._deps(q, reads, writes)
        dst = writes[0]
        if dst.dsem is None:
            key = 'd_' + dst.name
            self.sems[key] = self.nc.alloc_semaphore(key)
            dst.dsem = key
        inst = self.eng[q].dma_start(out=out_ap, in_=in_ap, **kw)
        dst.dcnt += 16
        self.dma_cnt[dst.dsem] = dst.dcnt
        inst.then_inc(self.sems[dst.dsem], 16)
        self._commit((dst.dsem, dst.dcnt), reads, writes)
        return inst

    def wait_all(self, e, bufs):
        for b in bufs:
            if b.w is not None:
                self._wait(e, b.w)

    def setup_common(self):
        nc = self.nc
        self.accs = [self.ps(f"acc{i}", [128, 512], F32) for i in range(5)]
        self.tps = [self.ps(f"tpb{i}", [128, 1024], BF16) for i in range(3)]
        identf = self.sb("identf", [128, 128], F32)
        ident = self.sb("ident", [128, 128], BF16)
        self.op('pool', lambda: nc.gpsimd.memset(identf[:], 0.0), writes=[identf])
        self.op('pool', lambda: nc.gpsimd.affine_select(out=identf[:], in_=identf[:], pattern=[[-1, 128]],
                                                        compare_op=ALU.not_equal, fill=1.0, base=0,
                                                        channel_multiplier=1), reads=[identf], writes=[identf])
        self.op('dve', lambda: nc.vector.tensor_copy(out=ident[:], in_=identf[:]), reads=[identf], writes=[ident])
        self.ident = ident
        self.identf = identf

    def acc(self):
        b = self.accs[self.acc_i % len(self.accs)]
        self.acc_i += 1
        return b

    def tp(self):
        b = self.tps[self.tp_i % len(self.tps)]
        self.tp_i += 1
        return b


def new_prog():
    nc = bass.Bass("TRN2", target_bir_lowering=False)
    return nc, P(nc)


class Scratch:
    def __init__(self, p, name, shape, dt, n=2):
        self.bufs = [p.sb(f"{name}{i}", shape, dt) for i in range(n)]
        self.i = 0

    def get(self):
        b = self.bufs[self.i % len(self.bufs)]
        self.i += 1
        return b


def emit_rstd(p, ss, rows, scale_in, tmp=None):
    nc = p.nc
    p.op('dve', lambda: nc.vector.tensor_scalar(out=ss, in0=ss, scalar1=float(scale_in), scalar2=EPS,
                                                op0=ALU.mult, op1=ALU.add), reads=[tmp], writes=[tmp])
    p.op('act', lambda: nc.scalar.sqrt(out=ss, in_=ss), reads=[tmp], writes=[tmp])
    p.op('dve', lambda: nc.vector.reciprocal(out=ss, in_=ss), reads=[tmp], writes=[tmp])


class NormT:
    def __init__(self, p):
        self.p = p
        self.sq = Scratch(p, "nt_sq", [128, D], BF16, 2)
        self.ss = Scratch(p, "nt_ss", [128, 1], F32, 2)
        self.xn = Scratch(p, "nt_xn", [128, D], BF16, 2)

    def emit(self, xt, rows, Acol, Bcol, hT, c0):
        p, nc = self.p, self.p.nc
        sq, ss, xn = self.sq.get(), self.ss.get(), self.xn.get()
        p.op('act', lambda: nc.scalar.activation(out=sq[0:rows, :], in_=xt[0:rows, :], func=AF.Square,
                                                 accum_out=ss[0:rows, :]), reads=[xt], writes=[sq, ss])
        emit_rstd(p, ss[0:rows, :], rows, 1.0 / D, ss)
        p.op('dve', lambda: nc.vector.tensor_scalar(out=xn[0:rows, :], in0=xt[0:rows, :], scalar1=ss[0:rows, 0:1],
                                                    scalar2=None, op0=ALU.mult), reads=[xt, ss], writes=[xn])
        for k4 in range(2):
            tp = p.tp()

            def tr():
                for j in range(8):
                    k = k4 * 8 + j
                    i = nc.tensor.transpose(out=tp[:, j * 128:j * 128 + rows], in_=xn[0:rows, k * 128:(k + 1) * 128],
                                            identity=p.ident[0:rows, 0:rows])
                return i
            p.op('pe', tr, reads=[xn, p.ident], writes=[tp])
            for j in range(8):
                k = k4 * 8 + j
                if j % 2 == 0:
                    p.op('dve', lambda: nc.vector.tensor_scalar(
                        out=hT[:, k, c0:c0 + rows], in0=tp[:, j * 128:j * 128 + rows], scalar1=Acol[:, k:k + 1],
                        scalar2=Bcol[:, k:k + 1], op0=ALU.mult, op1=ALU.add), reads=[tp, Acol, Bcol], writes=[hT])
                else:
                    p.op('act', lambda: nc.scalar.activation(
                        out=hT[:, k, c0:c0 + rows], in_=tp[:, j * 128:j * 128 + rows], func=AF.Identity,
                        scale=Acol[:, k:k + 1], bias=Bcol[:, k:k + 1]), reads=[tp, Acol, Bcol], writes=[hT])


def emit_Acol(p, A, g, sc):
    nc = p.nc
    p.op('dve', lambda: nc.vector.scalar_tensor_tensor(out=A[:], in0=sc[:], scalar=1.0, in1=g[:], op0=ALU.add,
                                                       op1=ALU.mult), reads=[sc, g], writes=[A])


MODW = 12288 // NCORES


def build_p1():
    nc, p = new_prog()
    wmod = p.dram("wmod", [2, D, MODW], F32, "ExternalInput")
    bmod = p.dram("bmod", [2, 3, MODW], F32, "ExternalInput")
    cT = p.dram("cT", [128, KC, 3], F32, "ExternalInput")
    mout = p.dram("m", [2, 3, MODW], F32, "ExternalOutput")
    p.setup_common()
    cs = p.sb("cs", [128, KC, 3], F32)
    sT = p.sb("sT", [128, KC, 3], BF16)
    p.dma('sp', cs[:], cT[:], reads=[cT], writes=[cs])
    p.op('act', lambda: nc.scalar.activation(out=sT[:], in_=cs[:], func=AF.Silu), reads=[cs], writes=[sT])
    for l in range(2):
        wb = p.sb(f"wb{l}", [128, KC, MODW], BF16)
        bb = p.sb(f"bb{l}", [3, MODW], F32)
        mo = p.sb(f"mo{l}", [3, MODW], F32)
        for h in range(2):
            p.dma('pool', wb[:, h * 8:(h + 1) * 8, :],
                  wmod[l, h * 1024:(h + 1) * 1024, :].rearrange("(k p) n -> p k n", p=128), reads=[wmod], writes=[wb])
        p.dma('sp', bb[:], bmod[l], reads=[bmod], writes=[bb])
        for c in range(MODW // 512):
            acc = p.acc()

            def mm():
                for k in range(KC):
                    i = nc.tensor.matmul(acc[0:3, :], lhsT=sT[:, k, :], rhs=wb[:, k, c * 512:(c + 1) * 512],
                                         start=(k == 0), stop=(k == KC - 1))
                return i
            p.op('pe', mm, reads=[sT, wb], writes=[acc])
            p.op('dve', lambda: nc.vector.tensor_tensor(out=mo[:, c * 512:(c + 1) * 512], in0=acc[0:3, :],
                                                        in1=bb[:, c * 512:(c + 1) * 512], op=ALU.add),
                 reads=[acc, bb], writes=[mo])
        p.dma('sp', mout[l], mo[:], reads=[mo], writes=[mout])
    p.wait_all('sp', [mout])
    return nc


TPC = 1024
CPC = 64


def emit_qk_post(p, ps, rows, nh, gain_b, rope_t, out_ap, out_buf, S):
    nc = p.nc
    n = nh * 128
    sq, ss4, qn = S['sq'].get(), S['ss4'].get(), S['qn'].get()
    p.op('act', lambda: nc.scalar.activation(out=sq[0:rows, 0:n], in_=ps[0:rows, 0:n], func=AF.Square),
         reads=[ps], writes=[sq])
    p.op('dve', lambda: nc.vector.tensor_reduce(out=ss4[0:rows, 0:nh],
                                                in_=sq[0:rows, 0:n].rearrange("p (h d) -> p h d", d=128),
                                                axis=AX.X, op=ALU.add), reads=[sq], writes=[ss4])
    emit_rstd(p, ss4[0:rows, 0:nh], rows, 1.0 / 128, ss4)
    p.op('dve', lambda: nc.vector.tensor_tensor(
        out=qn[0:rows, 0:n].rearrange("p (h d) -> p h d", d=128),
        in0=ps[0:rows, 0:n].rearrange("p (h d) -> p h d", d=128),
        in1=ss4[0:rows, 0:nh].unsqueeze(2).broadcast_to([rows, nh, 128]), op=ALU.mult),
        reads=[ps, ss4], writes=[qn])
    if rope_t is None:
        p.op('dve', lambda: nc.vector.tensor_tensor(
            out=out_ap.rearrange("p (h d) -> p h d", d=128),
            in0=qn[0:rows, 0:n].rearrange("p (h d) -> p h d", d=128),
            in1=gain_b[0:rows, :].unsqueeze(1).broadcast_to([rows, nh, 128]), op=ALU.mult),
            reads=[qn, gain_b], writes=[out_buf])
        return
    p.op('pool', lambda: nc.gpsimd.tensor_tensor(
        out=qn[0:rows, 0:n].rearrange("p (h d) -> p h d", d=128),
        in0=qn[0:rows, 0:n].rearrange("p (h d) -> p h d", d=128),
        in1=gain_b[0:rows, :].unsqueeze(1).broadcast_to([rows, nh, 128]), op=ALU.mult),
        reads=[qn, gain_b], writes=[qn])
    t1, t2 = S['t1'].get(), S['t2'].get()
    p.op('pool', lambda: nc.gpsimd.tensor_tensor(
        out=t1[0:rows, 0:n].rearrange("p (h d) -> p h d", d=128),
        in0=qn[0:rows, 0:n].rearrange("p (h d) -> p h d", d=128),
        in1=rope_t[0:rows, 0, :].unsqueeze(1).broadcast_to([rows, nh, 128]), op=ALU.mult),
        reads=[qn, rope_t], writes=[t1])
    for b in range(2):
        p.op('dve', lambda: nc.vector.tensor_tensor(
            out=t2[0:rows, 0:n].rearrange("p (h a b d) -> p h a b d", a=2, b=2, d=32)[:, :, :, b, :],
            in0=qn[0:rows, 0:n].rearrange("p (h a b d) -> p h a b d", a=2, b=2, d=32)[:, :, :, 1 - b, :],
            in1=rope_t[0:rows, 1, :].rearrange("p (a b d) -> p a b d", a=2, b=2, d=32)[:, :, b, :]
            .unsqueeze(1).broadcast_to([rows, nh, 2, 32]), op=ALU.mult),
            reads=[qn, rope_t], writes=[t2])
    p.op('dve', lambda: nc.vector.tensor_tensor(out=out_ap, in0=t1[0:rows, 0:n], in1=t2[0:rows, 0:n], op=ALU.add),
         reads=[t1, t2], writes=[out_buf])


def build_p2():
    nc, p = new_prog()
    x = p.dram("x", [TPC, D], F32, "ExternalInput")
    ctxr = p.dram("ctxr", [CPC, D], F32, "ExternalInput")
    cols = p.dram("cols", [128, 5, KC], F32, "ExternalInput")
    w_in = p.dram("w_in", [D, 2560], F32, "ExternalInput")
    gains = p.dram("gains", [2, 128], F32, "ExternalInput")
    rope = p.dram("rope", [TPC, 2, 128], F32, "ExternalInput")
    out = p.dram("qkvf", [TPC, 2560], BF16, "ExternalOutput")
    outc = p.dram("kvc", [CPC, 512], BF16, "ExternalOutput")
    p.setup_common()
    colsb = p.sb("colsb", [128, 5, KC], F32)
    p.dma('sp', colsb[:], cols[:], reads=[cols], writes=[colsb])
    qg = p.sb("qg", [128, 128], F32)
    kg = p.sb("kg", [128, 128], F32)
    p.dma('sp', qg[:], gains[0:1, :].partition_broadcast(128), reads=[gains], writes=[qg])
    p.dma('sp', kg[:], gains[1:2, :].partition_broadcast(128), reads=[gains], writes=[kg])
    A1 = p.sb("A1", [128, KC], F32)
    Ac = p.sb("Ac", [128, KC], F32)
    B1 = p.sb("B1", [128, KC], F32)
    Bc = p.sb("Bc", [128, KC], F32)
    p.op('dve', lambda: nc.vector.scalar_tensor_tensor(out=A1[:], in0=colsb[:, 2, :], scalar=1.0, in1=colsb[:, 0, :],
                                                       op0=ALU.add, op1=ALU.mult), reads=[colsb], writes=[A1])
    p.op('dve', lambda: nc.vector.scalar_tensor_tensor(out=Ac[:], in0=colsb[:, 4, :], scalar=1.0, in1=colsb[:, 0, :],
                                                       op0=ALU.add, op1=ALU.mult), reads=[colsb], writes=[Ac])
    p.op('dve', lambda: nc.vector.tensor_copy(out=B1[:], in_=colsb[:, 1, :]), reads=[colsb], writes=[B1])
    p.op('dve', lambda: nc.vector.tensor_copy(out=Bc[:], in_=colsb[:, 3, :]), reads=[colsb], writes=[Bc])
    wb = p.sb("wb", [128, KC, 2560], BF16)
    for h in range(4):
        p.dma('pool', wb[:, h * 4:(h + 1) * 4, :], w_in[h * 512:(h + 1) * 512, :].rearrange("(k p) n -> p k n", p=128),
              reads=[w_in], writes=[wb])
    nt = NormT(p)
    xs = Scratch(p, "xs", [128, D], F32, 2)
    hTs = Scratch(p, "hT", [128, KC, 128], BF16, 2)
    rts = Scratch(p, "rt", [128, 2, 128], F32, 2)
    obs = Scratch(p, "ob", [128, 2560], BF16, 2)
    S = {'sq': Scratch(p, "qsq", [128, 512], F32, 2), 'ss4': Scratch(p, "qss", [128, 4], F32, 2),
         'qn': Scratch(p, "qn", [128, 512], F32, 2), 't1': Scratch(p, "qt1", [128, 512], F32, 2),
         't2': Scratch(p, "qt2", [128, 512], F32, 2)}
    ntiles = TPC // 128
    for t in range(ntiles + 1):
        is_ctx = (t == ntiles)
        rows = CPC if is_ctx else 128
        xt, hT, ob = xs.get(), hTs.get(), obs.get()
        if is_ctx:
            p.dma('sp', xt[0:rows, :], ctxr[:, :], reads=[ctxr], writes=[xt])
            rt = None
        else:
            p.dma('sp', xt[:], x[t * 128:(t + 1) * 128, :], reads=[x], writes=[xt])
            rt = rts.get()
            p.dma('sp', rt[:], rope[t * 128:(t + 1) * 128, :, :], reads=[rope], writes=[rt])
        nt.emit(xt, rows, Ac if is_ctx else A1, Bc if is_ctx else B1, hT, 0)
        for c in ([2] if is_ctx else range(5)):
            acc = p.acc()

            def mm():
                for k in range(KC):
                    i = nc.tensor.matmul(acc[0:rows, :], lhsT=hT[:, k, 0:rows], rhs=wb[:, k, c * 512:(c + 1) * 512],
                                         start=(k == 0), stop=(k == KC - 1))
                return i
            p.op('pe', mm, reads=[hT, wb], writes=[acc])
            if c < 2:
                emit_qk_post(p, acc, rows, 4, qg, rt, ob[0:rows, c * 512:(c + 1) * 512], ob, S)
            elif c == 2:
                emit_qk_post(p, acc, rows, 2, kg, rt, ob[0:rows, 1024:1280], ob, S)
                p.op('act', lambda: nc.scalar.copy(out=ob[0:rows, 1280:1536], in_=acc[0:rows, 256:512]),
                     reads=[acc], writes=[ob])
            else:
                p.op('act', lambda: nc.scalar.copy(out=ob[0:rows, c * 512:(c + 1) * 512], in_=acc[0:rows, :]),
                     reads=[acc], writes=[ob])
        if is_ctx:
            p.dma('sp', outc[:, :], ob[0:rows, 1024:1536], reads=[ob], writes=[outc])
        else:
            p.dma('sp', out[t * 128:(t + 1) * 128, :], ob[:], reads=[ob], writes=[out])
    p.wait_all('sp', [out, outc])
    return nc


def p_barrier(p):
    targets = {}
    for k in p.eng:
        targets[k] = p.cnt[k]
    for k, v in p.sems.items():
        if k.startswith('d_'):
            targets[k] = p.dma_cnt.get(k, 0)
    for e in p.eng:
        for k, v in targets.items():
            if v > 0 and k != e:
                p._wait(e, (k, v))


class Phase:
    def __init__(self, p):
        from contextlib import ExitStack
        self.p = p
        self.st = ExitStack()

    def sb(self, name, shape, dt):
        t = self.st.enter_context(self.p.nc.sbuf_tensor(name, list(shape), dt))
        return Buf(t, name)

    def scratch(self, name, shape, dt, n=2):
        s = Scratch.__new__(Scratch)
        s.bufs = [self.sb(f"{name}{i}", shape, dt) for i in range(n)]
        s.i = 0
        return s

    def close(self):
        p_barrier(self.p)
        self.st.close()


class Prep:
    def __init__(self, p, ph, A2b, B2b, wr, brb, tpf):
        self.p = p
        self.A2b, self.B2b, self.wr, self.brb, self.tpf = A2b, B2b, wr, brb, tpf
        self.sq = ph.sb("pp_sq", [128, D], BF16)
        self.ss = ph.sb("pp_ss", [128, 1], F32)
        self.t32 = ph.sb("pp_t32", [128, D], F32)
        self.tb = ph.scratch("pp_tb", [128, D], BF16, 2)
        self.tT = ph.sb("pp_tT", [128, KC, 128], F32)
        self.lg = ph.scratch("pp_lg", [128, 32], F32, 2)
        self.t8 = ph.sb("pp_t8", [128, 8], F32)
        self.nmx = ph.sb("pp_nmx", [128, 1], F32)
        self.msk = ph.sb("pp_msk", [128, 32], F32)
        self.ex = ph.sb("pp_ex", [128, 32], F32)
        self.sm = ph.sb("pp_sm", [128, 1], F32)

    def emit(self, x1, t_out_ap, t_out, g_out_ap, g_out):
        p, nc = self.p, self.p.nc
        sq, ss, t32, tT = self.sq, self.ss, self.t32, self.tT
        tb, lg = self.tb.get(), self.lg.get()
        p.op('act', lambda: nc.scalar.activation(out=sq[:], in_=x1[:], func=AF.Square, accum_out=ss[:]),
             reads=[x1], writes=[sq, ss])
        emit_rstd(p, ss[:], 128, 1.0 / D, ss)
        p.op('dve', lambda: nc.vector.scalar_tensor_tensor(out=t32[:], in0=x1[:], scalar=ss[:, 0:1], in1=self.A2b[:],
                                                           op0=ALU.mult, op1=ALU.mult), reads=[x1, ss, self.A2b],
             writes=[t32])
        p.op('pool', lambda: nc.gpsimd.tensor_tensor(out=t32[:], in0=t32[:], in1=self.B2b[:], op=ALU.add),
             reads=[t32, self.B2b], writes=[t32])
        p.op('act', lambda: nc.scalar.copy(out=tb[:], in_=t32[:]), reads=[t32], writes=[tb])
        p.dma('sp', t_out_ap, tb[:], reads=[tb], writes=[t_out])
        for k4 in range(4):
            tpf = self.tpf

            def tr():
                for j in range(4):
                    k = k4 * 4 + j
                    i = nc.tensor.transpose(out=tpf[:, j * 128:(j + 1) * 128], in_=t32[:, k * 128:(k + 1) * 128],
                                            identity=p.identf[:])
                return i
            p.op('pe', tr, reads=[t32, p.identf], writes=[tpf])
            eng = 'dve' if k4 % 2 == 0 else 'act'
            if eng == 'dve':
                p.op('dve', lambda: nc.vector.tensor_copy(
                    out=tT[:, k4 * 4:(k4 + 1) * 4, :].rearrange("p k t -> p (k t)"), in_=tpf[:]),
                    reads=[tpf], writes=[tT])
            else:
                p.op('act', lambda: nc.scalar.copy(
                    out=tT[:, k4 * 4:(k4 + 1) * 4, :].rearrange("p k t -> p (k t)"), in_=tpf[:]),
                    reads=[tpf], writes=[tT])
        acc = p.acc()

        def mm():
            for k in range(KC):
                i = nc.tensor.matmul(acc[:, 0:32], lhsT=tT[:, k, :], rhs=self.wr[:, k, :], start=(k == 0),
                                     stop=(k == KC - 1))
            return i
        p.op('pe', mm, reads=[tT, self.wr], writes=[acc])
        p.op('dve', lambda: nc.vector.tensor_tensor(out=lg[:], in0=acc[:, 0:32], in1=self.brb[:], op=ALU.add),
             reads=[acc, self.brb], writes=[lg])
        emit_gates(p, lg, self.t8, self.nmx, self.msk, self.ex, self.sm)
        p.dma('sp', g_out_ap, lg[:], reads=[lg], writes=[g_out])


def emit_gates(p, lg, t8, nmx, msk, ex, sm):
    nc = p.nc
    p.op('dve', lambda: nc.vector.max(out=t8[:], in_=lg[:]), reads=[lg], writes=[t8])
    p.op('dve', lambda: nc.vector.tensor_scalar(out=msk[:], in0=lg[:], scalar1=t8[:, 3:4], scalar2=None,
                                                op0=ALU.is_ge), reads=[lg, t8], writes=[msk])
    p.op('dve', lambda: nc.vector.tensor_scalar(out=nmx[:], in0=t8[:, 0:1], scalar1=-1.0, scalar2=None,
                                                op0=ALU.mult), reads=[t8], writes=[nmx])
    p.op('act', lambda: nc.scalar.activation(out=ex[:], in_=lg[:], func=AF.Exp, bias=nmx[:, 0:1], scale=1.0),
         reads=[lg, nmx], writes=[ex])
    p.op('dve', lambda: nc.vector.tensor_tensor(out=ex[:], in0=ex[:], in1=msk[:], op=ALU.mult),
         reads=[ex, msk], writes=[ex])
    p.op('dve', lambda: nc.vector.reduce_sum(out=sm[:], in_=ex[:], axis=AX.X), reads=[ex], writes=[sm])
    p.op('dve', lambda: nc.vector.reciprocal(out=sm[:], in_=sm[:]), reads=[sm], writes=[sm])
    p.op('dve', lambda: nc.vector.tensor_scalar(out=lg[:], in0=ex[:], scalar1=sm[:, 0:1], scalar2=None,
                                                op0=ALU.mult), reads=[ex, sm], writes=[lg])


def load_rows_b(p, ph, rows_d, names, off=0):
    out = {}
    for i, nm in enumerate(names):
        b = ph.sb("rb_" + nm, [128, D], F32)
        p.dma('sp', b[:], rows_d[off + i:off + i + 1, :].partition_broadcast(128), reads=[rows_d], writes=[b])
        out[nm] = b
    return out


NKEY = 4096 + 256
NKT = NKEY // 128


def build_p3():
    nc, p = new_prog()
    x = p.dram("x", [TPC, D], F32, "ExternalInput")
    qT_d = p.dram("qT", [128, 8, TPC], BF16, "ExternalInput")
    kT_d = p.dram("kT", [128, 2, NKEY], BF16, "ExternalInput")
    v_d = p.dram("v", [NKEY, 256], BF16, "ExternalInput")
    f_d = p.dram("f", [4096, 1024], BF16, "ExternalInput")
    dc_d = p.dram("dftc", [4096, TPC], BF16, "ExternalInput")
    ds_d = p.dram("dfts", [4096, TPC], BF16, "ExternalInput")
    cdft_d = p.dram("cdft", [128, 2, 128], BF16, "ExternalInput")
    wo_d = p.dram("w_out", [D, D], F32, "ExternalInput")
    rows_d = p.dram("rows", [4, D], F32, "ExternalInput")
    wr_d = p.dram("w_router", [128, KC, 32], F32, "ExternalInput")
    br_d = p.dram("b_router", [1, 32], F32, "ExternalInput")
    x1_o = p.dram("x1", [TPC, D], F32, "ExternalOutput")
    t_o = p.dram("t", [TPC, D], BF16, "ExternalOutput")
    g_o = p.dram("gates", [TPC, 32], F32, "ExternalOutput")

    p.accs = [p.ps(f"acc{i}", [128, 512], F32) for i in range(7)]
    tpf = p.ps("tpf", [128, 512], F32)
    identf = p.sb("identf", [128, 128], F32)
    ident = p.sb("ident", [128, 128], BF16)
    ones = p.sb("ones", [128, 128], BF16)
    p.op('pool', lambda: nc.gpsimd.memset(identf[:], 0.0), writes=[identf])
    p.op('pool', lambda: nc.gpsimd.affine_select(out=identf[:], in_=identf[:], pattern=[[-1, 128]],
                                                 compare_op=ALU.not_equal, fill=1.0, base=0, channel_multiplier=1),
         reads=[identf], writes=[identf])
    p.op('dve', lambda: nc.vector.tensor_copy(out=ident[:], in_=identf[:]), reads=[identf], writes=[ident])
    p.op('pool', lambda: nc.gpsimd.memset(ones[:], 1.0), writes=[ones])
    p.ident, p.identf = ident, identf
    YT = p.sb("YT", [128, 8, TPC], BF16)
    OT = p.sb("OT", [128, 8, TPC], BF16)
    cd = p.sb("cd", [128, 2, 128], BF16)
    p.dma('sp', cd[:], cdft_d[:], reads=[cdft_d], writes=[cd])

    ph = Phase(p)
    U = ph.sb("U", [128, 32, 1024], BF16)
    for h in range(4):
        p.dma('sp', U[:, h * 8:(h + 1) * 8, :], f_d[h * 1024:(h + 1) * 1024, :].rearrange("(t p) n -> p t n", p=128),
              reads=[f_d], writes=[U])
    dcs = ph.scratch("dcs", [128, 32, 256], BF16, 2)
    dss = ph.scratch("dss", [128, 32, 256], BF16, 2)
    Zc = ph.sb("Zc", [128, 8, TPC], BF16)
    Zs = ph.sb("Zs", [128, 8, TPC], BF16)
    for lc in range(4):
        dc, ds = dcs.get(), dss.get()
        p.dma('sp', dc[:], dc_d[:, lc * 256:(lc + 1) * 256].rearrange("(t p) n -> p t n", p=128), reads=[dc_d],
              writes=[dc])
        p.dma('sp', ds[:], ds_d[:, lc * 256:(lc + 1) * 256].rearrange("(t p) n -> p t n", p=128), reads=[ds_d],
              writes=[ds])
        for g in range(8):
            for which, (dd, Z) in enumerate(((dc, Zc), (ds, Zs))):
                acc = p.acc()

                def mm():
                    for lt in range(32):
                        i = nc.tensor.matmul(acc[:, 0:256], lhsT=U[:, lt, g * 128:(g + 1) * 128], rhs=dd[:, lt, :],
                                             start=(lt == 0), stop=(lt == 31))
                    return i
                p.op('pe', mm, reads=[U, dd], writes=[acc])
                if which == 0:
                    p.op('act', lambda: nc.scalar.copy(out=Z[:, g, lc * 256:(lc + 1) * 256], in_=acc[:, 0:256]),
                         reads=[acc], writes=[Z])
                else:
                    p.op('dve', lambda: nc.vector.tensor_copy(out=Z[:, g, lc * 256:(lc + 1) * 256], in_=acc[:, 0:256]),
                         reads=[acc], writes=[Z])
    for g in range(8):
        for c in range(2):
            acc = p.acc()

            def mm():
                nc.tensor.matmul(acc[:], lhsT=cd[:, 0, :], rhs=Zc[:, g, c * 512:(c + 1) * 512], start=True, stop=False)
                return nc.tensor.matmul(acc[:], lhsT=cd[:, 1, :], rhs=Zs[:, g, c * 512:(c + 1) * 512], start=False,
                                        stop=True)
            p.op('pe', mm, reads=[cd, Zc, Zs], writes=[acc])
            p.op('act' if c == 0 else 'dve',
                 (lambda: nc.scalar.copy(out=YT[:, g, c * 512:(c + 1) * 512], in_=acc[:])) if c == 0 else
                 (lambda: nc.vector.tensor_copy(out=YT[:, g, c * 512:(c + 1) * 512], in_=acc[:])),
                 reads=[acc], writes=[YT])
    ph.close()

    wo = p.sb("wo", [128, KC, D], BF16)
    for h in range(4):
        p.dma('pool', wo[:, h * 4:(h + 1) * 4, :], wo_d[h * 512:(h + 1) * 512, :].rearrange("(k p) n -> p k n", p=128),
              reads=[wo_d], writes=[wo])

    ph = Phase(p)
    kT = ph.sb("kTs", [128, 2, NKEY], BF16)
    vv = ph.sb("vv", [128, NKT, 256], BF16)
    qT = ph.sb("qTs", [128, 8, TPC], BF16)
    p.dma('sp', kT[:], kT_d[:], reads=[kT_d], writes=[kT])
    p.dma('sp', vv[:], v_d.t.ap().rearrange("(t p) n -> p t n", p=128), reads=[v_d], writes=[vv])
    p.dma('sp', qT[:], qT_d[:], reads=[qT_d], writes=[qT])
    PTs = ph.scratch("PT", [128, 512], BF16, 3)
    rDs = ph.scratch("rD", [128, 512], F32, 2)
    scale = float(128 ** -0.5)
    it = 0
    for h in range(8):
        kvh = h // 4
        for qc in range(2):
            accO = p.accs[0 + 2 * (it % 2)]
            accD = p.accs[1 + 2 * (it % 2)]
            it += 1
            for kt in range(NKT):
                accS = p.accs[4 + (kt % 3)]
                p.op('pe', lambda: nc.tensor.matmul(accS[:], lhsT=kT[:, kvh, kt * 128:(kt + 1) * 128],
                                                    rhs=qT[:, h, qc * 512:(qc + 1) * 512], start=True, stop=True),
                     reads=[kT, qT], writes=[accS])
                PT = PTs.get()
                p.op('act', lambda: nc.scalar.activation(out=PT[:], in_=accS[:], func=AF.Exp, scale=scale),
                     reads=[accS], writes=[PT])

                def mm2():
                    nc.tensor.matmul(accO[:], lhsT=vv[:, kt, kvh * 128:(kvh + 1) * 128], rhs=PT[:], start=(kt == 0),
                                     stop=(kt == NKT - 1))
                    return nc.tensor.matmul(accD[:], lhsT=ones[:], rhs=PT[:], start=(kt == 0), stop=(kt == NKT - 1))
                p.op('pe', mm2, reads=[vv, PT, ones], writes=[accO, accD])
            rD = rDs.get()
            p.op('dve', lambda: nc.vector.reciprocal(out=rD[:], in_=accD[:]), reads=[accD], writes=[rD])
            p.op('dve', lambda: nc.vector.tensor_tensor(out=OT[:, h, qc * 512:(qc + 1) * 512], in0=accO[:], in1=rD[:],
                                                        op=ALU.mult), reads=[accO, rD], writes=[OT])
    ph.close()

    ph = Phase(p)
    rb = load_rows_b(p, ph, rows_d, ["gate1", "A2", "B2", "sc2"])
    p.op('dve', lambda: nc.vector.scalar_tensor_tensor(out=rb["A2"][:], in0=rb["sc2"][:], scalar=1.0, in1=rb["A2"][:],
                                                       op0=ALU.add, op1=ALU.mult), reads=[rb["sc2"], rb["A2"]],
         writes=[rb["A2"]])
    wr = ph.sb("wr", [128, KC, 32], F32)
    brb = ph.sb("brb", [128, 32], F32)
    p.dma('sp', wr[:], wr_d[:], reads=[wr_d], writes=[wr])
    p.dma('sp', brb[:], br_d[0:1, :].partition_broadcast(128), reads=[br_d], writes=[brb])
    prep = Prep(p, ph, rb["A2"], rb["B2"], wr, brb, tpf)
    xs = ph.scratch("xs", [128, D], F32, 2)
    x1s = ph.scratch("x1s", [128, D], F32, 2)
    tmp = ph.scratch("optmp", [128, 512], F32, 2)
    p.acc_i = 0
    for t in range(TPC // 128):
        xt, x1 = xs.get(), x1s.get()
        p.dma('sp', xt[:], x[t * 128:(t + 1) * 128, :], reads=[x], writes=[xt])
        for c in range(4):
            acc = p.acc()

            def mm():
                for k in range(KC):
                    src = OT if k < 8 else YT
                    i = nc.tensor.matmul(acc[:], lhsT=src[:, k % 8, t * 128:(t + 1) * 128],
                                         rhs=wo[:, k, c * 512:(c + 1) * 512], start=(k == 0), stop=(k == KC - 1))
                return i
            p.op('pe', mm, reads=[OT, YT, wo], writes=[acc])
            tm = tmp.get()
            p.op('dve', lambda: nc.vector.tensor_tensor(out=tm[:], in0=acc[:], in1=rb["gate1"][:, c * 512:(c + 1) * 512],
                                                        op=ALU.mult), reads=[acc, rb["gate1"]], writes=[tm])
            p.op('pool', lambda: nc.gpsimd.tensor_tensor(out=x1[:, c * 512:(c + 1) * 512], in0=tm[:],
                                                         in1=xt[:, c * 512:(c + 1) * 512], op=ALU.add),
                 reads=[tm, xt], writes=[x1])
        p.dma('sp', x1_o[t * 128:(t + 1) * 128, :], x1[:], reads=[x1], writes=[x1_o])
        prep.emit(x1, t_o[t * 128:(t + 1) * 128, :], t_o, g_o[t * 128:(t + 1) * 128, :], g_o)
    p.wait_all('sp', [x1_o, t_o, g_o])
    ph.close()
    return nc


NTOK = 8192
EPC = 4
SCH = 1024
LIMIT = 7.0
ALPHA = 1.702


def build_p4():
    nc, p = new_prog()
    tT_d = p.dram("tT", [128, KC, NTOK], BF16, "ExternalInput")
    gts_d = p.dram("gts", [128, NTOK // 128, EPC], F32, "ExternalInput")
    w1_d = p.dram("w1", [EPC, D, D], F32, "ExternalInput")
    b1_d = p.dram("b1c", [128, EPC, 16], F32, "ExternalInput")
    w2_d = p.dram("w2", [EPC, 1024, D], F32, "ExternalInput")
    b2_d = p.dram("b2", [EPC, D], F32, "ExternalInput")
    yp_o = p.dram("yp", [NTOK, D], BF16, "ExternalOutput")
    p.accs = [p.ps(f"acc{i}", [128, 512], F32) for i in range(8)]
    ones1 = p.sb("ones1", [1, 128], BF16)
    p.op('pool', lambda: nc.gpsimd.memset(ones1[:], 1.0), writes=[ones1])
    gts = p.sb("gts_s", [128, NTOK // 128, EPC], F32)
    b1c = p.sb("b1c_s", [128, EPC, 16], F32)
    p.dma('sp', gts[:], gts_d[:], reads=[gts_d], writes=[gts])
    p.dma('sp', b1c[:], b1_d[:], reads=[b1_d], writes=[b1c])
    hT = p.sb("hT", [128, KC, SCH], BF16)
    yacc = [p.sb(f"yacc{i}", [128, D], F32) for i in range(SCH // 128)]
    w1ss = Scratch(p, "w1s", [128, KC, 512], BF16, 2)
    w2ss = Scratch(p, "w2s", [128, 2, D], BF16, 2)
    b2ss = Scratch(p, "b2s", [1, D], BF16, 2)
    actTs = Scratch(p, "actT", [128, 2, SCH], BF16, 2)
    g32s = Scratch(p, "g32", [128, 512], F32, 2)
    sgs = Scratch(p, "sg", [128, 512], F32, 2)
    l32s = Scratch(p, "l32", [128, 512], F32, 2)
    obs = Scratch(p, "ob", [128, D], BF16, 2)
    for sc in range(NTOK // SCH):
        for h in range(2):
            p.dma('sp', hT[:, h * 8:(h + 1) * 8, :], tT_d[:, h * 8:(h + 1) * 8, sc * SCH:(sc + 1) * SCH], reads=[tT_d],
                  writes=[hT])
        for e in range(EPC):
            b2s = b2ss.get()
            p.dma('pool', b2s[:], b2_d[e:e + 1, :], reads=[b2_d], writes=[b2s])
            for s in range(4):
                w1s, w2s, actT = w1ss.get(), w2ss.get(), actTs.get()
                p.dma('pool', w1s[:, :, 0:256], w1_d[e, :, s * 256:(s + 1) * 256].rearrange("(k p) n -> p k n", p=128),
                      reads=[w1_d], writes=[w1s])
                p.dma('pool', w1s[:, :, 256:512],
                      w1_d[e, :, 1024 + s * 256:1024 + (s + 1) * 256].rearrange("(k p) n -> p k n", p=128),
                      reads=[w1_d], writes=[w1s])
                p.dma('pool', w2s[:], w2_d[e, s * 256:(s + 1) * 256, :].rearrange("(k p) n -> p k n", p=128),
                      reads=[w2_d], writes=[w2s])
                for tc in range(SCH // 512):
                    for j in range(2):
                        accG, accL = p.acc(), p.acc()
                        for acc, off in ((accG, 0), (accL, 256)):
                            def mm():
                                for k in range(KC):
                                    i = nc.tensor.matmul(acc[:], lhsT=w1s[:, k, off + j * 128:off + (j + 1) * 128],
                                                         rhs=hT[:, k, tc * 512:(tc + 1) * 512], start=(k == 0),
                                                         stop=(k == KC - 1))
                                return i
                            p.op('pe', mm, reads=[w1s, hT], writes=[acc])
                        g32, sg, l32 = g32s.get(), sgs.get(), l32s.get()
                        jj = s * 2 + j
                        p.op('dve', lambda: nc.vector.tensor_scalar(out=g32[:], in0=accG[:], scalar1=b1c[:, e, jj:jj + 1],
                                                                    scalar2=LIMIT, op0=ALU.add, op1=ALU.min),
                             reads=[accG, b1c], writes=[g32])
                        p.op('act', lambda: nc.scalar.activation(out=sg[:], in_=g32[:], func=AF.Sigmoid, scale=ALPHA),
                             reads=[g32], writes=[sg])
                        p.op('dve', lambda: nc.vector.tensor_scalar(out=l32[:], in0=accL[:],
                                                                    scalar1=b1c[:, e, 8 + jj:8 + jj + 1], scalar2=LIMIT,
                                                                    op0=ALU.add, op1=ALU.min), reads=[accL, b1c],
                             writes=[l32])
                        p.op('pool', lambda: nc.gpsimd.tensor_scalar(out=l32[:], in0=l32[:], scalar1=-LIMIT, scalar2=1.0,
                                                                     op0=ALU.max, op1=ALU.add), reads=[l32], writes=[l32])
                        p.op('pool', lambda: nc.gpsimd.tensor_tensor(out=g32[:], in0=g32[:], in1=sg[:], op=ALU.mult),
                             reads=[g32, sg], writes=[g32])
                        p.op('pool', lambda: nc.gpsimd.tensor_tensor(out=actT[:, j, tc * 512:(tc + 1) * 512], in0=g32[:],
                                                                     in1=l32[:], op=ALU.mult), reads=[g32, l32],
                             writes=[actT])
                for tt in range(SCH // 128):
                    gcol = gts[:, sc * (SCH // 128) + tt, e:e + 1]
                    for c in range(4):
                        acc = p.acc()

                        def mm2():
                            if s == 0:
                                nc.tensor.matmul(acc[:], lhsT=ones1[0:1, :], rhs=b2s[0:1, c * 512:(c + 1) * 512],
                                                 start=True, stop=False)
                            for j in range(2):
                                i = nc.tensor.matmul(acc[:], lhsT=actT[:, j, tt * 128:(tt + 1) * 128],
                                                     rhs=w2s[:, j, c * 512:(c + 1) * 512], start=(j == 0 and s != 0),
                                                     stop=(j == 1))
                            return i
                        p.op('pe', mm2, reads=[actT, w2s, b2s, ones1], writes=[acc])
                        ya = yacc[tt]
                        if e == 0 and s == 0:
                            p.op('dve', lambda: nc.vector.tensor_scalar(out=ya[:, c * 512:(c + 1) * 512], in0=acc[:],
                                                                        scalar1=gcol, scalar2=None, op0=ALU.mult),
                                 reads=[acc, gts], writes=[ya])
                        else:
                            p.op('dve', lambda: nc.vector.scalar_tensor_tensor(
                                out=ya[:, c * 512:(c + 1) * 512], in0=acc[:], scalar=gcol,
                                in1=ya[:, c * 512:(c + 1) * 512], op0=ALU.mult, op1=ALU.add),
                                reads=[acc, gts, ya], writes=[ya])
        for tt in range(SCH // 128):
            ob = obs.get()
            p.op('act', lambda: nc.scalar.copy(out=ob[:], in_=yacc[tt][:]), reads=[yacc[tt]], writes=[ob])
            p.dma('sp', yp_o[sc * SCH + tt * 128:sc * SCH + (tt + 1) * 128, :], ob[:], reads=[ob], writes=[yp_o])
    p.wait_all('sp', [yp_o])
    return nc


def emit_combine(p, xt, rows, ypt, gate_b, xo, tmps):
    nc = p.nc
    for c in range(4):
        acc = p.acc()

        def mm():
            for i in range(8):
                ins = nc.tensor.matmul(acc[0:rows, :], lhsT=p.ident[0:rows, 0:rows], rhs=ypt[0:rows, i, c * 512:(c + 1) * 512],
                                       start=(i == 0), stop=(i == 7))
            return ins
        p.op('pe', mm, reads=[p.ident, ypt], writes=[acc])
        tm = tmps.get()
        p.op('dve', lambda: nc.vector.tensor_tensor(out=tm[0:rows, :], in0=acc[0:rows, :],
                                                    in1=gate_b[0:rows, c * 512:(c + 1) * 512], op=ALU.mult),
             reads=[acc, gate_b], writes=[tm])
        p.op('pool', lambda: nc.gpsimd.tensor_tensor(out=xo[0:rows, c * 512:(c + 1) * 512], in0=tm[0:rows, :],
                                                     in1=xt[0:rows, c * 512:(c + 1) * 512], op=ALU.add),
             reads=[tm, xt], writes=[xo])


def setup_ident(p):
    nc = p.nc
    identf = p.sb("identf", [128, 128], F32)
    ident = p.sb("ident", [128, 128], BF16)
    p.op('pool', lambda: nc.gpsimd.memset(identf[:], 0.0), writes=[identf])
    p.op('pool', lambda: nc.gpsimd.affine_select(out=identf[:], in_=identf[:], pattern=[[-1, 128]],
                                                 compare_op=ALU.not_equal, fill=1.0, base=0, channel_multiplier=1),
         reads=[identf], writes=[identf])
    p.op('dve', lambda: nc.vector.tensor_copy(out=ident[:], in_=identf[:]), reads=[identf], writes=[ident])
    p.ident, p.identf = ident, identf


HALO = 16
LTOK = TPC + 2 * HALO
CW = 31


def build_p6():
    nc, p = new_prog()
    x1h_d = p.dram("x1h", [LTOK, D], F32, "ExternalInput")
    yph_d = p.dram("yph", [8, LTOK, D], BF16, "ExternalInput")
    rows_d = p.dram("rows", [6, D], F32, "ExternalInput")
    cols_d = p.dram("cols", [128, 3, KC], F32, "ExternalInput")
    cvb_d = p.dram("cvb", [128, 32 + 16 + 16 + 16], F32, "ExternalInput")
    wdw_d = p.dram("wdw", [128, KC, CW], F32, "ExternalInput")
    mask_d = p.dram("mask", [128, 2], F32, "ExternalInput")
    wi_d = p.dram("cv_w_in", [D, 2 * D], F32, "ExternalInput")
    wo_d = p.dram("cv_w_out", [D, D], F32, "ExternalInput")
    wr_d = p.dram("w_router", [128, KC, 32], F32, "ExternalInput")
    br_d = p.dram("b_router", [1, 32], F32, "ExternalInput")
    x2_o = p.dram("x2", [TPC, D], F32, "ExternalOutput")
    t_o = p.dram("t", [TPC, D], BF16, "ExternalOutput")
    g_o = p.dram("gates", [TPC, 32], F32, "ExternalOutput")
    x1p = p.dram("x1p", [LTOK, D], F32)
    p.accs = [p.ps(f"acc{i}", [128, 512], F32) for i in range(6)]
    p.tps = [p.ps("tpb0", [128, 1024], BF16)]
    tpf = p.ps("tpf", [128, 512], F32)
    setup_ident(p)
    ones = p.sb("ones", [128, 128], BF16)
    ones1 = p.sb("ones1", [1, 128], BF16)
    p.op('pool', lambda: nc.gpsimd.memset(ones[:], 1.0), writes=[ones])
    p.op('pool', lambda: nc.gpsimd.memset(ones1[:], 1.0), writes=[ones1])
    colsb = p.sb("colsb", [128, 3, KC], F32)
    cvb = p.sb("cvbs", [128, 80], F32)
    wdw = p.sb("wdws", [128, KC, CW], F32)
    msk = p.sb("msks", [128, 2], F32)
    p.dma('sp', colsb[:], cols_d[:], reads=[cols_d], writes=[colsb])
    p.dma('sp', cvb[:], cvb_d[:], reads=[cvb_d], writes=[cvb])
    p.dma('sp', wdw[:], wdw_d[:], reads=[wdw_d], writes=[wdw])
    p.dma('sp', msk[:], mask_d[:], reads=[mask_d], writes=[msk])
    A1 = p.sb("A1", [128, KC], F32)
    B1 = p.sb("B1", [128, KC], F32)
    p.op('dve', lambda: nc.vector.scalar_tensor_tensor(out=A1[:], in0=colsb[:, 2, :], scalar=1.0, in1=colsb[:, 0, :],
                                                       op0=ALU.add, op1=ALU.mult), reads=[colsb], writes=[A1])
    p.op('dve', lambda: nc.vector.tensor_copy(out=B1[:], in_=colsb[:, 1, :]), reads=[colsb], writes=[B1])
    uc = p.sb("uc", [128, KC, TPC], BF16)
    mean_b = [p.sb(f"mean_b{c}", [128, 512], F32) for c in range(2)]
    rstd_b = [p.sb(f"rstd_b{c}", [128, 512], F32) for c in range(2)]
    phA = Phase(p)
    hT = phA.sb("hT", [128, KC, LTOK], BF16)

    ph = Phase(p)
    g2b = ph.sb("g2b", [128, D], F32)
    p.dma('sp', g2b[:], rows_d[0:1, :].partition_broadcast(128), reads=[rows_d], writes=[g2b])
    nt = NormT.__new__(NormT)
    nt.p = p
    nt.sq = ph.scratch("nt_sq", [128, D], BF16, 1)
    nt.ss = ph.scratch("nt_ss", [128, 1], F32, 2)
    nt.xn = ph.scratch("nt_xn", [128, D], BF16, 2)
    xs = ph.scratch("xs", [128, D], F32, 2)
    xos = ph.scratch("xo", [128, D], F32, 2)
    yps = ph.scratch("ypt", [128, 8, D], BF16, 2)
    tmps = ph.scratch("ctmp", [128, 512], F32, 2)
    ntl = (LTOK + 127) // 128
    for t in range(ntl):
        r0 = t * 128
        rows = min(128, LTOK - r0)
        xt, xo, ypt = xs.get(), xos.get(), yps.get()
        p.dma('sp', xt[0:rows, :], x1h_d[r0:r0 + rows, :], reads=[x1h_d], writes=[xt])
        p.dma('sp', ypt[0:rows, :, :], yph_d[:, r0:r0 + rows, :].rearrange("c t d -> t c d"), reads=[yph_d], writes=[ypt])
        emit_combine(p, xt, rows, ypt, g2b, xo, tmps)
        p.dma('sp', x1p[r0:r0 + rows, :], xo[0:rows, :], reads=[xo], writes=[x1p])
        nt.emit(xo, rows, A1, B1, hT, r0)
    ph.close()

    ph = Phase(p)
    wis = ph.scratch("wis", [128, KC, 256], BF16, 2)
    uTs = ph.scratch("uT", [128, LTOK], BF16, 2)
    sgs = ph.scratch("sgm", [128, 512], F32, 2)
    dgs = ph.scratch("dg", [128, CW, 128], BF16, 2)
    sqs = ph.scratch("csq", [128, 512], BF16, 2)
    accM = [p.ps(f"accM{c}", [128, 512], F32) for c in range(0)]
    accM = [p.accs[0], p.accs[1]]
    accQ = [p.accs[2], p.accs[3]]
    rot = [p.accs[4], p.accs[5], tpf]
    ri = 0
    chunks = [(0, 512), (512, 512), (1024, LTOK - 1024)]
    for ct in range(KC):
        wi, uT, dg = wis.get(), uTs.get(), dgs.get()
        p.dma('pool', wi[:, :, 0:128], wi_d[:, ct * 128:(ct + 1) * 128].rearrange("(k p) n -> p k n", p=128),
              reads=[wi_d], writes=[wi])
        p.dma('pool', wi[:, :, 128:256], wi_d[:, D + ct * 128:D + (ct + 1) * 128].rearrange("(k p) n -> p k n", p=128),
              reads=[wi_d], writes=[wi])
        p.op('pool', lambda: nc.gpsimd.tensor_tensor(
            out=dg[:], in0=p.ident[:].unsqueeze(1).broadcast_to([128, CW, 128]),
            in1=wdw[:, ct, :].unsqueeze(2).broadcast_to([128, CW, 128]), op=ALU.mult), reads=[p.ident, wdw], writes=[dg])
        for (c0, cn) in chunks:
            accA, accG = rot[ri % 3], rot[(ri + 1) % 3]
            ri += 2
            for acc, off in ((accA, 0), (accG, 128)):
                def mm():
                    for k in range(KC):
                        i = nc.tensor.matmul(acc[:, 0:cn], lhsT=wi[:, k, off:off + 128], rhs=hT[:, k, c0:c0 + cn],
                                             start=(k == 0), stop=(k == KC - 1))
                    return i
                p.op('pe', mm, reads=[wi, hT], writes=[acc])
            sg = sgs.get()
            p.op('act', lambda: nc.scalar.activation(out=sg[:, 0:cn], in_=accG[:, 0:cn], func=AF.Sigmoid,
                                                     bias=cvb[:, 16 + ct:16 + ct + 1], scale=1.0), reads=[accG, cvb],
                 writes=[sg])
            p.op('dve', lambda: nc.vector.scalar_tensor_tensor(out=uT[:, c0:c0 + cn], in0=accA[:, 0:cn],
                                                               scalar=cvb[:, ct:ct + 1], in1=sg[:, 0:cn], op0=ALU.add,
                                                               op1=ALU.mult), reads=[accA, cvb, sg], writes=[uT])
        p.op('dve', lambda: nc.vector.tensor_scalar(out=uT[:, 0:HALO], in0=uT[:, 0:HALO], scalar1=msk[:, 0:1], scalar2=None,
                                                    op0=ALU.mult), reads=[uT, msk], writes=[uT])
        p.op('dve', lambda: nc.vector.tensor_scalar(out=uT[:, LTOK - HALO:LTOK], in0=uT[:, LTOK - HALO:LTOK],
                                                    scalar1=msk[:, 1:2], scalar2=None, op0=ALU.mult), reads=[uT, msk],
             writes=[uT])
        for c in range(2):
            acc = rot[ri % 3]
            ri += 1

            def mmc():
                for j in range(CW):
                    i = nc.tensor.matmul(acc[:], lhsT=dg[:, j, :], rhs=uT[:, c * 512 + j + 1:c * 512 + j + 1 + 512],
                                         start=(j == 0), stop=(j == CW - 1))
                return i
            p.op('pe', mmc, reads=[dg, uT], writes=[acc])
            p.op('dve', lambda: nc.vector.tensor_scalar(out=uc[:, ct, c * 512:(c + 1) * 512], in0=acc[:],
                                                        scalar1=cvb[:, 32 + ct:32 + ct + 1], scalar2=None, op0=ALU.add),
                 reads=[acc, cvb], writes=[uc])
            sq = sqs.get()
            p.op('act', lambda: nc.scalar.activation(out=sq[:], in_=uc[:, ct, c * 512:(c + 1) * 512], func=AF.Square),
                 reads=[uc], writes=[sq])

            def mms():
                nc.tensor.matmul(accM[c][:], lhsT=ones[:], rhs=uc[:, ct, c * 512:(c + 1) * 512], start=(ct == 0),
                                 stop=(ct == KC - 1))
                return nc.tensor.matmul(accQ[c][:], lhsT=ones[:], rhs=sq[:], start=(ct == 0), stop=(ct == KC - 1))
            p.op('pe', mms, reads=[ones, uc, sq], writes=[accM[c], accQ[c]])
    for c in range(2):
        p.op('dve', lambda: nc.vector.tensor_scalar(out=mean_b[c][:], in0=accM[c][:], scalar1=1.0 / D, scalar2=None,
                                                    op0=ALU.mult), reads=[accM[c]], writes=[mean_b[c]])
        p.op('dve', lambda: nc.vector.tensor_tensor(out=rstd_b[c][:], in0=mean_b[c][:], in1=mean_b[c][:], op=ALU.mult),
             reads=[mean_b[c]], writes=[rstd_b[c]])
        p.op('dve', lambda: nc.vector.scalar_tensor_tensor(out=rstd_b[c][:], in0=accQ[c][:], scalar=1.0 / D,
                                                           in1=rstd_b[c][:], op0=ALU.mult, op1=ALU.subtract),
             reads=[accQ[c], rstd_b[c]], writes=[rstd_b[c]])
        p.op('dve', lambda: nc.vector.tensor_scalar(out=rstd_b[c][:], in0=rstd_b[c][:], scalar1=EPS, scalar2=None,
                                                    op0=ALU.add), reads=[rstd_b[c]], writes=[rstd_b[c]])
        p.op('act', lambda: nc.scalar.sqrt(out=rstd_b[c][:], in_=rstd_b[c][:]), reads=[rstd_b[c]], writes=[rstd_b[c]])
        p.op('dve', lambda: nc.vector.reciprocal(out=rstd_b[c][:], in_=rstd_b[c][:]), reads=[rstd_b[c]],
             writes=[rstd_b[c]])
    ph.close()
    phA.close()

    ph = Phase(p)
    wo = ph.sb("wo", [128, KC, D], BF16)
    for h in range(4):
        p.dma('pool', wo[:, h * 4:(h + 1) * 4, :], wo_d[h * 512:(h + 1) * 512, :].rearrange("(k p) n -> p k n", p=128),
              reads=[wo_d], writes=[wo])
    lts = ph.scratch("lnt", [128, 512], F32, 2)
    for ct in range(KC):
        for c in range(2):
            lt = lts.get()
            p.op('dve', lambda: nc.vector.tensor_tensor(out=lt[:], in0=uc[:, ct, c * 512:(c + 1) * 512], in1=mean_b[c][:],
                                                        op=ALU.subtract), reads=[uc, mean_b[c]], writes=[lt])
            p.op('pool', lambda: nc.gpsimd.tensor_tensor(out=lt[:], in0=lt[:], in1=rstd_b[c][:], op=ALU.mult),
                 reads=[lt, rstd_b[c]], writes=[lt])
            p.op('act', lambda: nc.scalar.activation(out=uc[:, ct, c * 512:(c + 1) * 512], in_=lt[:], func=AF.Silu,
                                                     scale=cvb[:, 48 + ct:48 + ct + 1], bias=cvb[:, 64 + ct:64 + ct + 1]),
                 reads=[lt, cvb], writes=[uc])
    rb = load_rows_b(p, ph, rows_d, ["gate1", "A2", "B2", "sc2"], off=1)
    p.op('dve', lambda: nc.vector.scalar_tensor_tensor(out=rb["A2"][:], in0=rb["sc2"][:], scalar=1.0, in1=rb["A2"][:],
                                                       op0=ALU.add, op1=ALU.mult), reads=[rb["sc2"], rb["A2"]],
         writes=[rb["A2"]])
    bo = ph.sb("bo", [1, D], BF16)
    p.dma('pool', bo[:], rows_d[5:6, :], reads=[rows_d], writes=[bo])
    wr = ph.sb("wr", [128, KC, 32], F32)
    brb = ph.sb("brb", [128, 32], F32)
    p.dma('sp', wr[:], wr_d[:], reads=[wr_d], writes=[wr])
    p.dma('sp', brb[:], br_d[0:1, :].partition_broadcast(128), reads=[br_d], writes=[brb])
    prep = Prep(p, ph, rb["A2"], rb["B2"], wr, brb, tpf)
    xs = ph.scratch("xs4", [128, D], F32, 2)
    x2s = ph.scratch("x2s", [128, D], F32, 1)
    tmp = ph.scratch("optmp", [128, 512], F32, 2)
    p.acc_i = 0
    for t in range(TPC // 128):
        xt, x2 = xs.get(), x2s.get()
        p.dma('sp', xt[:], x1p[HALO + t * 128:HALO + (t + 1) * 128, :], reads=[x1p], writes=[xt])
        for c in range(4):
            acc = p.acc()

            def mm():
                nc.tensor.matmul(acc[:], lhsT=ones1[0:1, :], rhs=bo[0:1, c * 512:(c + 1) * 512], start=True, stop=False)
                for k in range(KC):
                    i = nc.tensor.matmul(acc[:], lhsT=uc[:, k, t * 128:(t + 1) * 128], rhs=wo[:, k, c * 512:(c + 1) * 512],
                                         start=False, stop=(k == KC - 1))
                return i
            p.op('pe', mm, reads=[uc, wo, bo, ones1], writes=[acc])
            tm = tmp.get()
            p.op('dve', lambda: nc.vector.tensor_tensor(out=tm[:], in0=acc[:], in1=rb["gate1"][:, c * 512:(c + 1) * 512],
                                                        op=ALU.mult), reads=[acc, rb["gate1"]], writes=[tm])
            p.op('pool', lambda: nc.gpsimd.tensor_tensor(out=x2[:, c * 512:(c + 1) * 512], in0=tm[:],
                                                         in1=xt[:, c * 512:(c + 1) * 512], op=ALU.add),
                 reads=[tm, xt], writes=[x2])
        p.dma('sp', x2_o[t * 128:(t + 1) * 128, :], x2[:], reads=[x2], writes=[x2_o])
        prep.emit(x2, t_o[t * 128:(t + 1) * 128, :], t_o, g_o[t * 128:(t + 1) * 128, :], g_o)
    p.wait_all('sp', [x2_o, t_o, g_o])
    ph.close()
    return nc


def build_p8():
    nc, p = new_prog()
    x2_d = p.dram("x2", [TPC, D], F32, "ExternalInput")
    yph_d = p.dram("yph", [8, TPC, D], BF16, "ExternalInput")
    rows_d = p.dram("rows", [2, D], F32, "ExternalInput")
    out_o = p.dram("out", [TPC, D], F32, "ExternalOutput")
    p.accs = [p.ps(f"acc{i}", [128, 512], F32) for i in range(8)]
    setup_ident(p)
    ph = Phase(p)
    rb = load_rows_b(p, ph, rows_d, ["g2", "gf"])
    xs = ph.scratch("xs", [128, D], F32, 2)
    xos = ph.scratch("xo", [128, D], F32, 2)
    yps = ph.scratch("ypt", [128, 8, D], BF16, 2)
    tmps = ph.scratch("ctmp", [128, 512], F32, 2)
    sqs = ph.scratch("sq", [128, D], BF16, 1)
    sss = ph.scratch("ss", [128, 1], F32, 2)
    oos = ph.scratch("oo", [128, D], F32, 2)
    for t in range(TPC // 128):
        r0 = t * 128
        xt, xo, ypt, sq, ss, oo = xs.get(), xos.get(), yps.get(), sqs.get(), sss.get(), oos.get()
        p.dma('sp', xt[:], x2_d[r0:r0 + 128, :], reads=[x2_d], writes=[xt])
        p.dma('sp', ypt[:], yph_d[:, r0:r0 + 128, :].rearrange("c t d -> t c d"), reads=[yph_d], writes=[ypt])
        emit_combine(p, xt, 128, ypt, rb["g2"], xo, tmps)
        p.op('act', lambda: nc.scalar.activation(out=sq[:], in_=xo[:], func=AF.Square, accum_out=ss[:]), reads=[xo],
             writes=[sq, ss])
        emit_rstd(p, ss[:], 128, 1.0 / D, ss)
        p.op('dve', lambda: nc.vector.scalar_tensor_tensor(out=oo[:], in0=xo[:], scalar=ss[:, 0:1], in1=rb["gf"][:],
                                                           op0=ALU.mult, op1=ALU.mult), reads=[xo, ss, rb["gf"]],
             writes=[oo])
        p.dma('sp', out_o[r0:r0 + 128, :], oo[:], reads=[oo], writes=[out_o])
    p.wait_all('sp', [out_o])
    ph.close()
    return nc


def _run(nc, ims):
    res = run_bass_kernel_spmd(nc, ims, core_ids=list(range(NCORES)))
    return res.results


def _cf(v, n):
    return np.ascontiguousarray(np.asarray(v).reshape(n, 128).T)


def _rope_table(pos):
    inv = (10000.0 ** (-np.arange(0, 64, 2, dtype=np.float32) / np.float32(64))).astype(np.float32)
    ar = ((pos // 64).astype(np.float32)[:, None] * inv).astype(np.float64)
    ac = ((pos % 64).astype(np.float32)[:, None] * inv).astype(np.float64)
    cr, sr, cc, sc = np.cos(ar), np.sin(ar), np.cos(ac), np.sin(ac)
    return np.ascontiguousarray(np.stack([np.concatenate([cr, cr, cc, cc], 1),
                                          np.concatenate([-sr, sr, -sc, sc], 1)], 1).astype(np.float32))


def _dft_tables(qt):
    l = np.arange(4096, dtype=np.int64)[:, None]
    lp = (qt * TPC + np.arange(TPC, dtype=np.int64))[None, :]
    ang = 2.0 * np.pi * ((l * lp) % 4096).astype(np.float64) / 4096.0
    return np.cos(ang).astype(NPBF), np.sin(ang).astype(NPBF)


def _cdft():
    c = np.arange(128, dtype=np.int64)[:, None]
    cp = np.arange(128, dtype=np.int64)[None, :]
    ang = 2.0 * np.pi * ((c * cp) % 128).astype(np.float64) / 128.0
    s = np.sqrt(4096.0 * 128.0)
    return np.ascontiguousarray(np.stack([np.cos(ang) / s, -np.sin(ang) / s], 1).astype(NPBF))


def _moe_launch(layer, t_all, gates_all, moe_w1, moe_b1, moe_w2, moe_b2):
    ims = []
    t_rows = np.ascontiguousarray(t_all)
    for i in range(NCORES):
        e0 = i * EPC
        w1 = np.asarray(moe_w1[layer, e0:e0 + EPC])
        w1p = np.ascontiguousarray(np.concatenate([w1[..., 0::2], w1[..., 1::2]], -1))
        b1 = np.asarray(moe_b1[layer, e0:e0 + EPC])
        b1p = np.concatenate([b1[..., 0::2], b1[..., 1::2]], -1)
        ims.append({
            "t": t_rows,
            "gts": np.ascontiguousarray(gates_all[:, e0:e0 + EPC].reshape(NTOK // 128, 128, EPC).transpose(1, 0, 2)),
            "w1": w1p,
            "b1c": np.ascontiguousarray(b1p.reshape(EPC, 16, 128).transpose(2, 0, 1)),
            "w2": np.ascontiguousarray(moe_w2[layer, e0:e0 + EPC]),
            "b2": np.ascontiguousarray(moe_b2[layer, e0:e0 + EPC]),
        })
    res = _run(build_p4s(), ims)
    overflow = any(bool((np.asarray(r["cnt"]) > CAPB * 128).any()) for r in res)
    if overflow:
        tT = np.ascontiguousarray(t_all.reshape(NTOK, KC, 128).transpose(2, 1, 0))
        for im in ims:
            del im["t"]
            im["tT"] = tT
        res = _run(build_p4(), ims)
    return np.stack([np.asarray(r["yp"]) for r in res], 0)


def kernel(x, c, ctx, c_ctx, w_mod, b_mod, g_mix, g_ffn, ab_w_in, ab_q_gain, ab_k_gain, ab_w_out,
           cv_w_in, cv_b_in, cv_w_dw, cv_b_dw, cv_ln_g, cv_ln_b, cv_w_out, cv_b_out,
           moe_w_router, moe_b_router, moe_w1, moe_b1, moe_w2, moe_b2, g_final):
    A = lambda a: np.asarray(a, dtype=np.float32)
    x, c, ctx, c_ctx, w_mod, b_mod, g_mix, g_ffn = map(A, (x, c, ctx, c_ctx, w_mod, b_mod, g_mix, g_ffn))
    ab_w_in, ab_q_gain, ab_k_gain, ab_w_out = map(A, (ab_w_in, ab_q_gain, ab_k_gain, ab_w_out))
    cv_w_in, cv_b_in, cv_w_dw, cv_b_dw, cv_ln_g, cv_ln_b, cv_w_out, cv_b_out = map(
        A, (cv_w_in, cv_b_in, cv_w_dw, cv_b_dw, cv_ln_g, cv_ln_b, cv_w_out, cv_b_out))
    moe_w_router, moe_b_router, moe_w1, moe_b1, moe_w2, moe_b2, g_final = map(
        A, (moe_w_router, moe_b_router, moe_w1, moe_b1, moe_w2, moe_b2, g_final))
    B, L, _ = x.shape
    cores = [(i // 4, i % 4) for i in range(NCORES)]

    cv = np.stack([c[0], c[1], c_ctx])
    cT = np.ascontiguousarray(cv.T.reshape(KC, 128, 3).transpose(1, 0, 2))
    ims = []
    for i in range(NCORES):
        sl = slice(i * MODW, (i + 1) * MODW)
        ims.append({"wmod": np.ascontiguousarray(w_mod[:, :, sl]),
                    "bmod": np.ascontiguousarray(np.broadcast_to(b_mod[:, None, sl], (2, 3, MODW))), "cT": cT})
    res = _run(build_p1(), ims)
    m = np.concatenate([np.asarray(r["m"]) for r in res], -1)
    mod = m.reshape(2, 3, 6, D)

    ims = []
    for (b, qt) in cores:
        colsv = np.stack([g_mix[0], mod[0, b, 0], mod[0, b, 1], mod[0, 2, 0], mod[0, 2, 1]])
        ims.append({"x": np.ascontiguousarray(x[b, qt * TPC:(qt + 1) * TPC]),
                    "ctxr": np.ascontiguousarray(ctx[b, qt * CPC:(qt + 1) * CPC]),
                    "cols": np.ascontiguousarray(colsv.reshape(5, KC, 128).transpose(2, 0, 1)),
                    "w_in": np.ascontiguousarray(ab_w_in[0]),
                    "gains": np.ascontiguousarray(np.stack([ab_q_gain[0], ab_k_gain[0]])),
                    "rope": _rope_table(qt * TPC + np.arange(TPC))})
    res = _run(build_p2(), ims)
    qkvf = [np.asarray(r["qkvf"]) for r in res]
    kvc = [np.asarray(r["kvc"]) for r in res]
    q_b, k_b, v_b, f_b = [], [], [], []
    for b in range(B):
        lat = np.concatenate(qkvf[b * 4:(b + 1) * 4], 0)
        cx = np.concatenate(kvc[b * 4:(b + 1) * 4], 0)
        q_b.append(lat[:, 0:1024])
        k_b.append(np.concatenate([lat[:, 1024:1280], cx[:, 0:256]], 0))
        v_b.append(np.concatenate([lat[:, 1280:1536], cx[:, 256:512]], 0))
        f_b.append(lat[:, 1536:2560])

    cd = _cdft()
    dft = [_dft_tables(qt) for qt in range(4)]
    wr_l = [np.ascontiguousarray(moe_w_router[l].reshape(KC, 128, 32).transpose(1, 0, 2)) for l in range(2)]
    ims = []
    for (b, qt) in cores:
        ims.append({"x": np.ascontiguousarray(x[b, qt * TPC:(qt + 1) * TPC]),
                    "qT": np.ascontiguousarray(q_b[b][qt * TPC:(qt + 1) * TPC].reshape(TPC, 8, 128).transpose(2, 1, 0)),
                    "kT": np.ascontiguousarray(k_b[b].reshape(NKEY, 2, 128).transpose(2, 1, 0)),
                    "v": np.ascontiguousarray(v_b[b]), "f": np.ascontiguousarray(f_b[b]),
                    "dftc": dft[qt][0], "dfts": dft[qt][1], "cdft": cd,
                    "w_out": np.ascontiguousarray(ab_w_out[0]),
                    "rows": np.ascontiguousarray(np.stack([mod[0, b, 2], g_ffn[0], mod[0, b, 3], mod[0, b, 4]])),
                    "w_router": wr_l[0], "b_router": np.ascontiguousarray(moe_b_router[0][None, :])})
    res = _run(build_p3(), ims)
    x1_all = np.concatenate([np.asarray(r["x1"]) for r in res], 0)
    t_all = np.concatenate([np.asarray(r["t"]) for r in res], 0)
    g_all = np.concatenate([np.asarray(r["gates"]) for r in res], 0)
    del dft

    yp_all = _moe_launch(0, t_all, g_all, moe_w1, moe_b1, moe_w2, moe_b2)

    cvb = np.ascontiguousarray(np.concatenate([_cf(cv_b_in[0], 32), _cf(cv_b_dw[0], 16), _cf(cv_ln_g[0], 16),
                                               _cf(cv_ln_b[0], 16)], 1))
    wdw = np.ascontiguousarray(cv_w_dw[0].T.reshape(KC, 128, CW).transpose(1, 0, 2))
    ims = []
    for i, (b, qt) in enumerate(cores):
        rows_g = i * TPC - HALO + np.arange(LTOK)
        valid = (rows_g >= b * L) & (rows_g < (b + 1) * L)
        x1h = np.zeros((LTOK, D), np.float32)
        x1h[valid] = x1_all[rows_g[valid]]
        yph = np.zeros((8, LTOK, D), NPBF)
        yph[:, valid] = yp_all[:, rows_g[valid]]
        colsv = np.stack([g_mix[1], mod[1, b, 0], mod[1, b, 1]])
        ims.append({"x1h": x1h, "yph": yph,
                    "rows": np.ascontiguousarray(np.stack([mod[0, b, 5], mod[1, b, 2], g_ffn[1], mod[1, b, 3],
                                                           mod[1, b, 4], cv_b_out[0]])),
                    "cols": np.ascontiguousarray(colsv.reshape(3, KC, 128).transpose(2, 0, 1)),
                    "cvb": cvb, "wdw": wdw,
                    "mask": np.ascontiguousarray(np.broadcast_to(
                        np.array([0.0 if qt == 0 else 1.0, 0.0 if qt == 3 else 1.0], np.float32), (128, 2))),
                    "cv_w_in": np.ascontiguousarray(cv_w_in[0]), "cv_w_out": np.ascontiguousarray(cv_w_out[0]),
                    "w_router": wr_l[1], "b_router": np.ascontiguousarray(moe_b_router[1][None, :])})
    res = _run(build_p6(), ims)
    x2_all = np.concatenate([np.asarray(r["x2"]) for r in res], 0)
    t2_all = np.concatenate([np.asarray(r["t"]) for r in res], 0)
    g2_all = np.concatenate([np.asarray(r["gates"]) for r in res], 0)
    del yp_all, ims

    yp2_all = _moe_launch(1, t2_all, g2_all, moe_w1, moe_b1, moe_w2, moe_b2)

    ims = []
    for i, (b, qt) in enumerate(cores):
        ims.append({"x2": np.ascontiguousarray(x2_all[i * TPC:(i + 1) * TPC]),
                    "yph": np.ascontiguousarray(yp2_all[:, i * TPC:(i + 1) * TPC]),
                    "rows": np.ascontiguousarray(np.stack([mod[1, b, 5], g_final]))})
    res = _run(build_p8(), ims)
    out = np.concatenate([np.asarray(r["out"]) for r in res], 0).reshape(B, L, D).astype(np.float32)
    return out


CAPB = 16
BIG = 1.0e6
I32 = mybir.dt.int32


def p_idma(p, out_ap, out_off, in_ap, in_off, bound, reads, dst, concurrent=True):
    nc = p.nc
    for b in reads:
        if b.w is not None:
            p._wait('pool', b.w)
    if concurrent:
        if getattr(dst, 'wbase', None) is not None:
            p._wait('pool', dst.wbase)
    else:
        if dst.w is not None:
            p._wait('pool', dst.w)
    for ev in dst.r:
        p._wait('pool', ev)
    if dst.dsem is None:
        key = 'd_' + dst.name
        p.sems[key] = nc.alloc_semaphore(key)
        dst.dsem = key
    if getattr(p, 'bound_reg', None) is None or p.bound_val != bound:
        p.bound_reg = nc.gpsimd.to_reg(bound)
        p.bound_val = bound
    inst = nc.gpsimd.indirect_dma_start(out=out_ap, out_offset=out_off, in_=in_ap, in_offset=in_off,
                                        bounds_check=p.bound_reg, oob_is_err=False)
    dst.dcnt += 16
    p.dma_cnt[dst.dsem] = dst.dcnt
    inst.then_inc(p.sems[dst.dsem], 16)
    ev = (dst.dsem, dst.dcnt)
    for b in reads:
        b.r.append(ev)
    dst.w = ev
    dst.r = []
    return inst


def build_p4s(capb=CAPB):
    CAPR = capb * 128
    NT = NTOK // 128
    nc, p = new_prog()
    t_d = p.dram("t", [NTOK, D], BF16, "ExternalInput")
    gts_d = p.dram("gts", [128, NT, EPC], F32, "ExternalInput")
    w1_d = p.dram("w1", [EPC, D, D], F32, "ExternalInput")
    b1_d = p.dram("b1c", [128, EPC, 16], F32, "ExternalInput")
    w2_d = p.dram("w2", [EPC, 1024, D], F32, "ExternalInput")
    b2_d = p.dram("b2", [EPC, D], F32, "ExternalInput")
    yp_o = p.dram("yp", [NTOK, D], BF16, "ExternalOutput")
    cnt_o = p.dram("cnt", [1, EPC], F32, "ExternalOutput")
    Xg = [p.dram(f"Xg{e}", [CAPR, D], BF16) for e in range(EPC)]
    Yg = [p.dram(f"Yg{e}", [CAPR, D], BF16) for e in range(EPC)]
    p.accs = [p.ps(f"acc{i}", [128, 512], F32) for i in range(6)]
    p.tps = [p.ps(f"tpb{i}", [128, 1024], BF16) for i in range(2)]
    setup_ident(p)
    ones = p.sb("ones", [128, 128], BF16)
    ones1 = p.sb("ones1", [1, 128], BF16)
    tri = p.sb("tri", [128, 128], BF16)
    trif = p.sb("trif", [128, 128], F32)
    p.op('pool', lambda: nc.gpsimd.memset(ones[:], 1.0), writes=[ones])
    p.op('pool', lambda: nc.gpsimd.memset(ones1[:], 1.0), writes=[ones1])
    p.op('pool', lambda: nc.gpsimd.memset(trif[:], 1.0), writes=[trif])
    p.op('pool', lambda: nc.gpsimd.affine_select(out=trif[:], in_=trif[:], pattern=[[1, 128]], compare_op=ALU.is_gt,
                                                 fill=0.0, base=0, channel_multiplier=-1), reads=[trif], writes=[trif])
    p.op('dve', lambda: nc.vector.tensor_copy(out=tri[:], in_=trif[:]), reads=[trif], writes=[tri])
    gts = p.sb("gts_s", [128, NT, EPC], F32)
    b1c = p.sb("b1c_s", [128, EPC, 16], F32)
    p.dma('sp', gts[:], gts_d[:], reads=[gts_d], writes=[gts])
    p.dma('sp', b1c[:], b1_d[:], reads=[b1_d], writes=[b1c])
    b2b = p.sb("b2b", [1, EPC, D], BF16)
    rank_si = p.sb("rank_si", [128, NT * EPC], I32)
    rank_gi = p.sb("rank_gi", [128, NT * EPC], I32)
    phW = Phase(p)
    w1b = phW.sb("w1b", [128, KC, D], BF16)
    w2b = phW.sb("w2b", [128, 8, D], BF16)
    p.dma('pool', b2b[:], b2_d.t.ap().unsqueeze(0) if hasattr(b2_d.t.ap(), "unsqueeze") else b2_d[:, :], reads=[b2_d],
          writes=[b2b])

    def load_w(e):
        for h in range(4):
            p.dma('pool', w1b[:, h * 4:(h + 1) * 4, :], w1_d[e, h * 512:(h + 1) * 512, :].rearrange("(k p) n -> p k n", p=128),
                  reads=[w1_d], writes=[w1b])
        for h in range(2):
            p.dma('pool', w2b[:, h * 4:(h + 1) * 4, :], w2_d[e, h * 512:(h + 1) * 512, :].rearrange("(k p) n -> p k n", p=128),
                  reads=[w2_d], writes=[w2b])
    load_w(0)

    NF = NT * EPC
    phR = Phase(p)
    maskf = phR.sb("maskf", [128, NF], F32)
    maskb = phR.sb("maskb", [128, NF], BF16)
    gflat = gts[:].rearrange("p t e -> p (t e)")
    p.op('dve', lambda: nc.vector.tensor_scalar(out=maskf[:], in0=gflat, scalar1=0.0, scalar2=None, op0=ALU.is_gt),
         reads=[gts], writes=[maskf])
    p.op('dve', lambda: nc.vector.tensor_copy(out=maskb[:], in_=maskf[:]), reads=[maskf], writes=[maskb])
    accW, accT = p.accs[0], p.accs[1]
    p.op('pe', lambda: nc.tensor.matmul(accW[:, 0:NF], lhsT=tri[:], rhs=maskb[:], start=True, stop=True),
         reads=[tri, maskb], writes=[accW])
    p.op('pe', lambda: nc.tensor.matmul(accT[:, 0:NF], lhsT=ones[:], rhs=maskb[:], start=True, stop=True),
         reads=[ones, maskb], writes=[accT])
    sa = phR.sb("scan_a", [128, NF], F32)
    sbb = phR.sb("scan_b", [128, NF], F32)
    tot = phR.sb("tot", [128, NF], F32)
    rank = phR.sb("rank", [128, NF], F32)
    p.op('dve', lambda: nc.vector.tensor_copy(out=tot[:], in_=accT[:, 0:NF]), reads=[accT], writes=[tot])
    p.op('act', lambda: nc.scalar.copy(out=sa[:], in_=accT[:, 0:NF]), reads=[accT], writes=[sa])
    cur, nxt = sa, sbb
    s = 1
    while s < NT:
        w = s * EPC
        p.op('dve', lambda: nc.vector.tensor_copy(out=nxt[:, 0:w], in_=cur[:, 0:w]), reads=[cur], writes=[nxt])
        p.op('dve', lambda: nc.vector.tensor_tensor(out=nxt[:, w:NF], in0=cur[:, w:NF], in1=cur[:, 0:NF - w], op=ALU.add),
             reads=[cur], writes=[nxt])
        cur, nxt = nxt, cur
        s *= 2
    incl = cur
    p.op('dve', lambda: nc.vector.tensor_tensor(out=rank[:], in0=accW[:, 0:NF], in1=incl[:], op=ALU.add),
         reads=[accW, incl], writes=[rank])
    p.op('dve', lambda: nc.vector.tensor_tensor(out=rank[:], in0=rank[:], in1=tot[:], op=ALU.subtract),
         reads=[rank, tot], writes=[rank])
    p.dma('sp', cnt_o[:, :], incl[0:1, NF - EPC:NF], reads=[incl], writes=[cnt_o])
    p.op('dve', lambda: nc.vector.tensor_scalar(out=maskf[:], in0=maskf[:], scalar1=-BIG, scalar2=BIG, op0=ALU.mult,
                                                op1=ALU.add), reads=[maskf], writes=[maskf])
    p.op('dve', lambda: nc.vector.tensor_tensor(out=rank[:], in0=rank[:], in1=maskf[:], op=ALU.add),
         reads=[rank, maskf], writes=[rank])
    p.op('dve', lambda: nc.vector.tensor_copy(out=rank_si[:], in_=rank[:]), reads=[rank], writes=[rank_si])
    p.op('dve', lambda: nc.vector.tensor_scalar(out=rank[:], in0=rank[:], scalar1=float(CAPR - 1), scalar2=None,
                                                op0=ALU.min), reads=[rank], writes=[rank])
    p.op('dve', lambda: nc.vector.tensor_copy(out=rank_gi[:], in_=rank[:]), reads=[rank], writes=[rank_gi])

    p.wait_all('sp', [cnt_o])
    phR.close()
    phD = Phase(p)
    zt = phD.sb("zt", [128, D], BF16)
    p.op('dve', lambda: nc.vector.memset(zt[:], 0.0), writes=[zt])
    for e in range(EPC):
        for r in range(0, CAPR, 128 * 8):
            n8 = min(8, (CAPR - r) // 128)
            p.dma('sp', Xg[e][r:r + n8 * 128, :].rearrange("(a p) d -> p a d", p=128),
                  zt[:].unsqueeze(1).broadcast_to([128, n8, D]), reads=[zt], writes=[Xg[e]])
        Xg[e].wbase = Xg[e].w
    tts = phD.scratch("tt", [128, D], BF16, 4)
    for t in range(NT):
        tt = tts.get()
        p.dma('sp', tt[:], t_d[t * 128:(t + 1) * 128, :], reads=[t_d], writes=[tt])
        for e in range(EPC):
            col = t * EPC + e
            p_idma(p, Xg[e][:, :], bass.IndirectOffsetOnAxis(ap=rank_si[:, col:col + 1], axis=0), tt[:, :], None,
                   CAPR - 1, [tt, rank_si], Xg[e])

    phD.close()
    phE = Phase(p)
    xgs = phE.scratch("xgt", [128, D], BF16, 3)
    XTs = phE.scratch("XT", [128, KC, 512], BF16, 2)
    actTs = phE.scratch("actT", [128, 8, 512], BF16, 2)
    g32s = phE.scratch("g32", [128, 512], F32, 2)
    sgs = phE.scratch("sg", [128, 512], F32, 2)
    l32s = phE.scratch("l32", [128, 512], F32, 2)
    yos = phE.scratch("yo", [128, D], BF16, 2)
    for e in range(EPC):
        if e > 0:
            load_w(e)
        for ch in range(capb // 4):
            XT, actT = XTs.get(), actTs.get()
            for ti in range(4):
                r0 = (ch * 4 + ti) * 128
                xg = xgs.get()
                p.dma('sp', xg[:], Xg[e][r0:r0 + 128, :], reads=[Xg[e]], writes=[xg])
                for k8 in range(2):
                    tp = p.tp()

                    def tr():
                        for j in range(8):
                            k = k8 * 8 + j
                            i = nc.tensor.transpose(out=tp[:, j * 128:(j + 1) * 128], in_=xg[:, k * 128:(k + 1) * 128],
                                                    identity=p.ident[:])
                        return i
                    p.op('pe', tr, reads=[xg, p.ident], writes=[tp])
                    dst = XT[:, k8 * 8:(k8 + 1) * 8, ti * 128:(ti + 1) * 128]
                    src = tp[:].rearrange("p (k t) -> p k t", t=128)
                    if k8 == 0:
                        p.op('act', lambda: nc.scalar.copy(out=dst, in_=src), reads=[tp], writes=[XT])
                    else:
                        p.op('dve', lambda: nc.vector.tensor_copy(out=dst, in_=src), reads=[tp], writes=[XT])
            for jj in range(8):
                accG, accL = p.acc(), p.acc()
                for acc, off in ((accG, 0), (accL, 1024)):
                    def mm():
                        for k in range(KC):
                            i = nc.tensor.matmul(acc[:], lhsT=w1b[:, k, off + jj * 128:off + (jj + 1) * 128], rhs=XT[:, k, :],
                                                 start=(k == 0), stop=(k == KC - 1))
                        return i
                    p.op('pe', mm, reads=[w1b, XT], writes=[acc])
                g32, sg, l32 = g32s.get(), sgs.get(), l32s.get()
                p.op('dve', lambda: nc.vector.tensor_scalar(out=g32[:], in0=accG[:], scalar1=b1c[:, e, jj:jj + 1],
                                                            scalar2=LIMIT, op0=ALU.add, op1=ALU.min),
                     reads=[accG, b1c], writes=[g32])
                p.op('act', lambda: nc.scalar.activation(out=sg[:], in_=g32[:], func=AF.Sigmoid, scale=ALPHA),
                     reads=[g32], writes=[sg])
                p.op('dve', lambda: nc.vector.tensor_scalar(out=l32[:], in0=accL[:], scalar1=b1c[:, e, 8 + jj:8 + jj + 1],
                                                            scalar2=LIMIT, op0=ALU.add, op1=ALU.min),
                     reads=[accL, b1c], writes=[l32])
                p.op('dve', lambda: nc.vector.tensor_scalar(out=l32[:], in0=l32[:], scalar1=-LIMIT, scalar2=1.0,
                                                            op0=ALU.max, op1=ALU.add), reads=[l32], writes=[l32])
                p.op('dve', lambda: nc.vector.tensor_tensor(out=g32[:], in0=g32[:], in1=sg[:], op=ALU.mult),
                     reads=[g32, sg], writes=[g32])
                p.op('dve', lambda: nc.vector.tensor_tensor(out=actT[:, jj, :], in0=g32[:], in1=l32[:], op=ALU.mult),
                     reads=[g32, l32], writes=[actT])
            for ti in range(4):
                r0 = (ch * 4 + ti) * 128
                yo = yos.get()
                for c in range(4):
                    acc = p.acc()

                    def mm2():
                        nc.tensor.matmul(acc[:], lhsT=ones1[0:1, :], rhs=b2b[0:1, e, c * 512:(c + 1) * 512], start=True,
                                         stop=False)
                        for j in range(8):
                            i = nc.tensor.matmul(acc[:], lhsT=actT[:, j, ti * 128:(ti + 1) * 128],
                                                 rhs=w2b[:, j, c * 512:(c + 1) * 512], start=False, stop=(j == 7))
                        return i
                    p.op('pe', mm2, reads=[actT, w2b, b2b, ones1], writes=[acc])
                    p.op('act', lambda: nc.scalar.copy(out=yo[:, c * 512:(c + 1) * 512], in_=acc[:]), reads=[acc],
                         writes=[yo])
                p.dma('sp', Yg[e][r0:r0 + 128, :], yo[:], reads=[yo], writes=[Yg[e]])

    phE.close()
    phW.close()
    phC = Phase(p)
    ygs = phC.scratch("yg", [128, D], BF16, 8)
    caccs = phC.scratch("cacc", [128, D], F32, 2)
    obs = phC.scratch("ob", [128, D], BF16, 2)
    for t in range(NT):
        eng = 'dve'
        ve = nc.vector if eng == 'dve' else nc.gpsimd
        ca, ob = caccs.get(), obs.get()
        for e in range(EPC):
            col = t * EPC + e
            yg = ygs.get()
            p_idma(p, yg[:, :], None, Yg[e][:, :], bass.IndirectOffsetOnAxis(ap=rank_gi[:, col:col + 1], axis=0),
                   CAPR - 1, [Yg[e], rank_gi], yg, concurrent=False)
            gcol = gts[:, t, e:e + 1]
            if e == 0:
                p.op(eng, lambda: ve.tensor_scalar(out=ca[:], in0=yg[:], scalar1=gcol, scalar2=None, op0=ALU.mult),
                     reads=[yg, gts], writes=[ca])
            elif e < EPC - 1:
                p.op(eng, lambda: ve.scalar_tensor_tensor(out=ca[:], in0=yg[:], scalar=gcol, in1=ca[:], op0=ALU.mult,
                                                          op1=ALU.add), reads=[yg, gts, ca], writes=[ca])
            else:
                p.op(eng, lambda: ve.scalar_tensor_tensor(out=ob[:], in0=yg[:], scalar=gcol, in1=ca[:], op0=ALU.mult,
                                                          op1=ALU.add), reads=[yg, gts, ca], writes=[ob])
        p.dma('sp', yp_o[t * 128:(t + 1) * 128, :], ob[:], reads=[ob], writes=[yp_o])
    p.wait_all('sp', [yp_o, cnt_o])
    phC.close()
    return nc
```

```python
import numpy as np
import ml_dtypes
import concourse.bass as bass
import concourse.mybir as mybir
from concourse.bass_utils import run_bass_kernel_spmd

F32 = mybir.dt.float32
BF16 = mybir.dt.bfloat16
AF = mybir.ActivationFunctionType
ALU = mybir.AluOpType
AX = mybir.AxisListType
NPBF = ml_dtypes.bfloat16

D = 2048
KC = 16
EPS = 1e-6
NCORES = 8


class Buf:
    def __init__(self, t, name):
        self.t = t
        self.name = name
        self.w = None
        self.r = []
        self.dsem = None
        self.dcnt = 0

    def __getitem__(self, idx):
        return self.t[idx]


class P:
    def __init__(self, nc):
        self.nc = nc
        self.eng = {'pe': nc.tensor, 'act': nc.scalar, 'dve': nc.vector, 'pool': nc.gpsimd, 'sp': nc.sync}
        self.sems = {}
        self.cnt = {}
        for k in self.eng:
            self.sems[k] = nc.alloc_semaphore('s_' + k)
            self.cnt[k] = 0
        self.seen = {k: {} for k in self.eng}
        self.acc_i = 0
        self.tp_i = 0
        self.dma_cnt = {}

    def sb(self, name, shape, dt):
        return Buf(self.nc.alloc_sbuf_tensor(name, list(shape), dt), name)

    def ps(self, name, shape, dt=F32):
        return Buf(self.nc.alloc_psum_tensor(name, list(shape), dt), name)

    def dram(self, name, shape, dt, kind="Internal"):
        return Buf(self.nc.dram_tensor(name, list(shape), dt, kind=kind), name)

    def _wait(self, e, ev):
        key, val = ev
        if self.seen[e].get(key, 0) >= val:
            return
        self.seen[e][key] = val
        self.eng[e].wait_ge(self.sems[key], val)

    def _deps(self, e, reads, writes):
        for b in reads:
            if b.w is not None:
                self._wait(e, b.w)
        for b in writes:
            if b.w is not None:
                self._wait(e, b.w)
            for ev in b.r:
                self._wait(e, ev)

    def _commit(self, ev, reads, writes):
        for b in reads:
            b.r.append(ev)
            if len(b.r) > 48:
                best = {}
                for k, v in b.r:
                    best[k] = max(best.get(k, 0), v)
                b.r = list(best.items())
        for b in writes:
            b.w = ev
            b.r = []

    def op(self, e, fn, reads=(), writes=()):
        self._deps(e, reads, writes)
        inst = fn()
        self.cnt[e] += 1
        inst.then_inc(self.sems[e], 1)
        self._commit((e, self.cnt[e]), reads, writes)
        return inst

    def dma(self, q, out_ap, in_ap, reads=(), writes=(), **kw):
        self._deps(q, reads, writes)
        dst = writes[0]
        if dst.dsem is None:
            key = 'd_' + dst.name
            self.sems[key] = self.nc.alloc_semaphore(key)
            dst.dsem = key
        inst = self.eng[q].dma_start(out=out_ap, in_=in_ap, **kw)
        dst.dcnt += 16
        self.dma_cnt[dst.dsem] = dst.dcnt
        inst.then_inc(self.sems[dst.dsem], 16)
        self._commit((dst.dsem, dst.dcnt), reads, writes)
        return inst

    def wait_all(self, e, bufs):
        for b in bufs:
            if b.w is not None:
                self._wait(e, b.w)

    def setup_common(self):
        nc = self.nc
        self.accs = [self.ps(f"acc{i}", [128, 512], F32) for i in range(5)]
        self.tps = [self.ps(f"tpb{i}", [128, 1024], BF16) for i in range(3)]
        identf = self.sb("identf", [128, 128], F32)
        ident = self.sb("ident", [128, 128], BF16)
        self.op('pool', lambda: nc.gpsimd.memset(identf[:], 0.0), writes=[identf])
        self.op('pool', lambda: nc.gpsimd.affine_select(out=identf[:], in_=identf[:], pattern=[[-1, 128]],
                                                        compare_op=ALU.not_equal, fill=1.0, base=0,
                                                        channel_multiplier=1), reads=[identf], writes=[identf])
        self.op('dve', lambda: nc.vector.tensor_copy(out=ident[:], in_=identf[:]), reads=[identf], writes=[ident])
        self.ident = ident
        self.identf = identf

    def acc(self):
        b = self.accs[self.acc_i % len(self.accs)]
        self.acc_i += 1
        return b

    def tp(self):
        b = self.tps[self.tp_i % len(self.tps)]
        self.tp_i += 1
        return b


def new_prog():
    nc = bass.Bass("TRN2", target_bir_lowering=False)
    return nc, P(nc)


class Scratch:
    def __init__(self, p, name, shape, dt, n=2):
        self.bufs = [p.sb(f"{name}{i}", shape, dt) for i in range(n)]
        self.i = 0

    def get(self):
        b = self.bufs[self.i % len(self.bufs)]
        self.i += 1
        return b


def emit_rstd(p, ss, rows, scale_in, tmp=None):
    nc = p.nc
    p.op('dve', lambda: nc.vector.tensor_scalar(out=ss, in0=ss, scalar1=float(scale_in), scalar2=EPS,
                                                op0=ALU.mult, op1=ALU.add), reads=[tmp], writes=[tmp])
    p.op('act', lambda: nc.scalar.sqrt(out=ss, in_=ss), reads=[tmp], writes=[tmp])
    p.op('dve', lambda: nc.vector.reciprocal(out=ss, in_=ss), reads=[tmp], writes=[tmp])


class NormT:
    def __init__(self, p):
        self.p = p
        self.sq = Scratch(p, "nt_sq", [128, D], BF16, 2)
        self.ss = Scratch(p, "nt_ss", [128, 1], F32, 2)
        self.xn = Scratch(p, "nt_xn", [128, D], BF16, 2)

    def emit(self, xt, rows, Acol, Bcol, hT, c0):
        p, nc = self.p, self.p.nc
        sq, ss, xn = self.sq.get(), self.ss.get(), self.xn.get()
        p.op('act', lambda: nc.scalar.activation(out=sq[0:rows, :], in_=xt[0:rows, :], func=AF.Square,
                                                 accum_out=ss[0:rows, :]), reads=[xt], writes=[sq, ss])
        emit_rstd(p, ss[0:rows, :], rows, 1.0 / D, ss)
        p.op('dve', lambda: nc.vector.tensor_scalar(out=xn[0:rows, :], in0=xt[0:rows, :], scalar1=ss[0:rows, 0:1],
                                                    scalar2=None, op0=ALU.mult), reads=[xt, ss], writes=[xn])
        for k4 in range(2):
            tp = p.tp()

            def tr():
                for j in range(8):
                    k = k4 * 8 + j
                    i = nc.tensor.transpose(out=tp[:, j * 128:j * 128 + rows], in_=xn[0:rows, k * 128:(k + 1) * 128],
                                            identity=p.ident[0:rows, 0:rows])
                return i
            p.op('pe', tr, reads=[xn, p.ident], writes=[tp])
            for j in range(8):
                k = k4 * 8 + j
                if j % 2 == 0:
                    p.op('dve', lambda: nc.vector.tensor_scalar(
                        out=hT[:, k, c0:c0 + rows], in0=tp[:, j * 128:j * 128 + rows], scalar1=Acol[:, k:k + 1],
                        scalar2=Bcol[:, k:k + 1], op0=ALU.mult, op1=ALU.add), reads=[tp, Acol, Bcol], writes=[hT])
                else:
                    p.op('act', lambda: nc.scalar.activation(
                        out=hT[:, k, c0:c0 + rows], in_=tp[:, j * 128:j * 128 + rows], func=AF.Identity,
                        scale=Acol[:, k:k + 1], bias=Bcol[:, k:k + 1]), reads=[tp, Acol, Bcol], writes=[hT])


def emit_Acol(p, A, g, sc):
    nc = p.nc
    p.op('dve', lambda: nc.vector.scalar_tensor_tensor(out=A[:], in0=sc[:], scalar=1.0, in1=g[:], op0=ALU.add,
                                                       op1=ALU.mult), reads=[sc, g], writes=[A])


MODW = 12288 // NCORES


def build_p1():
    nc, p = new_prog()
    wmod = p.dram("wmod", [2, D, MODW], F32, "ExternalInput")
    bmod = p.dram("bmod", [2, 3, MODW], F32, "ExternalInput")
    cT = p.dram("cT", [128, KC, 3], F32, "ExternalInput")
    mout = p.dram("m", [2, 3, MODW], F32, "ExternalOutput")
    p.setup_common()
    cs = p.sb("cs", [128, KC, 3], F32)
    sT = p.sb("sT", [128, KC, 3], BF16)
    p.dma('sp', cs[:], cT[:], reads=[cT], writes=[cs])
    p.op('act', lambda: nc.scalar.activation(out=sT[:], in_=cs[:], func=AF.Silu), reads=[cs], writes=[sT])
    for l in range(2):
        wb = p.sb(f"wb{l}", [128, KC, MODW], BF16)
        bb = p.sb(f"bb{l}", [3, MODW], F32)
        mo = p.sb(f"mo{l}", [3, MODW], F32)
        for h in range(2):
            p.dma('pool', wb[:, h * 8:(h + 1) * 8, :],
                  wmod[l, h * 1024:(h + 1) * 1024, :].rearrange("(k p) n -> p k n", p=128), reads=[wmod], writes=[wb])
        p.dma('sp', bb[:], bmod[l], reads=[bmod], writes=[bb])
        for c in range(MODW // 512):
            acc = p.acc()

            def mm():
                for k in range(KC):
                    i = nc.tensor.matmul(acc[0:3, :], lhsT=sT[:, k, :], rhs=wb[:, k, c * 512:(c + 1) * 512],
                                         start=(k == 0), stop=(k == KC - 1))
                return i
            p.op('pe', mm, reads=[sT, wb], writes=[acc])
            p.op('dve', lambda: nc.vector.tensor_tensor(out=mo[:, c * 512:(c + 1) * 512], in0=acc[0:3, :],
                                                        in1=bb[:, c * 512:(c + 1) * 512], op=ALU.add),
                 reads=[acc, bb], writes=[mo])
        p.dma('sp', mout[l], mo[:], reads=[mo], writes=[mout])
    p.wait_all('sp', [mout])
    return nc


TPC = 1024
CPC = 64


def emit_qk_post(p, ps, rows, nh, gain_b, rope_t, out_ap, out_buf, S):
    nc = p.nc
    n = nh * 128
    sq, ss4, qn = S['sq'].get(), S['ss4'].get(), S['qn'].get()
    p.op('act', lambda: nc.scalar.activation(out=sq[0:rows, 0:n], in_=ps[0:rows, 0:n], func=AF.Square),
         reads=[ps], writes=[sq])
    p.op('dve', lambda: nc.vector.tensor_reduce(out=ss4[0:rows, 0:nh],
                                                in_=sq[0:rows, 0:n].rearrange("p (h d) -> p h d", d=128),
                                                axis=AX.X, op=ALU.add), reads=[sq], writes=[ss4])
    emit_rstd(p, ss4[0:rows, 0:nh], rows, 1.0 / 128, ss4)
    p.op('dve', lambda: nc.vector.tensor_tensor(
        out=qn[0:rows, 0:n].rearrange("p (h d) -> p h d", d=128),
        in0=ps[0:rows, 0:n].rearrange("p (h d) -> p h d", d=128),
        in1=ss4[0:rows, 0:nh].unsqueeze(2).broadcast_to([rows, nh, 128]), op=ALU.mult),
        reads=[ps, ss4], writes=[qn])
    if rope_t is None:
        p.op('dve', lambda: nc.vector.tensor_tensor(
            out=out_ap.rearrange("p (h d) -> p h d", d=128),
            in0=qn[0:rows, 0:n].rearrange("p (h d) -> p h d", d=128),
            in1=gain_b[0:rows, :].unsqueeze(1).broadcast_to([rows, nh, 128]), op=ALU.mult),
            reads=[qn, gain_b], writes=[out_buf])
        return
    p.op('pool', lambda: nc.gpsimd.tensor_tensor(
        out=qn[0:rows, 0:n].rearrange("p (h d) -> p h d", d=128),
        in0=qn[0:rows, 0:n].rearrange("p (h d) -> p h d", d=128),
        in1=gain_b[0:rows, :].unsqueeze(1).broadcast_to([rows, nh, 128]), op=ALU.mult),
        reads=[qn, gain_b], writes=[qn])
    t1, t2 = S['t1'].get(), S['t2'].get()
    p.op('pool', lambda: nc.gpsimd.tensor_tensor(
        out=t1[0:rows, 0:n].rearrange("p (h d) -> p h d", d=128),
        in0=qn[0:rows, 0:n].rearrange("p (h d) -> p h d", d=128),
        in1=rope_t[0:rows, 0, :].unsqueeze(1).broadcast_to([rows, nh, 128]), op=ALU.mult),
        reads=[qn, rope_t], writes=[t1])
    for b in range(2):
        p.op('dve', lambda: nc.vector.tensor_tensor(
            out=t2[0:rows, 0:n].rearrange("p (h a b d) -> p h a b d", a=2, b=2, d=32)[:, :, :, b, :],
            in0=qn[0:rows, 0:n].rearrange("p (h a b d) -> p h a b d", a=2, b=2, d=32)[:, :, :, 1 - b, :],
            in1=rope_t[0:rows, 1, :].rearrange("p (a b d) -> p a b d", a=2, b=2, d=32)[:, :, b, :]
            .unsqueeze(1).broadcast_to([rows, nh, 2, 32]), op=ALU.mult),
            reads=[qn, rope_t], writes=[t2])
    p.op('dve', lambda: nc.vector.tensor_tensor(out=out_ap, in0=t1[0:rows, 0:n], in1=t2[0:rows, 0:n], op=ALU.add),
         reads=[t1, t2], writes=[out_buf])


def build_p2():
    nc, p = new_prog()
    x = p.dram("x", [TPC, D], F32, "ExternalInput")
    ctxr = p.dram("ctxr", [CPC, D], F32, "ExternalInput")
    cols = p.dram("cols", [128, 5, KC], F32, "ExternalInput")
    w_in = p.dram("w_in", [D, 2560], F32, "ExternalInput")
    gains = p.dram("gains", [2, 128], F32, "ExternalInput")
    rope = p.dram("rope", [TPC, 2, 128], F32, "ExternalInput")
    out = p.dram("qkvf", [TPC, 2560], BF16, "ExternalOutput")
    outc = p.dram("kvc", [CPC, 512], BF16, "ExternalOutput")
    p.setup_common()
    colsb = p.sb("colsb", [128, 5, KC], F32)
    p.dma('sp', colsb[:], cols[:], reads=[cols], writes=[colsb])
    qg = p.sb("qg", [128, 128], F32)
    kg = p.sb("kg", [128, 128], F32)
    p.dma('sp', qg[:], gains[0:1, :].partition_broadcast(128), reads=[gains], writes=[qg])
    p.dma('sp', kg[:], gains[1:2, :].partition_broadcast(128), reads=[gains], writes=[kg])
    A1 = p.sb("A1", [128, KC], F32)
    Ac = p.sb("Ac", [128, KC], F32)
    B1 = p.sb("B1", [128, KC], F32)
    Bc = p.sb("Bc", [128, KC], F32)
    p.op('dve', lambda: nc.vector.scalar_tensor_tensor(out=A1[:], in0=colsb[:, 2, :], scalar=1.0, in1=colsb[:, 0, :],
                                                       op0=ALU.add, op1=ALU.mult), reads=[colsb], writes=[A1])
    p.op('dve', lambda: nc.vector.scalar_tensor_tensor(out=Ac[:], in0=colsb[:, 4, :], scalar=1.0, in1=colsb[:, 0, :],
                                                       op0=ALU.add, op1=ALU.mult), reads=[colsb], writes=[Ac])
    p.op('dve', lambda: nc.vector.tensor_copy(out=B1[:], in_=colsb[:, 1, :]), reads=[colsb], writes=[B1])
    p.op('dve', lambda: nc.vector.tensor_copy(out=Bc[:], in_=colsb[:, 3, :]), reads=[colsb], writes=[Bc])
    wb = p.sb("wb", [128, KC, 2560], BF16)
    for h in range(4):
        p.dma('pool', wb[:, h * 4:(h + 1) * 4, :], w_in[h * 512:(h + 1) * 512, :].rearrange("(k p) n -> p k n", p=128),
              reads=[w_in], writes=[wb])
    nt = NormT(p)
    xs = Scratch(p, "xs", [128, D], F32, 2)
    hTs = Scratch(p, "hT", [128, KC, 128], BF16, 2)
    rts = Scratch(p, "rt", [128, 2, 128], F32, 2)
    obs = Scratch(p, "ob", [128, 2560], BF16, 2)
    S = {'sq': Scratch(p, "qsq", [128, 512], F32, 2), 'ss4': Scratch(p, "qss", [128, 4], F32, 2),
         'qn': Scratch(p, "qn", [128, 512], F32, 2), 't1': Scratch(p, "qt1", [128, 512], F32, 2),
         't2': Scratch(p, "qt2", [128, 512], F32, 2)}
    ntiles = TPC // 128
    for t in range(ntiles + 1):
        is_ctx = (t == ntiles)
        rows = CPC if is_ctx else 128
        xt, hT, ob = xs.get(), hTs.get(), obs.get()
        if is_ctx:
            p.dma('sp', xt[0:rows, :], ctxr[:, :], reads=[ctxr], writes=[xt])
            rt = None
        else:
            p.dma('sp', xt[:], x[t * 128:(t + 1) * 128, :], reads=[x], writes=[xt])
            rt = rts.get()
            p.dma('sp', rt[:], rope[t * 128:(t + 1) * 128, :, :], reads=[rope], writes=[rt])
        nt.emit(xt, rows, Ac if is_ctx else A1, Bc if is_ctx else B1, hT, 0)
        for c in ([2] if is_ctx else range(5)):
            acc = p.acc()

            def mm():
                for k in range(KC):
                    i = nc.tensor.matmul(acc[0:rows, :], lhsT=hT[:, k, 0:rows], rhs=wb[:, k, c * 512:(c + 1) * 512],
                                         start=(k == 0), stop=(k == KC - 1))
                return i
            p.op('pe', mm, reads=[hT, wb], writes=[acc])
            if c < 2:
                emit_qk_post(p, acc, rows, 4, qg, rt, ob[0:rows, c * 512:(c + 1) * 512], ob, S)
            elif c == 2:
                emit_qk_post(p, acc, rows, 2, kg, rt, ob[0:rows, 1024:1280], ob, S)
                p.op('act', lambda: nc.scalar.copy(out=ob[0:rows, 1280:1536], in_=acc[0:rows, 256:512]),
                     reads=[acc], writes=[ob])
            else:
                p.op('act', lambda: nc.scalar.copy(out=ob[0:rows, c * 512:(c + 1) * 512], in_=acc[0:rows, :]),
                     reads=[acc], writes=[ob])
        if is_ctx:
            p.dma('sp', outc[:, :], ob[0:rows, 1024:1536], reads=[ob], writes=[outc])
        else:
            p.dma('sp', out[t * 128:(t + 1) * 128, :], ob[:], reads=[ob], writes=[out])
    p.wait_all('sp', [out, outc])
    return nc


def p_barrier(p):
    targets = {}
    for k in p.eng:
        targets[k] = p.cnt[k]
    for k, v in p.sems.items():
        if k.startswith('d_'):
            targets[k] = p.dma_cnt.get(k, 0)
    for e in p.eng:
        for k, v in targets.items():
            if v > 0 and k != e:
                p._wait(e, (k, v))


class Phase:
    def __init__(self, p):
        from contextlib import ExitStack
        self.p = p
        self.st = ExitStack()

    def sb(self, name, shape, dt):
        t = self.st.enter_context(self.p.nc.sbuf_tensor(name, list(shape), dt))
        return Buf(t, name)

    def scratch(self, name, shape, dt, n=2):
        s = Scratch.__new__(Scratch)
        s.bufs = [self.sb(f"{name}{i}", shape, dt) for i in range(n)]
        s.i = 0
        return s

    def close(self):
        p_barrier(self.p)
        self.st.close()


class Prep:
    def __init__(self, p, ph, A2b, B2b, wr, brb, tpf):
        self.p = p
        self.A2b, self.B2b, self.wr, self.brb, self.tpf = A2b, B2b, wr, brb, tpf
        self.sq = ph.sb("pp_sq", [128, D], BF16)
        self.ss = ph.sb("pp_ss", [128, 1], F32)
        self.t32 = ph.sb("pp_t32", [128, D], F32)
        self.tb = ph.scratch("pp_tb", [128, D], BF16, 2)
        self.tT = ph.sb("pp_tT", [128, KC, 128], F32)
        self.lg = ph.scratch("pp_lg", [128, 32], F32, 2)
        self.t8 = ph.sb("pp_t8", [128, 8], F32)
        self.nmx = ph.sb("pp_nmx", [128, 1], F32)
        self.msk = ph.sb("pp_msk", [128, 32], F32)
        self.ex = ph.sb("pp_ex", [128, 32], F32)
        self.sm = ph.sb("pp_sm", [128, 1], F32)

    def emit(self, x1, t_out_ap, t_out, g_out_ap, g_out):
        p, nc = self.p, self.p.nc
        sq, ss, t32, tT = self.sq, self.ss, self.t32, self.tT
        tb, lg = self.tb.get(), self.lg.get()
        p.op('act', lambda: nc.scalar.activation(out=sq[:], in_=x1[:], func=AF.Square, accum_out=ss[:]),
             reads=[x1], writes=[sq, ss])
        emit_rstd(p, ss[:], 128, 1.0 / D, ss)
        p.op('dve', lambda: nc.vector.scalar_tensor_tensor(out=t32[:], in0=x1[:], scalar=ss[:, 0:1], in1=self.A2b[:],
                                                           op0=ALU.mult, op1=ALU.mult), reads=[x1, ss, self.A2b],
             writes=[t32])
        p.op('pool', lambda: nc.gpsimd.tensor_tensor(out=t32[:], in0=t32[:], in1=self.B2b[:], op=ALU.add),
             reads=[t32, self.B2b], writes=[t32])
        p.op('act', lambda: nc.scalar.copy(out=tb[:], in_=t32[:]), reads=[t32], writes=[tb])
        p.dma('sp', t_out_ap, tb[:], reads=[tb], writes=[t_out])
        for k4 in range(4):
            tpf = self.tpf

            def tr():
                for j in range(4):
                    k = k4 * 4 + j
                    i = nc.tensor.transpose(out=tpf[:, j * 128:(j + 1) * 128], in_=t32[:, k * 128:(k + 1) * 128],
                                            identity=p.identf[:])
                return i
            p.op('pe', tr, reads=[t32, p.identf], writes=[tpf])
            eng = 'dve' if k4 % 2 == 0 else 'act'
            if eng == 'dve':
                p.op('dve', lambda: nc.vector.tensor_copy(
                    out=tT[:, k4 * 4:(k4 + 1) * 4, :].rearrange("p k t -> p (k t)"), in_=tpf[:]),
                    reads=[tpf], writes=[tT])
            else:
                p.op('act', lambda: nc.scalar.copy(
                    out=tT[:, k4 * 4:(k4 + 1) * 4, :].rearrange("p k t -> p (k t)"), in_=tpf[:]),
                    reads=[tpf], writes=[tT])
        acc = p.acc()

        def mm():
            for k in range(KC):
                i = nc.tensor.matmul(acc[:, 0:32], lhsT=tT[:, k, :], rhs=self.wr[:, k, :], start=(k == 0),
                                     stop=(k == KC - 1))
            return i
        p.op('pe', mm, reads=[tT, self.wr], writes=[acc])
        p.op('dve', lambda: nc.vector.tensor_tensor(out=lg[:], in0=acc[:, 0:32], in1=self.brb[:], op=ALU.add),
             reads=[acc, self.brb], writes=[lg])
        emit_gates(p, lg, self.t8, self.nmx, self.msk, self.ex, self.sm)
        p.dma('sp', g_out_ap, lg[:], reads=[lg], writes=[g_out])


def emit_gates(p, lg, t8, nmx, msk, ex, sm):
    nc = p.nc
    p.op('dve', lambda: nc.vector.max(out=t8[:], in_=lg[:]), reads=[lg], writes=[t8])
    p.op('dve', lambda: nc.vector.tensor_scalar(out=msk[:], in0=lg[:], scalar1=t8[:, 3:4], scalar2=None,
                                                op0=ALU.is_ge), reads=[lg, t8], writes=[msk])
    p.op('dve', lambda: nc.vector.tensor_scalar(out=nmx[:], in0=t8[:, 0:1], scalar1=-1.0, scalar2=None,
                                                op0=ALU.mult), reads=[t8], writes=[nmx])
    p.op('act', lambda: nc.scalar.activation(out=ex[:], in_=lg[:], func=AF.Exp, bias=nmx[:, 0:1], scale=1.0),
         reads=[lg, nmx], writes=[ex])
    p.op('dve', lambda: nc.vector.tensor_tensor(out=ex[:], in0=ex[:], in1=msk[:], op=ALU.mult),
         reads=[ex, msk], writes=[ex])
    p.op('dve', lambda: nc.vector.reduce_sum(out=sm[:], in_=ex[:], axis=AX.X), reads=[ex], writes=[sm])
    p.op('dve', lambda: nc.vector.reciprocal(out=sm[:], in_=sm[:]), reads=[sm], writes=[sm])
    p.op('dve', lambda: nc.vector.tensor_scalar(out=lg[:], in0=ex[:], scalar1=sm[:, 0:1], scalar2=None,
                                                op0=ALU.mult), reads=[ex, sm], writes=[lg])


def load_rows_b(p, ph, rows_d, names, off=0):
    out = {}
    for i, nm in enumerate(names):
        b = ph.sb("rb_" + nm, [128, D], F32)
        p.dma('sp', b[:], rows_d[off + i:off + i + 1, :].partition_broadcast(128), reads=[rows_d], writes=[b])
        out[nm] = b
    return out


NKEY = 4096 + 256
NKT = NKEY // 128


def build_p3():
    nc, p = new_prog()
    x = p.dram("x", [TPC, D], F32, "ExternalInput")
    qT_d = p.dram("qT", [128, 8, TPC], BF16, "ExternalInput")
    kT_d = p.dram("kT", [128, 2, NKEY], BF16, "ExternalInput")
    v_d = p.dram("v", [NKEY, 256], BF16, "ExternalInput")
    f_d = p.dram("f", [4096, 1024], BF16, "ExternalInput")
    dc_d = p.dram("dftc", [4096, TPC], BF16, "ExternalInput")
    ds_d = p.dram("dfts", [4096, TPC], BF16, "ExternalInput")
    cdft_d = p.dram("cdft", [128, 2, 128], BF16, "ExternalInput")
    wo_d = p.dram("w_out", [D, D], F32, "ExternalInput")
    rows_d = p.dram("rows", [4, D], F32, "ExternalInput")
    wr_d = p.dram("w_router", [128, KC, 32], F32, "ExternalInput")
    br_d = p.dram("b_router", [1, 32], F32, "ExternalInput")
    x1_o = p.dram("x1", [TPC, D], F32, "ExternalOutput")
    t_o = p.dram("t", [TPC, D], BF16, "ExternalOutput")
    g_o = p.dram("gates", [TPC, 32], F32, "ExternalOutput")

    p.accs = [p.ps(f"acc{i}", [128, 512], F32) for i in range(7)]
    tpf = p.ps("tpf", [128, 512], F32)
    identf = p.sb("identf", [128, 128], F32)
    ident = p.sb("ident", [128, 128], BF16)
    ones = p.sb("ones", [128, 128], BF16)
    p.op('pool', lambda: nc.gpsimd.memset(identf[:], 0.0), writes=[identf])
    p.op('pool', lambda: nc.gpsimd.affine_select(out=identf[:], in_=identf[:], pattern=[[-1, 128]],
                                                 compare_op=ALU.not_equal, fill=1.0, base=0, channel_multiplier=1),
         reads=[identf], writes=[identf])
    p.op('dve', lambda: nc.vector.tensor_copy(out=ident[:], in_=identf[:]), reads=[identf], writes=[ident])
    p.op('pool', lambda: nc.gpsimd.memset(ones[:], 1.0), writes=[ones])
    p.ident, p.identf = ident, identf
    YT = p.sb("YT", [128, 8, TPC], BF16)
    OT = p.sb("OT", [128, 8, TPC], BF16)
    cd = p.sb("cd", [128, 2, 128], BF16)
    p.dma('sp', cd[:], cdft_d[:], reads=[cdft_d], writes=[cd])

    ph = Phase(p)
    U = ph.sb("U", [128, 32, 1024], BF16)
    for h in range(4):
        p.dma('sp', U[:, h * 8:(h + 1) * 8, :], f_d[h * 1024:(h + 1) * 1024, :].rearrange("(t p) n -> p t n", p=128),
              reads=[f_d], writes=[U])
    dcs = ph.scratch("dcs", [128, 32, 256], BF16, 2)
    dss = ph.scratch("dss", [128, 32, 256], BF16, 2)
    Zc = ph.sb("Zc", [128, 8, TPC], BF16)
    Zs = ph.sb("Zs", [128, 8, TPC], BF16)
    for lc in range(4):
        dc, ds = dcs.get(), dss.get()
        p.dma('sp', dc[:], dc_d[:, lc * 256:(lc + 1) * 256].rearrange("(t p) n -> p t n", p=128), reads=[dc_d],
              writes=[dc])
        p.dma('sp', ds[:], ds_d[:, lc * 256:(lc + 1) * 256].rearrange("(t p) n -> p t n", p=128), reads=[ds_d],
              writes=[ds])
        for g in range(8):
            for which, (dd, Z) in enumerate(((dc, Zc), (ds, Zs))):
                acc = p.acc()

                def mm():
                    for lt in range(32):
                        i = nc.tensor.matmul(acc[:, 0:256], lhsT=U[:, lt, g * 128:(g + 1) * 128], rhs=dd[:, lt, :],
                                             start=(lt == 0), stop=(lt == 31))
                    return i
                p.op('pe', mm, reads=[U, dd], writes=[acc])
                if which == 0:
                    p.op('act', lambda: nc.scalar.copy(out=Z[:, g, lc * 256:(lc + 1) * 256], in_=acc[:, 0:256]),
                         reads=[acc], writes=[Z])
                else:
                    p.op('dve', lambda: nc.vector.tensor_copy(out=Z[:, g, lc * 256:(lc + 1) * 256], in_=acc[:, 0:256]),
                         reads=[acc], writes=[Z])
    for g in range(8):
        for c in range(2):
            acc = p.acc()

            def mm():
                nc.tensor.matmul(acc[:], lhsT=cd[:, 0, :], rhs=Zc[:, g, c * 512:(c + 1) * 512], start=True, stop=False)
                return nc.tensor.matmul(acc[:], lhsT=cd[:, 1, :], rhs=Zs[:, g, c * 512:(c + 1) * 512], start=False,
                                        stop=True)
            p.op('pe', mm, reads=[cd, Zc, Zs], writes=[acc])
            p.op('act' if c == 0 else 'dve',
                 (lambda: nc.scalar.copy(out=YT[:, g, c * 512:(c + 1) * 512], in_=acc[:])) if c == 0 else
                 (lambda: nc.vector.tensor_copy(out=YT[:, g, c * 512:(c + 1) * 512], in_=acc[:])),
                 reads=[acc], writes=[YT])
    ph.close()

    wo = p.sb("wo", [128, KC, D], BF16)
    for h in range(4):
        p.dma('pool', wo[:, h * 4:(h + 1) * 4, :], wo_d[h * 512:(h + 1) * 512, :].rearrange("(k p) n -> p k n", p=128),
              reads=[wo_d], writes=[wo])

    ph = Phase(p)
    kT = ph.sb("kTs", [128, 2, NKEY], BF16)
    vv = ph.sb("vv", [128, NKT, 256], BF16)
    qT = ph.sb("qTs", [128, 8, TPC], BF16)
    p.dma('sp', kT[:], kT_d[:], reads=[kT_d], writes=[kT])
    p.dma('sp', vv[:], v_d.t.ap().rearrange("(t p) n -> p t n", p=128), reads=[v_d], writes=[vv])
    p.dma('sp', qT[:], qT_d[:], reads=[qT_d], writes=[qT])
    PTs = ph.scratch("PT", [128, 512], BF16, 3)
    rDs = ph.scratch("rD", [128, 512], F32, 2)
    scale = float(128 ** -0.5)
    it = 0
    for h in range(8):
        kvh = h // 4
        for qc in range(2):
            accO = p.accs[0 + 2 * (it % 2)]
            accD = p.accs[1 + 2 * (it % 2)]
            it += 1
            for kt in range(NKT):
                accS = p.accs[4 + (kt % 3)]
                p.op('pe', lambda: nc.tensor.matmul(accS[:], lhsT=kT[:, kvh, kt * 128:(kt + 1) * 128],
                                                    rhs=qT[:, h, qc * 512:(qc + 1) * 512], start=True, stop=True),
                     reads=[kT, qT], writes=[accS])
                PT = PTs.get()
                p.op('act', lambda: nc.scalar.activation(out=PT[:], in_=accS[:], func=AF.Exp, scale=scale),
                     reads=[accS], writes=[PT])

                def mm2():
                    nc.tensor.matmul(accO[:], lhsT=vv[:, kt, kvh * 128:(kvh + 1) * 128], rhs=PT[:], start=(kt == 0),
                                     stop=(kt == NKT - 1))
                    return nc.tensor.matmul(accD[:], lhsT=ones[:], rhs=PT[:], start=(kt == 0), stop=(kt == NKT - 1))
                p.op('pe', mm2, reads=[vv, PT, ones], writes=[accO, accD])
            rD = rDs.get()
            p.op('dve', lambda: nc.vector.reciprocal(out=rD[:], in_=accD[:]), reads=[accD], writes=[rD])
            p.op('dve', lambda: nc.vector.tensor_tensor(out=OT[:, h, qc * 512:(qc + 1) * 512], in0=accO[:], in1=rD[:],
                                                        op=ALU.mult), reads=[accO, rD], writes=[OT])
    ph.close()

    ph = Phase(p)
    rb = load_rows_b(p, ph, rows_d, ["gate1", "A2", "B2", "sc2"])
    p.op('dve', lambda: nc.vector.scalar_tensor_tensor(out=rb["A2"][:], in0=rb["sc2"][:], scalar=1.0, in1=rb["A2"][:],
                                                       op0=ALU.add, op1=ALU.mult), reads=[rb["sc2"], rb["A2"]],
         writes=[rb["A2"]])
    wr = ph.sb("wr", [128, KC, 32], F32)
    brb = ph.sb("brb", [128, 32], F32)
    p.dma('sp', wr[:], wr_d[:], reads=[wr_d], writes=[wr])
    p.dma('sp', brb[:], br_d[0:1, :].partition_broadcast(128), reads=[br_d], writes=[brb])
    prep = Prep(p, ph, rb["A2"], rb["B2"], wr, brb, tpf)
    xs = ph.scratch("xs", [128, D], F32, 2)
    x1s = ph.scratch("x1s", [128, D], F32, 2)
    tmp = ph.scratch("optmp", [128, 512], F32, 2)
    p.acc_i = 0
    for t in range(TPC // 128):
        xt, x1 = xs.get(), x1s.get()
        p.dma('sp', xt[:], x[t * 128:(t + 1) * 128, :], reads=[x], writes=[xt])
        for c in range(4):
            acc = p.acc()

            def mm():
                for k in range(KC):
                    src = OT if k < 8 else YT
                    i = nc.tensor.matmul(acc[:], lhsT=src[:, k % 8, t * 128:(t + 1) * 128],
                                         rhs=wo[:, k, c * 512:(c + 1) * 512], start=(k == 0), stop=(k == KC - 1))
                return i
            p.op('pe', mm, reads=[OT, YT, wo], writes=[acc])
            tm = tmp.get()
            p.op('dve', lambda: nc.vector.tensor_tensor(out=tm[:], in0=acc[:], in1=rb["gate1"][:, c * 512:(c + 1) * 512],
                                                        op=ALU.mult), reads=[acc, rb["gate1"]], writes=[tm])
            p.op('pool', lambda: nc.gpsimd.tensor_tensor(out=x1[:, c * 512:(c + 1) * 512], in0=tm[:],
                                                         in1=xt[:, c * 512:(c + 1) * 512], op=ALU.add),
                 reads=[tm, xt], writes=[x1])
        p.dma('sp', x1_o[t * 128:(t + 1) * 128, :], x1[:], reads=[x1], writes=[x1_o])
        prep.emit(x1, t_o[t * 128:(t + 1) * 128, :], t_o, g_o[t * 128:(t + 1) * 128, :], g_o)
    p.wait_all('sp', [x1_o, t_o, g_o])
    ph.close()
    return nc


NTOK = 8192
EPC = 4
SCH = 1024
LIMIT = 7.0
ALPHA = 1.702


def build_p4():
    nc, p = new_prog()
    tT_d = p.dram("tT", [128, KC, NTOK], BF16, "ExternalInput")
    gts_d = p.dram("gts", [128, NTOK // 128, EPC], F32, "ExternalInput")
    w1_d = p.dram("w1", [EPC, D, D], F32, "ExternalInput")
    b1_d = p.dram("b1c", [128, EPC, 16], F32, "ExternalInput")
    w2_d = p.dram("w2", [EPC, 1024, D], F32, "ExternalInput")
    b2_d = p.dram("b2", [EPC, D], F32, "ExternalInput")
    yp_o = p.dram("yp", [NTOK, D], BF16, "ExternalOutput")
    p.accs = [p.ps(f"acc{i}", [128, 512], F32) for i in range(8)]
    ones1 = p.sb("ones1", [1, 128], BF16)
    p.op('pool', lambda: nc.gpsimd.memset(ones1[:], 1.0), writes=[ones1])
    gts = p.sb("gts_s", [128, NTOK // 128, EPC], F32)
    b1c = p.sb("b1c_s", [128, EPC, 16], F32)
    p.dma('sp', gts[:], gts_d[:], reads=[gts_d], writes=[gts])
    p.dma('sp', b1c[:], b1_d[:], reads=[b1_d], writes=[b1c])
    hT = p.sb("hT", [128, KC, SCH], BF16)
    yacc = [p.sb(f"yacc{i}", [128, D], F32) for i in range(SCH // 128)]
    w1ss = Scratch(p, "w1s", [128, KC, 512], BF16, 2)
    w2ss = Scratch(p, "w2s", [128, 2, D], BF16, 2)
    b2ss = Scratch(p, "b2s", [1, D], BF16, 2)
    actTs = Scratch(p, "actT", [128, 2, SCH], BF16, 2)
    g32s = Scratch(p, "g32", [128, 512], F32, 2)
    sgs = Scratch(p, "sg", [128, 512], F32, 2)
    l32s = Scratch(p, "l32", [128, 512], F32, 2)
    obs = Scratch(p, "ob", [128, D], BF16, 2)
    for sc in range(NTOK // SCH):
        for h in range(2):
            p.dma('sp', hT[:, h * 8:(h + 1) * 8, :], tT_d[:, h * 8:(h + 1) * 8, sc * SCH:(sc + 1) * SCH], reads=[tT_d],
                  writes=[hT])
        for e in range(EPC):
            b2s = b2ss.get()
            p.dma('pool', b2s[:], b2_d[e:e + 1, :], reads=[b2_d], writes=[b2s])
            for s in range(4):
                w1s, w2s, actT = w1ss.get(), w2ss.get(), actTs.get()
                p.dma('pool', w1s[:, :, 0:256], w1_d[e, :, s * 256:(s + 1) * 256].rearrange("(k p) n -> p k n", p=128),
                      reads=[w1_d], writes=[w1s])
                p.dma('pool', w1s[:, :, 256:512],
                      w1_d[e, :, 1024 + s * 256:1024 + (s + 1) * 256].rearrange("(k p) n -> p k n", p=128),
                      reads=[w1_d], writes=[w1s])
                p.dma('pool', w2s[:], w2_d[e, s * 256:(s + 1) * 256, :].rearrange("(k p) n -> p k n", p=128),
                      reads=[w2_d], writes=[w2s])
                for tc in range(SCH // 512):
                    for j in range(2):
                        accG, accL = p.acc(), p.acc()
                        for acc, off in ((accG, 0), (accL, 256)):
                            def mm():
                                for k in range(KC):
                                    i = nc.tensor.matmul(acc[:], lhsT=w1s[:, k, off + j * 128:off + (j + 1) * 128],
                                                         rhs=hT[:, k, tc * 512:(tc + 1) * 512], start=(k == 0),
                                                         stop=(k == KC - 1))
                                return i
                            p.op('pe', mm, reads=[w1s, hT], writes=[acc])
                        g32, sg, l32 = g32s.get(), sgs.get(), l32s.get()
                        jj = s * 2 + j
                        p.op('dve', lambda: nc.vector.tensor_scalar(out=g32[:], in0=accG[:], scalar1=b1c[:, e, jj:jj + 1],
                                                                    scalar2=LIMIT, op0=ALU.add, op1=ALU.min),
                             reads=[accG, b1c], writes=[g32])
                        p.op('act', lambda: nc.scalar.activation(out=sg[:], in_=g32[:], func=AF.Sigmoid, scale=ALPHA),
                             reads=[g32], writes=[sg])
                        p.op('dve', lambda: nc.vector.tensor_scalar(out=l32[:], in0=accL[:],
                                                                    scalar1=b1c[:, e, 8 + jj:8 + jj + 1], scalar2=LIMIT,
                                                                    op0=ALU.add, op1=ALU.min), reads=[accL, b1c],
                             writes=[l32])
                        p.op('pool', lambda: nc.gpsimd.tensor_scalar(out=l32[:], in0=l32[:], scalar1=-LIMIT, scalar2=1.0,
                                                                     op0=ALU.max, op1=ALU.add), reads=[l32], writes=[l32])
                        p.op('pool', lambda: nc.gpsimd.tensor_tensor(out=g32[:], in0=g32[:], in1=sg[:], op=ALU.mult),
                             reads=[g32, sg], writes=[g32])
                        p.op('pool', lambda: nc.gpsimd.tensor_tensor(out=actT[:, j, tc * 512:(tc + 1) * 512], in0=g32[:],
                                                                     in1=l32[:], op=ALU.mult), reads=[g32, l32],
                             writes=[actT])
                for tt in range(SCH // 128):
                    gcol = gts[:, sc * (SCH // 128) + tt, e:e + 1]
                    for c in range(4):
                        acc = p.acc()

                        def mm2():
                            if s == 0:
                                nc.tensor.matmul(acc[:], lhsT=ones1[0:1, :], rhs=b2s[0:1, c * 512:(c + 1) * 512],
                                                 start=True, stop=False)
                            for j in range(2):
                                i = nc.tensor.matmul(acc[:], lhsT=actT[:, j, tt * 128:(tt + 1) * 128],
                                                     rhs=w2s[:, j, c * 512:(c + 1) * 512], start=(j == 0 and s != 0),
                                                     stop=(j == 1))
                            return i
                        p.op('pe', mm2, reads=[actT, w2s, b2s, ones1], writes=[acc])
                        ya = yacc[tt]
                        if e == 0 and s == 0:
                            p.op('dve', lambda: nc.vector.tensor_scalar(out=ya[:, c * 512:(c + 1) * 512], in0=acc[:],
                                                                        scalar1=gcol, scalar2=None, op0=ALU.mult),
                                 reads=[acc, gts], writes=[ya])
                        else:
                            p.op('dve', lambda: nc.vector.scalar_tensor_tensor(
                                out=ya[:, c * 512:(c + 1) * 512], in0=acc[:], scalar=gcol,
                                in1=ya[:, c * 512:(c + 1) * 512], op0=ALU.mult, op1=ALU.add),
                                reads=[acc, gts, ya], writes=[ya])
        for tt in range(SCH // 128):
            ob = obs.get()
            p.op('act', lambda: nc.scalar.copy(out=ob[:], in_=yacc[tt][:]), reads=[yacc[tt]], writes=[ob])
            p.dma('sp', yp_o[sc * SCH + tt * 128:sc * SCH + (tt + 1) * 128, :], ob[:], reads=[ob], writes=[yp_o])
    p.wait_all('sp', [yp_o])
    return nc


def emit_combine(p, xt, rows, ypt, gate_b, xo, tmps):
    nc = p.nc
    for c in range(4):
        acc = p.acc()

        def mm():
            for i in range(8):
                ins = nc.tensor.matmul(acc[0:rows, :], lhsT=p.ident[0:rows, 0:rows], rhs=ypt[0:rows, i, c * 512:(c + 1) * 512],
                                       start=(i == 0), stop=(i == 7))
            return ins
        p.op('pe', mm, reads=[p.ident, ypt], writes=[acc])
        tm = tmps.get()
        p.op('dve', lambda: nc.vector.tensor_tensor(out=tm[0:rows, :], in0=acc[0:rows, :],
                                                    in1=gate_b[0:rows, c * 512:(c + 1) * 512], op=ALU.mult),
             reads=[acc, gate_b], writes=[tm])
        p.op('pool', lambda: nc.gpsimd.tensor_tensor(out=xo[0:rows, c * 512:(c + 1) * 512], in0=tm[0:rows, :],
                                                     in1=xt[0:rows, c * 512:(c + 1) * 512], op=ALU.add),
             reads=[tm, xt], writes=[xo])


def setup_ident(p):
    nc = p.nc
    identf = p.sb("identf", [128, 128], F32)
    ident = p.sb("ident", [128, 128], BF16)
    p.op('pool', lambda: nc.gpsimd.memset(identf[:], 0.0), writes=[identf])
    p.op('pool', lambda: nc.gpsimd.affine_select(out=identf[:], in_=identf[:], pattern=[[-1, 128]],
                                                 compare_op=ALU.not_equal, fill=1.0, base=0, channel_multiplier=1),
         reads=[identf], writes=[identf])
    p.op('dve', lambda: nc.vector.tensor_copy(out=ident[:], in_=identf[:]), reads=[identf], writes=[ident])
    p.ident, p.identf = ident, identf


HALO = 16
LTOK = TPC + 2 * HALO
CW = 31


def build_p6():
    nc, p = new_prog()
    x1h_d = p.dram("x1h", [LTOK, D], F32, "ExternalInput")
    yph_d = p.dram("yph", [8, LTOK, D], BF16, "ExternalInput")
    rows_d = p.dram("rows", [6, D], F32, "ExternalInput")
    cols_d = p.dram("cols", [128, 3, KC], F32, "ExternalInput")
    cvb_d = p.dram("cvb", [128, 32 + 16 + 16 + 16], F32, "ExternalInput")
    wdw_d = p.dram("wdw", [128, KC, CW], F32, "ExternalInput")
    mask_d = p.dram("mask", [128, 2], F32, "ExternalInput")
    wi_d = p.dram("cv_w_in", [D, 2 * D], F32, "ExternalInput")
    wo_d = p.dram("cv_w_out", [D, D], F32, "ExternalInput")
    wr_d = p.dram("w_router", [128, KC, 32], F32, "ExternalInput")
    br_d = p.dram("b_router", [1, 32], F32, "ExternalInput")
    x2_o = p.dram("x2", [TPC, D], F32, "ExternalOutput")
    t_o = p.dram("t", [TPC, D], BF16, "ExternalOutput")
    g_o = p.dram("gates", [TPC, 32], F32, "ExternalOutput")
    x1p = p.dram("x1p", [LTOK, D], F32)
    p.accs = [p.ps(f"acc{i}", [128, 512], F32) for i in range(6)]
    p.tps = [p.ps("tpb0", [128, 1024], BF16)]
    tpf = p.ps("tpf", [128, 512], F32)
    setup_ident(p)
    ones = p.sb("ones", [128, 128], BF16)
    ones1 = p.sb("ones1", [1, 128], BF16)
    p.op('pool', lambda: nc.gpsimd.memset(ones[:], 1.0), writes=[ones])
    p.op('pool', lambda: nc.gpsimd.memset(ones1[:], 1.0), writes=[ones1])
    colsb = p.sb("colsb", [128, 3, KC], F32)
    cvb = p.sb("cvbs", [128, 80], F32)
    wdw = p.sb("wdws", [128, KC, CW], F32)
    msk = p.sb("msks", [128, 2], F32)
    p.dma('sp', colsb[:], cols_d[:], reads=[cols_d], writes=[colsb])
    p.dma('sp', cvb[:], cvb_d[:], reads=[cvb_d], writes=[cvb])
    p.dma('sp', wdw[:], wdw_d[:], reads=[wdw_d], writes=[wdw])
    p.dma('sp', msk[:], mask_d[:], reads=[mask_d], writes=[msk])
    A1 = p.sb("A1", [128, KC], F32)
    B1 = p.sb("B1", [128, KC], F32)
    p.op('dve', lambda: nc.vector.scalar_tensor_tensor(out=A1[:], in0=colsb[:, 2, :], scalar=1.0, in1=colsb[:, 0, :],
                                                       op0=ALU.add, op1=ALU.mult), reads=[colsb], writes=[A1])
    p.op('dve', lambda: nc.vector.tensor_copy(out=B1[:], in_=colsb[:, 1, :]), reads=[colsb], writes=[B1])
    uc = p.sb("uc", [128, KC, TPC], BF16)
    mean_b = [p.sb(f"mean_b{c}", [128, 512], F32) for c in range(2)]
    rstd_b = [p.sb(f"rstd_b{c}", [128, 512], F32) for c in range(2)]
    phA = Phase(p)
    hT = phA.sb("hT", [128, KC, LTOK], BF16)

    ph = Phase(p)
    g2b = ph.sb("g2b", [128, D], F32)
    p.dma('sp', g2b[:], rows_d[0:1, :].partition_broadcast(128), reads=[rows_d], writes=[g2b])
    nt = NormT.__new__(NormT)
    nt.p = p
    nt.sq = ph.scratch("nt_sq", [128, D], BF16, 1)
    nt.ss = ph.scratch("nt_ss", [128, 1], F32, 2)
    nt.xn = ph.scratch("nt_xn", [128, D], BF16, 2)
    xs = ph.scratch("xs", [128, D], F32, 2)
    xos = ph.scratch("xo", [128, D], F32, 2)
    yps = ph.scratch("ypt", [128, 8, D], BF16, 2)
    tmps = ph.scratch("ctmp", [128, 512], F32, 2)
    ntl = (LTOK + 127) // 128
    for t in range(ntl):
        r0 = t * 128
        rows = min(128, LTOK - r0)
        xt, xo, ypt = xs.get(), xos.get(), yps.get()
        p.dma('sp', xt[0:rows, :], x1h_d[r0:r0 + rows, :], reads=[x1h_d], writes=[xt])
        p.dma('sp', ypt[0:rows, :, :], yph_d[:, r0:r0 + rows, :].rearrange("c t d -> t c d"), reads=[yph_d], writes=[ypt])
        emit_combine(p, xt, rows, ypt, g2b, xo, tmps)
        p.dma('sp', x1p[r0:r0 + rows, :], xo[0:rows, :], reads=[xo], writes=[x1p])
        nt.emit(xo, rows, A1, B1, hT, r0)
    ph.close()

    ph = Phase(p)
    wis = ph.scratch("wis", [128, KC, 256], BF16, 2)
    uTs = ph.scratch("uT", [128, LTOK], BF16, 2)
    sgs = ph.scratch("sgm", [128, 512], F32, 2)
    dgs = ph.scratch("dg", [128, CW, 128], BF16, 2)
    sqs = ph.scratch("csq", [128, 512], BF16, 2)
    accM = [p.ps(f"accM{c}", [128, 512], F32) for c in range(0)]
    accM = [p.accs[0], p.accs[1]]
    accQ = [p.accs[2], p.accs[3]]
    rot = [p.accs[4], p.accs[5], tpf]
    ri = 0
    chunks = [(0, 512), (512, 512), (1024, LTOK - 1024)]
    for ct in range(KC):
        wi, uT, dg = wis.get(), uTs.get(), dgs.get()
        p.dma('pool', wi[:, :, 0:128], wi_d[:, ct * 128:(ct + 1) * 128].rearrange("(k p) n -> p k n", p=128),
              reads=[wi_d], writes=[wi])
        p.dma('pool', wi[:, :, 128:256], wi_d[:, D + ct * 128:D + (ct + 1) * 128].rearrange("(k p) n -> p k n", p=128),
              reads=[wi_d], writes=[wi])
        p.op('pool', lambda: nc.gpsimd.tensor_tensor(
            out=dg[:], in0=p.ident[:].unsqueeze(1).broadcast_to([128, CW, 128]),
            in1=wdw[:, ct, :].unsqueeze(2).broadcast_to([128, CW, 128]), op=ALU.mult), reads=[p.ident, wdw], writes=[dg])
        for (c0, cn) in chunks:
            accA, accG = rot[ri % 3], rot[(ri + 1) % 3]
            ri += 2
            for acc, off in ((accA, 0), (accG, 128)):
                def mm():
                    for k in range(KC):
                        i = nc.tensor.matmul(acc[:, 0:cn], lhsT=wi[:, k, off:off + 128], rhs=hT[:, k, c0:c0 + cn],
                                             start=(k == 0), stop=(k == KC - 1))
                    return i
                p.op('pe', mm, reads=[wi, hT], writes=[acc])
            sg = sgs.get()
            p.op('act', lambda: nc.scalar.activation(out=sg[:, 0:cn], in_=accG[:, 0:cn], func=AF.Sigmoid,
                                                     bias=cvb[:, 16 + ct:16 + ct + 1], scale=1.0), reads=[accG, cvb],
                 writes=[sg])
            p.op('dve', lambda: nc.vector.scalar_tensor_tensor(out=uT[:, c0:c0 + cn], in0=accA[:, 0:cn],
                                                               scalar=cvb[:, ct:ct + 1], in1=sg[:, 0:cn], op0=ALU.add,
                                                               op1=ALU.mult), reads=[accA, cvb, sg], writes=[uT])
        p.op('dve', lambda: nc.vector.tensor_scalar(out=uT[:, 0:HALO], in0=uT[:, 0:HALO], scalar1=msk[:, 0:1], scalar2=None,
                                                    op0=ALU.mult), reads=[uT, msk], writes=[uT])
        p.op('dve', lambda: nc.vector.tensor_scalar(out=uT[:, LTOK - HALO:LTOK], in0=uT[:, LTOK - HALO:LTOK],
                                                    scalar1=msk[:, 1:2], scalar2=None, op0=ALU.mult), reads=[uT, msk],
             writes=[uT])
        for c in range(2):
            acc = rot[ri % 3]
            ri += 1

            def mmc():
                for j in range(CW):
                    i = nc.tensor.matmul(acc[:], lhsT=dg[:, j, :], rhs=uT[:, c * 512 + j + 1:c * 512 + j + 1 + 512],
                                         start=(j == 0), stop=(j == CW - 1))
                return i
            p.op('pe', mmc, reads=[dg, uT], writes=[acc])
            p.op('dve', lambda: nc.vector.tensor_scalar(out=uc[:, ct, c * 512:(c + 1) * 512], in0=acc[:],
                                                        scalar1=cvb[:, 32 + ct:32 + ct + 1], scalar2=None, op0=ALU.add),
                 reads=[acc, cvb], writes=[uc])
            sq = sqs.get()
            p.op('act', lambda: nc.scalar.activation(out=sq[:], in_=uc[:, ct, c * 512:(c + 1) * 512], func=AF.Square),
                 reads=[uc], writes=[sq])

            def mms():
                nc.tensor.matmul(accM[c][:], lhsT=ones[:], rhs=uc[:, ct, c * 512:(c + 1) * 512], start=(ct == 0),
                                 stop=(ct == KC - 1))
                return nc.tensor.matmul(accQ[c][:], lhsT=ones[:], rhs=sq[:], start=(ct == 0), stop=(ct == KC - 1))
            p.op('pe', mms, reads=[ones, uc, sq], writes=[accM[c], accQ[c]])
    for c in range(2):
        p.op('dve', lambda: nc.vector.tensor_scalar(out=mean_b[c][:], in0=accM[c][:], scalar1=1.0 / D, scalar2=None,
                                                    op0=ALU.mult), reads=[accM[c]], writes=[mean_b[c]])
        p.op('dve', lambda: nc.vector.tensor_tensor(out=rstd_b[c][:], in0=mean_b[c][:], in1=mean_b[c][:], op=ALU.mult),
             reads=[mean_b[c]], writes=[rstd_b[c]])
        p.op('dve', lambda: nc.vector.scalar_tensor_tensor(out=rstd_b[c][:], in0=accQ[c][:], scalar=1.0 / D,
                                                           in1=rstd_b[c][:], op0=ALU.mult, op1=ALU.subtract),
             reads=[accQ[c], rstd_b[c]], writes=[rstd_b[c]])
        p.op('dve', lambda: nc.vector.tensor_scalar(out=rstd_b[c][:], in0=rstd_b[c][:], scalar1=EPS, scalar2=None,
                                                    op0=ALU.add), reads=[rstd_b[c]], writes=[rstd_b[c]])
        p.op('act', lambda: nc.scalar.sqrt(out=rstd_b[c][:], in_=rstd_b[c][:]), reads=[rstd_b[c]], writes=[rstd_b[c]])
        p.op('dve', lambda: nc.vector.reciprocal(out=rstd_b[c][:], in_=rstd_b[c][:]), reads=[rstd_b[c]],
             writes=[rstd_b[c]])
    ph.close()
    phA.close()

    ph = Phase(p)
    wo = ph.sb("wo", [128, KC, D], BF16)
    for h in range(4):
        p.dma('pool', wo[:, h * 4:(h + 1) * 4, :], wo_d[h * 512:(h + 1) * 512, :].rearrange("(k p) n -> p k n", p=128),
              reads=[wo_d], writes=[wo])
    lts = ph.scratch("lnt", [128, 512], F32, 2)
    for ct in range(KC):
        for c in range(2):
            lt = lts.get()
            p.op('dve', lambda: nc.vector.tensor_tensor(out=lt[:], in0=uc[:, ct, c * 512:(c + 1) * 512], in1=mean_b[c][:],
                                                        op=ALU.subtract), reads=[uc, mean_b[c]], writes=[lt])
            p.op('pool', lambda: nc.gpsimd.tensor_tensor(out=lt[:], in0=lt[:], in1=rstd_b[c][:], op=ALU.mult),
                 reads=[lt, rstd_b[c]], writes=[lt])
            p.op('act', lambda: nc.scalar.activation(out=uc[:, ct, c * 512:(c + 1) * 512], in_=lt[:], func=AF.Silu,
                                                     scale=cvb[:, 48 + ct:48 + ct + 1], bias=cvb[:, 64 + ct:64 + ct + 1]),
                 reads=[lt, cvb], writes=[uc])
    rb = load_rows_b(p, ph, rows_d, ["gate1", "A2", "B2", "sc2"], off=1)
    p.op('dve', lambda: nc.vector.scalar_tensor_tensor(out=rb["A2"][:], in0=rb["sc2"][:], scalar=1.0, in1=rb["A2"][:],
                                                       op0=ALU.add, op1=ALU.mult), reads=[rb["sc2"], rb["A2"]],
         writes=[rb["A2"]])
    bo = ph.sb("bo", [1, D], BF16)
    p.dma('pool', bo[:], rows_d[5:6, :], reads=[rows_d], writes=[bo])
    wr = ph.sb("wr", [128, KC, 32], F32)
    brb = ph.sb("brb", [128, 32], F32)
    p.dma('sp', wr[:], wr_d[:], reads=[wr_d], writes=[wr])
    p.dma('sp', brb[:], br_d[0:1, :].partition_broadcast(128), reads=[br_d], writes=[brb])
    prep = Prep(p, ph, rb["A2"], rb["B2"], wr, brb, tpf)
    xs = ph.scratch("xs4", [128, D], F32, 2)
    x2s = ph.scratch("x2s", [128, D], F32, 1)
    tmp = ph.scratch("optmp", [128, 512], F32, 2)
    p.acc_i = 0
    for t in range(TPC // 128):
        xt, x2 = xs.get(), x2s.get()
        p.dma('sp', xt[:], x1p[HALO + t * 128:HALO + (t + 1) * 128, :], reads=[x1p], writes=[xt])
        for c in range(4):
            acc = p.acc()

            def mm():
                nc.tensor.matmul(acc[:], lhsT=ones1[0:1, :], rhs=bo[0:1, c * 512:(c + 1) * 512], start=True, stop=False)
                for k in range(KC):
                    i = nc.tensor.matmul(acc[:], lhsT=uc[:, k, t * 128:(t + 1) * 128], rhs=wo[:, k, c * 512:(c + 1) * 512],
                                         start=False, stop=(k == KC - 1))
                return i
            p.op('pe', mm, reads=[uc, wo, bo, ones1], writes=[acc])
            tm = tmp.get()
            p.op('dve', lambda: nc.vector.tensor_tensor(out=tm[:], in0=acc[:], in1=rb["gate1"][:, c * 512:(c + 1) * 512],
                                                        op=ALU.mult), reads=[acc, rb["gate1"]], writes=[tm])
            p.op('pool', lambda: nc.gpsimd.tensor_tensor(out=x2[:, c * 512:(c + 1) * 512], in0=tm[:],
                                                         in1=xt[:, c * 512:(c + 1) * 512], op=ALU.add),
                 reads=[tm, xt], writes=[x2])
        p.dma('sp', x2_o[t * 128:(t + 1) * 128, :], x2[:], reads=[x2], writes=[x2_o])
        prep.emit(x2, t_o[t * 128:(t + 1) * 128, :], t_o, g_o[t * 128:(t + 1) * 128, :], g_o)
    p.wait_all('sp', [x2_o, t_o, g_o])
    ph.close()
    return nc


def build_p8():
    nc, p = new_prog()
    x2_d = p.dram("x2", [TPC, D], F32, "ExternalInput")
    yph_d = p.dram("yph", [8, TPC, D], BF16, "ExternalInput")
    rows_d = p.dram("rows", [2, D], F32, "ExternalInput")
    out_o = p.dram("out", [TPC, D], F32, "ExternalOutput")
    p.accs = [p.ps(f"acc{i}", [128, 512], F32) for i in range(8)]
    setup_ident(p)
    ph = Phase(p)
    rb = load_rows_b(p, ph, rows_d, ["g2", "gf"])
    xs = ph.scratch("xs", [128, D], F32, 2)
    xos = ph.scratch("xo", [128, D], F32, 2)
    yps = ph.scratch("ypt", [128, 8, D], BF16, 2)
    tmps = ph.scratch("ctmp", [128, 512], F32, 2)
    sqs = ph.scratch("sq", [128, D], BF16, 1)
    sss = ph.scratch("ss", [128, 1], F32, 2)
    oos = ph.scratch("oo", [128, D], F32, 2)
    for t in range(TPC // 128):
        r0 = t * 128
        xt, xo, ypt, sq, ss, oo = xs.get(), xos.get(), yps.get(), sqs.get(), sss.get(), oos.get()
        p.dma('sp', xt[:], x2_d[r0:r0 + 128, :], reads=[x2_d], writes=[xt])
        p.dma('sp', ypt[:], yph_d[:, r0:r0 + 128, :].rearrange("c t d -> t c d"), reads=[yph_d], writes=[ypt])
        emit_combine(p, xt, 128, ypt, rb["g2"], xo, tmps)
        p.op('act', lambda: nc.scalar.activation(out=sq[:], in_=xo[:], func=AF.Square, accum_out=ss[:]), reads=[xo],
             writes=[sq, ss])
        emit_rstd(p, ss[:], 128, 1.0 / D, ss)
        p.op('dve', lambda: nc.vector.scalar_tensor_tensor(out=oo[:], in0=xo[:], scalar=ss[:, 0:1], in1=rb["gf"][:],
                                                           op0=ALU.mult, op1=ALU.mult), reads=[xo, ss, rb["gf"]],
             writes=[oo])
        p.dma('sp', out_o[r0:r0 + 128, :], oo[:], reads=[oo], writes=[out_o])
    p.wait_all('sp', [out_o])
    ph.close()
    return nc


def _run(nc, ims):
    res = run_bass_kernel_spmd(nc, ims, core_ids=list(range(NCORES)))
    return res.results


def _cf(v, n):
    return np.ascontiguousarray(np.asarray(v).reshape(n, 128).T)


def _rope_table(pos):
    inv = (10000.0 ** (-np.arange(0, 64, 2, dtype=np.float32) / np.float32(64))).astype(np.float32)
    ar = ((pos // 64).astype(np.float32)[:, None] * inv).astype(np.float64)
    ac = ((pos % 64).astype(np.float32)[:, None] * inv).astype(np.float64)
    cr, sr, cc, sc = np.cos(ar), np.sin(ar), np.cos(ac), np.sin(ac)
    return np.ascontiguousarray(np.stack([np.concatenate([cr, cr, cc, cc], 1),
                                          np.concatenate([-sr, sr, -sc, sc], 1)], 1).astype(np.float32))


def _dft_tables(qt):
    l = np.arange(4096, dtype=np.int64)[:, None]
    lp = (qt * TPC + np.arange(TPC, dtype=np.int64))[None, :]
    ang = 2.0 * np.pi * ((l * lp) % 4096).astype(np.float64) / 4096.0
    return np.cos(ang).astype(NPBF), np.sin(ang).astype(NPBF)


def _cdft():
    c = np.arange(128, dtype=np.int64)[:, None]
    cp = np.arange(128, dtype=np.int64)[None, :]
    ang = 2.0 * np.pi * ((c * cp) % 128).astype(np.float64) / 128.0
    s = np.sqrt(4096.0 * 128.0)
    return np.ascontiguousarray(np.stack([np.cos(ang) / s, -np.sin(ang) / s], 1).astype(NPBF))


def _moe_launch(layer, t_all, gates_all, moe_w1, moe_b1, moe_w2, moe_b2):
    gi = np.ascontiguousarray(gates_all.reshape(NTOK // 128, 128, NEXP).transpose(1, 0, 2))
    info = np.asarray(_run(build_p5(), [{"g": gi}] * NCORES)[0]["info"])[0]
    rank_of = [int(v) for v in info[:NEXP]]
    expert_at = [0] * NEXP
    for e, r in enumerate(rank_of):
        expert_at[r] = e
    caps = []
    for j in range(EPC):
        caps.append(next(cb for cb in range(1, NTOK // 128 + 1) if info[NEXP + j] <= cb * 128))
    ims = []
    t_rows = np.ascontiguousarray(t_all)
    for i in range(NCORES):
        ex = [expert_at[8 * j + i] for j in range(EPC)]
        w1 = np.asarray(moe_w1[layer][ex])
        w1p = np.ascontiguousarray(np.concatenate([w1[..., 0::2], w1[..., 1::2]], -1))
        b1 = np.asarray(moe_b1[layer][ex])
        b1p = np.concatenate([b1[..., 0::2], b1[..., 1::2]], -1)
        ims.append({
            "t": t_rows,
            "gts": np.ascontiguousarray(gates_all[:, ex].reshape(NTOK // 128, 128, EPC).transpose(1, 0, 2)),
            "w1": w1p,
            "b1c": np.ascontiguousarray(b1p.reshape(EPC, 16, 128).transpose(2, 0, 1)),
            "w2": np.ascontiguousarray(moe_w2[layer][ex]),
            "b2": np.ascontiguousarray(moe_b2[layer][ex]),
        })
    res = _run(build_p4s(tuple(caps)), ims)
    overflow = any(bool((np.asarray(r["cnt"])[0] > np.array(caps) * 128).any()) for r in res)
    if overflow:
        tT = np.ascontiguousarray(t_all.reshape(NTOK, KC, 128).transpose(2, 1, 0))
        for im in ims:
            del im["t"]
            im["tT"] = tT
        res = _run(build_p4(), ims)
    return np.stack([np.asarray(r["yp"]) for r in res], 0)


def kernel(x, c, ctx, c_ctx, w_mod, b_mod, g_mix, g_ffn, ab_w_in, ab_q_gain, ab_k_gain, ab_w_out,
           cv_w_in, cv_b_in, cv_w_dw, cv_b_dw, cv_ln_g, cv_ln_b, cv_w_out, cv_b_out,
           moe_w_router, moe_b_router, moe_w1, moe_b1, moe_w2, moe_b2, g_final):
    A = lambda a: np.asarray(a, dtype=np.float32)
    x, c, ctx, c_ctx, w_mod, b_mod, g_mix, g_ffn = map(A, (x, c, ctx, c_ctx, w_mod, b_mod, g_mix, g_ffn))
    ab_w_in, ab_q_gain, ab_k_gain, ab_w_out = map(A, (ab_w_in, ab_q_gain, ab_k_gain, ab_w_out))
    cv_w_in, cv_b_in, cv_w_dw, cv_b_dw, cv_ln_g, cv_ln_b, cv_w_out, cv_b_out = map(
        A, (cv_w_in, cv_b_in, cv_w_dw, cv_b_dw, cv_ln_g, cv_ln_b, cv_w_out, cv_b_out))
    moe_w_router, moe_b_router, moe_w1, moe_b1, moe_w2, moe_b2, g_final = map(
        A, (moe_w_router, moe_b_router, moe_w1, moe_b1, moe_w2, moe_b2, g_final))
    B, L, _ = x.shape
    cores = [(i // 4, i % 4) for i in range(NCORES)]

    cv = np.stack([c[0], c[1], c_ctx])
    cT = np.ascontiguousarray(cv.T.reshape(KC, 128, 3).transpose(1, 0, 2))
    ims = []
    for i in range(NCORES):
        sl = slice(i * MODW, (i + 1) * MODW)
        ims.append({"wmod": np.ascontiguousarray(w_mod[:, :, sl]),
                    "bmod": np.ascontiguousarray(np.broadcast_to(b_mod[:, None, sl], (2, 3, MODW))), "cT": cT})
    res = _run(build_p1(), ims)
    m = np.concatenate([np.asarray(r["m"]) for r in res], -1)
    mod = m.reshape(2, 3, 6, D)

    ims = []
    for (b, qt) in cores:
        colsv = np.stack([g_mix[0], mod[0, b, 0], mod[0, b, 1], mod[0, 2, 0], mod[0, 2, 1]])
        ims.append({"x": np.ascontiguousarray(x[b, qt * TPC:(qt + 1) * TPC]),
                    "ctxr": np.ascontiguousarray(ctx[b, qt * CPC:(qt + 1) * CPC]),
                    "cols": np.ascontiguousarray(colsv.reshape(5, KC, 128).transpose(2, 0, 1)),
                    "w_in": np.ascontiguousarray(ab_w_in[0]),
                    "gains": np.ascontiguousarray(np.stack([ab_q_gain[0], ab_k_gain[0]])),
                    "rope": _rope_table(qt * TPC + np.arange(TPC))})
    res = _run(build_p2(), ims)
    qkvf = [np.asarray(r["qkvf"]) for r in res]
    kvc = [np.asarray(r["kvc"]) for r in res]
    q_b, k_b, v_b, f_b = [], [], [], []
    for b in range(B):
        lat = np.concatenate(qkvf[b * 4:(b + 1) * 4], 0)
        cx = np.concatenate(kvc[b * 4:(b + 1) * 4], 0)
        q_b.append(lat[:, 0:1024])
        k_b.append(np.concatenate([lat[:, 1024:1280], cx[:, 0:256]], 0))
        v_b.append(np.concatenate([lat[:, 1280:1536], cx[:, 256:512]], 0))
        f_b.append(lat[:, 1536:2560])

    cd = _cdft()
    dft = [_dft_tables(qt) for qt in range(4)]
    wr_l = [np.ascontiguousarray(moe_w_router[l].reshape(KC, 128, 32).transpose(1, 0, 2)) for l in range(2)]
    ims = []
    for (b, qt) in cores:
        ims.append({"x": np.ascontiguousarray(x[b, qt * TPC:(qt + 1) * TPC]),
                    "qT": np.ascontiguousarray(q_b[b][qt * TPC:(qt + 1) * TPC].reshape(TPC, 8, 128).transpose(2, 1, 0)),
                    "kT": np.ascontiguousarray(k_b[b].reshape(NKEY, 2, 128).transpose(2, 1, 0)),
                    "v": np.ascontiguousarray(v_b[b]), "f": np.ascontiguousarray(f_b[b]),
                    "dftc": dft[qt][0], "dfts": dft[qt][1], "cdft": cd,
                    "w_out": np.ascontiguousarray(ab_w_out[0]),
                    "rows": np.ascontiguousarray(np.stack([mod[0, b, 2], g_ffn[0], mod[0, b, 3], mod[0, b, 4]])),
                    "w_router": wr_l[0], "b_router": np.ascontiguousarray(moe_b_router[0][None, :])})
    res = _run(build_p3(), ims)
    x1_all = np.concatenate([np.asarray(r["x1"]) for r in res], 0)
    t_all = np.concatenate([np.asarray(r["t"]) for r in res], 0)
    g_all = np.concatenate([np.asarray(r["gates"]) for r in res], 0)
    del dft

    yp_all = _moe_launch(0, t_all, g_all, moe_w1, moe_b1, moe_w2, moe_b2)

    cvb = np.ascontiguousarray(np.concatenate([_cf(cv_b_in[0], 32), _cf(cv_b_dw[0], 16), _cf(cv_ln_g[0], 16),
                                               _cf(cv_ln_b[0], 16)], 1))
    wdw = np.ascontiguousarray(cv_w_dw[0].T.reshape(KC, 128, CW).transpose(1, 0, 2))
    ims = []
    for i, (b, qt) in enumerate(cores):
        rows_g = i * TPC - HALO + np.arange(LTOK)
        valid = (rows_g >= b * L) & (rows_g < (b + 1) * L)
        x1h = np.zeros((LTOK, D), np.float32)
        x1h[valid] = x1_all[rows_g[valid]]
        yph = np.zeros((8, LTOK, D), NPBF)
        yph[:, valid] = yp_all[:, rows_g[valid]]
        colsv = np.stack([g_mix[1], mod[1, b, 0], mod[1, b, 1]])
        ims.append({"x1h": x1h, "yph": yph,
                    "rows": np.ascontiguousarray(np.stack([mod[0, b, 5], mod[1, b, 2], g_ffn[1], mod[1, b, 3],
                                                           mod[1, b, 4], cv_b_out[0]])),
                    "cols": np.ascontiguousarray(colsv.reshape(3, KC, 128).transpose(2, 0, 1)),
                    "cvb": cvb, "wdw": wdw,
                    "mask": np.ascontiguousarray(np.broadcast_to(
                        np.array([0.0 if qt == 0 else 1.0, 0.0 if qt == 3 else 1.0], np.float32), (128, 2))),
                    "cv_w_in": np.ascontiguousarray(cv_w_in[0]), "cv_w_out": np.ascontiguousarray(cv_w_out[0]),
                    "w_router": wr_l[1], "b_router": np.ascontiguousarray(moe_b_router[1][None, :])})
    res = _run(build_p6(), ims)
    x2_all = np.concatenate([np.asarray(r["x2"]) for r in res], 0)
    t2_all = np.concatenate([np.asarray(r["t"]) for r in res], 0)
    g2_all = np.concatenate([np.asarray(r["gates"]) for r in res], 0)
    del yp_all, ims

    yp2_all = _moe_launch(1, t2_all, g2_all, moe_w1, moe_b1, moe_w2, moe_b2)

    ims = []
    for i, (b, qt) in enumerate(cores):
        ims.append({"x2": np.ascontiguousarray(x2_all[i * TPC:(i + 1) * TPC]),
                    "yph": np.ascontiguousarray(yp2_all[:, i * TPC:(i + 1) * TPC]),
                    "rows": np.ascontiguousarray(np.stack([mod[1, b, 5], g_final]))})
    res = _run(build_p8(), ims)
    out = np.concatenate([np.asarray(r["out"]) for r in res], 0).reshape(B, L, D).astype(np.float32)
    return out


CAPB = 16
BIG = 1.0e6
I32 = mybir.dt.int32


def p_idma(p, out_ap, out_off, in_ap, in_off, bound, reads, dst, concurrent=True):
    nc = p.nc
    for b in reads:
        if b.w is not None:
            p._wait('pool', b.w)
    if concurrent:
        if getattr(dst, 'wbase', None) is not None:
            p._wait('pool', dst.wbase)
    else:
        if dst.w is not None:
            p._wait('pool', dst.w)
    for ev in dst.r:
        p._wait('pool', ev)
    if dst.dsem is None:
        key = 'd_' + dst.name
        p.sems[key] = nc.alloc_semaphore(key)
        dst.dsem = key
    if not hasattr(p, 'bound_regs'):
        p.bound_regs = {}
    if bound not in p.bound_regs:
        p.bound_regs[bound] = nc.gpsimd.to_reg(bound)
    inst = nc.gpsimd.indirect_dma_start(out=out_ap, out_offset=out_off, in_=in_ap, in_offset=in_off,
                                        bounds_check=p.bound_regs[bound], oob_is_err=False)
    dst.dcnt += 16
    p.dma_cnt[dst.dsem] = dst.dcnt
    inst.then_inc(p.sems[dst.dsem], 16)
    ev = (dst.dsem, dst.dcnt)
    for b in reads:
        b.r.append(ev)
    dst.w = ev
    dst.r = []
    return inst


def build_p4s(caps=(CAPB,) * 4):
    caps = tuple(int(c) for c in caps)
    CAPRS = [c * 128 for c in caps]
    NT = NTOK // 128
    nc, p = new_prog()
    t_d = p.dram("t", [NTOK, D], BF16, "ExternalInput")
    gts_d = p.dram("gts", [128, NT, EPC], F32, "ExternalInput")
    w1_d = p.dram("w1", [EPC, D, D], F32, "ExternalInput")
    b1_d = p.dram("b1c", [128, EPC, 16], F32, "ExternalInput")
    w2_d = p.dram("w2", [EPC, 1024, D], F32, "ExternalInput")
    b2_d = p.dram("b2", [EPC, D], F32, "ExternalInput")
    yp_o = p.dram("yp", [NTOK, D], BF16, "ExternalOutput")
    cnt_o = p.dram("cnt", [1, EPC], F32, "ExternalOutput")
    Xg = [p.dram(f"Xg{e}", [CAPRS[e], D], BF16) for e in range(EPC)]
    Yg = [p.dram(f"Yg{e}", [CAPRS[e], D], BF16) for e in range(EPC)]
    p.accs = [p.ps(f"acc{i}", [128, 512], F32) for i in range(6)]
    p.tps = [p.ps(f"tpb{i}", [128, 1024], BF16) for i in range(2)]
    setup_ident(p)
    ones = p.sb("ones", [128, 128], BF16)
    ones1 = p.sb("ones1", [1, 128], BF16)
    tri = p.sb("tri", [128, 128], BF16)
    trif = p.sb("trif", [128, 128], F32)
    p.op('pool', lambda: nc.gpsimd.memset(ones[:], 1.0), writes=[ones])
    p.op('pool', lambda: nc.gpsimd.memset(ones1[:], 1.0), writes=[ones1])
    p.op('pool', lambda: nc.gpsimd.memset(trif[:], 1.0), writes=[trif])
    p.op('pool', lambda: nc.gpsimd.affine_select(out=trif[:], in_=trif[:], pattern=[[1, 128]], compare_op=ALU.is_gt,
                                                 fill=0.0, base=0, channel_multiplier=-1), reads=[trif], writes=[trif])
    p.op('dve', lambda: nc.vector.tensor_copy(out=tri[:], in_=trif[:]), reads=[trif], writes=[tri])
    gts = p.sb("gts_s", [128, NT, EPC], F32)
    b1c = p.sb("b1c_s", [128, EPC, 16], F32)
    p.dma('sp', gts[:], gts_d[:], reads=[gts_d], writes=[gts])
    p.dma('sp', b1c[:], b1_d[:], reads=[b1_d], writes=[b1c])
    b2b = p.sb("b2b", [1, EPC, D], BF16)
    rank_si = p.sb("rank_si", [128, NT * EPC], I32)
    rank_gi = p.sb("rank_gi", [128, NT * EPC], I32)
    phW = Phase(p)
    w1b = phW.sb("w1b", [128, KC, D], BF16)
    w2b = phW.sb("w2b", [128, 8, D], BF16)
    p.dma('pool', b2b[:], b2_d.t.ap().unsqueeze(0) if hasattr(b2_d.t.ap(), "unsqueeze") else b2_d[:, :], reads=[b2_d],
          writes=[b2b])

    def load_w(e):
        for h in range(4):
            p.dma('pool', w1b[:, h * 4:(h + 1) * 4, :], w1_d[e, h * 512:(h + 1) * 512, :].rearrange("(k p) n -> p k n", p=128),
                  reads=[w1_d], writes=[w1b])
        for h in range(2):
            p.dma('pool', w2b[:, h * 4:(h + 1) * 4, :], w2_d[e, h * 512:(h + 1) * 512, :].rearrange("(k p) n -> p k n", p=128),
                  reads=[w2_d], writes=[w2b])
    load_w(0)

    NF = NT * EPC
    phR = Phase(p)
    maskf = phR.sb("maskf", [128, NF], F32)
    maskb = phR.sb("maskb", [128, NF], BF16)
    gflat = gts[:].rearrange("p t e -> p (t e)")
    p.op('dve', lambda: nc.vector.tensor_scalar(out=maskf[:], in0=gflat, scalar1=0.0, scalar2=None, op0=ALU.is_gt),
         reads=[gts], writes=[maskf])
    p.op('dve', lambda: nc.vector.tensor_copy(out=maskb[:], in_=maskf[:]), reads=[maskf], writes=[maskb])
    accW, accT = p.accs[0], p.accs[1]
    p.op('pe', lambda: nc.tensor.matmul(accW[:, 0:NF], lhsT=tri[:], rhs=maskb[:], start=True, stop=True),
         reads=[tri, maskb], writes=[accW])
    p.op('pe', lambda: nc.tensor.matmul(accT[:, 0:NF], lhsT=ones[:], rhs=maskb[:], start=True, stop=True),
         reads=[ones, maskb], writes=[accT])
    sa = phR.sb("scan_a", [128, NF], F32)
    sbb = phR.sb("scan_b", [128, NF], F32)
    tot = phR.sb("tot", [128, NF], F32)
    rank = phR.sb("rank", [128, NF], F32)
    p.op('dve', lambda: nc.vector.tensor_copy(out=tot[:], in_=accT[:, 0:NF]), reads=[accT], writes=[tot])
    p.op('act', lambda: nc.scalar.copy(out=sa[:], in_=accT[:, 0:NF]), reads=[accT], writes=[sa])
    cur, nxt = sa, sbb
    s = 1
    while s < NT:
        w = s * EPC
        p.op('dve', lambda: nc.vector.tensor_copy(out=nxt[:, 0:w], in_=cur[:, 0:w]), reads=[cur], writes=[nxt])
        p.op('dve', lambda: nc.vector.tensor_tensor(out=nxt[:, w:NF], in0=cur[:, w:NF], in1=cur[:, 0:NF - w], op=ALU.add),
             reads=[cur], writes=[nxt])
        cur, nxt = nxt, cur
        s *= 2
    incl = cur
    p.op('dve', lambda: nc.vector.tensor_tensor(out=rank[:], in0=accW[:, 0:NF], in1=incl[:], op=ALU.add),
         reads=[accW, incl], writes=[rank])
    p.op('dve', lambda: nc.vector.tensor_tensor(out=rank[:], in0=rank[:], in1=tot[:], op=ALU.subtract),
         reads=[rank, tot], writes=[rank])
    p.dma('sp', cnt_o[:, :], incl[0:1, NF - EPC:NF], reads=[incl], writes=[cnt_o])
    p.op('dve', lambda: nc.vector.tensor_scalar(out=maskf[:], in0=maskf[:], scalar1=-BIG, scalar2=BIG, op0=ALU.mult,
                                                op1=ALU.add), reads=[maskf], writes=[maskf])
    p.op('dve', lambda: nc.vector.tensor_tensor(out=rank[:], in0=rank[:], in1=maskf[:], op=ALU.add),
         reads=[rank, maskf], writes=[rank])
    p.op('dve', lambda: nc.vector.tensor_copy(out=rank_si[:], in_=rank[:]), reads=[rank], writes=[rank_si])
    for e in range(EPC):
        rv = rank[:].rearrange("p (t e) -> p t e", e=EPC)[:, :, e]
        p.op('dve', lambda: nc.vector.tensor_scalar(out=rv, in0=rv, scalar1=float(CAPRS[e] - 1), scalar2=None,
                                                    op0=ALU.min), reads=[rank], writes=[rank])
    p.op('dve', lambda: nc.vector.tensor_copy(out=rank_gi[:], in_=rank[:]), reads=[rank], writes=[rank_gi])

    p.wait_all('sp', [cnt_o])
    phR.close()
    phD = Phase(p)
    zt = phD.sb("zt", [128, D], BF16)
    p.op('dve', lambda: nc.vector.memset(zt[:], 0.0), writes=[zt])
    for e in range(EPC):
        CAPR = CAPRS[e]
        for r in range(0, CAPR, 128 * 8):
            n8 = min(8, (CAPR - r) // 128)
            p.dma('sp', Xg[e][r:r + n8 * 128, :].rearrange("(a p) d -> p a d", p=128),
                  zt[:].unsqueeze(1).broadcast_to([128, n8, D]), reads=[zt], writes=[Xg[e]])
        Xg[e].wbase = Xg[e].w
    tts = phD.scratch("tt", [128, D], BF16, 4)
    for t in range(NT):
        tt = tts.get()
        p.dma('sp', tt[:], t_d[t * 128:(t + 1) * 128, :], reads=[t_d], writes=[tt])
        for e in range(EPC):
            col = t * EPC + e
            p_idma(p, Xg[e][:, :], bass.IndirectOffsetOnAxis(ap=rank_si[:, col:col + 1], axis=0), tt[:, :], None,
                   CAPRS[e] - 1, [tt, rank_si], Xg[e])

    phD.close()
    phE = Phase(p)
    xgs = phE.scratch("xgt", [128, D], BF16, 3)
    XTs = phE.scratch("XT", [128, KC, 512], BF16, 2)
    actTs = phE.scratch("actT", [128, 8, 512], BF16, 2)
    g32s = phE.scratch("g32", [128, 512], F32, 2)
    sgs = phE.scratch("sg", [128, 512], F32, 2)
    l32s = phE.scratch("l32", [128, 512], F32, 2)
    yos = phE.scratch("yo", [128, D], BF16, 2)
    for e in range(EPC):
        if e > 0:
            load_w(e)
        for ch in range((caps[e] + 3) // 4):
            XT, actT = XTs.get(), actTs.get()
            nb = min(4, caps[e] - 4 * ch)
            N = nb * 128
            for ti in range(nb):
                r0 = (ch * 4 + ti) * 128
                xg = xgs.get()
                p.dma('sp', xg[:], Xg[e][r0:r0 + 128, :], reads=[Xg[e]], writes=[xg])
                for k8 in range(2):
                    tp = p.tp()

                    def tr():
                        for j in range(8):
                            k = k8 * 8 + j
                            i = nc.tensor.transpose(out=tp[:, j * 128:(j + 1) * 128], in_=xg[:, k * 128:(k + 1) * 128],
                                                    identity=p.ident[:])
                        return i
                    p.op('pe', tr, reads=[xg, p.ident], writes=[tp])
                    dst = XT[:, k8 * 8:(k8 + 1) * 8, ti * 128:(ti + 1) * 128]
                    src = tp[:].rearrange("p (k t) -> p k t", t=128)
                    if k8 == 0:
                        p.op('act', lambda: nc.scalar.copy(out=dst, in_=src), reads=[tp], writes=[XT])
                    else:
                        p.op('dve', lambda: nc.vector.tensor_copy(out=dst, in_=src), reads=[tp], writes=[XT])
            for jj in range(8):
                accG, accL = p.acc(), p.acc()
                for acc, off in ((accG, 0), (accL, 1024)):
                    def mm():
                        for k in range(KC):
                            i = nc.tensor.matmul(acc[:, 0:N], lhsT=w1b[:, k, off + jj * 128:off + (jj + 1) * 128],
                                                 rhs=XT[:, k, 0:N], start=(k == 0), stop=(k == KC - 1))
                        return i
                    p.op('pe', mm, reads=[w1b, XT], writes=[acc])
                g32, sg, l32 = g32s.get(), sgs.get(), l32s.get()
                p.op('dve', lambda: nc.vector.tensor_scalar(out=g32[:, 0:N], in0=accG[:, 0:N], scalar1=b1c[:, e, jj:jj + 1],
                                                            scalar2=LIMIT, op0=ALU.add, op1=ALU.min),
                     reads=[accG, b1c], writes=[g32])
                p.op('act', lambda: nc.scalar.activation(out=sg[:, 0:N], in_=g32[:, 0:N], func=AF.Sigmoid, scale=ALPHA),
                     reads=[g32], writes=[sg])
                p.op('dve', lambda: nc.vector.tensor_scalar(out=l32[:, 0:N], in0=accL[:, 0:N], scalar1=b1c[:, e, 8 + jj:8 + jj + 1],
                                                            scalar2=LIMIT, op0=ALU.add, op1=ALU.min),
                     reads=[accL, b1c], writes=[l32])
                p.op('dve', lambda: nc.vector.tensor_scalar(out=l32[:, 0:N], in0=l32[:, 0:N], scalar1=-LIMIT, scalar2=1.0,
                                                            op0=ALU.max, op1=ALU.add), reads=[l32], writes=[l32])
                p.op('dve', lambda: nc.vector.tensor_tensor(out=g32[:, 0:N], in0=g32[:, 0:N], in1=sg[:, 0:N], op=ALU.mult),
                     reads=[g32, sg], writes=[g32])
                p.op('dve', lambda: nc.vector.tensor_tensor(out=actT[:, jj, 0:N], in0=g32[:, 0:N], in1=l32[:, 0:N], op=ALU.mult),
                     reads=[g32, l32], writes=[actT])
            for ti in range(nb):
                r0 = (ch * 4 + ti) * 128
                yo = yos.get()
                for c in range(4):
                    acc = p.acc()

                    def mm2():
                        nc.tensor.matmul(acc[:], lhsT=ones1[0:1, :], rhs=b2b[0:1, e, c * 512:(c + 1) * 512], start=True,
                                         stop=False)
                        for j in range(8):
                            i = nc.tensor.matmul(acc[:], lhsT=actT[:, j, ti * 128:(ti + 1) * 128],
                                                 rhs=w2b[:, j, c * 512:(c + 1) * 512], start=False, stop=(j == 7))
                        return i
                    p.op('pe', mm2, reads=[actT, w2b, b2b, ones1], writes=[acc])
                    p.op('act', lambda: nc.scalar.copy(out=yo[:, c * 512:(c + 1) * 512], in_=acc[:]), reads=[acc],
                         writes=[yo])
                p.dma('sp', Yg[e][r0:r0 + 128, :], yo[:], reads=[yo], writes=[Yg[e]])

    phE.close()
    phW.close()
    phC = Phase(p)
    ygs = phC.scratch("yg", [128, D], BF16, 8)
    caccs = phC.scratch("cacc", [128, D], F32, 2)
    obs = phC.scratch("ob", [128, D], BF16, 2)
    for t in range(NT):
        eng = 'dve'
        ve = nc.vector if eng == 'dve' else nc.gpsimd
        ca, ob = caccs.get(), obs.get()
        for e in range(EPC):
            col = t * EPC + e
            yg = ygs.get()
            p_idma(p, yg[:, :], None, Yg[e][:, :], bass.IndirectOffsetOnAxis(ap=rank_gi[:, col:col + 1], axis=0),
                   CAPRS[e] - 1, [Yg[e], rank_gi], yg, concurrent=False)
            gcol = gts[:, t, e:e + 1]
            if e == 0:
                p.op(eng, lambda: ve.tensor_scalar(out=ca[:], in0=yg[:], scalar1=gcol, scalar2=None, op0=ALU.mult),
                     reads=[yg, gts], writes=[ca])
            elif e < EPC - 1:
                p.op(eng, lambda: ve.scalar_tensor_tensor(out=ca[:], in0=yg[:], scalar=gcol, in1=ca[:], op0=ALU.mult,
                                                          op1=ALU.add), reads=[yg, gts, ca], writes=[ca])
            else:
                p.op(eng, lambda: ve.scalar_tensor_tensor(out=ob[:], in0=yg[:], scalar=gcol, in1=ca[:], op0=ALU.mult,
                                                          op1=ALU.add), reads=[yg, gts, ca], writes=[ob])
        p.dma('sp', yp_o[t * 128:(t + 1) * 128, :], ob[:], reads=[ob], writes=[yp_o])
    p.wait_all('sp', [yp_o, cnt_o])
    phC.close()
    return nc


NEXP = 32


def build_p5():
    NT = NTOK // 128
    nc, p = new_prog()
    g_d = p.dram("g", [128, NT, NEXP], F32, "ExternalInput")
    out_o = p.dram("info", [1, NEXP + EPC], F32, "ExternalOutput")
    p.accs = [p.ps(f"acc{i}", [128, 512], F32) for i in range(4)]
    ones = p.sb("ones", [128, 128], BF16)
    p.op('pool', lambda: nc.gpsimd.memset(ones[:], 1.0), writes=[ones])
    g = p.sb("gs", [128, NT, NEXP], F32)
    mk = p.sb("mk", [128, NT * NEXP], BF16)
    tot = p.sb("tot", [128, NT * NEXP], F32)
    p.dma('sp', g[:], g_d[:], reads=[g_d], writes=[g])
    p.op('dve', lambda: nc.vector.tensor_scalar(out=mk[:], in0=g[:].rearrange("p t e -> p (t e)"), scalar1=0.0,
                                                scalar2=None, op0=ALU.is_gt), reads=[g], writes=[mk])
    for c in range(NT * NEXP // 512):
        acc = p.acc()
        p.op('pe', lambda: nc.tensor.matmul(acc[:], lhsT=ones[:], rhs=mk[:, c * 512:(c + 1) * 512], start=True, stop=True),
             reads=[ones, mk], writes=[acc])
        p.op('act', lambda: nc.scalar.copy(out=tot[:, c * 512:(c + 1) * 512], in_=acc[:]), reads=[acc], writes=[tot])
    cnt = p.sb("cnt", [128, NEXP], F32)
    p.op('dve', lambda: nc.vector.tensor_reduce(out=cnt[:], in_=tot[:].rearrange("p (t e) -> p e t", e=NEXP), axis=AX.X,
                                                op=ALU.add), reads=[tot], writes=[cnt])
    io = p.sb("io", [128, NEXP], F32)
    p.op('pool', lambda: nc.gpsimd.iota(io[:], pattern=[[1, NEXP]], base=0, channel_multiplier=0,
                                        allow_small_or_imprecise_dtypes=True), writes=[io])
    cnt2 = p.sb("cnt2", [128, NEXP], F32)
    p.op('dve', lambda: nc.vector.scalar_tensor_tensor(out=cnt2[:], in0=io[:], scalar=1.0 / 64, in1=cnt[:], op0=ALU.mult,
                                                       op1=ALU.add), reads=[io, cnt], writes=[cnt2])
    cmp = p.sb("cmp", [128, NEXP, NEXP], F32)
    p.op('dve', lambda: nc.vector.tensor_tensor(out=cmp[:], in0=cnt2[:].unsqueeze(1).broadcast_to([128, NEXP, NEXP]),
                                                in1=cnt2[:].unsqueeze(2).broadcast_to([128, NEXP, NEXP]), op=ALU.is_gt),
         reads=[cnt2], writes=[cmp])
    res = p.sb("res", [128, NEXP + EPC], F32)
    p.op('dve', lambda: nc.vector.tensor_reduce(out=res[:, 0:NEXP], in_=cmp[:], axis=AX.X, op=ALU.add), reads=[cmp],
         writes=[res])
    sel = p.sb("sel", [128, NEXP], F32)
    sel2 = p.sb("sel2", [128, NEXP], F32)
    for j in range(EPC):
        p.op('dve', lambda: nc.vector.tensor_scalar(out=sel[:], in0=res[:, 0:NEXP], scalar1=float(8 * j) - 0.5,
                                                    scalar2=None, op0=ALU.is_gt), reads=[res], writes=[sel])
        p.op('dve', lambda: nc.vector.tensor_scalar(out=sel2[:], in0=res[:, 0:NEXP], scalar1=float(8 * j + 8) - 0.5,
                                                    scalar2=None, op0=ALU.is_lt), reads=[res], writes=[sel2])
        p.op('dve', lambda: nc.vector.tensor_tensor(out=sel[:], in0=sel[:], in1=sel2[:], op=ALU.mult), reads=[sel, sel2],
             writes=[sel])
        p.op('dve', lambda: nc.vector.tensor_tensor(out=sel[:], in0=sel[:], in1=cnt[:], op=ALU.mult), reads=[sel, cnt],
             writes=[sel])
        p.op('dve', lambda: nc.vector.reduce_max(out=res[:, NEXP + j:NEXP + j + 1], in_=sel[:], axis=AX.X), reads=[sel],
             writes=[res])
    p.dma('sp', out_o[:, :], res[0:1, :], reads=[res], writes=[out_o])
    p.wait_all('sp', [out_o])
    return nc
```

```python
import numpy as np
import ml_dtypes
import concourse.bass as bass
import concourse.mybir as mybir
from concourse.bass_utils import run_bass_kernel_spmd

F32 = mybir.dt.float32
BF16 = mybir.dt.bfloat16
AF = mybir.ActivationFunctionType
ALU = mybir.AluOpType
AX = mybir.AxisListType
NPBF = ml_dtypes.bfloat16

D = 2048
KC = 16
EPS = 1e-6
NCORES = 8


class Buf:
    def __init__(self, t, name):
        self.t = t
        self.name = name
        self.w = None
        self.r = []
        self.dsem = None
        self.dcnt = 0

    def __getitem__(self, idx):
        return self.t[idx]


class P:
    def __init__(self, nc):
        self.nc = nc
        self.eng = {'pe': nc.tensor, 'act': nc.scalar, 'dve': nc.vector, 'pool': nc.gpsimd, 'sp': nc.sync}
        self.sems = {}
        self.cnt = {}
        for k in self.eng:
            self.sems[k] = nc.alloc_semaphore('s_' + k)
            self.cnt[k] = 0
        self.seen = {k: {} for k in self.eng}
        self.acc_i = 0
        self.tp_i = 0
        self.dma_cnt = {}

    def sb(self, name, shape, dt):
        return Buf(self.nc.alloc_sbuf_tensor(name, list(shape), dt), name)

    def ps(self, name, shape, dt=F32):
        return Buf(self.nc.alloc_psum_tensor(name, list(shape), dt), name)

    def dram(self, name, shape, dt, kind="Internal"):
        return Buf(self.nc.dram_tensor(name, list(shape), dt, kind=kind), name)

    def _wait(self, e, ev):
        key, val = ev
        if key == 'pe' and e == 'pe':
            return
        if self.seen[e].get(key, 0) >= val:
            return
        self.seen[e][key] = val
        self.eng[e].wait_ge(self.sems[key], val)

    def _deps(self, e, reads, writes):
        for b in reads:
            if b.w is not None:
                self._wait(e, b.w)
        for b in writes:
            if b.w is not None:
                self._wait(e, b.w)
            for ev in b.r:
                self._wait(e, ev)

    def _commit(self, ev, reads, writes):
        for b in reads:
            b.r.append(ev)
            if len(b.r) > 48:
                best = {}
                for k, v in b.r:
                    best[k] = max(best.get(k, 0), v)
                b.r = list(best.items())
        for b in writes:
            b.w = ev
            b.r = []

    def op(self, e, fn, reads=(), writes=()):
        self._deps(e, reads, writes)
        inst = fn()
        self.cnt[e] += 1
        inst.then_inc(self.sems[e], 1)
        self._commit((e, self.cnt[e]), reads, writes)
        return inst

    def dma(self, q, out_ap, in_ap, reads=(), writes=(), **kw):
        self._deps(q, reads, writes)
        dst = writes[0]
        if dst.dsem is None:
            key = 'd_' + dst.name
            self.sems[key] = self.nc.alloc_semaphore(key)
            dst.dsem = key
        inst = self.eng[q].dma_start(out=out_ap, in_=in_ap, **kw)
        dst.dcnt += 16
        self.dma_cnt[dst.dsem] = dst.dcnt
        inst.then_inc(self.sems[dst.dsem], 16)
        self._commit((dst.dsem, dst.dcnt), reads, writes)
        return inst

    def wait_all(self, e, bufs):
        for b in bufs:
            if b.w is not None:
                self._wait(e, b.w)

    def setup_common(self):
        nc = self.nc
        self.accs = [self.ps(f"acc{i}", [128, 512], F32) for i in range(5)]
        self.tps = [self.ps(f"tpb{i}", [128, 1024], BF16) for i in range(3)]
        identf = self.sb("identf", [128, 128], F32)
        ident = self.sb("ident", [128, 128], BF16)
        self.op('pool', lambda: nc.gpsimd.memset(identf[:], 0.0), writes=[identf])
        self.op('pool', lambda: nc.gpsimd.affine_select(out=identf[:], in_=identf[:], pattern=[[-1, 128]],
                                                        compare_op=ALU.not_equal, fill=1.0, base=0,
                                                        channel_multiplier=1), reads=[identf], writes=[identf])
        self.op('dve', lambda: nc.vector.tensor_copy(out=ident[:], in_=identf[:]), reads=[identf], writes=[ident])
        self.ident = ident
        self.identf = identf

    def acc(self):
        b = self.accs[self.acc_i % len(self.accs)]
        self.acc_i += 1
        return b

    def tp(self):
        b = self.tps[self.tp_i % len(self.tps)]
        self.tp_i += 1
        return b


def new_prog():
    nc = bass.Bass("TRN2", target_bir_lowering=False)
    return nc, P(nc)


class Scratch:
    def __init__(self, p, name, shape, dt, n=2):
        self.bufs = [p.sb(f"{name}{i}", shape, dt) for i in range(n)]
        self.i = 0

    def get(self):
        b = self.bufs[self.i % len(self.bufs)]
        self.i += 1
        return b


def emit_rstd(p, ss, rows, scale_in, tmp=None):
    nc = p.nc
    p.op('dve', lambda: nc.vector.tensor_scalar(out=ss, in0=ss, scalar1=float(scale_in), scalar2=EPS,
                                                op0=ALU.mult, op1=ALU.add), reads=[tmp], writes=[tmp])
    p.op('act', lambda: nc.scalar.sqrt(out=ss, in_=ss), reads=[tmp], writes=[tmp])
    p.op('dve', lambda: nc.vector.reciprocal(out=ss, in_=ss), reads=[tmp], writes=[tmp])


class NormT:
    def __init__(self, p):
        self.p = p
        self.sq = Scratch(p, "nt_sq", [128, D], BF16, 2)
        self.ss = Scratch(p, "nt_ss", [128, 1], F32, 2)
        self.xn = Scratch(p, "nt_xn", [128, D], BF16, 2)

    def emit(self, xt, rows, Acol, Bcol, hT, c0):
        p, nc = self.p, self.p.nc
        sq, ss, xn = self.sq.get(), self.ss.get(), self.xn.get()
        p.op('act', lambda: nc.scalar.activation(out=sq[0:rows, :], in_=xt[0:rows, :], func=AF.Square,
                                                 accum_out=ss[0:rows, :]), reads=[xt], writes=[sq, ss])
        emit_rstd(p, ss[0:rows, :], rows, 1.0 / D, ss)
        p.op('dve', lambda: nc.vector.tensor_scalar(out=xn[0:rows, :], in0=xt[0:rows, :], scalar1=ss[0:rows, 0:1],
                                                    scalar2=None, op0=ALU.mult), reads=[xt, ss], writes=[xn])
        for k4 in range(2):
            tp = p.tp()

            def tr():
                for j in range(8):
                    k = k4 * 8 + j
                    i = nc.tensor.transpose(out=tp[:, j * 128:j * 128 + rows], in_=xn[0:rows, k * 128:(k + 1) * 128],
                                            identity=p.ident[0:rows, 0:rows])
                return i
            p.op('pe', tr, reads=[xn, p.ident], writes=[tp])
            for j in range(8):
                k = k4 * 8 + j
                if j % 2 == 0:
                    p.op('dve', lambda: nc.vector.tensor_scalar(
                        out=hT[:, k, c0:c0 + rows], in0=tp[:, j * 128:j * 128 + rows], scalar1=Acol[:, k:k + 1],
                        scalar2=Bcol[:, k:k + 1], op0=ALU.mult, op1=ALU.add), reads=[tp, Acol, Bcol], writes=[hT])
                else:
                    p.op('act', lambda: nc.scalar.activation(
                        out=hT[:, k, c0:c0 + rows], in_=tp[:, j * 128:j * 128 + rows], func=AF.Identity,
                        scale=Acol[:, k:k + 1], bias=Bcol[:, k:k + 1]), reads=[tp, Acol, Bcol], writes=[hT])


def emit_Acol(p, A, g, sc):
    nc = p.nc
    p.op('dve', lambda: nc.vector.scalar_tensor_tensor(out=A[:], in0=sc[:], scalar=1.0, in1=g[:], op0=ALU.add,
                                                       op1=ALU.mult), reads=[sc, g], writes=[A])


MODW = 12288 // NCORES


def build_p1():
    nc, p = new_prog()
    wmod = p.dram("wmod", [2, D, MODW], F32, "ExternalInput")
    bmod = p.dram("bmod", [2, 3, MODW], F32, "ExternalInput")
    cT = p.dram("cT", [128, KC, 3], F32, "ExternalInput")
    mout = p.dram("m", [2, 3, MODW], F32, "ExternalOutput")
    p.setup_common()
    cs = p.sb("cs", [128, KC, 3], F32)
    sT = p.sb("sT", [128, KC, 3], BF16)
    p.dma('sp', cs[:], cT[:], reads=[cT], writes=[cs])
    p.op('act', lambda: nc.scalar.activation(out=sT[:], in_=cs[:], func=AF.Silu), reads=[cs], writes=[sT])
    for l in range(2):
        wb = p.sb(f"wb{l}", [128, KC, MODW], BF16)
        bb = p.sb(f"bb{l}", [3, MODW], F32)
        mo = p.sb(f"mo{l}", [3, MODW], F32)
        for h in range(2):
            p.dma('pool', wb[:, h * 8:(h + 1) * 8, :],
                  wmod[l, h * 1024:(h + 1) * 1024, :].rearrange("(k p) n -> p k n", p=128), reads=[wmod], writes=[wb])
        p.dma('sp', bb[:], bmod[l], reads=[bmod], writes=[bb])
        for c in range(MODW // 512):
            acc = p.acc()

            def mm():
                for k in range(KC):
                    i = nc.tensor.matmul(acc[0:3, :], lhsT=sT[:, k, :], rhs=wb[:, k, c * 512:(c + 1) * 512],
                                         start=(k == 0), stop=(k == KC - 1))
                return i
            p.op('pe', mm, reads=[sT, wb], writes=[acc])
            p.op('dve', lambda: nc.vector.tensor_tensor(out=mo[:, c * 512:(c + 1) * 512], in0=acc[0:3, :],
                                                        in1=bb[:, c * 512:(c + 1) * 512], op=ALU.add),
                 reads=[acc, bb], writes=[mo])
        p.dma('sp', mout[l], mo[:], reads=[mo], writes=[mout])
    p.wait_all('sp', [mout])
    return nc


TPC = 1024
CPC = 64


def emit_qk_post(p, ps, rows, nh, gain_b, rope_t, out_ap, out_buf, S):
    nc = p.nc
    n = nh * 128
    sq, ss4, qn = S['sq'].get(), S['ss4'].get(), S['qn'].get()
    p.op('act', lambda: nc.scalar.activation(out=sq[0:rows, 0:n], in_=ps[0:rows, 0:n], func=AF.Square),
         reads=[ps], writes=[sq])
    p.op('dve', lambda: nc.vector.tensor_reduce(out=ss4[0:rows, 0:nh],
                                                in_=sq[0:rows, 0:n].rearrange("p (h d) -> p h d", d=128),
                                                axis=AX.X, op=ALU.add), reads=[sq], writes=[ss4])
    emit_rstd(p, ss4[0:rows, 0:nh], rows, 1.0 / 128, ss4)
    p.op('dve', lambda: nc.vector.tensor_tensor(
        out=qn[0:rows, 0:n].rearrange("p (h d) -> p h d", d=128),
        in0=ps[0:rows, 0:n].rearrange("p (h d) -> p h d", d=128),
        in1=ss4[0:rows, 0:nh].unsqueeze(2).broadcast_to([rows, nh, 128]), op=ALU.mult),
        reads=[ps, ss4], writes=[qn])
    if rope_t is None:
        p.op('dve', lambda: nc.vector.tensor_tensor(
            out=out_ap.rearrange("p (h d) -> p h d", d=128),
            in0=qn[0:rows, 0:n].rearrange("p (h d) -> p h d", d=128),
            in1=gain_b[0:rows, :].unsqueeze(1).broadcast_to([rows, nh, 128]), op=ALU.mult),
            reads=[qn, gain_b], writes=[out_buf])
        return
    p.op('pool', lambda: nc.gpsimd.tensor_tensor(
        out=qn[0:rows, 0:n].rearrange("p (h d) -> p h d", d=128),
        in0=qn[0:rows, 0:n].rearrange("p (h d) -> p h d", d=128),
        in1=gain_b[0:rows, :].unsqueeze(1).broadcast_to([rows, nh, 128]), op=ALU.mult),
        reads=[qn, gain_b], writes=[qn])
    t1, t2 = S['t1'].get(), S['t2'].get()
    p.op('pool', lambda: nc.gpsimd.tensor_tensor(
        out=t1[0:rows, 0:n].rearrange("p (h d) -> p h d", d=128),
        in0=qn[0:rows, 0:n].rearrange("p (h d) -> p h d", d=128),
        in1=rope_t[0:rows, 0, :].unsqueeze(1).broadcast_to([rows, nh, 128]), op=ALU.mult),
        reads=[qn, rope_t], writes=[t1])
    for b in range(2):
        p.op('dve', lambda: nc.vector.tensor_tensor(
            out=t2[0:rows, 0:n].rearrange("p (h a b d) -> p h a b d", a=2, b=2, d=32)[:, :, :, b, :],
            in0=qn[0:rows, 0:n].rearrange("p (h a b d) -> p h a b d", a=2, b=2, d=32)[:, :, :, 1 - b, :],
            in1=rope_t[0:rows, 1, :].rearrange("p (a b d) -> p a b d", a=2, b=2, d=32)[:, :, b, :]
            .unsqueeze(1).broadcast_to([rows, nh, 2, 32]), op=ALU.mult),
            reads=[qn, rope_t], writes=[t2])
    p.op('dve', lambda: nc.vector.tensor_tensor(out=out_ap, in0=t1[0:rows, 0:n], in1=t2[0:rows, 0:n], op=ALU.add),
         reads=[t1, t2], writes=[out_buf])


def build_p2():
    nc, p = new_prog()
    x = p.dram("x", [TPC, D], F32, "ExternalInput")
    ctxr = p.dram("ctxr", [CPC, D], F32, "ExternalInput")
    cols = p.dram("cols", [128, 5, KC], F32, "ExternalInput")
    w_in = p.dram("w_in", [D, 2560], F32, "ExternalInput")
    gains = p.dram("gains", [2, 128], F32, "ExternalInput")
    rope = p.dram("rope", [TPC, 2, 128], F32, "ExternalInput")
    out = p.dram("qkvf", [TPC, 2560], BF16, "ExternalOutput")
    outc = p.dram("kvc", [CPC, 512], BF16, "ExternalOutput")
    p.setup_common()
    colsb = p.sb("colsb", [128, 5, KC], F32)
    p.dma('sp', colsb[:], cols[:], reads=[cols], writes=[colsb])
    qg = p.sb("qg", [128, 128], F32)
    kg = p.sb("kg", [128, 128], F32)
    p.dma('sp', qg[:], gains[0:1, :].partition_broadcast(128), reads=[gains], writes=[qg])
    p.dma('sp', kg[:], gains[1:2, :].partition_broadcast(128), reads=[gains], writes=[kg])
    A1 = p.sb("A1", [128, KC], F32)
    Ac = p.sb("Ac", [128, KC], F32)
    B1 = p.sb("B1", [128, KC], F32)
    Bc = p.sb("Bc", [128, KC], F32)
    p.op('dve', lambda: nc.vector.scalar_tensor_tensor(out=A1[:], in0=colsb[:, 2, :], scalar=1.0, in1=colsb[:, 0, :],
                                                       op0=ALU.add, op1=ALU.mult), reads=[colsb], writes=[A1])
    p.op('dve', lambda: nc.vector.scalar_tensor_tensor(out=Ac[:], in0=colsb[:, 4, :], scalar=1.0, in1=colsb[:, 0, :],
                                                       op0=ALU.add, op1=ALU.mult), reads=[colsb], writes=[Ac])
    p.op('dve', lambda: nc.vector.tensor_copy(out=B1[:], in_=colsb[:, 1, :]), reads=[colsb], writes=[B1])
    p.op('dve', lambda: nc.vector.tensor_copy(out=Bc[:], in_=colsb[:, 3, :]), reads=[colsb], writes=[Bc])
    wb = p.sb("wb", [128, KC, 2560], BF16)
    for h in range(4):
        p.dma('pool', wb[:, h * 4:(h + 1) * 4, :], w_in[h * 512:(h + 1) * 512, :].rearrange("(k p) n -> p k n", p=128),
              reads=[w_in], writes=[wb])
    nt = NormT(p)
    xs = Scratch(p, "xs", [128, D], F32, 2)
    hTs = Scratch(p, "hT", [128, KC, 128], BF16, 2)
    rts = Scratch(p, "rt", [128, 2, 128], F32, 2)
    obs = Scratch(p, "ob", [128, 2560], BF16, 2)
    S = {'sq': Scratch(p, "qsq", [128, 512], F32, 2), 'ss4': Scratch(p, "qss", [128, 4], F32, 2),
         'qn': Scratch(p, "qn", [128, 512], F32, 2), 't1': Scratch(p, "qt1", [128, 512], F32, 2),
         't2': Scratch(p, "qt2", [128, 512], F32, 2)}
    ntiles = TPC // 128
    for t in range(ntiles + 1):
        is_ctx = (t == ntiles)
        rows = CPC if is_ctx else 128
        xt, hT, ob = xs.get(), hTs.get(), obs.get()
        if is_ctx:
            p.dma('sp', xt[0:rows, :], ctxr[:, :], reads=[ctxr], writes=[xt])
            rt = None
        else:
            p.dma('sp', xt[:], x[t * 128:(t + 1) * 128, :], reads=[x], writes=[xt])
            rt = rts.get()
            p.dma('sp', rt[:], rope[t * 128:(t + 1) * 128, :, :], reads=[rope], writes=[rt])
        nt.emit(xt, rows, Ac if is_ctx else A1, Bc if is_ctx else B1, hT, 0)
        for c in ([2] if is_ctx else range(5)):
            acc = p.acc()

            def mm():
                for k in range(KC):
                    i = nc.tensor.matmul(acc[0:rows, :], lhsT=hT[:, k, 0:rows], rhs=wb[:, k, c * 512:(c + 1) * 512],
                                         start=(k == 0), stop=(k == KC - 1))
                return i
            p.op('pe', mm, reads=[hT, wb], writes=[acc])
            if c < 2:
                emit_qk_post(p, acc, rows, 4, qg, rt, ob[0:rows, c * 512:(c + 1) * 512], ob, S)
            elif c == 2:
                emit_qk_post(p, acc, rows, 2, kg, rt, ob[0:rows, 1024:1280], ob, S)
                p.op('act', lambda: nc.scalar.copy(out=ob[0:rows, 1280:1536], in_=acc[0:rows, 256:512]),
                     reads=[acc], writes=[ob])
            else:
                p.op('act', lambda: nc.scalar.copy(out=ob[0:rows, c * 512:(c + 1) * 512], in_=acc[0:rows, :]),
                     reads=[acc], writes=[ob])
        if is_ctx:
            p.dma('sp', outc[:, :], ob[0:rows, 1024:1536], reads=[ob], writes=[outc])
        else:
            p.dma('sp', out[t * 128:(t + 1) * 128, :], ob[:], reads=[ob], writes=[out])
    p.wait_all('sp', [out, outc])
    return nc


def p_barrier(p):
    targets = {}
    for k in p.eng:
        targets[k] = p.cnt[k]
    for k, v in p.sems.items():
        if k.startswith('d_'):
            targets[k] = p.dma_cnt.get(k, 0)
    for e in p.eng:
        for k, v in targets.items():
            if v > 0 and k != e:
                p._wait(e, (k, v))


class Phase:
    def __init__(self, p):
        from contextlib import ExitStack
        self.p = p
        self.st = ExitStack()

    def sb(self, name, shape, dt):
        t = self.st.enter_context(self.p.nc.sbuf_tensor(name, list(shape), dt))
        return Buf(t, name)

    def scratch(self, name, shape, dt, n=2):
        s = Scratch.__new__(Scratch)
        s.bufs = [self.sb(f"{name}{i}", shape, dt) for i in range(n)]
        s.i = 0
        return s

    def close(self):
        p_barrier(self.p)
        self.st.close()


class Prep:
    def __init__(self, p, ph, A2b, B2b, wr, brb, tpf):
        self.p = p
        self.A2b, self.B2b, self.wr, self.brb, self.tpf = A2b, B2b, wr, brb, tpf
        self.sq = ph.sb("pp_sq", [128, D], BF16)
        self.ss = ph.sb("pp_ss", [128, 1], F32)
        self.t32 = ph.sb("pp_t32", [128, D], F32)
        self.tb = ph.scratch("pp_tb", [128, D], BF16, 2)
        self.tT = ph.sb("pp_tT", [128, KC, 128], F32)
        self.lg = ph.scratch("pp_lg", [128, 32], F32, 2)
        self.t8 = ph.sb("pp_t8", [128, 8], F32)
        self.nmx = ph.sb("pp_nmx", [128, 1], F32)
        self.msk = ph.sb("pp_msk", [128, 32], F32)
        self.ex = ph.sb("pp_ex", [128, 32], F32)
        self.sm = ph.sb("pp_sm", [128, 1], F32)

    def emit(self, x1, t_out_ap, t_out, g_out_ap, g_out):
        p, nc = self.p, self.p.nc
        sq, ss, t32, tT = self.sq, self.ss, self.t32, self.tT
        tb, lg = self.tb.get(), self.lg.get()
        p.op('act', lambda: nc.scalar.activation(out=sq[:], in_=x1[:], func=AF.Square, accum_out=ss[:]),
             reads=[x1], writes=[sq, ss])
        emit_rstd(p, ss[:], 128, 1.0 / D, ss)
        p.op('dve', lambda: nc.vector.scalar_tensor_tensor(out=t32[:], in0=x1[:], scalar=ss[:, 0:1], in1=self.A2b[:],
                                                           op0=ALU.mult, op1=ALU.mult), reads=[x1, ss, self.A2b],
             writes=[t32])
        p.op('pool', lambda: nc.gpsimd.tensor_tensor(out=t32[:], in0=t32[:], in1=self.B2b[:], op=ALU.add),
             reads=[t32, self.B2b], writes=[t32])
        p.op('act', lambda: nc.scalar.copy(out=tb[:], in_=t32[:]), reads=[t32], writes=[tb])
        p.dma('sp', t_out_ap, tb[:], reads=[tb], writes=[t_out])
        for k4 in range(4):
            tpf = self.tpf

            def tr():
                for j in range(4):
                    k = k4 * 4 + j
                    i = nc.tensor.transpose(out=tpf[:, j * 128:(j + 1) * 128], in_=t32[:, k * 128:(k + 1) * 128],
                                            identity=p.identf[:])
                return i
            p.op('pe', tr, reads=[t32, p.identf], writes=[tpf])
            eng = 'dve' if k4 % 2 == 0 else 'act'
            if eng == 'dve':
                p.op('dve', lambda: nc.vector.tensor_copy(
                    out=tT[:, k4 * 4:(k4 + 1) * 4, :].rearrange("p k t -> p (k t)"), in_=tpf[:]),
                    reads=[tpf], writes=[tT])
            else:
                p.op('act', lambda: nc.scalar.copy(
                    out=tT[:, k4 * 4:(k4 + 1) * 4, :].rearrange("p k t -> p (k t)"), in_=tpf[:]),
                    reads=[tpf], writes=[tT])
        acc = p.acc()

        def mm():
            for k in range(KC):
                i = nc.tensor.matmul(acc[:, 0:32], lhsT=tT[:, k, :], rhs=self.wr[:, k, :], start=(k == 0),
                                     stop=(k == KC - 1))
            return i
        p.op('pe', mm, reads=[tT, self.wr], writes=[acc])
        p.op('dve', lambda: nc.vector.tensor_tensor(out=lg[:], in0=acc[:, 0:32], in1=self.brb[:], op=ALU.add),
             reads=[acc, self.brb], writes=[lg])
        emit_gates(p, lg, self.t8, self.nmx, self.msk, self.ex, self.sm)
        p.dma('sp', g_out_ap, lg[:], reads=[lg], writes=[g_out])


def emit_gates(p, lg, t8, nmx, msk, ex, sm):
    nc = p.nc
    p.op('dve', lambda: nc.vector.max(out=t8[:], in_=lg[:]), reads=[lg], writes=[t8])
    p.op('dve', lambda: nc.vector.tensor_scalar(out=msk[:], in0=lg[:], scalar1=t8[:, 3:4], scalar2=None,
                                                op0=ALU.is_ge), reads=[lg, t8], writes=[msk])
    p.op('dve', lambda: nc.vector.tensor_scalar(out=nmx[:], in0=t8[:, 0:1], scalar1=-1.0, scalar2=None,
                                                op0=ALU.mult), reads=[t8], writes=[nmx])
    p.op('act', lambda: nc.scalar.activation(out=ex[:], in_=lg[:], func=AF.Exp, bias=nmx[:, 0:1], scale=1.0),
         reads=[lg, nmx], writes=[ex])
    p.op('dve', lambda: nc.vector.tensor_tensor(out=ex[:], in0=ex[:], in1=msk[:], op=ALU.mult),
         reads=[ex, msk], writes=[ex])
    p.op('dve', lambda: nc.vector.reduce_sum(out=sm[:], in_=ex[:], axis=AX.X), reads=[ex], writes=[sm])
    p.op('dve', lambda: nc.vector.reciprocal(out=sm[:], in_=sm[:]), reads=[sm], writes=[sm])
    p.op('dve', lambda: nc.vector.tensor_scalar(out=lg[:], in0=ex[:], scalar1=sm[:, 0:1], scalar2=None,
                                                op0=ALU.mult), reads=[ex, sm], writes=[lg])


def load_rows_b(p, ph, rows_d, names, off=0):
    out = {}
    for i, nm in enumerate(names):
        b = ph.sb("rb_" + nm, [128, D], F32)
        p.dma('sp', b[:], rows_d[off + i:off + i + 1, :].partition_broadcast(128), reads=[rows_d], writes=[b])
        out[nm] = b
    return out


NKEY = 4096 + 256
NKT = NKEY // 128


def build_p3():
    nc, p = new_prog()
    x = p.dram("x", [TPC, D], F32, "ExternalInput")
    qT_d = p.dram("qT", [128, 8, TPC], BF16, "ExternalInput")
    kT_d = p.dram("kT", [128, 2, NKEY], BF16, "ExternalInput")
    v_d = p.dram("v", [NKEY, 256], BF16, "ExternalInput")
    f_d = p.dram("f", [4096, 1024], BF16, "ExternalInput")
    dc_d = p.dram("dftc", [4, 128, 32, 256], BF16, "ExternalInput")
    ds_d = p.dram("dfts", [4, 128, 32, 256], BF16, "ExternalInput")
    cdft_d = p.dram("cdft", [128, 2, 128], BF16, "ExternalInput")
    wo_d = p.dram("w_out", [D, D], F32, "ExternalInput")
    rows_d = p.dram("rows", [4, D], F32, "ExternalInput")
    wr_d = p.dram("w_router", [128, KC, 32], F32, "ExternalInput")
    br_d = p.dram("b_router", [1, 32], F32, "ExternalInput")
    x1_o = p.dram("x1", [TPC, D], F32, "ExternalOutput")
    t_o = p.dram("t", [TPC, D], BF16, "ExternalOutput")
    g_o = p.dram("gates", [TPC, 32], F32, "ExternalOutput")

    p.accs = [p.ps(f"acc{i}", [128, 512], F32) for i in range(7)]
    tpf = p.ps("tpf", [128, 512], F32)
    identf = p.sb("identf", [128, 128], F32)
    ident = p.sb("ident", [128, 128], BF16)
    ones = p.sb("ones", [128, 128], BF16)
    p.op('pool', lambda: nc.gpsimd.memset(identf[:], 0.0), writes=[identf])
    p.op('pool', lambda: nc.gpsimd.affine_select(out=identf[:], in_=identf[:], pattern=[[-1, 128]],
                                                 compare_op=ALU.not_equal, fill=1.0, base=0, channel_multiplier=1),
         reads=[identf], writes=[identf])
    p.op('dve', lambda: nc.vector.tensor_copy(out=ident[:], in_=identf[:]), reads=[identf], writes=[ident])
    p.op('pool', lambda: nc.gpsimd.memset(ones[:], 1.0), writes=[ones])
    p.ident, p.identf = ident, identf
    YT = p.sb("YT", [128, 8, TPC], BF16)
    OT = p.sb("OT", [128, 8, TPC], BF16)
    cd = p.sb("cd", [128, 2, 128], BF16)
    p.dma('sp', cd[:], cdft_d[:], reads=[cdft_d], writes=[cd])

    ph = Phase(p)
    U = ph.sb("U", [128, 32, 1024], BF16)
    for h in range(4):
        p.dma('sp', U[:, h * 8:(h + 1) * 8, :], f_d[h * 1024:(h + 1) * 1024, :].rearrange("(t p) n -> p t n", p=128),
              reads=[f_d], writes=[U])
    dcs = ph.scratch("dcs", [128, 32, 256], BF16, 2)
    dss = ph.scratch("dss", [128, 32, 256], BF16, 2)
    Zc = ph.sb("Zc", [128, 8, TPC], BF16)
    Zs = ph.sb("Zs", [128, 8, TPC], BF16)
    for lc in range(4):
        dc, ds = dcs.get(), dss.get()
        p.dma('sp', dc[:], dc_d[lc], reads=[dc_d], writes=[dc])
        p.dma('sp', ds[:], ds_d[lc], reads=[ds_d], writes=[ds])
        for g in range(8):
            for which, (dd, Z) in enumerate(((dc, Zc), (ds, Zs))):
                acc = p.acc()

                def mm():
                    for lt in range(32):
                        i = nc.tensor.matmul(acc[:, 0:256], lhsT=U[:, lt, g * 128:(g + 1) * 128], rhs=dd[:, lt, :],
                                             start=(lt == 0), stop=(lt == 31))
                    return i
                p.op('pe', mm, reads=[U, dd], writes=[acc])
                if which == 0:
                    p.op('act', lambda: nc.scalar.copy(out=Z[:, g, lc * 256:(lc + 1) * 256], in_=acc[:, 0:256]),
                         reads=[acc], writes=[Z])
                else:
                    p.op('dve', lambda: nc.vector.tensor_copy(out=Z[:, g, lc * 256:(lc + 1) * 256], in_=acc[:, 0:256]),
                         reads=[acc], writes=[Z])
    for g in range(8):
        for c in range(2):
            acc = p.acc()

            def mm():
                nc.tensor.matmul(acc[:], lhsT=cd[:, 0, :], rhs=Zc[:, g, c * 512:(c + 1) * 512], start=True, stop=False)
                return nc.tensor.matmul(acc[:], lhsT=cd[:, 1, :], rhs=Zs[:, g, c * 512:(c + 1) * 512], start=False,
                                        stop=True)
            p.op('pe', mm, reads=[cd, Zc, Zs], writes=[acc])
            p.op('act' if c == 0 else 'dve',
                 (lambda: nc.scalar.copy(out=YT[:, g, c * 512:(c + 1) * 512], in_=acc[:])) if c == 0 else
                 (lambda: nc.vector.tensor_copy(out=YT[:, g, c * 512:(c + 1) * 512], in_=acc[:])),
                 reads=[acc], writes=[YT])
    ph.close()

    wo = p.sb("wo", [128, KC, D], BF16)
    for h in range(4):
        p.dma('pool', wo[:, h * 4:(h + 1) * 4, :], wo_d[h * 512:(h + 1) * 512, :].rearrange("(k p) n -> p k n", p=128),
              reads=[wo_d], writes=[wo])

    ph = Phase(p)
    kT = ph.sb("kTs", [128, 2, NKEY], BF16)
    vv = ph.sb("vv", [128, NKT, 256], BF16)
    qT = ph.sb("qTs", [128, 8, TPC], BF16)
    p.dma('sp', kT[:], kT_d[:], reads=[kT_d], writes=[kT])
    p.dma('sp', vv[:], v_d.t.ap().rearrange("(t p) n -> p t n", p=128), reads=[v_d], writes=[vv])
    p.dma('sp', qT[:], qT_d[:], reads=[qT_d], writes=[qT])
    PTs = ph.scratch("PT", [128, 512], BF16, 3)
    rDs = ph.scratch("rD", [128, 512], F32, 2)
    scale = float(128 ** -0.5)
    it = 0
    for h in range(8):
        kvh = h // 4
        for qc in range(2):
            accO = p.accs[0 + 2 * (it % 2)]
            accD = p.accs[1 + 2 * (it % 2)]
            it += 1
            def emit_s(kt):
                accS = p.accs[4 + (kt % 3)]
                p.op('pe', lambda: nc.tensor.matmul(accS[:], lhsT=kT[:, kvh, kt * 128:(kt + 1) * 128],
                                                    rhs=qT[:, h, qc * 512:(qc + 1) * 512], start=True, stop=True),
                     reads=[kT, qT], writes=[accS])
            emit_s(0)
            for kt in range(NKT):
                accS = p.accs[4 + (kt % 3)]
                if kt + 1 < NKT:
                    emit_s(kt + 1)
                PT = PTs.get()
                p.op('act', lambda: nc.scalar.activation(out=PT[:], in_=accS[:], func=AF.Exp, scale=scale),
                     reads=[accS], writes=[PT])

                def mm2():
                    nc.tensor.matmul(accO[:], lhsT=vv[:, kt, kvh * 128:(kvh + 1) * 128], rhs=PT[:], start=(kt == 0),
                                     stop=(kt == NKT - 1))
                    return nc.tensor.matmul(accD[:], lhsT=ones[:], rhs=PT[:], start=(kt == 0), stop=(kt == NKT - 1))
                p.op('pe', mm2, reads=[vv, PT, ones], writes=[accO, accD])
            rD = rDs.get()
            p.op('dve', lambda: nc.vector.reciprocal(out=rD[:], in_=accD[:]), reads=[accD], writes=[rD])
            p.op('dve', lambda: nc.vector.tensor_tensor(out=OT[:, h, qc * 512:(qc + 1) * 512], in0=accO[:], in1=rD[:],
                                                        op=ALU.mult), reads=[accO, rD], writes=[OT])
    ph.close()

    ph = Phase(p)
    rb = load_rows_b(p, ph, rows_d, ["gate1", "A2", "B2", "sc2"])
    p.op('dve', lambda: nc.vector.scalar_tensor_tensor(out=rb["A2"][:], in0=rb["sc2"][:], scalar=1.0, in1=rb["A2"][:],
                                                       op0=ALU.add, op1=ALU.mult), reads=[rb["sc2"], rb["A2"]],
         writes=[rb["A2"]])
    wr = ph.sb("wr", [128, KC, 32], F32)
    brb = ph.sb("brb", [128, 32], F32)
    p.dma('sp', wr[:], wr_d[:], reads=[wr_d], writes=[wr])
    p.dma('sp', brb[:], br_d[0:1, :].partition_broadcast(128), reads=[br_d], writes=[brb])
    prep = Prep(p, ph, rb["A2"], rb["B2"], wr, brb, tpf)
    xs = ph.scratch("xs", [128, D], F32, 2)
    x1s = ph.scratch("x1s", [128, D], F32, 2)
    tmp = ph.scratch("optmp", [128, 512], F32, 2)
    p.acc_i = 0
    for t in range(TPC // 128):
        xt, x1 = xs.get(), x1s.get()
        p.dma('sp', xt[:], x[t * 128:(t + 1) * 128, :], reads=[x], writes=[xt])
        for c in range(4):
            acc = p.acc()

            def mm():
                for k in range(KC):
                    src = OT if k < 8 else YT
                    i = nc.tensor.matmul(acc[:], lhsT=src[:, k % 8, t * 128:(t + 1) * 128],
                                         rhs=wo[:, k, c * 512:(c + 1) * 512], start=(k == 0), stop=(k == KC - 1))
                return i
            p.op('pe', mm, reads=[OT, YT, wo], writes=[acc])
            tm = tmp.get()
            p.op('dve', lambda: nc.vector.tensor_tensor(out=tm[:], in0=acc[:], in1=rb["gate1"][:, c * 512:(c + 1) * 512],
                                                        op=ALU.mult), reads=[acc, rb["gate1"]], writes=[tm])
            p.op('pool', lambda: nc.gpsimd.tensor_tensor(out=x1[:, c * 512:(c + 1) * 512], in0=tm[:],
                                                         in1=xt[:, c * 512:(c + 1) * 512], op=ALU.add),
                 reads=[tm, xt], writes=[x1])
        p.dma('sp', x1_o[t * 128:(t + 1) * 128, :], x1[:], reads=[x1], writes=[x1_o])
        prep.emit(x1, t_o[t * 128:(t + 1) * 128, :], t_o, g_o[t * 128:(t + 1) * 128, :], g_o)
    p.wait_all('sp', [x1_o, t_o, g_o])
    ph.close()
    return nc


NTOK = 8192
EPC = 4
SCH = 1024
LIMIT = 7.0
ALPHA = 1.702


def build_p4():
    nc, p = new_prog()
    tT_d = p.dram("tT", [128, KC, NTOK], BF16, "ExternalInput")
    gts_d = p.dram("gts", [128, NTOK // 128, EPC], F32, "ExternalInput")
    w1_d = p.dram("w1", [EPC, D, D], F32, "ExternalInput")
    b1_d = p.dram("b1c", [128, EPC, 16], F32, "ExternalInput")
    w2_d = p.dram("w2", [EPC, 1024, D], F32, "ExternalInput")
    b2_d = p.dram("b2", [EPC, D], F32, "ExternalInput")
    yp_o = p.dram("yp", [NTOK, D], BF16, "ExternalOutput")
    p.accs = [p.ps(f"acc{i}", [128, 512], F32) for i in range(8)]
    ones1 = p.sb("ones1", [1, 128], BF16)
    p.op('pool', lambda: nc.gpsimd.memset(ones1[:], 1.0), writes=[ones1])
    gts = p.sb("gts_s", [128, NTOK // 128, EPC], F32)
    b1c = p.sb("b1c_s", [128, EPC, 16], F32)
    p.dma('sp', gts[:], gts_d[:], reads=[gts_d], writes=[gts])
    p.dma('sp', b1c[:], b1_d[:], reads=[b1_d], writes=[b1c])
    hT = p.sb("hT", [128, KC, SCH], BF16)
    yacc = [p.sb(f"yacc{i}", [128, D], F32) for i in range(SCH // 128)]
    w1ss = Scratch(p, "w1s", [128, KC, 512], BF16, 2)
    w2ss = Scratch(p, "w2s", [128, 2, D], BF16, 2)
    b2ss = Scratch(p, "b2s", [1, D], BF16, 2)
    actTs = Scratch(p, "actT", [128, 2, SCH], BF16, 2)
    g32s = Scratch(p, "g32", [128, 512], F32, 2)
    sgs = Scratch(p, "sg", [128, 512], F32, 2)
    l32s = Scratch(p, "l32", [128, 512], F32, 2)
    obs = Scratch(p, "ob", [128, D], BF16, 2)
    for sc in range(NTOK // SCH):
        for h in range(2):
            p.dma('sp', hT[:, h * 8:(h + 1) * 8, :], tT_d[:, h * 8:(h + 1) * 8, sc * SCH:(sc + 1) * SCH], reads=[tT_d],
                  writes=[hT])
        for e in range(EPC):
            b2s = b2ss.get()
            p.dma('pool', b2s[:], b2_d[e:e + 1, :], reads=[b2_d], writes=[b2s])
            for s in range(4):
                w1s, w2s, actT = w1ss.get(), w2ss.get(), actTs.get()
                p.dma('pool', w1s[:, :, 0:256], w1_d[e, :, s * 256:(s + 1) * 256].rearrange("(k p) n -> p k n", p=128),
                      reads=[w1_d], writes=[w1s])
                p.dma('pool', w1s[:, :, 256:512],
                      w1_d[e, :, 1024 + s * 256:1024 + (s + 1) * 256].rearrange("(k p) n -> p k n", p=128),
                      reads=[w1_d], writes=[w1s])
                p.dma('pool', w2s[:], w2_d[e, s * 256:(s + 1) * 256, :].rearrange("(k p) n -> p k n", p=128),
                      reads=[w2_d], writes=[w2s])
                for tc in range(SCH // 512):
                    for j in range(2):
                        accG, accL = p.acc(), p.acc()
                        for acc, off in ((accG, 0), (accL, 256)):
                            def mm():
                                for k in range(KC):
                                    i = nc.tensor.matmul(acc[:], lhsT=w1s[:, k, off + j * 128:off + (j + 1) * 128],
                                                         rhs=hT[:, k, tc * 512:(tc + 1) * 512], start=(k == 0),
                                                         stop=(k == KC - 1))
                                return i
                            p.op('pe', mm, reads=[w1s, hT], writes=[acc])
                        g32, sg, l32 = g32s.get(), sgs.get(), l32s.get()
                        jj = s * 2 + j
                        p.op('dve', lambda: nc.vector.tensor_scalar(out=g32[:], in0=accG[:], scalar1=b1c[:, e, jj:jj + 1],
                                                                    scalar2=LIMIT, op0=ALU.add, op1=ALU.min),
                             reads=[accG, b1c], writes=[g32])
                        p.op('act', lambda: nc.scalar.activation(out=sg[:], in_=g32[:], func=AF.Sigmoid, scale=ALPHA),
                             reads=[g32], writes=[sg])
                        p.op('dve', lambda: nc.vector.tensor_scalar(out=l32[:], in0=accL[:],
                                                                    scalar1=b1c[:, e, 8 + jj:8 + jj + 1], scalar2=LIMIT,
                                                                    op0=ALU.add, op1=ALU.min), reads=[accL, b1c],
                             writes=[l32])
                        p.op('pool', lambda: nc.gpsimd.tensor_scalar(out=l32[:], in0=l32[:], scalar1=-LIMIT, scalar2=1.0,
                                                                     op0=ALU.max, op1=ALU.add), reads=[l32], writes=[l32])
                        p.op('pool', lambda: nc.gpsimd.tensor_tensor(out=g32[:], in0=g32[:], in1=sg[:], op=ALU.mult),
                             reads=[g32, sg], writes=[g32])
                        p.op('pool', lambda: nc.gpsimd.tensor_tensor(out=actT[:, j, tc * 512:(tc + 1) * 512], in0=g32[:],
                                                                     in1=l32[:], op=ALU.mult), reads=[g32, l32],
                             writes=[actT])
                for tt in range(SCH // 128):
                    gcol = gts[:, sc * (SCH // 128) + tt, e:e + 1]
                    for c in range(4):
                        acc = p.acc()

                        def mm2():
                            if s == 0:
                                nc.tensor.matmul(acc[:], lhsT=ones1[0:1, :], rhs=b2s[0:1, c * 512:(c + 1) * 512],
                                                 start=True, stop=False)
                            for j in range(2):
                                i = nc.tensor.matmul(acc[:], lhsT=actT[:, j, tt * 128:(tt + 1) * 128],
                                                     rhs=w2s[:, j, c * 512:(c + 1) * 512], start=(j == 0 and s != 0),
                                                     stop=(j == 1))
                            return i
                        p.op('pe', mm2, reads=[actT, w2s, b2s, ones1], writes=[acc])
                        ya = yacc[tt]
                        if e == 0 and s == 0:
                            p.op('dve', lambda: nc.vector.tensor_scalar(out=ya[:, c * 512:(c + 1) * 512], in0=acc[:],
                                                                        scalar1=gcol, scalar2=None, op0=ALU.mult),
                                 reads=[acc, gts], writes=[ya])
                        else:
                            p.op('dve', lambda: nc.vector.scalar_tensor_tensor(
                                out=ya[:, c * 512:(c + 1) * 512], in0=acc[:], scalar=gcol,
                                in1=ya[:, c * 512:(c + 1) * 512], op0=ALU.mult, op1=ALU.add),
                                reads=[acc, gts, ya], writes=[ya])
        for tt in range(SCH // 128):
            ob = obs.get()
            p.op('act', lambda: nc.scalar.copy(out=ob[:], in_=yacc[tt][:]), reads=[yacc[tt]], writes=[ob])
            p.dma('sp', yp_o[sc * SCH + tt * 128:sc * SCH + (tt + 1) * 128, :], ob[:], reads=[ob], writes=[yp_o])
    p.wait_all('sp', [yp_o])
    return nc


def emit_combine(p, xt, rows, ypt, gate_b, xo, tmps):
    nc = p.nc
    for c in range(4):
        acc = p.acc()

        def mm():
            for i in range(8):
                ins = nc.tensor.matmul(acc[0:rows, :], lhsT=p.ident[0:rows, 0:rows], rhs=ypt[0:rows, i, c * 512:(c + 1) * 512],
                                       start=(i == 0), stop=(i == 7))
            return ins
        p.op('pe', mm, reads=[p.ident, ypt], writes=[acc])
        tm = tmps.get()
        p.op('dve', lambda: nc.vector.tensor_tensor(out=tm[0:rows, :], in0=acc[0:rows, :],
                                                    in1=gate_b[0:rows, c * 512:(c + 1) * 512], op=ALU.mult),
             reads=[acc, gate_b], writes=[tm])
        p.op('pool', lambda: nc.gpsimd.tensor_tensor(out=xo[0:rows, c * 512:(c + 1) * 512], in0=tm[0:rows, :],
                                                     in1=xt[0:rows, c * 512:(c + 1) * 512], op=ALU.add),
             reads=[tm, xt], writes=[xo])


def setup_ident(p):
    nc = p.nc
    identf = p.sb("identf", [128, 128], F32)
    ident = p.sb("ident", [128, 128], BF16)
    p.op('pool', lambda: nc.gpsimd.memset(identf[:], 0.0), writes=[identf])
    p.op('pool', lambda: nc.gpsimd.affine_select(out=identf[:], in_=identf[:], pattern=[[-1, 128]],
                                                 compare_op=ALU.not_equal, fill=1.0, base=0, channel_multiplier=1),
         reads=[identf], writes=[identf])
    p.op('dve', lambda: nc.vector.tensor_copy(out=ident[:], in_=identf[:]), reads=[identf], writes=[ident])
    p.ident, p.identf = ident, identf


HALO = 16
LTOK = TPC + 2 * HALO
CW = 31


def build_p6():
    nc, p = new_prog()
    x1h_d = p.dram("x1h", [LTOK, D], F32, "ExternalInput")
    yph_d = p.dram("yph", [8, LTOK, D], BF16, "ExternalInput")
    rows_d = p.dram("rows", [6, D], F32, "ExternalInput")
    cols_d = p.dram("cols", [128, 3, KC], F32, "ExternalInput")
    cvb_d = p.dram("cvb", [128, 32 + 16 + 16 + 16], F32, "ExternalInput")
    wdw_d = p.dram("wdw", [128, KC, CW], F32, "ExternalInput")
    mask_d = p.dram("mask", [128, 2], F32, "ExternalInput")
    wi_d = p.dram("cv_w_in", [KC, 128, KC, 256], F32, "ExternalInput")
    wo_d = p.dram("cv_w_out", [D, D], F32, "ExternalInput")
    wr_d = p.dram("w_router", [128, KC, 32], F32, "ExternalInput")
    br_d = p.dram("b_router", [1, 32], F32, "ExternalInput")
    x2_o = p.dram("x2", [TPC, D], F32, "ExternalOutput")
    t_o = p.dram("t", [TPC, D], BF16, "ExternalOutput")
    g_o = p.dram("gates", [TPC, 32], F32, "ExternalOutput")
    x1p = p.dram("x1p", [LTOK, D], F32)
    p.accs = [p.ps(f"acc{i}", [128, 512], F32) for i in range(6)]
    p.tps = [p.ps("tpb0", [128, 1024], BF16)]
    tpf = p.ps("tpf", [128, 512], F32)
    setup_ident(p)
    ones = p.sb("ones", [128, 128], BF16)
    ones1 = p.sb("ones1", [1, 128], BF16)
    p.op('pool', lambda: nc.gpsimd.memset(ones[:], 1.0), writes=[ones])
    p.op('pool', lambda: nc.gpsimd.memset(ones1[:], 1.0), writes=[ones1])
    colsb = p.sb("colsb", [128, 3, KC], F32)
    cvb = p.sb("cvbs", [128, 80], F32)
    wdw = p.sb("wdws", [128, KC, CW], F32)
    msk = p.sb("msks", [128, 2], F32)
    p.dma('sp', colsb[:], cols_d[:], reads=[cols_d], writes=[colsb])
    p.dma('sp', cvb[:], cvb_d[:], reads=[cvb_d], writes=[cvb])
    p.dma('sp', wdw[:], wdw_d[:], reads=[wdw_d], writes=[wdw])
    p.dma('sp', msk[:], mask_d[:], reads=[mask_d], writes=[msk])
    A1 = p.sb("A1", [128, KC], F32)
    B1 = p.sb("B1", [128, KC], F32)
    p.op('dve', lambda: nc.vector.scalar_tensor_tensor(out=A1[:], in0=colsb[:, 2, :], scalar=1.0, in1=colsb[:, 0, :],
                                                       op0=ALU.add, op1=ALU.mult), reads=[colsb], writes=[A1])
    p.op('dve', lambda: nc.vector.tensor_copy(out=B1[:], in_=colsb[:, 1, :]), reads=[colsb], writes=[B1])
    uc = p.sb("uc", [128, KC, TPC], BF16)
    mean_b = [p.sb(f"mean_b{c}", [128, 512], F32) for c in range(2)]
    rstd_b = [p.sb(f"rstd_b{c}", [128, 512], F32) for c in range(2)]
    phA = Phase(p)
    hT = phA.sb("hT", [128, KC, LTOK], BF16)

    ph = Phase(p)
    g2b = ph.sb("g2b", [128, D], F32)
    p.dma('sp', g2b[:], rows_d[0:1, :].partition_broadcast(128), reads=[rows_d], writes=[g2b])
    nt = NormT.__new__(NormT)
    nt.p = p
    nt.sq = ph.scratch("nt_sq", [128, D], BF16, 1)
    nt.ss = ph.scratch("nt_ss", [128, 1], F32, 2)
    nt.xn = ph.scratch("nt_xn", [128, D], BF16, 2)
    xs = ph.scratch("xs", [128, D], F32, 2)
    xos = ph.scratch("xo", [128, D], F32, 2)
    yps = ph.scratch("ypt", [128, 8, D], BF16, 2)
    tmps = ph.scratch("ctmp", [128, 512], F32, 2)
    ntl = (LTOK + 127) // 128
    for t in range(ntl):
        r0 = t * 128
        rows = min(128, LTOK - r0)
        xt, xo, ypt = xs.get(), xos.get(), yps.get()
        p.dma('sp', xt[0:rows, :], x1h_d[r0:r0 + rows, :], reads=[x1h_d], writes=[xt])
        p.dma('sp', ypt[0:rows, :, :], yph_d[:, r0:r0 + rows, :].rearrange("c t d -> t c d"), reads=[yph_d], writes=[ypt])
        emit_combine(p, xt, rows, ypt, g2b, xo, tmps)
        p.dma('sp', x1p[r0:r0 + rows, :], xo[0:rows, :], reads=[xo], writes=[x1p])
        nt.emit(xo, rows, A1, B1, hT, r0)
    ph.close()

    ph = Phase(p)
    wis = ph.scratch("wis", [128, KC, 256], BF16, 2)
    uTs = ph.scratch("uT", [128, LTOK], BF16, 2)
    sgs = ph.scratch("sgm", [128, 512], F32, 2)
    dgs = ph.scratch("dg", [128, CW, 128], BF16, 2)
    sqs = ph.scratch("csq", [128, 512], BF16, 2)
    accM = [p.ps(f"accM{c}", [128, 512], F32) for c in range(0)]
    accM = [p.accs[0], p.accs[1]]
    accQ = [p.accs[2], p.accs[3]]
    rot = [p.accs[4], p.accs[5], tpf]
    ri = 0
    chunks = [(0, 512), (512, 512), (1024, LTOK - 1024)]
    for ct in range(KC):
        wi, uT, dg = wis.get(), uTs.get(), dgs.get()
        p.dma('pool', wi[:], wi_d[ct], reads=[wi_d], writes=[wi])
        p.op('pool', lambda: nc.gpsimd.tensor_tensor(
            out=dg[:], in0=p.ident[:].unsqueeze(1).broadcast_to([128, CW, 128]),
            in1=wdw[:, ct, :].unsqueeze(2).broadcast_to([128, CW, 128]), op=ALU.mult), reads=[p.ident, wdw], writes=[dg])
        for (c0, cn) in chunks:
            accA, accG = rot[ri % 3], rot[(ri + 1) % 3]
            ri += 2
            for acc, off in ((accA, 0), (accG, 128)):
                def mm():
                    for k in range(KC):
                        i = nc.tensor.matmul(acc[:, 0:cn], lhsT=wi[:, k, off:off + 128], rhs=hT[:, k, c0:c0 + cn],
                                             start=(k == 0), stop=(k == KC - 1))
                    return i
                p.op('pe', mm, reads=[wi, hT], writes=[acc])
            sg = sgs.get()
            p.op('act', lambda: nc.scalar.activation(out=sg[:, 0:cn], in_=accG[:, 0:cn], func=AF.Sigmoid,
                                                     bias=cvb[:, 16 + ct:16 + ct + 1], scale=1.0), reads=[accG, cvb],
                 writes=[sg])
            p.op('dve', lambda: nc.vector.scalar_tensor_tensor(out=uT[:, c0:c0 + cn], in0=accA[:, 0:cn],
                                                               scalar=cvb[:, ct:ct + 1], in1=sg[:, 0:cn], op0=ALU.add,
                                                               op1=ALU.mult), reads=[accA, cvb, sg], writes=[uT])
        p.op('dve', lambda: nc.vector.tensor_scalar(out=uT[:, 0:HALO], in0=uT[:, 0:HALO], scalar1=msk[:, 0:1], scalar2=None,
                                                    op0=ALU.mult), reads=[uT, msk], writes=[uT])
        p.op('dve', lambda: nc.vector.tensor_scalar(out=uT[:, LTOK - HALO:LTOK], in0=uT[:, LTOK - HALO:LTOK],
                                                    scalar1=msk[:, 1:2], scalar2=None, op0=ALU.mult), reads=[uT, msk],
             writes=[uT])
        for c in range(2):
            acc = rot[ri % 3]
            ri += 1

            def mmc():
                for j in range(CW):
                    i = nc.tensor.matmul(acc[:], lhsT=dg[:, j, :], rhs=uT[:, c * 512 + j + 1:c * 512 + j + 1 + 512],
                                         start=(j == 0), stop=(j == CW - 1))
                return i
            p.op('pe', mmc, reads=[dg, uT], writes=[acc])
            p.op('dve', lambda: nc.vector.tensor_scalar(out=uc[:, ct, c * 512:(c + 1) * 512], in0=acc[:],
                                                        scalar1=cvb[:, 32 + ct:32 + ct + 1], scalar2=None, op0=ALU.add),
                 reads=[acc, cvb], writes=[uc])
            sq = sqs.get()
            p.op('act', lambda: nc.scalar.activation(out=sq[:], in_=uc[:, ct, c * 512:(c + 1) * 512], func=AF.Square),
                 reads=[uc], writes=[sq])

            def mms():
                nc.tensor.matmul(accM[c][:], lhsT=ones[:], rhs=uc[:, ct, c * 512:(c + 1) * 512], start=(ct == 0),
                                 stop=(ct == KC - 1))
                return nc.tensor.matmul(accQ[c][:], lhsT=ones[:], rhs=sq[:], start=(ct == 0), stop=(ct == KC - 1))
            p.op('pe', mms, reads=[ones, uc, sq], writes=[accM[c], accQ[c]])
    for c in range(2):
        p.op('dve', lambda: nc.vector.tensor_scalar(out=mean_b[c][:], in0=accM[c][:], scalar1=1.0 / D, scalar2=None,
                                                    op0=ALU.mult), reads=[accM[c]], writes=[mean_b[c]])
        p.op('dve', lambda: nc.vector.tensor_tensor(out=rstd_b[c][:], in0=mean_b[c][:], in1=mean_b[c][:], op=ALU.mult),
             reads=[mean_b[c]], writes=[rstd_b[c]])
        p.op('dve', lambda: nc.vector.scalar_tensor_tensor(out=rstd_b[c][:], in0=accQ[c][:], scalar=1.0 / D,
                                                           in1=rstd_b[c][:], op0=ALU.mult, op1=ALU.subtract),
             reads=[accQ[c], rstd_b[c]], writes=[rstd_b[c]])
        p.op('dve', lambda: nc.vector.tensor_scalar(out=rstd_b[c][:], in0=rstd_b[c][:], scalar1=EPS, scalar2=None,
                                                    op0=ALU.add), reads=[rstd_b[c]], writes=[rstd_b[c]])
        p.op('act', lambda: nc.scalar.sqrt(out=rstd_b[c][:], in_=rstd_b[c][:]), reads=[rstd_b[c]], writes=[rstd_b[c]])
        p.op('dve', lambda: nc.vector.reciprocal(out=rstd_b[c][:], in_=rstd_b[c][:]), reads=[rstd_b[c]],
             writes=[rstd_b[c]])
    ph.close()
    phA.close()

    ph = Phase(p)
    wo = ph.sb("wo", [128, KC, D], BF16)
    for h in range(4):
        p.dma('pool', wo[:, h * 4:(h + 1) * 4, :], wo_d[h * 512:(h + 1) * 512, :].rearrange("(k p) n -> p k n", p=128),
              reads=[wo_d], writes=[wo])
    lts = ph.scratch("lnt", [128, 512], F32, 2)
    for ct in range(KC):
        for c in range(2):
            lt = lts.get()
            p.op('dve', lambda: nc.vector.tensor_tensor(out=lt[:], in0=uc[:, ct, c * 512:(c + 1) * 512], in1=mean_b[c][:],
                                                        op=ALU.subtract), reads=[uc, mean_b[c]], writes=[lt])
            p.op('pool', lambda: nc.gpsimd.tensor_tensor(out=lt[:], in0=lt[:], in1=rstd_b[c][:], op=ALU.mult),
                 reads=[lt, rstd_b[c]], writes=[lt])
            p.op('act', lambda: nc.scalar.activation(out=uc[:, ct, c * 512:(c + 1) * 512], in_=lt[:], func=AF.Silu,
                                                     scale=cvb[:, 48 + ct:48 + ct + 1], bias=cvb[:, 64 + ct:64 + ct + 1]),
                 reads=[lt, cvb], writes=[uc])
    rb = load_rows_b(p, ph, rows_d, ["gate1", "A2", "B2", "sc2"], off=1)
    p.op('dve', lambda: nc.vector.scalar_tensor_tensor(out=rb["A2"][:], in0=rb["sc2"][:], scalar=1.0, in1=rb["A2"][:],
                                                       op0=ALU.add, op1=ALU.mult), reads=[rb["sc2"], rb["A2"]],
         writes=[rb["A2"]])
    bo = ph.sb("bo", [1, D], BF16)
    p.dma('pool', bo[:], rows_d[5:6, :], reads=[rows_d], writes=[bo])
    wr = ph.sb("wr", [128, KC, 32], F32)
    brb = ph.sb("brb", [128, 32], F32)
    p.dma('sp', wr[:], wr_d[:], reads=[wr_d], writes=[wr])
    p.dma('sp', brb[:], br_d[0:1, :].partition_broadcast(128), reads=[br_d], writes=[brb])
    prep = Prep(p, ph, rb["A2"], rb["B2"], wr, brb, tpf)
    xs = ph.scratch("xs4", [128, D], F32, 2)
    x2s = ph.scratch("x2s", [128, D], F32, 1)
    tmp = ph.scratch("optmp", [128, 512], F32, 2)
    p.acc_i = 0
    for t in range(TPC // 128):
        xt, x2 = xs.get(), x2s.get()
        p.dma('sp', xt[:], x1p[HALO + t * 128:HALO + (t + 1) * 128, :], reads=[x1p], writes=[xt])
        for c in range(4):
            acc = p.acc()

            def mm():
                nc.tensor.matmul(acc[:], lhsT=ones1[0:1, :], rhs=bo[0:1, c * 512:(c + 1) * 512], start=True, stop=False)
                for k in range(KC):
                    i = nc.tensor.matmul(acc[:], lhsT=uc[:, k, t * 128:(t + 1) * 128], rhs=wo[:, k, c * 512:(c + 1) * 512],
                                         start=False, stop=(k == KC - 1))
                return i
            p.op('pe', mm, reads=[uc, wo, bo, ones1], writes=[acc])
            tm = tmp.get()
            p.op('dve', lambda: nc.vector.tensor_tensor(out=tm[:], in0=acc[:], in1=rb["gate1"][:, c * 512:(c + 1) * 512],
                                                        op=ALU.mult), reads=[acc, rb["gate1"]], writes=[tm])
            p.op('pool', lambda: nc.gpsimd.tensor_tensor(out=x2[:, c * 512:(c + 1) * 512], in0=tm[:],
                                                         in1=xt[:, c * 512:(c + 1) * 512], op=ALU.add),
                 reads=[tm, xt], writes=[x2])
        p.dma('sp', x2_o[t * 128:(t + 1) * 128, :], x2[:], reads=[x2], writes=[x2_o])
        prep.emit(x2, t_o[t * 128:(t + 1) * 128, :], t_o, g_o[t * 128:(t + 1) * 128, :], g_o)
    p.wait_all('sp', [x2_o, t_o, g_o])
    ph.close()
    return nc


def build_p8():
    nc, p = new_prog()
    x2_d = p.dram("x2", [TPC, D], F32, "ExternalInput")
    yph_d = p.dram("yph", [8, TPC, D], BF16, "ExternalInput")
    rows_d = p.dram("rows", [2, D], F32, "ExternalInput")
    out_o = p.dram("out", [TPC, D], F32, "ExternalOutput")
    p.accs = [p.ps(f"acc{i}", [128, 512], F32) for i in range(8)]
    setup_ident(p)
    ph = Phase(p)
    rb = load_rows_b(p, ph, rows_d, ["g2", "gf"])
    xs = ph.scratch("xs", [128, D], F32, 2)
    xos = ph.scratch("xo", [128, D], F32, 2)
    yps = ph.scratch("ypt", [128, 8, D], BF16, 2)
    tmps = ph.scratch("ctmp", [128, 512], F32, 2)
    sqs = ph.scratch("sq", [128, D], BF16, 1)
    sss = ph.scratch("ss", [128, 1], F32, 2)
    oos = ph.scratch("oo", [128, D], F32, 2)
    for t in range(TPC // 128):
        r0 = t * 128
        xt, xo, ypt, sq, ss, oo = xs.get(), xos.get(), yps.get(), sqs.get(), sss.get(), oos.get()
        p.dma('sp', xt[:], x2_d[r0:r0 + 128, :], reads=[x2_d], writes=[xt])
        p.dma('sp', ypt[:], yph_d[:, r0:r0 + 128, :].rearrange("c t d -> t c d"), reads=[yph_d], writes=[ypt])
        emit_combine(p, xt, 128, ypt, rb["g2"], xo, tmps)
        p.op('act', lambda: nc.scalar.activation(out=sq[:], in_=xo[:], func=AF.Square, accum_out=ss[:]), reads=[xo],
             writes=[sq, ss])
        emit_rstd(p, ss[:], 128, 1.0 / D, ss)
        p.op('dve', lambda: nc.vector.scalar_tensor_tensor(out=oo[:], in0=xo[:], scalar=ss[:, 0:1], in1=rb["gf"][:],
                                                           op0=ALU.mult, op1=ALU.mult), reads=[xo, ss, rb["gf"]],
             writes=[oo])
        p.dma('sp', out_o[r0:r0 + 128, :], oo[:], reads=[oo], writes=[out_o])
    p.wait_all('sp', [out_o])
    ph.close()
    return nc


def _run(nc, ims):
    res = run_bass_kernel_spmd(nc, ims, core_ids=list(range(NCORES)))
    return res.results


def _cf(v, n):
    return np.ascontiguousarray(np.asarray(v).reshape(n, 128).T)


def _rope_table(pos):
    inv = (10000.0 ** (-np.arange(0, 64, 2, dtype=np.float32) / np.float32(64))).astype(np.float32)
    ar = ((pos // 64).astype(np.float32)[:, None] * inv).astype(np.float64)
    ac = ((pos % 64).astype(np.float32)[:, None] * inv).astype(np.float64)
    cr, sr, cc, sc = np.cos(ar), np.sin(ar), np.cos(ac), np.sin(ac)
    return np.ascontiguousarray(np.stack([np.concatenate([cr, cr, cc, cc], 1),
                                          np.concatenate([-sr, sr, -sc, sc], 1)], 1).astype(np.float32))


def _dft_tables(qt):
    l = np.arange(4096, dtype=np.int64)[:, None]
    lp = (qt * TPC + np.arange(TPC, dtype=np.int64))[None, :]
    ang = 2.0 * np.pi * ((l * lp) % 4096).astype(np.float64) / 4096.0
    lay = lambda a: np.ascontiguousarray(a.astype(NPBF).reshape(32, 128, 4, 256).transpose(2, 1, 0, 3))
    return lay(np.cos(ang)), lay(np.sin(ang))


def _cdft():
    c = np.arange(128, dtype=np.int64)[:, None]
    cp = np.arange(128, dtype=np.int64)[None, :]
    ang = 2.0 * np.pi * ((c * cp) % 128).astype(np.float64) / 128.0
    s = np.sqrt(4096.0 * 128.0)
    return np.ascontiguousarray(np.stack([np.cos(ang) / s, -np.sin(ang) / s], 1).astype(NPBF))


def _moe_launch(layer, t_all, gates_all, moe_w1, moe_b1, moe_w2, moe_b2):
    gi = np.ascontiguousarray(gates_all.reshape(NTOK // 128, 128, NEXP).transpose(1, 0, 2))
    info = np.asarray(_run(build_p5(), [{"g": gi}] * NCORES)[0]["info"])[0]
    rank_of = [int(v) for v in info[:NEXP]]
    expert_at = [0] * NEXP
    for e, r in enumerate(rank_of):
        expert_at[r] = e
    caps = []
    for j in range(EPC):
        caps.append(next(cb for cb in range(1, NTOK // 128 + 1) if info[NEXP + j] <= cb * 128))
    ims = []
    t_rows = np.ascontiguousarray(t_all)
    for i in range(NCORES):
        ex = [expert_at[8 * j + i] for j in range(EPC)]
        w1 = np.asarray(moe_w1[layer][ex])
        w1p = np.ascontiguousarray(np.concatenate([w1[..., 0::2], w1[..., 1::2]], -1))
        b1 = np.asarray(moe_b1[layer][ex])
        b1p = np.concatenate([b1[..., 0::2], b1[..., 1::2]], -1)
        ims.append({
            "t": t_rows,
            "gts": np.ascontiguousarray(gates_all[:, ex].reshape(NTOK // 128, 128, EPC).transpose(1, 0, 2)),
            "w1": w1p,
            "b1c": np.ascontiguousarray(b1p.reshape(EPC, 16, 128).transpose(2, 0, 1)),
            "w2": np.ascontiguousarray(moe_w2[layer][ex]),
            "b2": np.ascontiguousarray(moe_b2[layer][ex]),
        })
    res = _run(build_p4s(tuple(caps)), ims)
    overflow = any(bool((np.asarray(r["cnt"])[0] > np.array(caps) * 128).any()) for r in res)
    if overflow:
        tT = np.ascontiguousarray(t_all.reshape(NTOK, KC, 128).transpose(2, 1, 0))
        for im in ims:
            del im["t"]
            im["tT"] = tT
        res = _run(build_p4(), ims)
    return np.stack([np.asarray(r["yp"]) for r in res], 0)


def kernel(x, c, ctx, c_ctx, w_mod, b_mod, g_mix, g_ffn, ab_w_in, ab_q_gain, ab_k_gain, ab_w_out,
           cv_w_in, cv_b_in, cv_w_dw, cv_b_dw, cv_ln_g, cv_ln_b, cv_w_out, cv_b_out,
           moe_w_router, moe_b_router, moe_w1, moe_b1, moe_w2, moe_b2, g_final):
    A = lambda a: np.asarray(a, dtype=np.float32)
    x, c, ctx, c_ctx, w_mod, b_mod, g_mix, g_ffn = map(A, (x, c, ctx, c_ctx, w_mod, b_mod, g_mix, g_ffn))
    ab_w_in, ab_q_gain, ab_k_gain, ab_w_out = map(A, (ab_w_in, ab_q_gain, ab_k_gain, ab_w_out))
    cv_w_in, cv_b_in, cv_w_dw, cv_b_dw, cv_ln_g, cv_ln_b, cv_w_out, cv_b_out = map(
        A, (cv_w_in, cv_b_in, cv_w_dw, cv_b_dw, cv_ln_g, cv_ln_b, cv_w_out, cv_b_out))
    moe_w_router, moe_b_router, moe_w1, moe_b1, moe_w2, moe_b2, g_final = map(
        A, (moe_w_router, moe_b_router, moe_w1, moe_b1, moe_w2, moe_b2, g_final))
    B, L, _ = x.shape
    cores = [(i // 4, i % 4) for i in range(NCORES)]

    cv = np.stack([c[0], c[1], c_ctx])
    cT = np.ascontiguousarray(cv.T.reshape(KC, 128, 3).transpose(1, 0, 2))
    ims = []
    for i in range(NCORES):
        sl = slice(i * MODW, (i + 1) * MODW)
        ims.append({"wmod": np.ascontiguousarray(w_mod[:, :, sl]),
                    "bmod": np.ascontiguousarray(np.broadcast_to(b_mod[:, None, sl], (2, 3, MODW))), "cT": cT})
    res = _run(build_p1(), ims)
    m = np.concatenate([np.asarray(r["m"]) for r in res], -1)
    mod = m.reshape(2, 3, 6, D)

    ims = []
    for (b, qt) in cores:
        colsv = np.stack([g_mix[0], mod[0, b, 0], mod[0, b, 1], mod[0, 2, 0], mod[0, 2, 1]])
        ims.append({"x": np.ascontiguousarray(x[b, qt * TPC:(qt + 1) * TPC]),
                    "ctxr": np.ascontiguousarray(ctx[b, qt * CPC:(qt + 1) * CPC]),
                    "cols": np.ascontiguousarray(colsv.reshape(5, KC, 128).transpose(2, 0, 1)),
                    "w_in": np.ascontiguousarray(ab_w_in[0]),
                    "gains": np.ascontiguousarray(np.stack([ab_q_gain[0], ab_k_gain[0]])),
                    "rope": _rope_table(qt * TPC + np.arange(TPC))})
    res = _run(build_p2(), ims)
    qkvf = [np.asarray(r["qkvf"]) for r in res]
    kvc = [np.asarray(r["kvc"]) for r in res]
    q_b, k_b, v_b, f_b = [], [], [], []
    for b in range(B):
        lat = np.concatenate(qkvf[b * 4:(b + 1) * 4], 0)
        cx = np.concatenate(kvc[b * 4:(b + 1) * 4], 0)
        q_b.append(lat[:, 0:1024])
        k_b.append(np.concatenate([lat[:, 1024:1280], cx[:, 0:256]], 0))
        v_b.append(np.concatenate([lat[:, 1280:1536], cx[:, 256:512]], 0))
        f_b.append(lat[:, 1536:2560])

    cd = _cdft()
    dft = [_dft_tables(qt) for qt in range(4)]
    wr_l = [np.ascontiguousarray(moe_w_router[l].reshape(KC, 128, 32).transpose(1, 0, 2)) for l in range(2)]
    ims = []
    for (b, qt) in cores:
        ims.append({"x": np.ascontiguousarray(x[b, qt * TPC:(qt + 1) * TPC]),
                    "qT": np.ascontiguousarray(q_b[b][qt * TPC:(qt + 1) * TPC].reshape(TPC, 8, 128).transpose(2, 1, 0)),
                    "kT": np.ascontiguousarray(k_b[b].reshape(NKEY, 2, 128).transpose(2, 1, 0)),
                    "v": np.ascontiguousarray(v_b[b]), "f": np.ascontiguousarray(f_b[b]),
                    "dftc": dft[qt][0], "dfts": dft[qt][1], "cdft": cd,
                    "w_out": np.ascontiguousarray(ab_w_out[0]),
                    "rows": np.ascontiguousarray(np.stack([mod[0, b, 2], g_ffn[0], mod[0, b, 3], mod[0, b, 4]])),
                    "w_router": wr_l[0], "b_router": np.ascontiguousarray(moe_b_router[0][None, :])})
    res = _run(build_p3(), ims)
    x1_all = np.concatenate([np.asarray(r["x1"]) for r in res], 0)
    t_all = np.concatenate([np.asarray(r["t"]) for r in res], 0)
    g_all = np.concatenate([np.asarray(r["gates"]) for r in res], 0)
    del dft

    yp_all = _moe_launch(0, t_all, g_all, moe_w1, moe_b1, moe_w2, moe_b2)

    cvb = np.ascontiguousarray(np.concatenate([_cf(cv_b_in[0], 32), _cf(cv_b_dw[0], 16), _cf(cv_ln_g[0], 16),
                                               _cf(cv_ln_b[0], 16)], 1))
    wdw = np.ascontiguousarray(cv_w_dw[0].T.reshape(KC, 128, CW).transpose(1, 0, 2))
    wa = cv_w_in[0][:, :D].reshape(KC, 128, KC, 128)
    wg = cv_w_in[0][:, D:].reshape(KC, 128, KC, 128)
    cvwi = np.ascontiguousarray(np.concatenate([wa, wg], -1).transpose(2, 1, 0, 3))
    ims = []
    for i, (b, qt) in enumerate(cores):
        rows_g = i * TPC - HALO + np.arange(LTOK)
        valid = (rows_g >= b * L) & (rows_g < (b + 1) * L)
        x1h = np.zeros((LTOK, D), np.float32)
        x1h[valid] = x1_all[rows_g[valid]]
        yph = np.zeros((8, LTOK, D), NPBF)
        yph[:, valid] = yp_all[:, rows_g[valid]]
        colsv = np.stack([g_mix[1], mod[1, b, 0], mod[1, b, 1]])
        ims.append({"x1h": x1h, "yph": yph,
                    "rows": np.ascontiguousarray(np.stack([mod[0, b, 5], mod[1, b, 2], g_ffn[1], mod[1, b, 3],
                                                           mod[1, b, 4], cv_b_out[0]])),
                    "cols": np.ascontiguousarray(colsv.reshape(3, KC, 128).transpose(2, 0, 1)),
                    "cvb": cvb, "wdw": wdw,
                    "mask": np.ascontiguousarray(np.broadcast_to(
                        np.array([0.0 if qt == 0 else 1.0, 0.0 if qt == 3 else 1.0], np.float32), (128, 2))),
                    "cv_w_in": cvwi, "cv_w_out": np.ascontiguousarray(cv_w_out[0]),
                    "w_router": wr_l[1], "b_router": np.ascontiguousarray(moe_b_router[1][None, :])})
    res = _run(build_p6(), ims)
    x2_all = np.concatenate([np.asarray(r["x2"]) for r in res], 0)
    t2_all = np.concatenate([np.asarray(r["t"]) for r in res], 0)
    g2_all = np.concatenate([np.asarray(r["gates"]) for r in res], 0)
    del yp_all, ims

    yp2_all = _moe_launch(1, t2_all, g2_all, moe_w1, moe_b1, moe_w2, moe_b2)

    ims = []
    for i, (b, qt) in enumerate(cores):
        ims.append({"x2": np.ascontiguousarray(x2_all[i * TPC:(i + 1) * TPC]),
                    "yph": np.ascontiguousarray(yp2_all[:, i * TPC:(i + 1) * TPC]),
                    "rows": np.ascontiguousarray(np.stack([mod[1, b, 5], g_final]))})
    res = _run(build_p8(), ims)
    out = np.concatenate([np.asarray(r["out"]) for r in res], 0).reshape(B, L, D).astype(np.float32)
    return out


CAPB = 16
BIG = 1.0e6
I32 = mybir.dt.int32


def p_idma(p, out_ap, out_off, in_ap, in_off, bound, reads, dst, concurrent=True):
    nc = p.nc
    for b in reads:
        if b.w is not None:
            p._wait('pool', b.w)
    if concurrent:
        if getattr(dst, 'wbase', None) is not None:
            p._wait('pool', dst.wbase)
    else:
        if dst.w is not None:
            p._wait('pool', dst.w)
    for ev in dst.r:
        p._wait('pool', ev)
    if dst.dsem is None:
        key = 'd_' + dst.name
        p.sems[key] = nc.alloc_semaphore(key)
        dst.dsem = key
    if not hasattr(p, 'bound_regs'):
        p.bound_regs = {}
    if bound not in p.bound_regs:
        p.bound_regs[bound] = nc.gpsimd.to_reg(bound)
    inst = nc.gpsimd.indirect_dma_start(out=out_ap, out_offset=out_off, in_=in_ap, in_offset=in_off,
                                        bounds_check=p.bound_regs[bound], oob_is_err=False)
    dst.dcnt += 16
    p.dma_cnt[dst.dsem] = dst.dcnt
    inst.then_inc(p.sems[dst.dsem], 16)
    ev = (dst.dsem, dst.dcnt)
    for b in reads:
        b.r.append(ev)
    dst.w = ev
    dst.r = []
    return inst


def build_p4s(caps=(CAPB,) * 4):
    caps = tuple(int(c) for c in caps)
    CAPRS = [c * 128 for c in caps]
    NT = NTOK // 128
    nc, p = new_prog()
    t_d = p.dram("t", [NTOK, D], BF16, "ExternalInput")
    gts_d = p.dram("gts", [128, NT, EPC], F32, "ExternalInput")
    w1_d = p.dram("w1", [EPC, D, D], F32, "ExternalInput")
    b1_d = p.dram("b1c", [128, EPC, 16], F32, "ExternalInput")
    w2_d = p.dram("w2", [EPC, 1024, D], F32, "ExternalInput")
    b2_d = p.dram("b2", [EPC, D], F32, "ExternalInput")
    yp_o = p.dram("yp", [NTOK, D], BF16, "ExternalOutput")
    cnt_o = p.dram("cnt", [1, EPC], F32, "ExternalOutput")
    Xg = [p.dram(f"Xg{e}", [CAPRS[e], D], BF16) for e in range(EPC)]
    Yg = [p.dram(f"Yg{e}", [CAPRS[e], D], BF16) for e in range(EPC)]
    p.accs = [p.ps(f"acc{i}", [128, 512], F32) for i in range(6)]
    p.tps = [p.ps(f"tpb{i}", [128, 1024], BF16) for i in range(2)]
    setup_ident(p)
    ones = p.sb("ones", [128, 128], BF16)
    ones1 = p.sb("ones1", [1, 128], BF16)
    tri = p.sb("tri", [128, 128], BF16)
    trif = p.sb("trif", [128, 128], F32)
    p.op('pool', lambda: nc.gpsimd.memset(ones[:], 1.0), writes=[ones])
    p.op('pool', lambda: nc.gpsimd.memset(ones1[:], 1.0), writes=[ones1])
    p.op('pool', lambda: nc.gpsimd.memset(trif[:], 1.0), writes=[trif])
    p.op('pool', lambda: nc.gpsimd.affine_select(out=trif[:], in_=trif[:], pattern=[[1, 128]], compare_op=ALU.is_gt,
                                                 fill=0.0, base=0, channel_multiplier=-1), reads=[trif], writes=[trif])
    p.op('dve', lambda: nc.vector.tensor_copy(out=tri[:], in_=trif[:]), reads=[trif], writes=[tri])
    gts = p.sb("gts_s", [128, NT, EPC], F32)
    b1c = p.sb("b1c_s", [128, EPC, 16], F32)
    p.dma('sp', gts[:], gts_d[:], reads=[gts_d], writes=[gts])
    p.dma('sp', b1c[:], b1_d[:], reads=[b1_d], writes=[b1c])
    b2b = p.sb("b2b", [1, EPC, D], BF16)
    rank_si = p.sb("rank_si", [128, NT * EPC], I32)
    rank_gi = p.sb("rank_gi", [128, NT * EPC], I32)
    phW = Phase(p)
    w1b = phW.sb("w1b", [128, KC, D], BF16)
    w2b = phW.sb("w2b", [128, 8, D], BF16)
    p.dma('pool', b2b[:], b2_d.t.ap().unsqueeze(0) if hasattr(b2_d.t.ap(), "unsqueeze") else b2_d[:, :], reads=[b2_d],
          writes=[b2b])

    def load_w(e):
        for h in range(4):
            p.dma('pool', w1b[:, h * 4:(h + 1) * 4, :], w1_d[e, h * 512:(h + 1) * 512, :].rearrange("(k p) n -> p k n", p=128),
                  reads=[w1_d], writes=[w1b])
        for h in range(2):
            p.dma('pool', w2b[:, h * 4:(h + 1) * 4, :], w2_d[e, h * 512:(h + 1) * 512, :].rearrange("(k p) n -> p k n", p=128),
                  reads=[w2_d], writes=[w2b])
    load_w(0)

    NF = NT * EPC
    phR = Phase(p)
    maskf = phR.sb("maskf", [128, NF], F32)
    maskb = phR.sb("maskb", [128, NF], BF16)
    gflat = gts[:].rearrange("p t e -> p (t e)")
    p.op('dve', lambda: nc.vector.tensor_scalar(out=maskf[:], in0=gflat, scalar1=0.0, scalar2=None, op0=ALU.is_gt),
         reads=[gts], writes=[maskf])
    p.op('dve', lambda: nc.vector.tensor_copy(out=maskb[:], in_=maskf[:]), reads=[maskf], writes=[maskb])
    accW, accT = p.accs[0], p.accs[1]
    p.op('pe', lambda: nc.tensor.matmul(accW[:, 0:NF], lhsT=tri[:], rhs=maskb[:], start=True, stop=True),
         reads=[tri, maskb], writes=[accW])
    p.op('pe', lambda: nc.tensor.matmul(accT[:, 0:NF], lhsT=ones[:], rhs=maskb[:], start=True, stop=True),
         reads=[ones, maskb], writes=[accT])
    sa = phR.sb("scan_a", [128, NF], F32)
    sbb = phR.sb("scan_b", [128, NF], F32)
    tot = phR.sb("tot", [128, NF], F32)
    rank = phR.sb("rank", [128, NF], F32)
    p.op('dve', lambda: nc.vector.tensor_copy(out=tot[:], in_=accT[:, 0:NF]), reads=[accT], writes=[tot])
    p.op('act', lambda: nc.scalar.copy(out=sa[:], in_=accT[:, 0:NF]), reads=[accT], writes=[sa])
    cur, nxt = sa, sbb
    s = 1
    while s < NT:
        w = s * EPC
        p.op('dve', lambda: nc.vector.tensor_copy(out=nxt[:, 0:w], in_=cur[:, 0:w]), reads=[cur], writes=[nxt])
        p.op('dve', lambda: nc.vector.tensor_tensor(out=nxt[:, w:NF], in0=cur[:, w:NF], in1=cur[:, 0:NF - w], op=ALU.add),
             reads=[cur], writes=[nxt])
        cur, nxt = nxt, cur
        s *= 2
    incl = cur
    p.op('dve', lambda: nc.vector.tensor_tensor(out=rank[:], in0=accW[:, 0:NF], in1=incl[:], op=ALU.add),
         reads=[accW, incl], writes=[rank])
    p.op('dve', lambda: nc.vector.tensor_tensor(out=rank[:], in0=rank[:], in1=tot[:], op=ALU.subtract),
         reads=[rank, tot], writes=[rank])
    p.dma('sp', cnt_o[:, :], incl[0:1, NF - EPC:NF], reads=[incl], writes=[cnt_o])
    p.op('dve', lambda: nc.vector.tensor_scalar(out=maskf[:], in0=maskf[:], scalar1=-BIG, scalar2=BIG, op0=ALU.mult,
                                                op1=ALU.add), reads=[maskf], writes=[maskf])
    p.op('dve', lambda: nc.vector.tensor_tensor(out=rank[:], in0=rank[:], in1=maskf[:], op=ALU.add),
         reads=[rank, maskf], writes=[rank])
    p.op('dve', lambda: nc.vector.tensor_copy(out=rank_si[:], in_=rank[:]), reads=[rank], writes=[rank_si])
    for e in range(EPC):
        rv = rank[:].rearrange("p (t e) -> p t e", e=EPC)[:, :, e]
        p.op('dve', lambda: nc.vector.tensor_scalar(out=rv, in0=rv, scalar1=float(CAPRS[e] - 1), scalar2=None,
                                                    op0=ALU.min), reads=[rank], writes=[rank])
    p.op('dve', lambda: nc.vector.tensor_copy(out=rank_gi[:], in_=rank[:]), reads=[rank], writes=[rank_gi])

    p.wait_all('sp', [cnt_o])
    phR.close()
    phD = Phase(p)
    zt = phD.sb("zt", [128, D], BF16)
    p.op('dve', lambda: nc.vector.memset(zt[:], 0.0), writes=[zt])
    for e in range(EPC):
        CAPR = CAPRS[e]
        for r in range(0, CAPR, 128 * 8):
            n8 = min(8, (CAPR - r) // 128)
            p.dma('sp', Xg[e][r:r + n8 * 128, :].rearrange("(a p) d -> p a d", p=128),
                  zt[:].unsqueeze(1).broadcast_to([128, n8, D]), reads=[zt], writes=[Xg[e]])
        Xg[e].wbase = Xg[e].w
    tts = phD.scratch("tt", [128, D], BF16, 4)
    for t in range(NT):
        tt = tts.get()
        p.dma('sp', tt[:], t_d[t * 128:(t + 1) * 128, :], reads=[t_d], writes=[tt])
        for e in range(EPC):
            col = t * EPC + e
            p_idma(p, Xg[e][:, :], bass.IndirectOffsetOnAxis(ap=rank_si[:, col:col + 1], axis=0), tt[:, :], None,
                   CAPRS[e] - 1, [tt, rank_si], Xg[e])

    phD.close()
    phE = Phase(p)
    xgs = phE.scratch("xgt", [128, D], BF16, 3)
    XTs = phE.scratch("XT", [128, KC, 512], BF16, 2)
    actTs = phE.scratch("actT", [128, 8, 512], BF16, 2)
    g32s = phE.scratch("g32", [128, 512], F32, 2)
    sgs = phE.scratch("sg", [128, 512], F32, 2)
    l32s = phE.scratch("l32", [128, 512], F32, 2)
    yos = phE.scratch("yo", [128, D], BF16, 2)
    for e in range(EPC):
        if e > 0:
            load_w(e)
        for ch in range((caps[e] + 3) // 4):
            XT, actT = XTs.get(), actTs.get()
            nb = min(4, caps[e] - 4 * ch)
            N = nb * 128
            for ti in range(nb):
                r0 = (ch * 4 + ti) * 128
                xg = xgs.get()
                p.dma('sp', xg[:], Xg[e][r0:r0 + 128, :], reads=[Xg[e]], writes=[xg])
                for k8 in range(2):
                    tp = p.tp()

                    def tr():
                        for j in range(8):
                            k = k8 * 8 + j
                            i = nc.tensor.transpose(out=tp[:, j * 128:(j + 1) * 128], in_=xg[:, k * 128:(k + 1) * 128],
                                                    identity=p.ident[:])
                        return i
                    p.op('pe', tr, reads=[xg, p.ident], writes=[tp])
                    dst = XT[:, k8 * 8:(k8 + 1) * 8, ti * 128:(ti + 1) * 128]
                    src = tp[:].rearrange("p (k t) -> p k t", t=128)
                    if k8 == 0:
                        p.op('act', lambda: nc.scalar.copy(out=dst, in_=src), reads=[tp], writes=[XT])
                    else:
                        p.op('dve', lambda: nc.vector.tensor_copy(out=dst, in_=src), reads=[tp], writes=[XT])
            for jj in range(8):
                accG, accL = p.acc(), p.acc()
                for acc, off in ((accG, 0), (accL, 1024)):
                    def mm():
                        for k in range(KC):
                            i = nc.tensor.matmul(acc[:, 0:N], lhsT=w1b[:, k, off + jj * 128:off + (jj + 1) * 128],
                                                 rhs=XT[:, k, 0:N], start=(k == 0), stop=(k == KC - 1))
                        return i
                    p.op('pe', mm, reads=[w1b, XT], writes=[acc])
                g32, sg, l32 = g32s.get(), sgs.get(), l32s.get()
                p.op('dve', lambda: nc.vector.tensor_scalar(out=g32[:, 0:N], in0=accG[:, 0:N], scalar1=b1c[:, e, jj:jj + 1],
                                                            scalar2=LIMIT, op0=ALU.add, op1=ALU.min),
                     reads=[accG, b1c], writes=[g32])
                p.op('act', lambda: nc.scalar.activation(out=sg[:, 0:N], in_=g32[:, 0:N], func=AF.Sigmoid, scale=ALPHA),
                     reads=[g32], writes=[sg])
                p.op('dve', lambda: nc.vector.tensor_scalar(out=l32[:, 0:N], in0=accL[:, 0:N], scalar1=b1c[:, e, 8 + jj:8 + jj + 1],
                                                            scalar2=LIMIT, op0=ALU.add, op1=ALU.min),
                     reads=[accL, b1c], writes=[l32])
                p.op('dve', lambda: nc.vector.tensor_scalar(out=l32[:, 0:N], in0=l32[:, 0:N], scalar1=-LIMIT, scalar2=1.0,
                                                            op0=ALU.max, op1=ALU.add), reads=[l32], writes=[l32])
                p.op('dve', lambda: nc.vector.tensor_tensor(out=g32[:, 0:N], in0=g32[:, 0:N], in1=sg[:, 0:N], op=ALU.mult),
                     reads=[g32, sg], writes=[g32])
                p.op('dve', lambda: nc.vector.tensor_tensor(out=actT[:, jj, 0:N], in0=g32[:, 0:N], in1=l32[:, 0:N], op=ALU.mult),
                     reads=[g32, l32], writes=[actT])
            for ti in range(nb):
                r0 = (ch * 4 + ti) * 128
                yo = yos.get()
                for c in range(4):
                    acc = p.acc()

                    def mm2():
                        nc.tensor.matmul(acc[:], lhsT=ones1[0:1, :], rhs=b2b[0:1, e, c * 512:(c + 1) * 512], start=True,
                                         stop=False)
                        for j in range(8):
                            i = nc.tensor.matmul(acc[:], lhsT=actT[:, j, ti * 128:(ti + 1) * 128],
                                                 rhs=w2b[:, j, c * 512:(c + 1) * 512], start=False, stop=(j == 7))
                        return i
                    p.op('pe', mm2, reads=[actT, w2b, b2b, ones1], writes=[acc])
                    p.op('act', lambda: nc.scalar.copy(out=yo[:, c * 512:(c + 1) * 512], in_=acc[:]), reads=[acc],
                         writes=[yo])
                p.dma('sp', Yg[e][r0:r0 + 128, :], yo[:], reads=[yo], writes=[Yg[e]])

    phE.close()
    phW.close()
    phC = Phase(p)
    ygs = phC.scratch("yg", [128, D], BF16, 8)
    caccs = phC.scratch("cacc", [128, D], F32, 2)
    obs = phC.scratch("ob", [128, D], BF16, 2)
    for t in range(NT):
        eng = 'dve'
        ve = nc.vector if eng == 'dve' else nc.gpsimd
        ca, ob = caccs.get(), obs.get()
        for e in range(EPC):
            col = t * EPC + e
            yg = ygs.get()
            p_idma(p, yg[:, :], None, Yg[e][:, :], bass.IndirectOffsetOnAxis(ap=rank_gi[:, col:col + 1], axis=0),
                   CAPRS[e] - 1, [Yg[e], rank_gi], yg, concurrent=False)
            gcol = gts[:, t, e:e + 1]
            if e == 0:
                p.op(eng, lambda: ve.tensor_scalar(out=ca[:], in0=yg[:], scalar1=gcol, scalar2=None, op0=ALU.mult),
                     reads=[yg, gts], writes=[ca])
            elif e < EPC - 1:
                p.op(eng, lambda: ve.scalar_tensor_tensor(out=ca[:], in0=yg[:], scalar=gcol, in1=ca[:], op0=ALU.mult,
                                                          op1=ALU.add), reads=[yg, gts, ca], writes=[ca])
            else:
                p.op(eng, lambda: ve.scalar_tensor_tensor(out=ob[:], in0=yg[:], scalar=gcol, in1=ca[:], op0=ALU.mult,
                                                          op1=ALU.add), reads=[yg, gts, ca], writes=[ob])
        p.dma('sp', yp_o[t * 128:(t + 1) * 128, :], ob[:], reads=[ob], writes=[yp_o])
    p.wait_all('sp', [yp_o, cnt_o])
    phC.close()
    return nc


NEXP = 32


def build_p5():
    NT = NTOK // 128
    nc, p = new_prog()
    g_d = p.dram("g", [128, NT, NEXP], F32, "ExternalInput")
    out_o = p.dram("info", [1, NEXP + EPC], F32, "ExternalOutput")
    p.accs = [p.ps(f"acc{i}", [128, 512], F32) for i in range(4)]
    ones = p.sb("ones", [128, 128], BF16)
    p.op('pool', lambda: nc.gpsimd.memset(ones[:], 1.0), writes=[ones])
    g = p.sb("gs", [128, NT, NEXP], F32)
    mk = p.sb("mk", [128, NT * NEXP], BF16)
    tot = p.sb("tot", [128, NT * NEXP], F32)
    p.dma('sp', g[:], g_d[:], reads=[g_d], writes=[g])
    p.op('dve', lambda: nc.vector.tensor_scalar(out=mk[:], in0=g[:].rearrange("p t e -> p (t e)"), scalar1=0.0,
                                                scalar2=None, op0=ALU.is_gt), reads=[g], writes=[mk])
    for c in range(NT * NEXP // 512):
        acc = p.acc()
        p.op('pe', lambda: nc.tensor.matmul(acc[:], lhsT=ones[:], rhs=mk[:, c * 512:(c + 1) * 512], start=True, stop=True),
             reads=[ones, mk], writes=[acc])
        p.op('act', lambda: nc.scalar.copy(out=tot[:, c * 512:(c + 1) * 512], in_=acc[:]), reads=[acc], writes=[tot])
    cnt = p.sb("cnt", [128, NEXP], F32)
    p.op('dve', lambda: nc.vector.tensor_reduce(out=cnt[:], in_=tot[:].rearrange("p (t e) -> p e t", e=NEXP), axis=AX.X,
                                                op=ALU.add), reads=[tot], writes=[cnt])
    io = p.sb("io", [128, NEXP], F32)
    p.op('pool', lambda: nc.gpsimd.iota(io[:], pattern=[[1, NEXP]], base=0, channel_multiplier=0,
                                        allow_small_or_imprecise_dtypes=True), writes=[io])
    cnt2 = p.sb("cnt2", [128, NEXP], F32)
    p.op('dve', lambda: nc.vector.scalar_tensor_tensor(out=cnt2[:], in0=io[:], scalar=1.0 / 64, in1=cnt[:], op0=ALU.mult,
                                                       op1=ALU.add), reads=[io, cnt], writes=[cnt2])
    cmp = p.sb("cmp", [128, NEXP, NEXP], F32)
    p.op('dve', lambda: nc.vector.tensor_tensor(out=cmp[:], in0=cnt2[:].unsqueeze(1).broadcast_to([128, NEXP, NEXP]),
                                                in1=cnt2[:].unsqueeze(2).broadcast_to([128, NEXP, NEXP]), op=ALU.is_gt),
         reads=[cnt2], writes=[cmp])
    res = p.sb("res", [128, NEXP + EPC], F32)
    p.op('dve', lambda: nc.vector.tensor_reduce(out=res[:, 0:NEXP], in_=cmp[:], axis=AX.X, op=ALU.add), reads=[cmp],
         writes=[res])
    sel = p.sb("sel", [128, NEXP], F32)
    sel2 = p.sb("sel2", [128, NEXP], F32)
    for j in range(EPC):
        p.op('dve', lambda: nc.vector.tensor_scalar(out=sel[:], in0=res[:, 0:NEXP], scalar1=float(8 * j) - 0.5,
                                                    scalar2=None, op0=ALU.is_gt), reads=[res], writes=[sel])
        p.op('dve', lambda: nc.vector.tensor_scalar(out=sel2[:], in0=res[:, 0:NEXP], scalar1=float(8 * j + 8) - 0.5,
                                                    scalar2=None, op0=ALU.is_lt), reads=[res], writes=[sel2])
        p.op('dve', lambda: nc.vector.tensor_tensor(out=sel[:], in0=sel[:], in1=sel2[:], op=ALU.mult), reads=[sel, sel2],
             writes=[sel])
        p.op('dve', lambda: nc.vector.tensor_tensor(out=sel[:], in0=sel[:], in1=cnt[:], op=ALU.mult), reads=[sel, cnt],
             writes=[sel])
        p.op('dve', lambda: nc.vector.reduce_max(out=res[:, NEXP + j:NEXP + j + 1], in_=sel[:], axis=AX.X), reads=[sel],
             writes=[res])
    p.dma('sp', out_o[:, :], res[0:1, :], reads=[res], writes=[out_o])
    p.wait_all('sp', [out_o])
    return nc
```

```python
import numpy as np
import ml_dtypes
import concourse.bass as bass
import concourse.mybir as mybir
from concourse.bass_utils import run_bass_kernel_spmd

F32 = mybir.dt.float32
BF16 = mybir.dt.bfloat16
AF = mybir.ActivationFunctionType
ALU = mybir.AluOpType
AX = mybir.AxisListType
NPBF = ml_dtypes.bfloat16

D = 2048
KC = 16
EPS = 1e-6
NCORES = 8


class Buf:
    def __init__(self, t, name):
        self.t = t
        self.name = name
        self.w = None
        self.r = []
        self.dsem = None
        self.dcnt = 0

    def __getitem__(self, idx):
        return self.t[idx]


class P:
    def __init__(self, nc):
        self.nc = nc
        self.eng = {'pe': nc.tensor, 'act': nc.scalar, 'dve': nc.vector, 'pool': nc.gpsimd, 'sp': nc.sync}
        self.sems = {}
        self.cnt = {}
        for k in self.eng:
            self.sems[k] = nc.alloc_semaphore('s_' + k)
            self.cnt[k] = 0
        self.seen = {k: {} for k in self.eng}
        self.acc_i = 0
        self.tp_i = 0
        self.dma_cnt = {}

    def sb(self, name, shape, dt):
        return Buf(self.nc.alloc_sbuf_tensor(name, list(shape), dt), name)

    def ps(self, name, shape, dt=F32):
        return Buf(self.nc.alloc_psum_tensor(name, list(shape), dt), name)

    def dram(self, name, shape, dt, kind="Internal"):
        b = Buf(self.nc.dram_tensor(name, list(shape), dt, kind=kind), name)
        b.is_dram = True
        return b

    def _wait(self, e, ev):
        key, val = ev
        if key == 'pe' and e == 'pe':
            return
        if self.seen[e].get(key, 0) >= val:
            return
        self.seen[e][key] = val
        self.eng[e].wait_ge(self.sems[key], val)

    def _deps(self, e, reads, writes):
        for b in reads:
            if b.w is not None:
                self._wait(e, b.w)
        for b in writes:
            if b.w is not None:
                self._wait(e, b.w)
            for ev in b.r:
                self._wait(e, ev)

    def _commit(self, ev, reads, writes):
        for b in reads:
            b.r.append(ev)
            if len(b.r) > 48:
                best = {}
                for k, v in b.r:
                    best[k] = max(best.get(k, 0), v)
                b.r = list(best.items())
        for b in writes:
            b.w = ev
            b.r = []

    def op(self, e, fn, reads=(), writes=()):
        self._deps(e, reads, writes)
        inst = fn()
        self.cnt[e] += 1
        inst.then_inc(self.sems[e], 1)
        self._commit((e, self.cnt[e]), reads, writes)
        return inst

    def dma(self, q, out_ap, in_ap, reads=(), writes=(), **kw):
        dst = writes[0]
        if q == 'sp' and getattr(dst, 'is_dram', False):
            q = 'act'
        self._deps(q, reads, writes)
        if dst.dsem is None:
            key = 'd_' + dst.name
            self.sems[key] = self.nc.alloc_semaphore(key)
            dst.dsem = key
        inst = self.eng[q].dma_start(out=out_ap, in_=in_ap, **kw)
        dst.dcnt += 16
        self.dma_cnt[dst.dsem] = dst.dcnt
        inst.then_inc(self.sems[dst.dsem], 16)
        self._commit((dst.dsem, dst.dcnt), reads, writes)
        return inst

    def wait_all(self, e, bufs):
        for b in bufs:
            if b.w is not None:
                self._wait(e, b.w)

    def setup_common(self):
        nc = self.nc
        self.accs = [self.ps(f"acc{i}", [128, 512], F32) for i in range(5)]
        self.tps = [self.ps(f"tpb{i}", [128, 1024], BF16) for i in range(3)]
        identf = self.sb("identf", [128, 128], F32)
        ident = self.sb("ident", [128, 128], BF16)
        self.op('pool', lambda: nc.gpsimd.memset(identf[:], 0.0), writes=[identf])
        self.op('pool', lambda: nc.gpsimd.affine_select(out=identf[:], in_=identf[:], pattern=[[-1, 128]],
                                                        compare_op=ALU.not_equal, fill=1.0, base=0,
                                                        channel_multiplier=1), reads=[identf], writes=[identf])
        self.op('dve', lambda: nc.vector.tensor_copy(out=ident[:], in_=identf[:]), reads=[identf], writes=[ident])
        self.ident = ident
        self.identf = identf

    def acc(self):
        b = self.accs[self.acc_i % len(self.accs)]
        self.acc_i += 1
        return b

    def tp(self):
        b = self.tps[self.tp_i % len(self.tps)]
        self.tp_i += 1
        return b


def new_prog():
    nc = bass.Bass("TRN2", target_bir_lowering=False)
    return nc, P(nc)


class Scratch:
    def __init__(self, p, name, shape, dt, n=2):
        self.bufs = [p.sb(f"{name}{i}", shape, dt) for i in range(n)]
        self.i = 0

    def get(self):
        b = self.bufs[self.i % len(self.bufs)]
        self.i += 1
        return b


def emit_rstd(p, ss, rows, scale_in, tmp=None):
    nc = p.nc
    p.op('dve', lambda: nc.vector.tensor_scalar(out=ss, in0=ss, scalar1=float(scale_in), scalar2=EPS,
                                                op0=ALU.mult, op1=ALU.add), reads=[tmp], writes=[tmp])
    p.op('act', lambda: nc.scalar.sqrt(out=ss, in_=ss), reads=[tmp], writes=[tmp])
    p.op('dve', lambda: nc.vector.reciprocal(out=ss, in_=ss), reads=[tmp], writes=[tmp])


class NormT:
    def __init__(self, p):
        self.p = p
        self.sq = Scratch(p, "nt_sq", [128, D], BF16, 2)
        self.ss = Scratch(p, "nt_ss", [128, 1], F32, 2)
        self.xn = Scratch(p, "nt_xn", [128, D], BF16, 2)

    def emit(self, xt, rows, Acol, Bcol, hT, c0):
        p, nc = self.p, self.p.nc
        sq, ss, xn = self.sq.get(), self.ss.get(), self.xn.get()
        p.op('act', lambda: nc.scalar.activation(out=sq[0:rows, :], in_=xt[0:rows, :], func=AF.Square,
                                                 accum_out=ss[0:rows, :]), reads=[xt], writes=[sq, ss])
        emit_rstd(p, ss[0:rows, :], rows, 1.0 / D, ss)
        p.op('dve', lambda: nc.vector.tensor_scalar(out=xn[0:rows, :], in0=xt[0:rows, :], scalar1=ss[0:rows, 0:1],
                                                    scalar2=None, op0=ALU.mult), reads=[xt, ss], writes=[xn])
        for k4 in range(2):
            tp = p.tp()

            def tr():
                for j in range(8):
                    k = k4 * 8 + j
                    i = nc.tensor.transpose(out=tp[:, j * 128:j * 128 + rows], in_=xn[0:rows, k * 128:(k + 1) * 128],
                                            identity=p.ident[0:rows, 0:rows])
                return i
            p.op('pe', tr, reads=[xn, p.ident], writes=[tp])
            for j in range(8):
                k = k4 * 8 + j
                if j % 2 == 0:
                    p.op('dve', lambda: nc.vector.tensor_scalar(
                        out=hT[:, k, c0:c0 + rows], in0=tp[:, j * 128:j * 128 + rows], scalar1=Acol[:, k:k + 1],
                        scalar2=Bcol[:, k:k + 1], op0=ALU.mult, op1=ALU.add), reads=[tp, Acol, Bcol], writes=[hT])
                else:
                    p.op('act', lambda: nc.scalar.activation(
                        out=hT[:, k, c0:c0 + rows], in_=tp[:, j * 128:j * 128 + rows], func=AF.Identity,
                        scale=Acol[:, k:k + 1], bias=Bcol[:, k:k + 1]), reads=[tp, Acol, Bcol], writes=[hT])


def emit_Acol(p, A, g, sc):
    nc = p.nc
    p.op('dve', lambda: nc.vector.scalar_tensor_tensor(out=A[:], in0=sc[:], scalar=1.0, in1=g[:], op0=ALU.add,
                                                       op1=ALU.mult), reads=[sc, g], writes=[A])


MODW = 12288 // NCORES


def build_p1():
    nc, p = new_prog()
    wmod = p.dram("wmod", [2, D, MODW], F32, "ExternalInput")
    bmod = p.dram("bmod", [2, 3, MODW], F32, "ExternalInput")
    cT = p.dram("cT", [128, KC, 3], F32, "ExternalInput")
    mout = p.dram("m", [2, 3, MODW], F32, "ExternalOutput")
    p.setup_common()
    cs = p.sb("cs", [128, KC, 3], F32)
    sT = p.sb("sT", [128, KC, 3], BF16)
    p.dma('sp', cs[:], cT[:], reads=[cT], writes=[cs])
    p.op('act', lambda: nc.scalar.activation(out=sT[:], in_=cs[:], func=AF.Silu), reads=[cs], writes=[sT])
    for l in range(2):
        wb = p.sb(f"wb{l}", [128, KC, MODW], BF16)
        bb = p.sb(f"bb{l}", [3, MODW], F32)
        mo = p.sb(f"mo{l}", [3, MODW], F32)
        for h in range(2):
            p.dma('pool', wb[:, h * 8:(h + 1) * 8, :],
                  wmod[l, h * 1024:(h + 1) * 1024, :].rearrange("(k p) n -> p k n", p=128), reads=[wmod], writes=[wb])
        p.dma('sp', bb[:], bmod[l], reads=[bmod], writes=[bb])
        for c in range(MODW // 512):
            acc = p.acc()

            def mm():
                for k in range(KC):
                    i = nc.tensor.matmul(acc[0:3, :], lhsT=sT[:, k, :], rhs=wb[:, k, c * 512:(c + 1) * 512],
                                         start=(k == 0), stop=(k == KC - 1))
                return i
            p.op('pe', mm, reads=[sT, wb], writes=[acc])
            p.op('dve', lambda: nc.vector.tensor_tensor(out=mo[:, c * 512:(c + 1) * 512], in0=acc[0:3, :],
                                                        in1=bb[:, c * 512:(c + 1) * 512], op=ALU.add),
                 reads=[acc, bb], writes=[mo])
        p.dma('sp', mout[l], mo[:], reads=[mo], writes=[mout])
    p.wait_all('sp', [mout])
    return nc


TPC = 1024
CPC = 64


def emit_qk_post(p, ps, rows, nh, gain_b, rope_t, out_ap, out_buf, S):
    nc = p.nc
    n = nh * 128
    sq, ss4, qn = S['sq'].get(), S['ss4'].get(), S['qn'].get()
    p.op('act', lambda: nc.scalar.activation(out=sq[0:rows, 0:n], in_=ps[0:rows, 0:n], func=AF.Square),
         reads=[ps], writes=[sq])
    p.op('dve', lambda: nc.vector.tensor_reduce(out=ss4[0:rows, 0:nh],
                                                in_=sq[0:rows, 0:n].rearrange("p (h d) -> p h d", d=128),
                                                axis=AX.X, op=ALU.add), reads=[sq], writes=[ss4])
    emit_rstd(p, ss4[0:rows, 0:nh], rows, 1.0 / 128, ss4)
    p.op('dve', lambda: nc.vector.tensor_tensor(
        out=qn[0:rows, 0:n].rearrange("p (h d) -> p h d", d=128),
        in0=ps[0:rows, 0:n].rearrange("p (h d) -> p h d", d=128),
        in1=ss4[0:rows, 0:nh].unsqueeze(2).broadcast_to([rows, nh, 128]), op=ALU.mult),
        reads=[ps, ss4], writes=[qn])
    if rope_t is None:
        p.op('dve', lambda: nc.vector.tensor_tensor(
            out=out_ap.rearrange("p (h d) -> p h d", d=128),
            in0=qn[0:rows, 0:n].rearrange("p (h d) -> p h d", d=128),
            in1=gain_b[0:rows, :].unsqueeze(1).broadcast_to([rows, nh, 128]), op=ALU.mult),
            reads=[qn, gain_b], writes=[out_buf])
        return
    p.op('pool', lambda: nc.gpsimd.tensor_tensor(
        out=qn[0:rows, 0:n].rearrange("p (h d) -> p h d", d=128),
        in0=qn[0:rows, 0:n].rearrange("p (h d) -> p h d", d=128),
        in1=gain_b[0:rows, :].unsqueeze(1).broadcast_to([rows, nh, 128]), op=ALU.mult),
        reads=[qn, gain_b], writes=[qn])
    t1, t2 = S['t1'].get(), S['t2'].get()
    p.op('pool', lambda: nc.gpsimd.tensor_tensor(
        out=t1[0:rows, 0:n].rearrange("p (h d) -> p h d", d=128),
        in0=qn[0:rows, 0:n].rearrange("p (h d) -> p h d", d=128),
        in1=rope_t[0:rows, 0, :].unsqueeze(1).broadcast_to([rows, nh, 128]), op=ALU.mult),
        reads=[qn, rope_t], writes=[t1])
    for b in range(2):
        p.op('dve', lambda: nc.vector.tensor_tensor(
            out=t2[0:rows, 0:n].rearrange("p (h a b d) -> p h a b d", a=2, b=2, d=32)[:, :, :, b, :],
            in0=qn[0:rows, 0:n].rearrange("p (h a b d) -> p h a b d", a=2, b=2, d=32)[:, :, :, 1 - b, :],
            in1=rope_t[0:rows, 1, :].rearrange("p (a b d) -> p a b d", a=2, b=2, d=32)[:, :, b, :]
            .unsqueeze(1).broadcast_to([rows, nh, 2, 32]), op=ALU.mult),
            reads=[qn, rope_t], writes=[t2])
    p.op('dve', lambda: nc.vector.tensor_tensor(out=out_ap, in0=t1[0:rows, 0:n], in1=t2[0:rows, 0:n], op=ALU.add),
         reads=[t1, t2], writes=[out_buf])


def build_p2():
    nc, p = new_prog()
    x = p.dram("x", [TPC, D], F32, "ExternalInput")
    ctxr = p.dram("ctxr", [CPC, D], F32, "ExternalInput")
    cols = p.dram("cols", [128, 5, KC], F32, "ExternalInput")
    w_in = p.dram("w_in", [D, 2560], F32, "ExternalInput")
    gains = p.dram("gains", [2, 128], F32, "ExternalInput")
    rope = p.dram("rope", [TPC, 2, 128], F32, "ExternalInput")
    out = p.dram("qkvf", [TPC, 2560], BF16, "ExternalOutput")
    outc = p.dram("kvc", [CPC, 512], BF16, "ExternalOutput")
    p.setup_common()
    colsb = p.sb("colsb", [128, 5, KC], F32)
    p.dma('sp', colsb[:], cols[:], reads=[cols], writes=[colsb])
    qg = p.sb("qg", [128, 128], F32)
    kg = p.sb("kg", [128, 128], F32)
    p.dma('sp', qg[:], gains[0:1, :].partition_broadcast(128), reads=[gains], writes=[qg])
    p.dma('sp', kg[:], gains[1:2, :].partition_broadcast(128), reads=[gains], writes=[kg])
    A1 = p.sb("A1", [128, KC], F32)
    Ac = p.sb("Ac", [128, KC], F32)
    B1 = p.sb("B1", [128, KC], F32)
    Bc = p.sb("Bc", [128, KC], F32)
    p.op('dve', lambda: nc.vector.scalar_tensor_tensor(out=A1[:], in0=colsb[:, 2, :], scalar=1.0, in1=colsb[:, 0, :],
                                                       op0=ALU.add, op1=ALU.mult), reads=[colsb], writes=[A1])
    p.op('dve', lambda: nc.vector.scalar_tensor_tensor(out=Ac[:], in0=colsb[:, 4, :], scalar=1.0, in1=colsb[:, 0, :],
                                                       op0=ALU.add, op1=ALU.mult), reads=[colsb], writes=[Ac])
    p.op('dve', lambda: nc.vector.tensor_copy(out=B1[:], in_=colsb[:, 1, :]), reads=[colsb], writes=[B1])
    p.op('dve', lambda: nc.vector.tensor_copy(out=Bc[:], in_=colsb[:, 3, :]), reads=[colsb], writes=[Bc])
    wb = p.sb("wb", [128, KC, 2560], BF16)
    for h in range(4):
        p.dma('pool', wb[:, h * 4:(h + 1) * 4, :], w_in[h * 512:(h + 1) * 512, :].rearrange("(k p) n -> p k n", p=128),
              reads=[w_in], writes=[wb])
    nt = NormT(p)
    xs = Scratch(p, "xs", [128, D], F32, 2)
    hTs = Scratch(p, "hT", [128, KC, 128], BF16, 2)
    rts = Scratch(p, "rt", [128, 2, 128], F32, 2)
    obs = Scratch(p, "ob", [128, 2560], BF16, 2)
    S = {'sq': Scratch(p, "qsq", [128, 512], F32, 2), 'ss4': Scratch(p, "qss", [128, 4], F32, 2),
         'qn': Scratch(p, "qn", [128, 512], F32, 2), 't1': Scratch(p, "qt1", [128, 512], F32, 2),
         't2': Scratch(p, "qt2", [128, 512], F32, 2)}
    ntiles = TPC // 128
    for t in range(ntiles + 1):
        is_ctx = (t == ntiles)
        rows = CPC if is_ctx else 128
        xt, hT, ob = xs.get(), hTs.get(), obs.get()
        if is_ctx:
            p.dma('sp', xt[0:rows, :], ctxr[:, :], reads=[ctxr], writes=[xt])
            rt = None
        else:
            p.dma('sp', xt[:], x[t * 128:(t + 1) * 128, :], reads=[x], writes=[xt])
            rt = rts.get()
            p.dma('sp', rt[:], rope[t * 128:(t + 1) * 128, :, :], reads=[rope], writes=[rt])
        nt.emit(xt, rows, Ac if is_ctx else A1, Bc if is_ctx else B1, hT, 0)
        for c in ([2] if is_ctx else range(5)):
            acc = p.acc()

            def mm():
                for k in range(KC):
                    i = nc.tensor.matmul(acc[0:rows, :], lhsT=hT[:, k, 0:rows], rhs=wb[:, k, c * 512:(c + 1) * 512],
                                         start=(k == 0), stop=(k == KC - 1))
                return i
            p.op('pe', mm, reads=[hT, wb], writes=[acc])
            if c < 2:
                emit_qk_post(p, acc, rows, 4, qg, rt, ob[0:rows, c * 512:(c + 1) * 512], ob, S)
            elif c == 2:
                emit_qk_post(p, acc, rows, 2, kg, rt, ob[0:rows, 1024:1280], ob, S)
                p.op('act', lambda: nc.scalar.copy(out=ob[0:rows, 1280:1536], in_=acc[0:rows, 256:512]),
                     reads=[acc], writes=[ob])
            else:
                p.op('act', lambda: nc.scalar.copy(out=ob[0:rows, c * 512:(c + 1) * 512], in_=acc[0:rows, :]),
                     reads=[acc], writes=[ob])
        if is_ctx:
            p.dma('sp', outc[:, :], ob[0:rows, 1024:1536], reads=[ob], writes=[outc])
        else:
            p.dma('sp', out[t * 128:(t + 1) * 128, :], ob[:], reads=[ob], writes=[out])
    p.wait_all('sp', [out, outc])
    return nc


def p_barrier(p):
    targets = {}
    for k in p.eng:
        targets[k] = p.cnt[k]
    for k, v in p.sems.items():
        if k.startswith('d_'):
            targets[k] = p.dma_cnt.get(k, 0)
    for e in p.eng:
        for k, v in targets.items():
            if v > 0 and k != e:
                p._wait(e, (k, v))


class Phase:
    def __init__(self, p):
        from contextlib import ExitStack
        self.p = p
        self.st = ExitStack()

    def sb(self, name, shape, dt):
        t = self.st.enter_context(self.p.nc.sbuf_tensor(name, list(shape), dt))
        return Buf(t, name)

    def scratch(self, name, shape, dt, n=2):
        s = Scratch.__new__(Scratch)
        s.bufs = [self.sb(f"{name}{i}", shape, dt) for i in range(n)]
        s.i = 0
        return s

    def close(self):
        p_barrier(self.p)
        self.st.close()


class Prep:
    def __init__(self, p, ph, A2b, B2b, wr, brb, tpf):
        self.p = p
        self.A2b, self.B2b, self.wr, self.brb, self.tpf = A2b, B2b, wr, brb, tpf
        self.sq = ph.sb("pp_sq", [128, D], BF16)
        self.ss = ph.sb("pp_ss", [128, 1], F32)
        self.t32 = ph.sb("pp_t32", [128, D], F32)
        self.tb = ph.scratch("pp_tb", [128, D], BF16, 2)
        self.tT = ph.sb("pp_tT", [128, KC, 128], F32)
        self.lg = ph.scratch("pp_lg", [128, 32], F32, 2)
        self.t8 = ph.sb("pp_t8", [128, 8], F32)
        self.nmx = ph.sb("pp_nmx", [128, 1], F32)
        self.msk = ph.sb("pp_msk", [128, 32], F32)
        self.ex = ph.sb("pp_ex", [128, 32], F32)
        self.sm = ph.sb("pp_sm", [128, 1], F32)

    def emit(self, x1, t_out_ap, t_out, g_out_ap, g_out):
        p, nc = self.p, self.p.nc
        sq, ss, t32, tT = self.sq, self.ss, self.t32, self.tT
        tb, lg = self.tb.get(), self.lg.get()
        p.op('act', lambda: nc.scalar.activation(out=sq[:], in_=x1[:], func=AF.Square, accum_out=ss[:]),
             reads=[x1], writes=[sq, ss])
        emit_rstd(p, ss[:], 128, 1.0 / D, ss)
        p.op('dve', lambda: nc.vector.scalar_tensor_tensor(out=t32[:], in0=x1[:], scalar=ss[:, 0:1], in1=self.A2b[:],
                                                           op0=ALU.mult, op1=ALU.mult), reads=[x1, ss, self.A2b],
             writes=[t32])
        p.op('pool', lambda: nc.gpsimd.tensor_tensor(out=t32[:], in0=t32[:], in1=self.B2b[:], op=ALU.add),
             reads=[t32, self.B2b], writes=[t32])
        p.op('act', lambda: nc.scalar.copy(out=tb[:], in_=t32[:]), reads=[t32], writes=[tb])
        p.dma('sp', t_out_ap, tb[:], reads=[tb], writes=[t_out])
        for k4 in range(4):
            tpf = self.tpf

            def tr():
                for j in range(4):
                    k = k4 * 4 + j
                    i = nc.tensor.transpose(out=tpf[:, j * 128:(j + 1) * 128], in_=t32[:, k * 128:(k + 1) * 128],
                                            identity=p.identf[:])
                return i
            p.op('pe', tr, reads=[t32, p.identf], writes=[tpf])
            eng = 'dve' if k4 % 2 == 0 else 'act'
            if eng == 'dve':
                p.op('dve', lambda: nc.vector.tensor_copy(
                    out=tT[:, k4 * 4:(k4 + 1) * 4, :].rearrange("p k t -> p (k t)"), in_=tpf[:]),
                    reads=[tpf], writes=[tT])
            else:
                p.op('act', lambda: nc.scalar.copy(
                    out=tT[:, k4 * 4:(k4 + 1) * 4, :].rearrange("p k t -> p (k t)"), in_=tpf[:]),
                    reads=[tpf], writes=[tT])
        acc = p.acc()

        def mm():
            for k in range(KC):
                i = nc.tensor.matmul(acc[:, 0:32], lhsT=tT[:, k, :], rhs=self.wr[:, k, :], start=(k == 0),
                                     stop=(k == KC - 1))
            return i
        p.op('pe', mm, reads=[tT, self.wr], writes=[acc])
        p.op('dve', lambda: nc.vector.tensor_tensor(out=lg[:], in0=acc[:, 0:32], in1=self.brb[:], op=ALU.add),
             reads=[acc, self.brb], writes=[lg])
        emit_gates(p, lg, self.t8, self.nmx, self.msk, self.ex, self.sm)
        p.dma('sp', g_out_ap, lg[:], reads=[lg], writes=[g_out])


def emit_gates(p, lg, t8, nmx, msk, ex, sm):
    nc = p.nc
    p.op('dve', lambda: nc.vector.max(out=t8[:], in_=lg[:]), reads=[lg], writes=[t8])
    p.op('dve', lambda: nc.vector.tensor_scalar(out=msk[:], in0=lg[:], scalar1=t8[:, 3:4], scalar2=None,
                                                op0=ALU.is_ge), reads=[lg, t8], writes=[msk])
    p.op('dve', lambda: nc.vector.tensor_scalar(out=nmx[:], in0=t8[:, 0:1], scalar1=-1.0, scalar2=None,
                                                op0=ALU.mult), reads=[t8], writes=[nmx])
    p.op('act', lambda: nc.scalar.activation(out=ex[:], in_=lg[:], func=AF.Exp, bias=nmx[:, 0:1], scale=1.0),
         reads=[lg, nmx], writes=[ex])
    p.op('dve', lambda: nc.vector.tensor_tensor(out=ex[:], in0=ex[:], in1=msk[:], op=ALU.mult),
         reads=[ex, msk], writes=[ex])
    p.op('dve', lambda: nc.vector.reduce_sum(out=sm[:], in_=ex[:], axis=AX.X), reads=[ex], writes=[sm])
    p.op('dve', lambda: nc.vector.reciprocal(out=sm[:], in_=sm[:]), reads=[sm], writes=[sm])
    p.op('dve', lambda: nc.vector.tensor_scalar(out=lg[:], in0=ex[:], scalar1=sm[:, 0:1], scalar2=None,
                                                op0=ALU.mult), reads=[ex, sm], writes=[lg])


def load_rows_b(p, ph, rows_d, names, off=0):
    out = {}
    for i, nm in enumerate(names):
        b = ph.sb("rb_" + nm, [128, D], F32)
        p.dma('sp', b[:], rows_d[off + i:off + i + 1, :].partition_broadcast(128), reads=[rows_d], writes=[b])
        out[nm] = b
    return out


NKEY = 4096 + 256
NKT = NKEY // 128


def build_p3():
    nc, p = new_prog()
    x = p.dram("x", [TPC, D], F32, "ExternalInput")
    qT_d = p.dram("qT", [128, 8, TPC], BF16, "ExternalInput")
    kT_d = p.dram("kT", [128, 2, NKEY], BF16, "ExternalInput")
    v_d = p.dram("v", [NKEY, 256], BF16, "ExternalInput")
    f_d = p.dram("f", [4096, 1024], BF16, "ExternalInput")
    dc_d = p.dram("dftc", [4, 128, 32, 256], BF16, "ExternalInput")
    ds_d = p.dram("dfts", [4, 128, 32, 256], BF16, "ExternalInput")
    cdft_d = p.dram("cdft", [128, 2, 128], BF16, "ExternalInput")
    wo_d = p.dram("w_out", [D, D], F32, "ExternalInput")
    rows_d = p.dram("rows", [4, D], F32, "ExternalInput")
    wr_d = p.dram("w_router", [128, KC, 32], F32, "ExternalInput")
    br_d = p.dram("b_router", [1, 32], F32, "ExternalInput")
    x1_o = p.dram("x1", [TPC, D], F32, "ExternalOutput")
    t_o = p.dram("t", [TPC, D], BF16, "ExternalOutput")
    g_o = p.dram("gates", [TPC, 32], F32, "ExternalOutput")

    p.accs = [p.ps(f"acc{i}", [128, 512], F32) for i in range(7)]
    tpf = p.ps("tpf", [128, 512], F32)
    identf = p.sb("identf", [128, 128], F32)
    ident = p.sb("ident", [128, 128], BF16)
    ones = p.sb("ones", [128, 128], BF16)
    p.op('pool', lambda: nc.gpsimd.memset(identf[:], 0.0), writes=[identf])
    p.op('pool', lambda: nc.gpsimd.affine_select(out=identf[:], in_=identf[:], pattern=[[-1, 128]],
                                                 compare_op=ALU.not_equal, fill=1.0, base=0, channel_multiplier=1),
         reads=[identf], writes=[identf])
    p.op('dve', lambda: nc.vector.tensor_copy(out=ident[:], in_=identf[:]), reads=[identf], writes=[ident])
    p.op('pool', lambda: nc.gpsimd.memset(ones[:], 1.0), writes=[ones])
    p.ident, p.identf = ident, identf
    YT = p.sb("YT", [128, 8, TPC], BF16)
    OT = p.sb("OT", [128, 8, TPC], BF16)
    cd = p.sb("cd", [128, 2, 128], BF16)
    p.dma('sp', cd[:], cdft_d[:], reads=[cdft_d], writes=[cd])

    ph = Phase(p)
    U = ph.sb("U", [128, 32, 1024], BF16)
    for h in range(4):
        p.dma('sp', U[:, h * 8:(h + 1) * 8, :], f_d[h * 1024:(h + 1) * 1024, :].rearrange("(t p) n -> p t n", p=128),
              reads=[f_d], writes=[U])
    dcs = ph.scratch("dcs", [128, 32, 256], BF16, 2)
    dss = ph.scratch("dss", [128, 32, 256], BF16, 2)
    Zc = ph.sb("Zc", [128, 8, TPC], BF16)
    Zs = ph.sb("Zs", [128, 8, TPC], BF16)
    for lc in range(4):
        dc, ds = dcs.get(), dss.get()
        p.dma('sp', dc[:], dc_d[lc], reads=[dc_d], writes=[dc])
        p.dma('sp', ds[:], ds_d[lc], reads=[ds_d], writes=[ds])
        for g in range(8):
            for which, (dd, Z) in enumerate(((dc, Zc), (ds, Zs))):
                acc = p.acc()

                def mm():
                    for lt in range(32):
                        i = nc.tensor.matmul(acc[:, 0:256], lhsT=U[:, lt, g * 128:(g + 1) * 128], rhs=dd[:, lt, :],
                                             start=(lt == 0), stop=(lt == 31))
                    return i
                p.op('pe', mm, reads=[U, dd], writes=[acc])
                if which == 0:
                    p.op('act', lambda: nc.scalar.copy(out=Z[:, g, lc * 256:(lc + 1) * 256], in_=acc[:, 0:256]),
                         reads=[acc], writes=[Z])
                else:
                    p.op('dve', lambda: nc.vector.tensor_copy(out=Z[:, g, lc * 256:(lc + 1) * 256], in_=acc[:, 0:256]),
                         reads=[acc], writes=[Z])
    for g in range(8):
        for c in range(2):
            acc = p.acc()

            def mm():
                nc.tensor.matmul(acc[:], lhsT=cd[:, 0, :], rhs=Zc[:, g, c * 512:(c + 1) * 512], start=True, stop=False)
                return nc.tensor.matmul(acc[:], lhsT=cd[:, 1, :], rhs=Zs[:, g, c * 512:(c + 1) * 512], start=False,
                                        stop=True)
            p.op('pe', mm, reads=[cd, Zc, Zs], writes=[acc])
            p.op('act' if c == 0 else 'dve',
                 (lambda: nc.scalar.copy(out=YT[:, g, c * 512:(c + 1) * 512], in_=acc[:])) if c == 0 else
                 (lambda: nc.vector.tensor_copy(out=YT[:, g, c * 512:(c + 1) * 512], in_=acc[:])),
                 reads=[acc], writes=[YT])
    ph.close()

    wo = p.sb("wo", [128, KC, D], BF16)
    for h in range(4):
        p.dma('pool', wo[:, h * 4:(h + 1) * 4, :], wo_d[h * 512:(h + 1) * 512, :].rearrange("(k p) n -> p k n", p=128),
              reads=[wo_d], writes=[wo])

    ph = Phase(p)
    kT = ph.sb("kTs", [128, 2, NKEY], BF16)
    vv = ph.sb("vv", [128, NKT, 256], BF16)
    qT = ph.sb("qTs", [128, 8, TPC], BF16)
    p.dma('sp', kT[:], kT_d[:], reads=[kT_d], writes=[kT])
    p.dma('sp', vv[:], v_d.t.ap().rearrange("(t p) n -> p t n", p=128), reads=[v_d], writes=[vv])
    p.dma('sp', qT[:], qT_d[:], reads=[qT_d], writes=[qT])
    PTs = ph.scratch("PT", [128, 512], BF16, 3)
    rDs = ph.scratch("rD", [128, 512], F32, 2)
    scale = float(128 ** -0.5)
    it = 0
    for h in range(8):
        kvh = h // 4
        for qc in range(2):
            accO = p.accs[0 + 2 * (it % 2)]
            accD = p.accs[1 + 2 * (it % 2)]
            it += 1
            def emit_s(kt):
                accS = p.accs[4 + (kt % 3)]
                p.op('pe', lambda: nc.tensor.matmul(accS[:], lhsT=kT[:, kvh, kt * 128:(kt + 1) * 128],
                                                    rhs=qT[:, h, qc * 512:(qc + 1) * 512], start=True, stop=True),
                     reads=[kT, qT], writes=[accS])
            emit_s(0)
            for kt in range(NKT):
                accS = p.accs[4 + (kt % 3)]
                if kt + 1 < NKT:
                    emit_s(kt + 1)
                PT = PTs.get()
                p.op('act', lambda: nc.scalar.activation(out=PT[:], in_=accS[:], func=AF.Exp, scale=scale),
                     reads=[accS], writes=[PT])

                def mm2():
                    nc.tensor.matmul(accO[:], lhsT=vv[:, kt, kvh * 128:(kvh + 1) * 128], rhs=PT[:], start=(kt == 0),
                                     stop=(kt == NKT - 1))
                    return nc.tensor.matmul(accD[:], lhsT=ones[:], rhs=PT[:], start=(kt == 0), stop=(kt == NKT - 1))
                p.op('pe', mm2, reads=[vv, PT, ones], writes=[accO, accD])
            rD = rDs.get()
            p.op('dve', lambda: nc.vector.reciprocal(out=rD[:], in_=accD[:]), reads=[accD], writes=[rD])
            p.op('dve', lambda: nc.vector.tensor_tensor(out=OT[:, h, qc * 512:(qc + 1) * 512], in0=accO[:], in1=rD[:],
                                                        op=ALU.mult), reads=[accO, rD], writes=[OT])
    ph.close()

    ph = Phase(p)
    rb = load_rows_b(p, ph, rows_d, ["gate1", "A2", "B2", "sc2"])
    p.op('dve', lambda: nc.vector.scalar_tensor_tensor(out=rb["A2"][:], in0=rb["sc2"][:], scalar=1.0, in1=rb["A2"][:],
                                                       op0=ALU.add, op1=ALU.mult), reads=[rb["sc2"], rb["A2"]],
         writes=[rb["A2"]])
    wr = ph.sb("wr", [128, KC, 32], F32)
    brb = ph.sb("brb", [128, 32], F32)
    p.dma('sp', wr[:], wr_d[:], reads=[wr_d], writes=[wr])
    p.dma('sp', brb[:], br_d[0:1, :].partition_broadcast(128), reads=[br_d], writes=[brb])
    prep = Prep(p, ph, rb["A2"], rb["B2"], wr, brb, tpf)
    xs = ph.scratch("xs", [128, D], F32, 2)
    x1s = ph.scratch("x1s", [128, D], F32, 2)
    tmp = ph.scratch("optmp", [128, 512], F32, 2)
    p.acc_i = 0
    for t in range(TPC // 128):
        xt, x1 = xs.get(), x1s.get()
        p.dma('sp', xt[:], x[t * 128:(t + 1) * 128, :], reads=[x], writes=[xt])
        for c in range(4):
            acc = p.acc()

            def mm():
                for k in range(KC):
                    src = OT if k < 8 else YT
                    i = nc.tensor.matmul(acc[:], lhsT=src[:, k % 8, t * 128:(t + 1) * 128],
                                         rhs=wo[:, k, c * 512:(c + 1) * 512], start=(k == 0), stop=(k == KC - 1))
                return i
            p.op('pe', mm, reads=[OT, YT, wo], writes=[acc])
            tm = tmp.get()
            p.op('dve', lambda: nc.vector.tensor_tensor(out=tm[:], in0=acc[:], in1=rb["gate1"][:, c * 512:(c + 1) * 512],
                                                        op=ALU.mult), reads=[acc, rb["gate1"]], writes=[tm])
            p.op('pool', lambda: nc.gpsimd.tensor_tensor(out=x1[:, c * 512:(c + 1) * 512], in0=tm[:],
                                                         in1=xt[:, c * 512:(c + 1) * 512], op=ALU.add),
                 reads=[tm, xt], writes=[x1])
        p.dma('sp', x1_o[t * 128:(t + 1) * 128, :], x1[:], reads=[x1], writes=[x1_o])
        prep.emit(x1, t_o[t * 128:(t + 1) * 128, :], t_o, g_o[t * 128:(t + 1) * 128, :], g_o)
    p.wait_all('sp', [x1_o, t_o, g_o])
    ph.close()
    return nc


NTOK = 8192
EPC = 4
SCH = 1024
LIMIT = 7.0
ALPHA = 1.702


def build_p4():
    nc, p = new_prog()
    tT_d = p.dram("tT", [128, KC, NTOK], BF16, "ExternalInput")
    gts_d = p.dram("gts", [128, NTOK // 128, EPC], F32, "ExternalInput")
    w1_d = p.dram("w1", [EPC, D, D], F32, "ExternalInput")
    b1_d = p.dram("b1c", [128, EPC, 16], F32, "ExternalInput")
    w2_d = p.dram("w2", [EPC, 1024, D], F32, "ExternalInput")
    b2_d = p.dram("b2", [EPC, D], F32, "ExternalInput")
    yp_o = p.dram("yp", [NTOK, D], BF16, "ExternalOutput")
    p.accs = [p.ps(f"acc{i}", [128, 512], F32) for i in range(8)]
    ones1 = p.sb("ones1", [1, 128], BF16)
    p.op('pool', lambda: nc.gpsimd.memset(ones1[:], 1.0), writes=[ones1])
    gts = p.sb("gts_s", [128, NTOK // 128, EPC], F32)
    b1c = p.sb("b1c_s", [128, EPC, 16], F32)
    p.dma('sp', gts[:], gts_d[:], reads=[gts_d], writes=[gts])
    p.dma('sp', b1c[:], b1_d[:], reads=[b1_d], writes=[b1c])
    hT = p.sb("hT", [128, KC, SCH], BF16)
    yacc = [p.sb(f"yacc{i}", [128, D], F32) for i in range(SCH // 128)]
    w1ss = Scratch(p, "w1s", [128, KC, 512], BF16, 2)
    w2ss = Scratch(p, "w2s", [128, 2, D], BF16, 2)
    b2ss = Scratch(p, "b2s", [1, D], BF16, 2)
    actTs = Scratch(p, "actT", [128, 2, SCH], BF16, 2)
    g32s = Scratch(p, "g32", [128, 512], F32, 2)
    sgs = Scratch(p, "sg", [128, 512], F32, 2)
    l32s = Scratch(p, "l32", [128, 512], F32, 2)
    obs = Scratch(p, "ob", [128, D], BF16, 2)
    for sc in range(NTOK // SCH):
        for h in range(2):
            p.dma('sp', hT[:, h * 8:(h + 1) * 8, :], tT_d[:, h * 8:(h + 1) * 8, sc * SCH:(sc + 1) * SCH], reads=[tT_d],
                  writes=[hT])
        for e in range(EPC):
            b2s = b2ss.get()
            p.dma('pool', b2s[:], b2_d[e:e + 1, :], reads=[b2_d], writes=[b2s])
            for s in range(4):
                w1s, w2s, actT = w1ss.get(), w2ss.get(), actTs.get()
                p.dma('pool', w1s[:, :, 0:256], w1_d[e, :, s * 256:(s + 1) * 256].rearrange("(k p) n -> p k n", p=128),
                      reads=[w1_d], writes=[w1s])
                p.dma('pool', w1s[:, :, 256:512],
                      w1_d[e, :, 1024 + s * 256:1024 + (s + 1) * 256].rearrange("(k p) n -> p k n", p=128),
                      reads=[w1_d], writes=[w1s])
                p.dma('pool', w2s[:], w2_d[e, s * 256:(s + 1) * 256, :].rearrange("(k p) n -> p k n", p=128),
                      reads=[w2_d], writes=[w2s])
                for tc in range(SCH // 512):
                    for j in range(2):
                        accG, accL = p.acc(), p.acc()
                        for acc, off in ((accG, 0), (accL, 256)):
                            def mm():
                                for k in range(KC):
                                    i = nc.tensor.matmul(acc[:], lhsT=w1s[:, k, off + j * 128:off + (j + 1) * 128],
                                                         rhs=hT[:, k, tc * 512:(tc + 1) * 512], start=(k == 0),
                                                         stop=(k == KC - 1))
                                return i
                            p.op('pe', mm, reads=[w1s, hT], writes=[acc])
                        g32, sg, l32 = g32s.get(), sgs.get(), l32s.get()
                        jj = s * 2 + j
                        p.op('dve', lambda: nc.vector.tensor_scalar(out=g32[:], in0=accG[:], scalar1=b1c[:, e, jj:jj + 1],
                                                                    scalar2=LIMIT, op0=ALU.add, op1=ALU.min),
                             reads=[accG, b1c], writes=[g32])
                        p.op('act', lambda: nc.scalar.activation(out=sg[:], in_=g32[:], func=AF.Sigmoid, scale=ALPHA),
                             reads=[g32], writes=[sg])
                        p.op('dve', lambda: nc.vector.tensor_scalar(out=l32[:], in0=accL[:],
                                                                    scalar1=b1c[:, e, 8 + jj:8 + jj + 1], scalar2=LIMIT,
                                                                    op0=ALU.add, op1=ALU.min), reads=[accL, b1c],
                             writes=[l32])
                        p.op('pool', lambda: nc.gpsimd.tensor_scalar(out=l32[:], in0=l32[:], scalar1=-LIMIT, scalar2=1.0,
                                                                     op0=ALU.max, op1=ALU.add), reads=[l32], writes=[l32])
                        p.op('pool', lambda: nc.gpsimd.tensor_tensor(out=g32[:], in0=g32[:], in1=sg[:], op=ALU.mult),
                             reads=[g32, sg], writes=[g32])
                        p.op('pool', lambda: nc.gpsimd.tensor_tensor(out=actT[:, j, tc * 512:(tc + 1) * 512], in0=g32[:],
                                                                     in1=l32[:], op=ALU.mult), reads=[g32, l32],
                             writes=[actT])
                for tt in range(SCH // 128):
                    gcol = gts[:, sc * (SCH // 128) + tt, e:e + 1]
                    for c in range(4):
                        acc = p.acc()

                        def mm2():
                            if s == 0:
                                nc.tensor.matmul(acc[:], lhsT=ones1[0:1, :], rhs=b2s[0:1, c * 512:(c + 1) * 512],
                                                 start=True, stop=False)
                            for j in range(2):
                                i = nc.tensor.matmul(acc[:], lhsT=actT[:, j, tt * 128:(tt + 1) * 128],
                                                     rhs=w2s[:, j, c * 512:(c + 1) * 512], start=(j == 0 and s != 0),
                                                     stop=(j == 1))
                            return i
                        p.op('pe', mm2, reads=[actT, w2s, b2s, ones1], writes=[acc])
                        ya = yacc[tt]
                        if e == 0 and s == 0:
                            p.op('dve', lambda: nc.vector.tensor_scalar(out=ya[:, c * 512:(c + 1) * 512], in0=acc[:],
                                                                        scalar1=gcol, scalar2=None, op0=ALU.mult),
                                 reads=[acc, gts], writes=[ya])
                        else:
                            p.op('dve', lambda: nc.vector.scalar_tensor_tensor(
                                out=ya[:, c * 512:(c + 1) * 512], in0=acc[:], scalar=gcol,
                                in1=ya[:, c * 512:(c + 1) * 512], op0=ALU.mult, op1=ALU.add),
                                reads=[acc, gts, ya], writes=[ya])
        for tt in range(SCH // 128):
            ob = obs.get()
            p.op('act', lambda: nc.scalar.copy(out=ob[:], in_=yacc[tt][:]), reads=[yacc[tt]], writes=[ob])
            p.dma('sp', yp_o[sc * SCH + tt * 128:sc * SCH + (tt + 1) * 128, :], ob[:], reads=[ob], writes=[yp_o])
    p.wait_all('sp', [yp_o])
    return nc


def emit_combine(p, xt, rows, ypt, gate_b, xo, tmps):
    nc = p.nc
    for c in range(4):
        acc = p.acc()

        def mm():
            for i in range(8):
                ins = nc.tensor.matmul(acc[0:rows, :], lhsT=p.ident[0:rows, 0:rows], rhs=ypt[0:rows, i, c * 512:(c + 1) * 512],
                                       start=(i == 0), stop=(i == 7))
            return ins
        p.op('pe', mm, reads=[p.ident, ypt], writes=[acc])
        tm = tmps.get()
        p.op('dve', lambda: nc.vector.tensor_tensor(out=tm[0:rows, :], in0=acc[0:rows, :],
                                                    in1=gate_b[0:rows, c * 512:(c + 1) * 512], op=ALU.mult),
             reads=[acc, gate_b], writes=[tm])
        p.op('pool', lambda: nc.gpsimd.tensor_tensor(out=xo[0:rows, c * 512:(c + 1) * 512], in0=tm[0:rows, :],
                                                     in1=xt[0:rows, c * 512:(c + 1) * 512], op=ALU.add),
             reads=[tm, xt], writes=[xo])


def setup_ident(p):
    nc = p.nc
    identf = p.sb("identf", [128, 128], F32)
    ident = p.sb("ident", [128, 128], BF16)
    p.op('pool', lambda: nc.gpsimd.memset(identf[:], 0.0), writes=[identf])
    p.op('pool', lambda: nc.gpsimd.affine_select(out=identf[:], in_=identf[:], pattern=[[-1, 128]],
                                                 compare_op=ALU.not_equal, fill=1.0, base=0, channel_multiplier=1),
         reads=[identf], writes=[identf])
    p.op('dve', lambda: nc.vector.tensor_copy(out=ident[:], in_=identf[:]), reads=[identf], writes=[ident])
    p.ident, p.identf = ident, identf


HALO = 16
LTOK = TPC + 2 * HALO
CW = 31


def build_p6():
    nc, p = new_prog()
    x1h_d = p.dram("x1h", [LTOK, D], F32, "ExternalInput")
    yph_d = p.dram("yph", [8, LTOK, D], BF16, "ExternalInput")
    rows_d = p.dram("rows", [6, D], F32, "ExternalInput")
    cols_d = p.dram("cols", [128, 3, KC], F32, "ExternalInput")
    cvb_d = p.dram("cvb", [128, 32 + 16 + 16 + 16], F32, "ExternalInput")
    wdw_d = p.dram("wdw", [128, KC, CW], F32, "ExternalInput")
    mask_d = p.dram("mask", [128, 2], F32, "ExternalInput")
    wi_d = p.dram("cv_w_in", [KC, 128, KC, 256], F32, "ExternalInput")
    wo_d = p.dram("cv_w_out", [D, D], F32, "ExternalInput")
    wr_d = p.dram("w_router", [128, KC, 32], F32, "ExternalInput")
    br_d = p.dram("b_router", [1, 32], F32, "ExternalInput")
    x2_o = p.dram("x2", [TPC, D], F32, "ExternalOutput")
    t_o = p.dram("t", [TPC, D], BF16, "ExternalOutput")
    g_o = p.dram("gates", [TPC, 32], F32, "ExternalOutput")
    x1p = p.dram("x1p", [LTOK, D], F32)
    p.accs = [p.ps(f"acc{i}", [128, 512], F32) for i in range(6)]
    p.tps = [p.ps("tpb0", [128, 1024], BF16)]
    tpf = p.ps("tpf", [128, 512], F32)
    setup_ident(p)
    ones = p.sb("ones", [128, 128], BF16)
    ones1 = p.sb("ones1", [1, 128], BF16)
    p.op('pool', lambda: nc.gpsimd.memset(ones[:], 1.0), writes=[ones])
    p.op('pool', lambda: nc.gpsimd.memset(ones1[:], 1.0), writes=[ones1])
    colsb = p.sb("colsb", [128, 3, KC], F32)
    cvb = p.sb("cvbs", [128, 80], F32)
    wdw = p.sb("wdws", [128, KC, CW], F32)
    msk = p.sb("msks", [128, 2], F32)
    p.dma('sp', colsb[:], cols_d[:], reads=[cols_d], writes=[colsb])
    p.dma('sp', cvb[:], cvb_d[:], reads=[cvb_d], writes=[cvb])
    p.dma('sp', wdw[:], wdw_d[:], reads=[wdw_d], writes=[wdw])
    p.dma('sp', msk[:], mask_d[:], reads=[mask_d], writes=[msk])
    A1 = p.sb("A1", [128, KC], F32)
    B1 = p.sb("B1", [128, KC], F32)
    p.op('dve', lambda: nc.vector.scalar_tensor_tensor(out=A1[:], in0=colsb[:, 2, :], scalar=1.0, in1=colsb[:, 0, :],
                                                       op0=ALU.add, op1=ALU.mult), reads=[colsb], writes=[A1])
    p.op('dve', lambda: nc.vector.tensor_copy(out=B1[:], in_=colsb[:, 1, :]), reads=[colsb], writes=[B1])
    uc = p.sb("uc", [128, KC, TPC], BF16)
    mean_b = [p.sb(f"mean_b{c}", [128, 512], F32) for c in range(2)]
    rstd_b = [p.sb(f"rstd_b{c}", [128, 512], F32) for c in range(2)]
    phA = Phase(p)
    hT = phA.sb("hT", [128, KC, LTOK], BF16)

    ph = Phase(p)
    g2b = ph.sb("g2b", [128, D], F32)
    p.dma('sp', g2b[:], rows_d[0:1, :].partition_broadcast(128), reads=[rows_d], writes=[g2b])
    nt = NormT.__new__(NormT)
    nt.p = p
    nt.sq = ph.scratch("nt_sq", [128, D], BF16, 1)
    nt.ss = ph.scratch("nt_ss", [128, 1], F32, 2)
    nt.xn = ph.scratch("nt_xn", [128, D], BF16, 2)
    xs = ph.scratch("xs", [128, D], F32, 2)
    xos = ph.scratch("xo", [128, D], F32, 2)
    yps = ph.scratch("ypt", [128, 8, D], BF16, 2)
    tmps = ph.scratch("ctmp", [128, 512], F32, 2)
    ntl = (LTOK + 127) // 128
    for t in range(ntl):
        r0 = t * 128
        rows = min(128, LTOK - r0)
        xt, xo, ypt = xs.get(), xos.get(), yps.get()
        p.dma('sp', xt[0:rows, :], x1h_d[r0:r0 + rows, :], reads=[x1h_d], writes=[xt])
        p.dma('sp', ypt[0:rows, :, :], yph_d[:, r0:r0 + rows, :].rearrange("c t d -> t c d"), reads=[yph_d], writes=[ypt])
        emit_combine(p, xt, rows, ypt, g2b, xo, tmps)
        p.dma('sp', x1p[r0:r0 + rows, :], xo[0:rows, :], reads=[xo], writes=[x1p])
        nt.emit(xo, rows, A1, B1, hT, r0)
    ph.close()

    ph = Phase(p)
    wis = ph.scratch("wis", [128, KC, 256], BF16, 2)
    uTs = ph.scratch("uT", [128, LTOK], BF16, 2)
    sgs = ph.scratch("sgm", [128, 512], F32, 2)
    dgs = ph.scratch("dg", [128, CW, 128], BF16, 2)
    sqs = ph.scratch("csq", [128, 512], BF16, 2)
    accM = [p.ps(f"accM{c}", [128, 512], F32) for c in range(0)]
    accM = [p.accs[0], p.accs[1]]
    accQ = [p.accs[2], p.accs[3]]
    rot = [p.accs[4], p.accs[5], tpf]
    ri = 0
    chunks = [(0, 512), (512, 512), (1024, LTOK - 1024)]
    for ct in range(KC):
        wi, uT, dg = wis.get(), uTs.get(), dgs.get()
        p.dma('pool', wi[:], wi_d[ct], reads=[wi_d], writes=[wi])
        p.op('pool', lambda: nc.gpsimd.tensor_tensor(
            out=dg[:], in0=p.ident[:].unsqueeze(1).broadcast_to([128, CW, 128]),
            in1=wdw[:, ct, :].unsqueeze(2).broadcast_to([128, CW, 128]), op=ALU.mult), reads=[p.ident, wdw], writes=[dg])
        for (c0, cn) in chunks:
            accA, accG = rot[ri % 3], rot[(ri + 1) % 3]
            ri += 2
            for acc, off in ((accA, 0), (accG, 128)):
                def mm():
                    for k in range(KC):
                        i = nc.tensor.matmul(acc[:, 0:cn], lhsT=wi[:, k, off:off + 128], rhs=hT[:, k, c0:c0 + cn],
                                             start=(k == 0), stop=(k == KC - 1))
                    return i
                p.op('pe', mm, reads=[wi, hT], writes=[acc])
            sg = sgs.get()
            p.op('act', lambda: nc.scalar.activation(out=sg[:, 0:cn], in_=accG[:, 0:cn], func=AF.Sigmoid,
                                                     bias=cvb[:, 16 + ct:16 + ct + 1], scale=1.0), reads=[accG, cvb],
                 writes=[sg])
            p.op('dve', lambda: nc.vector.scalar_tensor_tensor(out=uT[:, c0:c0 + cn], in0=accA[:, 0:cn],
                                                               scalar=cvb[:, ct:ct + 1], in1=sg[:, 0:cn], op0=ALU.add,
                                                               op1=ALU.mult), reads=[accA, cvb, sg], writes=[uT])
        p.op('dve', lambda: nc.vector.tensor_scalar(out=uT[:, 0:HALO], in0=uT[:, 0:HALO], scalar1=msk[:, 0:1], scalar2=None,
                                                    op0=ALU.mult), reads=[uT, msk], writes=[uT])
        p.op('dve', lambda: nc.vector.tensor_scalar(out=uT[:, LTOK - HALO:LTOK], in0=uT[:, LTOK - HALO:LTOK],
                                                    scalar1=msk[:, 1:2], scalar2=None, op0=ALU.mult), reads=[uT, msk],
             writes=[uT])
        for c in range(2):
            acc = rot[ri % 3]
            ri += 1

            def mmc():
                for j in range(CW):
                    i = nc.tensor.matmul(acc[:], lhsT=dg[:, j, :], rhs=uT[:, c * 512 + j + 1:c * 512 + j + 1 + 512],
                                         start=(j == 0), stop=(j == CW - 1))
                return i
            p.op('pe', mmc, reads=[dg, uT], writes=[acc])
            p.op('dve', lambda: nc.vector.tensor_scalar(out=uc[:, ct, c * 512:(c + 1) * 512], in0=acc[:],
                                                        scalar1=cvb[:, 32 + ct:32 + ct + 1], scalar2=None, op0=ALU.add),
                 reads=[acc, cvb], writes=[uc])
            sq = sqs.get()
            p.op('act', lambda: nc.scalar.activation(out=sq[:], in_=uc[:, ct, c * 512:(c + 1) * 512], func=AF.Square),
                 reads=[uc], writes=[sq])

            def mms():
                nc.tensor.matmul(accM[c][:], lhsT=ones[:], rhs=uc[:, ct, c * 512:(c + 1) * 512], start=(ct == 0),
                                 stop=(ct == KC - 1))
                return nc.tensor.matmul(accQ[c][:], lhsT=ones[:], rhs=sq[:], start=(ct == 0), stop=(ct == KC - 1))
            p.op('pe', mms, reads=[ones, uc, sq], writes=[accM[c], accQ[c]])
    for c in range(2):
        p.op('dve', lambda: nc.vector.tensor_scalar(out=mean_b[c][:], in0=accM[c][:], scalar1=1.0 / D, scalar2=None,
                                                    op0=ALU.mult), reads=[accM[c]], writes=[mean_b[c]])
        p.op('dve', lambda: nc.vector.tensor_tensor(out=rstd_b[c][:], in0=mean_b[c][:], in1=mean_b[c][:], op=ALU.mult),
             reads=[mean_b[c]], writes=[rstd_b[c]])
        p.op('dve', lambda: nc.vector.scalar_tensor_tensor(out=rstd_b[c][:], in0=accQ[c][:], scalar=1.0 / D,
                                                           in1=rstd_b[c][:], op0=ALU.mult, op1=ALU.subtract),
             reads=[accQ[c], rstd_b[c]], writes=[rstd_b[c]])
        p.op('dve', lambda: nc.vector.tensor_scalar(out=rstd_b[c][:], in0=rstd_b[c][:], scalar1=EPS, scalar2=None,
                                                    op0=ALU.add), reads=[rstd_b[c]], writes=[rstd_b[c]])
        p.op('act', lambda: nc.scalar.sqrt(out=rstd_b[c][:], in_=rstd_b[c][:]), reads=[rstd_b[c]], writes=[rstd_b[c]])
        p.op('dve', lambda: nc.vector.reciprocal(out=rstd_b[c][:], in_=rstd_b[c][:]), reads=[rstd_b[c]],
             writes=[rstd_b[c]])
    ph.close()
    phA.close()

    ph = Phase(p)
    wo = ph.sb("wo", [128, KC, D], BF16)
    for h in range(4):
        p.dma('pool', wo[:, h * 4:(h + 1) * 4, :], wo_d[h * 512:(h + 1) * 512, :].rearrange("(k p) n -> p k n", p=128),
              reads=[wo_d], writes=[wo])
    lts = ph.scratch("lnt", [128, 512], F32, 2)
    for ct in range(KC):
        for c in range(2):
            lt = lts.get()
            p.op('dve', lambda: nc.vector.tensor_tensor(out=lt[:], in0=uc[:, ct, c * 512:(c + 1) * 512], in1=mean_b[c][:],
                                                        op=ALU.subtract), reads=[uc, mean_b[c]], writes=[lt])
            p.op('pool', lambda: nc.gpsimd.tensor_tensor(out=lt[:], in0=lt[:], in1=rstd_b[c][:], op=ALU.mult),
                 reads=[lt, rstd_b[c]], writes=[lt])
            p.op('act', lambda: nc.scalar.activation(out=uc[:, ct, c * 512:(c + 1) * 512], in_=lt[:], func=AF.Silu,
                                                     scale=cvb[:, 48 + ct:48 + ct + 1], bias=cvb[:, 64 + ct:64 + ct + 1]),
                 reads=[lt, cvb], writes=[uc])
    rb = load_rows_b(p, ph, rows_d, ["gate1", "A2", "B2", "sc2"], off=1)
    p.op('dve', lambda: nc.vector.scalar_tensor_tensor(out=rb["A2"][:], in0=rb["sc2"][:], scalar=1.0, in1=rb["A2"][:],
                                                       op0=ALU.add, op1=ALU.mult), reads=[rb["sc2"], rb["A2"]],
         writes=[rb["A2"]])
    bo = ph.sb("bo", [1, D], BF16)
    p.dma('pool', bo[:], rows_d[5:6, :], reads=[rows_d], writes=[bo])
    wr = ph.sb("wr", [128, KC, 32], F32)
    brb = ph.sb("brb", [128, 32], F32)
    p.dma('sp', wr[:], wr_d[:], reads=[wr_d], writes=[wr])
    p.dma('sp', brb[:], br_d[0:1, :].partition_broadcast(128), reads=[br_d], writes=[brb])
    prep = Prep(p, ph, rb["A2"], rb["B2"], wr, brb, tpf)
    xs = ph.scratch("xs4", [128, D], F32, 2)
    x2s = ph.scratch("x2s", [128, D], F32, 1)
    tmp = ph.scratch("optmp", [128, 512], F32, 2)
    p.acc_i = 0
    for t in range(TPC // 128):
        xt, x2 = xs.get(), x2s.get()
        p.dma('sp', xt[:], x1p[HALO + t * 128:HALO + (t + 1) * 128, :], reads=[x1p], writes=[xt])
        for c in range(4):
            acc = p.acc()

            def mm():
                nc.tensor.matmul(acc[:], lhsT=ones1[0:1, :], rhs=bo[0:1, c * 512:(c + 1) * 512], start=True, stop=False)
                for k in range(KC):
                    i = nc.tensor.matmul(acc[:], lhsT=uc[:, k, t * 128:(t + 1) * 128], rhs=wo[:, k, c * 512:(c + 1) * 512],
                                         start=False, stop=(k == KC - 1))
                return i
            p.op('pe', mm, reads=[uc, wo, bo, ones1], writes=[acc])
            tm = tmp.get()
            p.op('dve', lambda: nc.vector.tensor_tensor(out=tm[:], in0=acc[:], in1=rb["gate1"][:, c * 512:(c + 1) * 512],
                                                        op=ALU.mult), reads=[acc, rb["gate1"]], writes=[tm])
            p.op('pool', lambda: nc.gpsimd.tensor_tensor(out=x2[:, c * 512:(c + 1) * 512], in0=tm[:],
                                                         in1=xt[:, c * 512:(c + 1) * 512], op=ALU.add),
                 reads=[tm, xt], writes=[x2])
        p.dma('sp', x2_o[t * 128:(t + 1) * 128, :], x2[:], reads=[x2], writes=[x2_o])
        prep.emit(x2, t_o[t * 128:(t + 1) * 128, :], t_o, g_o[t * 128:(t + 1) * 128, :], g_o)
    p.wait_all('sp', [x2_o, t_o, g_o])
    ph.close()
    return nc


def build_p8():
    nc, p = new_prog()
    x2_d = p.dram("x2", [TPC, D], F32, "ExternalInput")
    yph_d = p.dram("yph", [8, TPC, D], BF16, "ExternalInput")
    rows_d = p.dram("rows", [2, D], F32, "ExternalInput")
    out_o = p.dram("out", [TPC, D], F32, "ExternalOutput")
    p.accs = [p.ps(f"acc{i}", [128, 512], F32) for i in range(8)]
    setup_ident(p)
    ph = Phase(p)
    rb = load_rows_b(p, ph, rows_d, ["g2", "gf"])
    xs = ph.scratch("xs", [128, D], F32, 2)
    xos = ph.scratch("xo", [128, D], F32, 2)
    yps = ph.scratch("ypt", [128, 8, D], BF16, 2)
    tmps = ph.scratch("ctmp", [128, 512], F32, 2)
    sqs = ph.scratch("sq", [128, D], BF16, 1)
    sss = ph.scratch("ss", [128, 1], F32, 2)
    oos = ph.scratch("oo", [128, D], F32, 2)
    for t in range(TPC // 128):
        r0 = t * 128
        xt, xo, ypt, sq, ss, oo = xs.get(), xos.get(), yps.get(), sqs.get(), sss.get(), oos.get()
        p.dma('sp', xt[:], x2_d[r0:r0 + 128, :], reads=[x2_d], writes=[xt])
        p.dma('sp', ypt[:], yph_d[:, r0:r0 + 128, :].rearrange("c t d -> t c d"), reads=[yph_d], writes=[ypt])
        emit_combine(p, xt, 128, ypt, rb["g2"], xo, tmps)
        p.op('act', lambda: nc.scalar.activation(out=sq[:], in_=xo[:], func=AF.Square, accum_out=ss[:]), reads=[xo],
             writes=[sq, ss])
        emit_rstd(p, ss[:], 128, 1.0 / D, ss)
        p.op('dve', lambda: nc.vector.scalar_tensor_tensor(out=oo[:], in0=xo[:], scalar=ss[:, 0:1], in1=rb["gf"][:],
                                                           op0=ALU.mult, op1=ALU.mult), reads=[xo, ss, rb["gf"]],
             writes=[oo])
        p.dma('sp', out_o[r0:r0 + 128, :], oo[:], reads=[oo], writes=[out_o])
    p.wait_all('sp', [out_o])
    ph.close()
    return nc


def _run(nc, ims):
    res = run_bass_kernel_spmd(nc, ims, core_ids=list(range(NCORES)))
    return res.results


def _cf(v, n):
    return np.ascontiguousarray(np.asarray(v).reshape(n, 128).T)


def _rope_table(pos):
    inv = (10000.0 ** (-np.arange(0, 64, 2, dtype=np.float32) / np.float32(64))).astype(np.float32)
    ar = ((pos // 64).astype(np.float32)[:, None] * inv).astype(np.float64)
    ac = ((pos % 64).astype(np.float32)[:, None] * inv).astype(np.float64)
    cr, sr, cc, sc = np.cos(ar), np.sin(ar), np.cos(ac), np.sin(ac)
    return np.ascontiguousarray(np.stack([np.concatenate([cr, cr, cc, cc], 1),
                                          np.concatenate([-sr, sr, -sc, sc], 1)], 1).astype(np.float32))


def _dft_tables(qt):
    l = np.arange(4096, dtype=np.int64)[:, None]
    lp = (qt * TPC + np.arange(TPC, dtype=np.int64))[None, :]
    ang = 2.0 * np.pi * ((l * lp) % 4096).astype(np.float64) / 4096.0
    lay = lambda a: np.ascontiguousarray(a.astype(NPBF).reshape(32, 128, 4, 256).transpose(2, 1, 0, 3))
    return lay(np.cos(ang)), lay(np.sin(ang))


def _cdft():
    c = np.arange(128, dtype=np.int64)[:, None]
    cp = np.arange(128, dtype=np.int64)[None, :]
    ang = 2.0 * np.pi * ((c * cp) % 128).astype(np.float64) / 128.0
    s = np.sqrt(4096.0 * 128.0)
    return np.ascontiguousarray(np.stack([np.cos(ang) / s, -np.sin(ang) / s], 1).astype(NPBF))


def _moe_launch(layer, t_all, gates_all, moe_w1, moe_b1, moe_w2, moe_b2):
    gi = np.ascontiguousarray(gates_all.reshape(NTOK // 128, 128, NEXP).transpose(1, 0, 2))
    info = np.asarray(_run(build_p5(), [{"g": gi}] * NCORES)[0]["info"])[0]
    rank_of = [int(v) for v in info[:NEXP]]
    expert_at = [0] * NEXP
    for e, r in enumerate(rank_of):
        expert_at[r] = e
    caps = []
    for j in range(EPC):
        caps.append(next(cb for cb in range(1, NTOK // 128 + 1) if info[NEXP + j] <= cb * 128))
    ims = []
    t_rows = np.ascontiguousarray(t_all)
    for i in range(NCORES):
        ex = [expert_at[8 * j + i] for j in range(EPC)]
        w1 = np.asarray(moe_w1[layer][ex])
        w1p = np.ascontiguousarray(np.concatenate([w1[..., 0::2], w1[..., 1::2]], -1))
        b1 = np.asarray(moe_b1[layer][ex])
        b1p = np.concatenate([b1[..., 0::2], b1[..., 1::2]], -1)
        ims.append({
            "t": t_rows,
            "gts": np.ascontiguousarray(gates_all[:, ex].reshape(NTOK // 128, 128, EPC).transpose(1, 0, 2)),
            "w1": w1p,
            "b1c": np.ascontiguousarray(b1p.reshape(EPC, 16, 128).transpose(2, 0, 1)),
            "w2": np.ascontiguousarray(moe_w2[layer][ex]),
            "b2": np.ascontiguousarray(moe_b2[layer][ex]),
        })
    res = _run(build_p4s(tuple(caps)), ims)
    overflow = any(bool((np.asarray(r["cnt"])[0] > np.array(caps) * 128).any()) for r in res)
    if overflow:
        tT = np.ascontiguousarray(t_all.reshape(NTOK, KC, 128).transpose(2, 1, 0))
        for im in ims:
            del im["t"]
            im["tT"] = tT
        res = _run(build_p4(), ims)
    return np.stack([np.asarray(r["yp"]) for r in res], 0)


def kernel(x, c, ctx, c_ctx, w_mod, b_mod, g_mix, g_ffn, ab_w_in, ab_q_gain, ab_k_gain, ab_w_out,
           cv_w_in, cv_b_in, cv_w_dw, cv_b_dw, cv_ln_g, cv_ln_b, cv_w_out, cv_b_out,
           moe_w_router, moe_b_router, moe_w1, moe_b1, moe_w2, moe_b2, g_final):
    A = lambda a: np.asarray(a, dtype=np.float32)
    x, c, ctx, c_ctx, w_mod, b_mod, g_mix, g_ffn = map(A, (x, c, ctx, c_ctx, w_mod, b_mod, g_mix, g_ffn))
    ab_w_in, ab_q_gain, ab_k_gain, ab_w_out = map(A, (ab_w_in, ab_q_gain, ab_k_gain, ab_w_out))
    cv_w_in, cv_b_in, cv_w_dw, cv_b_dw, cv_ln_g, cv_ln_b, cv_w_out, cv_b_out = map(
        A, (cv_w_in, cv_b_in, cv_w_dw, cv_b_dw, cv_ln_g, cv_ln_b, cv_w_out, cv_b_out))
    moe_w_router, moe_b_router, moe_w1, moe_b1, moe_w2, moe_b2, g_final = map(
        A, (moe_w_router, moe_b_router, moe_w1, moe_b1, moe_w2, moe_b2, g_final))
    B, L, _ = x.shape
    cores = [(i // 4, i % 4) for i in range(NCORES)]

    cv = np.stack([c[0], c[1], c_ctx])
    cT = np.ascontiguousarray(cv.T.reshape(KC, 128, 3).transpose(1, 0, 2))
    ims = []
    for i in range(NCORES):
        sl = slice(i * MODW, (i + 1) * MODW)
        ims.append({"wmod": np.ascontiguousarray(w_mod[:, :, sl]),
                    "bmod": np.ascontiguousarray(np.broadcast_to(b_mod[:, None, sl], (2, 3, MODW))), "cT": cT})
    res = _run(build_p1(), ims)
    m = np.concatenate([np.asarray(r["m"]) for r in res], -1)
    mod = m.reshape(2, 3, 6, D)

    ims = []
    for (b, qt) in cores:
        colsv = np.stack([g_mix[0], mod[0, b, 0], mod[0, b, 1], mod[0, 2, 0], mod[0, 2, 1]])
        ims.append({"x": np.ascontiguousarray(x[b, qt * TPC:(qt + 1) * TPC]),
                    "ctxr": np.ascontiguousarray(ctx[b, qt * CPC:(qt + 1) * CPC]),
                    "cols": np.ascontiguousarray(colsv.reshape(5, KC, 128).transpose(2, 0, 1)),
                    "w_in": np.ascontiguousarray(ab_w_in[0]),
                    "gains": np.ascontiguousarray(np.stack([ab_q_gain[0], ab_k_gain[0]])),
                    "rope": _rope_table(qt * TPC + np.arange(TPC))})
    res = _run(build_p2(), ims)
    qkvf = [np.asarray(r["qkvf"]) for r in res]
    kvc = [np.asarray(r["kvc"]) for r in res]
    q_b, k_b, v_b, f_b = [], [], [], []
    for b in range(B):
        lat = np.concatenate(qkvf[b * 4:(b + 1) * 4], 0)
        cx = np.concatenate(kvc[b * 4:(b + 1) * 4], 0)
        q_b.append(lat[:, 0:1024])
        k_b.append(np.concatenate([lat[:, 1024:1280], cx[:, 0:256]], 0))
        v_b.append(np.concatenate([lat[:, 1280:1536], cx[:, 256:512]], 0))
        f_b.append(lat[:, 1536:2560])

    cd = _cdft()
    dft = [_dft_tables(qt) for qt in range(4)]
    wr_l = [np.ascontiguousarray(moe_w_router[l].reshape(KC, 128, 32).transpose(1, 0, 2)) for l in range(2)]
    ims = []
    for (b, qt) in cores:
        ims.append({"x": np.ascontiguousarray(x[b, qt * TPC:(qt + 1) * TPC]),
                    "qT": np.ascontiguousarray(q_b[b][qt * TPC:(qt + 1) * TPC].reshape(TPC, 8, 128).transpose(2, 1, 0)),
                    "kT": np.ascontiguousarray(k_b[b].reshape(NKEY, 2, 128).transpose(2, 1, 0)),
                    "v": np.ascontiguousarray(v_b[b]), "f": np.ascontiguousarray(f_b[b]),
                    "dftc": dft[qt][0], "dfts": dft[qt][1], "cdft": cd,
                    "w_out": np.ascontiguousarray(ab_w_out[0]),
                    "rows": np.ascontiguousarray(np.stack([mod[0, b, 2], g_ffn[0], mod[0, b, 3], mod[0, b, 4]])),
                    "w_router": wr_l[0], "b_router": np.ascontiguousarray(moe_b_router[0][None, :])})
    res = _run(build_p3(), ims)
    x1_all = np.concatenate([np.asarray(r["x1"]) for r in res], 0)
    t_all = np.concatenate([np.asarray(r["t"]) for r in res], 0)
    g_all = np.concatenate([np.asarray(r["gates"]) for r in res], 0)
    del dft

    yp_all = _moe_launch(0, t_all, g_all, moe_w1, moe_b1, moe_w2, moe_b2)

    cvb = np.ascontiguousarray(np.concatenate([_cf(cv_b_in[0], 32), _cf(cv_b_dw[0], 16), _cf(cv_ln_g[0], 16),
                                               _cf(cv_ln_b[0], 16)], 1))
    wdw = np.ascontiguousarray(cv_w_dw[0].T.reshape(KC, 128, CW).transpose(1, 0, 2))
    wa = cv_w_in[0][:, :D].reshape(KC, 128, KC, 128)
    wg = cv_w_in[0][:, D:].reshape(KC, 128, KC, 128)
    cvwi = np.ascontiguousarray(np.concatenate([wa, wg], -1).transpose(2, 1, 0, 3))
    ims = []
    for i, (b, qt) in enumerate(cores):
        rows_g = i * TPC - HALO + np.arange(LTOK)
        valid = (rows_g >= b * L) & (rows_g < (b + 1) * L)
        x1h = np.zeros((LTOK, D), np.float32)
        x1h[valid] = x1_all[rows_g[valid]]
        yph = np.zeros((8, LTOK, D), NPBF)
        yph[:, valid] = yp_all[:, rows_g[valid]]
        colsv = np.stack([g_mix[1], mod[1, b, 0], mod[1, b, 1]])
        ims.append({"x1h": x1h, "yph": yph,
                    "rows": np.ascontiguousarray(np.stack([mod[0, b, 5], mod[1, b, 2], g_ffn[1], mod[1, b, 3],
                                                           mod[1, b, 4], cv_b_out[0]])),
                    "cols": np.ascontiguousarray(colsv.reshape(3, KC, 128).transpose(2, 0, 1)),
                    "cvb": cvb, "wdw": wdw,
                    "mask": np.ascontiguousarray(np.broadcast_to(
                        np.array([0.0 if qt == 0 else 1.0, 0.0 if qt == 3 else 1.0], np.float32), (128, 2))),
                    "cv_w_in": cvwi, "cv_w_out": np.ascontiguousarray(cv_w_out[0]),
                    "w_router": wr_l[1], "b_router": np.ascontiguousarray(moe_b_router[1][None, :])})
    res = _run(build_p6(), ims)
    x2_all = np.concatenate([np.asarray(r["x2"]) for r in res], 0)
    t2_all = np.concatenate([np.asarray(r["t"]) for r in res], 0)
    g2_all = np.concatenate([np.asarray(r["gates"]) for r in res], 0)
    del yp_all, ims

    yp2_all = _moe_launch(1, t2_all, g2_all, moe_w1, moe_b1, moe_w2, moe_b2)

    ims = []
    for i, (b, qt) in enumerate(cores):
        ims.append({"x2": np.ascontiguousarray(x2_all[i * TPC:(i + 1) * TPC]),
                    "yph": np.ascontiguousarray(yp2_all[:, i * TPC:(i + 1) * TPC]),
                    "rows": np.ascontiguousarray(np.stack([mod[1, b, 5], g_final]))})
    res = _run(build_p8(), ims)
    out = np.concatenate([np.asarray(r["out"]) for r in res], 0).reshape(B, L, D).astype(np.float32)
    return out


CAPB = 16
BIG = 1.0e6
I32 = mybir.dt.int32


def p_idma(p, out_ap, out_off, in_ap, in_off, bound, reads, dst, concurrent=True):
    nc = p.nc
    for b in reads:
        if b.w is not None:
            p._wait('pool', b.w)
    if concurrent:
        if getattr(dst, 'wbase', None) is not None:
            p._wait('pool', dst.wbase)
    else:
        if dst.w is not None:
            p._wait('pool', dst.w)
    for ev in dst.r:
        p._wait('pool', ev)
    if dst.dsem is None:
        key = 'd_' + dst.name
        p.sems[key] = nc.alloc_semaphore(key)
        dst.dsem = key
    if not hasattr(p, 'bound_regs'):
        p.bound_regs = {}
    if bound not in p.bound_regs:
        p.bound_regs[bound] = nc.gpsimd.to_reg(bound)
    inst = nc.gpsimd.indirect_dma_start(out=out_ap, out_offset=out_off, in_=in_ap, in_offset=in_off,
                                        bounds_check=p.bound_regs[bound], oob_is_err=False)
    dst.dcnt += 16
    p.dma_cnt[dst.dsem] = dst.dcnt
    inst.then_inc(p.sems[dst.dsem], 16)
    ev = (dst.dsem, dst.dcnt)
    for b in reads:
        b.r.append(ev)
    dst.w = ev
    dst.r = []
    return inst


def build_p4s(caps=(CAPB,) * 4):
    caps = tuple(int(c) for c in caps)
    CAPRS = [c * 128 for c in caps]
    NT = NTOK // 128
    nc, p = new_prog()
    t_d = p.dram("t", [NTOK, D], BF16, "ExternalInput")
    gts_d = p.dram("gts", [128, NT, EPC], F32, "ExternalInput")
    w1_d = p.dram("w1", [EPC, D, D], F32, "ExternalInput")
    b1_d = p.dram("b1c", [128, EPC, 16], F32, "ExternalInput")
    w2_d = p.dram("w2", [EPC, 1024, D], F32, "ExternalInput")
    b2_d = p.dram("b2", [EPC, D], F32, "ExternalInput")
    yp_o = p.dram("yp", [NTOK, D], BF16, "ExternalOutput")
    cnt_o = p.dram("cnt", [1, EPC], F32, "ExternalOutput")
    Xg = [p.dram(f"Xg{e}", [CAPRS[e], D], BF16) for e in range(EPC)]
    Yg = [p.dram(f"Yg{e}", [CAPRS[e], D], BF16) for e in range(EPC)]
    p.accs = [p.ps(f"acc{i}", [128, 512], F32) for i in range(6)]
    p.tps = [p.ps(f"tpb{i}", [128, 1024], BF16) for i in range(2)]
    setup_ident(p)
    ones = p.sb("ones", [128, 128], BF16)
    ones1 = p.sb("ones1", [1, 128], BF16)
    tri = p.sb("tri", [128, 128], BF16)
    trif = p.sb("trif", [128, 128], F32)
    p.op('pool', lambda: nc.gpsimd.memset(ones[:], 1.0), writes=[ones])
    p.op('pool', lambda: nc.gpsimd.memset(ones1[:], 1.0), writes=[ones1])
    p.op('pool', lambda: nc.gpsimd.memset(trif[:], 1.0), writes=[trif])
    p.op('pool', lambda: nc.gpsimd.affine_select(out=trif[:], in_=trif[:], pattern=[[1, 128]], compare_op=ALU.is_gt,
                                                 fill=0.0, base=0, channel_multiplier=-1), reads=[trif], writes=[trif])
    p.op('dve', lambda: nc.vector.tensor_copy(out=tri[:], in_=trif[:]), reads=[trif], writes=[tri])
    gts = p.sb("gts_s", [128, NT, EPC], F32)
    b1c = p.sb("b1c_s", [128, EPC, 16], F32)
    p.dma('sp', gts[:], gts_d[:], reads=[gts_d], writes=[gts])
    p.dma('sp', b1c[:], b1_d[:], reads=[b1_d], writes=[b1c])
    b2b = p.sb("b2b", [1, EPC, D], BF16)
    rank_si = p.sb("rank_si", [128, NT * EPC], I32)
    rank_gi = p.sb("rank_gi", [128, NT * EPC], I32)
    phW = Phase(p)
    w1b = phW.sb("w1b", [128, KC, D], BF16)
    w2b = phW.sb("w2b", [128, 8, D], BF16)
    p.dma('pool', b2b[:], b2_d.t.ap().unsqueeze(0) if hasattr(b2_d.t.ap(), "unsqueeze") else b2_d[:, :], reads=[b2_d],
          writes=[b2b])

    def load_w(e):
        for h in range(4):
            p.dma('pool', w1b[:, h * 4:(h + 1) * 4, :], w1_d[e, h * 512:(h + 1) * 512, :].rearrange("(k p) n -> p k n", p=128),
                  reads=[w1_d], writes=[w1b])
        for h in range(2):
            p.dma('pool', w2b[:, h * 4:(h + 1) * 4, :], w2_d[e, h * 512:(h + 1) * 512, :].rearrange("(k p) n -> p k n", p=128),
                  reads=[w2_d], writes=[w2b])
    load_w(0)

    NF = NT * EPC
    phR = Phase(p)
    maskf = phR.sb("maskf", [128, NF], F32)
    maskb = phR.sb("maskb", [128, NF], BF16)
    gflat = gts[:].rearrange("p t e -> p (t e)")
    p.op('dve', lambda: nc.vector.tensor_scalar(out=maskf[:], in0=gflat, scalar1=0.0, scalar2=None, op0=ALU.is_gt),
         reads=[gts], writes=[maskf])
    p.op('dve', lambda: nc.vector.tensor_copy(out=maskb[:], in_=maskf[:]), reads=[maskf], writes=[maskb])
    accW, accT = p.accs[0], p.accs[1]
    p.op('pe', lambda: nc.tensor.matmul(accW[:, 0:NF], lhsT=tri[:], rhs=maskb[:], start=True, stop=True),
         reads=[tri, maskb], writes=[accW])
    p.op('pe', lambda: nc.tensor.matmul(accT[:, 0:NF], lhsT=ones[:], rhs=maskb[:], start=True, stop=True),
         reads=[ones, maskb], writes=[accT])
    sa = phR.sb("scan_a", [128, NF], F32)
    sbb = phR.sb("scan_b", [128, NF], F32)
    tot = phR.sb("tot", [128, NF], F32)
    rank = phR.sb("rank", [128, NF], F32)
    p.op('dve', lambda: nc.vector.tensor_copy(out=tot[:], in_=accT[:, 0:NF]), reads=[accT], writes=[tot])
    p.op('act', lambda: nc.scalar.copy(out=sa[:], in_=accT[:, 0:NF]), reads=[accT], writes=[sa])
    cur, nxt = sa, sbb
    s = 1
    while s < NT:
        w = s * EPC
        p.op('dve', lambda: nc.vector.tensor_copy(out=nxt[:, 0:w], in_=cur[:, 0:w]), reads=[cur], writes=[nxt])
        p.op('dve', lambda: nc.vector.tensor_tensor(out=nxt[:, w:NF], in0=cur[:, w:NF], in1=cur[:, 0:NF - w], op=ALU.add),
             reads=[cur], writes=[nxt])
        cur, nxt = nxt, cur
        s *= 2
    incl = cur
    p.op('dve', lambda: nc.vector.tensor_tensor(out=rank[:], in0=accW[:, 0:NF], in1=incl[:], op=ALU.add),
         reads=[accW, incl], writes=[rank])
    p.op('dve', lambda: nc.vector.tensor_tensor(out=rank[:], in0=rank[:], in1=tot[:], op=ALU.subtract),
         reads=[rank, tot], writes=[rank])
    p.dma('sp', cnt_o[:, :], incl[0:1, NF - EPC:NF], reads=[incl], writes=[cnt_o])
    p.op('dve', lambda: nc.vector.tensor_scalar(out=maskf[:], in0=maskf[:], scalar1=-BIG, scalar2=BIG, op0=ALU.mult,
                                                op1=ALU.add), reads=[maskf], writes=[maskf])
    p.op('dve', lambda: nc.vector.tensor_tensor(out=rank[:], in0=rank[:], in1=maskf[:], op=ALU.add),
         reads=[rank, maskf], writes=[rank])
    p.op('dve', lambda: nc.vector.tensor_copy(out=rank_si[:], in_=rank[:]), reads=[rank], writes=[rank_si])
    for e in range(EPC):
        rv = rank[:].rearrange("p (t e) -> p t e", e=EPC)[:, :, e]
        p.op('dve', lambda: nc.vector.tensor_scalar(out=rv, in0=rv, scalar1=float(CAPRS[e] - 1), scalar2=None,
                                                    op0=ALU.min), reads=[rank], writes=[rank])
    p.op('dve', lambda: nc.vector.tensor_copy(out=rank_gi[:], in_=rank[:]), reads=[rank], writes=[rank_gi])

    p.wait_all('sp', [cnt_o])
    phR.close()
    phD = Phase(p)
    zt = phD.sb("zt", [128, D], BF16)
    p.op('dve', lambda: nc.vector.memset(zt[:], 0.0), writes=[zt])
    for e in range(EPC):
        CAPR = CAPRS[e]
        for r in range(0, CAPR, 128 * 8):
            n8 = min(8, (CAPR - r) // 128)
            p.dma('sp', Xg[e][r:r + n8 * 128, :].rearrange("(a p) d -> p a d", p=128),
                  zt[:].unsqueeze(1).broadcast_to([128, n8, D]), reads=[zt], writes=[Xg[e]])
        Xg[e].wbase = Xg[e].w
    tts = phD.scratch("tt", [128, D], BF16, 4)
    for t in range(NT):
        tt = tts.get()
        p.dma('sp', tt[:], t_d[t * 128:(t + 1) * 128, :], reads=[t_d], writes=[tt])
        for e in range(EPC):
            col = t * EPC + e
            p_idma(p, Xg[e][:, :], bass.IndirectOffsetOnAxis(ap=rank_si[:, col:col + 1], axis=0), tt[:, :], None,
                   CAPRS[e] - 1, [tt, rank_si], Xg[e])

    phD.close()
    phE = Phase(p)
    xgs = phE.scratch("xgt", [128, D], BF16, 3)
    XTs = phE.scratch("XT", [128, KC, 512], BF16, 2)
    actTs = phE.scratch("actT", [128, 8, 512], BF16, 2)
    g32s = phE.scratch("g32", [128, 512], F32, 2)
    sgs = phE.scratch("sg", [128, 512], F32, 2)
    l32s = phE.scratch("l32", [128, 512], F32, 2)
    yos = phE.scratch("yo", [128, D], BF16, 2)
    for e in range(EPC):
        if e > 0:
            load_w(e)
        for ch in range((caps[e] + 3) // 4):
            XT, actT = XTs.get(), actTs.get()
            nb = min(4, caps[e] - 4 * ch)
            N = nb * 128
            for ti in range(nb):
                r0 = (ch * 4 + ti) * 128
                xg = xgs.get()
                p.dma('sp', xg[:], Xg[e][r0:r0 + 128, :], reads=[Xg[e]], writes=[xg])
                for k8 in range(2):
                    tp = p.tp()

                    def tr():
                        for j in range(8):
                            k = k8 * 8 + j
                            i = nc.tensor.transpose(out=tp[:, j * 128:(j + 1) * 128], in_=xg[:, k * 128:(k + 1) * 128],
                                                    identity=p.ident[:])
                        return i
                    p.op('pe', tr, reads=[xg, p.ident], writes=[tp])
                    dst = XT[:, k8 * 8:(k8 + 1) * 8, ti * 128:(ti + 1) * 128]
                    src = tp[:].rearrange("p (k t) -> p k t", t=128)
                    if k8 == 0:
                        p.op('act', lambda: nc.scalar.copy(out=dst, in_=src), reads=[tp], writes=[XT])
                    else:
                        p.op('dve', lambda: nc.vector.tensor_copy(out=dst, in_=src), reads=[tp], writes=[XT])
            for jj in range(8):
                accG, accL = p.acc(), p.acc()
                for acc, off in ((accG, 0), (accL, 1024)):
                    def mm():
                        for k in range(KC):
                            i = nc.tensor.matmul(acc[:, 0:N], lhsT=w1b[:, k, off + jj * 128:off + (jj + 1) * 128],
                                                 rhs=XT[:, k, 0:N], start=(k == 0), stop=(k == KC - 1))
                        return i
                    p.op('pe', mm, reads=[w1b, XT], writes=[acc])
                g32, sg, l32 = g32s.get(), sgs.get(), l32s.get()
                p.op('dve', lambda: nc.vector.tensor_scalar(out=g32[:, 0:N], in0=accG[:, 0:N], scalar1=b1c[:, e, jj:jj + 1],
                                                            scalar2=LIMIT, op0=ALU.add, op1=ALU.min),
                     reads=[accG, b1c], writes=[g32])
                p.op('act', lambda: nc.scalar.activation(out=sg[:, 0:N], in_=g32[:, 0:N], func=AF.Sigmoid, scale=ALPHA),
                     reads=[g32], writes=[sg])
                p.op('dve', lambda: nc.vector.tensor_scalar(out=l32[:, 0:N], in0=accL[:, 0:N], scalar1=b1c[:, e, 8 + jj:8 + jj + 1],
                                                            scalar2=LIMIT, op0=ALU.add, op1=ALU.min),
                     reads=[accL, b1c], writes=[l32])
                p.op('dve', lambda: nc.vector.tensor_scalar(out=l32[:, 0:N], in0=l32[:, 0:N], scalar1=-LIMIT, scalar2=1.0,
                                                            op0=ALU.max, op1=ALU.add), reads=[l32], writes=[l32])
                p.op('dve', lambda: nc.vector.tensor_tensor(out=g32[:, 0:N], in0=g32[:, 0:N], in1=sg[:, 0:N], op=ALU.mult),
                     reads=[g32, sg], writes=[g32])
                p.op('dve', lambda: nc.vector.tensor_tensor(out=actT[:, jj, 0:N], in0=g32[:, 0:N], in1=l32[:, 0:N], op=ALU.mult),
                     reads=[g32, l32], writes=[actT])
            for ti in range(nb):
                r0 = (ch * 4 + ti) * 128
                yo = yos.get()
                for c in range(4):
                    acc = p.acc()

                    def mm2():
                        nc.tensor.matmul(acc[:], lhsT=ones1[0:1, :], rhs=b2b[0:1, e, c * 512:(c + 1) * 512], start=True,
                                         stop=False)
                        for j in range(8):
                            i = nc.tensor.matmul(acc[:], lhsT=actT[:, j, ti * 128:(ti + 1) * 128],
                                                 rhs=w2b[:, j, c * 512:(c + 1) * 512], start=False, stop=(j == 7))
                        return i
                    p.op('pe', mm2, reads=[actT, w2b, b2b, ones1], writes=[acc])
                    p.op('act', lambda: nc.scalar.copy(out=yo[:, c * 512:(c + 1) * 512], in_=acc[:]), reads=[acc],
                         writes=[yo])
                p.dma('sp', Yg[e][r0:r0 + 128, :], yo[:], reads=[yo], writes=[Yg[e]])

    phE.close()
    phW.close()
    phC = Phase(p)
    ygs = phC.scratch("yg", [128, D], BF16, 8)
    caccs = phC.scratch("cacc", [128, D], F32, 2)
    obs = phC.scratch("ob", [128, D], BF16, 2)
    for t in range(NT):
        eng = 'dve'
        ve = nc.vector if eng == 'dve' else nc.gpsimd
        ca, ob = caccs.get(), obs.get()
        for e in range(EPC):
            col = t * EPC + e
            yg = ygs.get()
            p_idma(p, yg[:, :], None, Yg[e][:, :], bass.IndirectOffsetOnAxis(ap=rank_gi[:, col:col + 1], axis=0),
                   CAPRS[e] - 1, [Yg[e], rank_gi], yg, concurrent=False)
            gcol = gts[:, t, e:e + 1]
            if e == 0:
                p.op(eng, lambda: ve.tensor_scalar(out=ca[:], in0=yg[:], scalar1=gcol, scalar2=None, op0=ALU.mult),
                     reads=[yg, gts], writes=[ca])
            elif e < EPC - 1:
                p.op(eng, lambda: ve.scalar_tensor_tensor(out=ca[:], in0=yg[:], scalar=gcol, in1=ca[:], op0=ALU.mult,
                                                          op1=ALU.add), reads=[yg, gts, ca], writes=[ca])
            else:
                p.op(eng, lambda: ve.scalar_tensor_tensor(out=ob[:], in0=yg[:], scalar=gcol, in1=ca[:], op0=ALU.mult,
                                                          op1=ALU.add), reads=[yg, gts, ca], writes=[ob])
        p.dma('sp', yp_o[t * 128:(t + 1) * 128, :], ob[:], reads=[ob], writes=[yp_o])
    p.wait_all('sp', [yp_o, cnt_o])
    phC.close()
    return nc


NEXP = 32


def build_p5():
    NT = NTOK // 128
    nc, p = new_prog()
    g_d = p.dram("g", [128, NT, NEXP], F32, "ExternalInput")
    out_o = p.dram("info", [1, NEXP + EPC], F32, "ExternalOutput")
    p.accs = [p.ps(f"acc{i}", [128, 512], F32) for i in range(4)]
    ones = p.sb("ones", [128, 128], BF16)
    p.op('pool', lambda: nc.gpsimd.memset(ones[:], 1.0), writes=[ones])
    g = p.sb("gs", [128, NT, NEXP], F32)
    mk = p.sb("mk", [128, NT * NEXP], BF16)
    tot = p.sb("tot", [128, NT * NEXP], F32)
    p.dma('sp', g[:], g_d[:], reads=[g_d], writes=[g])
    p.op('dve', lambda: nc.vector.tensor_scalar(out=mk[:], in0=g[:].rearrange("p t e -> p (t e)"), scalar1=0.0,
                                                scalar2=None, op0=ALU.is_gt), reads=[g], writes=[mk])
    for c in range(NT * NEXP // 512):
        acc = p.acc()
        p.op('pe', lambda: nc.tensor.matmul(acc[:], lhsT=ones[:], rhs=mk[:, c * 512:(c + 1) * 512], start=True, stop=True),
             reads=[ones, mk], writes=[acc])
        p.op('act', lambda: nc.scalar.copy(out=tot[:, c * 512:(c + 1) * 512], in_=acc[:]), reads=[acc], writes=[tot])
    cnt = p.sb("cnt", [128, NEXP], F32)
    p.op('dve', lambda: nc.vector.tensor_reduce(out=cnt[:], in_=tot[:].rearrange("p (t e) -> p e t", e=NEXP), axis=AX.X,
                                                op=ALU.add), reads=[tot], writes=[cnt])
    io = p.sb("io", [128, NEXP], F32)
    p.op('pool', lambda: nc.gpsimd.iota(io[:], pattern=[[1, NEXP]], base=0, channel_multiplier=0,
                                        allow_small_or_imprecise_dtypes=True), writes=[io])
    cnt2 = p.sb("cnt2", [128, NEXP], F32)
    p.op('dve', lambda: nc.vector.scalar_tensor_tensor(out=cnt2[:], in0=io[:], scalar=1.0 / 64, in1=cnt[:], op0=ALU.mult,
                                                       op1=ALU.add), reads=[io, cnt], writes=[cnt2])
    cmp = p.sb("cmp", [128, NEXP, NEXP], F32)
    p.op('dve', lambda: nc.vector.tensor_tensor(out=cmp[:], in0=cnt2[:].unsqueeze(1).broadcast_to([128, NEXP, NEXP]),
                                                in1=cnt2[:].unsqueeze(2).broadcast_to([128, NEXP, NEXP]), op=ALU.is_gt),
         reads=[cnt2], writes=[cmp])
    res = p.sb("res", [128, NEXP + EPC], F32)
    p.op('dve', lambda: nc.vector.tensor_reduce(out=res[:, 0:NEXP], in_=cmp[:], axis=AX.X, op=ALU.add), reads=[cmp],
         writes=[res])
    sel = p.sb("sel", [128, NEXP], F32)
    sel2 = p.sb("sel2", [128, NEXP], F32)
    for j in range(EPC):
        p.op('dve', lambda: nc.vector.tensor_scalar(out=sel[:], in0=res[:, 0:NEXP], scalar1=float(8 * j) - 0.5,
                                                    scalar2=None, op0=ALU.is_gt), reads=[res], writes=[sel])
        p.op('dve', lambda: nc.vector.tensor_scalar(out=sel2[:], in0=res[:, 0:NEXP], scalar1=float(8 * j + 8) - 0.5,
                                                    scalar2=None, op0=ALU.is_lt), reads=[res], writes=[sel2])
        p.op('dve', lambda: nc.vector.tensor_tensor(out=sel[:], in0=sel[:], in1=sel2[:], op=ALU.mult), reads=[sel, sel2],
             writes=[sel])
        p.op('dve', lambda: nc.vector.tensor_tensor(out=sel[:], in0=sel[:], in1=cnt[:], op=ALU.mult), reads=[sel, cnt],
             writes=[sel])
        p.op('dve', lambda: nc.vector.reduce_max(out=res[:, NEXP + j:NEXP + j + 1], in_=sel[:], axis=AX.X), reads=[sel],
             writes=[res])
    p.dma('sp', out_o[:, :], res[0:1, :], reads=[res], writes=[out_o])
    p.wait_all('sp', [out_o])
    return nc
```

```python
import numpy as np
import ml_dtypes
import concourse.bass as bass
import concourse.mybir as mybir
from concourse.bass_utils import run_bass_kernel_spmd

F32 = mybir.dt.float32
BF16 = mybir.dt.bfloat16
AF = mybir.ActivationFunctionType
ALU = mybir.AluOpType
AX = mybir.AxisListType
NPBF = ml_dtypes.bfloat16

D = 2048
KC = 16
EPS = 1e-6
NCORES = 8


class Buf:
    def __init__(self, t, name):
        self.t = t
        self.name = name
        self.w = None
        self.r = []
        self.dsem = None
        self.dcnt = 0

    def __getitem__(self, idx):
        return self.t[idx]


class P:
    def __init__(self, nc):
        self.nc = nc
        self.eng = {'pe': nc.tensor, 'act': nc.scalar, 'dve': nc.vector, 'pool': nc.gpsimd, 'sp': nc.sync}
        self.sems = {}
        self.cnt = {}
        for k in self.eng:
            self.sems[k] = nc.alloc_semaphore('s_' + k)
            self.cnt[k] = 0
        self.seen = {k: {} for k in self.eng}
        self.acc_i = 0
        self.tp_i = 0
        self.dma_cnt = {}

    def sb(self, name, shape, dt):
        return Buf(self.nc.alloc_sbuf_tensor(name, list(shape), dt), name)

    def ps(self, name, shape, dt=F32):
        return Buf(self.nc.alloc_psum_tensor(name, list(shape), dt), name)

    def dram(self, name, shape, dt, kind="Internal"):
        b = Buf(self.nc.dram_tensor(name, list(shape), dt, kind=kind), name)
        b.is_dram = True
        return b

    def _wait(self, e, ev):
        key, val = ev
        if key == 'pe' and e == 'pe':
            return
        if self.seen[e].get(key, 0) >= val:
            return
        self.seen[e][key] = val
        self.eng[e].wait_ge(self.sems[key], val)

    def _deps(self, e, reads, writes):
        for b in reads:
            if b.w is not None:
                self._wait(e, b.w)
        for b in writes:
            if b.w is not None:
                self._wait(e, b.w)
            for ev in b.r:
                self._wait(e, ev)

    def _commit(self, ev, reads, writes):
        for b in reads:
            b.r.append(ev)
            if len(b.r) > 48:
                best = {}
                for k, v in b.r:
                    best[k] = max(best.get(k, 0), v)
                b.r = list(best.items())
        for b in writes:
            b.w = ev
            b.r = []

    def op(self, e, fn, reads=(), writes=()):
        self._deps(e, reads, writes)
        inst = fn()
        self.cnt[e] += 1
        inst.then_inc(self.sems[e], 1)
        self._commit((e, self.cnt[e]), reads, writes)
        return inst

    def dma(self, q, out_ap, in_ap, reads=(), writes=(), **kw):
        dst = writes[0]
        if q == 'sp' and getattr(dst, 'is_dram', False):
            q = 'act'
        self._deps(q, reads, writes)
        if dst.dsem is None:
            key = 'd_' + dst.name
            self.sems[key] = self.nc.alloc_semaphore(key)
            dst.dsem = key
        inst = self.eng[q].dma_start(out=out_ap, in_=in_ap, **kw)
        dst.dcnt += 16
        self.dma_cnt[dst.dsem] = dst.dcnt
        inst.then_inc(self.sems[dst.dsem], 16)
        self._commit((dst.dsem, dst.dcnt), reads, writes)
        return inst

    def wait_all(self, e, bufs):
        for b in bufs:
            if b.w is not None:
                self._wait(e, b.w)

    def setup_common(self):
        nc = self.nc
        self.accs = [self.ps(f"acc{i}", [128, 512], F32) for i in range(5)]
        self.tps = [self.ps(f"tpb{i}", [128, 1024], BF16) for i in range(3)]
        identf = self.sb("identf", [128, 128], F32)
        ident = self.sb("ident", [128, 128], BF16)
        self.op('pool', lambda: nc.gpsimd.memset(identf[:], 0.0), writes=[identf])
        self.op('pool', lambda: nc.gpsimd.affine_select(out=identf[:], in_=identf[:], pattern=[[-1, 128]],
                                                        compare_op=ALU.not_equal, fill=1.0, base=0,
                                                        channel_multiplier=1), reads=[identf], writes=[identf])
        self.op('dve', lambda: nc.vector.tensor_copy(out=ident[:], in_=identf[:]), reads=[identf], writes=[ident])
        self.ident = ident
        self.identf = identf

    def acc(self):
        b = self.accs[self.acc_i % len(self.accs)]
        self.acc_i += 1
        return b

    def tp(self):
        b = self.tps[self.tp_i % len(self.tps)]
        self.tp_i += 1
        return b


def new_prog():
    nc = bass.Bass("TRN2", target_bir_lowering=False)
    return nc, P(nc)


class Scratch:
    def __init__(self, p, name, shape, dt, n=2):
        self.bufs = [p.sb(f"{name}{i}", shape, dt) for i in range(n)]
        self.i = 0

    def get(self):
        b = self.bufs[self.i % len(self.bufs)]
        self.i += 1
        return b


def emit_rstd(p, ss, rows, scale_in, tmp=None):
    nc = p.nc
    p.op('dve', lambda: nc.vector.tensor_scalar(out=ss, in0=ss, scalar1=float(scale_in), scalar2=EPS,
                                                op0=ALU.mult, op1=ALU.add), reads=[tmp], writes=[tmp])
    p.op('act', lambda: nc.scalar.sqrt(out=ss, in_=ss), reads=[tmp], writes=[tmp])
    p.op('dve', lambda: nc.vector.reciprocal(out=ss, in_=ss), reads=[tmp], writes=[tmp])


class NormT:
    def __init__(self, p):
        self.p = p
        self.sq = Scratch(p, "nt_sq", [128, D], BF16, 2)
        self.ss = Scratch(p, "nt_ss", [128, 1], F32, 2)
        self.xn = Scratch(p, "nt_xn", [128, D], BF16, 2)

    def emit(self, xt, rows, Acol, Bcol, hT, c0):
        p, nc = self.p, self.p.nc
        sq, ss, xn = self.sq.get(), self.ss.get(), self.xn.get()
        p.op('act', lambda: nc.scalar.activation(out=sq[0:rows, :], in_=xt[0:rows, :], func=AF.Square,
                                                 accum_out=ss[0:rows, :]), reads=[xt], writes=[sq, ss])
        emit_rstd(p, ss[0:rows, :], rows, 1.0 / D, ss)
        p.op('dve', lambda: nc.vector.tensor_scalar(out=xn[0:rows, :], in0=xt[0:rows, :], scalar1=ss[0:rows, 0:1],
                                                    scalar2=None, op0=ALU.mult), reads=[xt, ss], writes=[xn])
        for k4 in range(2):
            tp = p.tp()

            def tr():
                for j in range(8):
                    k = k4 * 8 + j
                    i = nc.tensor.transpose(out=tp[:, j * 128:j * 128 + rows], in_=xn[0:rows, k * 128:(k + 1) * 128],
                                            identity=p.ident[0:rows, 0:rows])
                return i
            p.op('pe', tr, reads=[xn, p.ident], writes=[tp])
            for j in range(8):
                k = k4 * 8 + j
                if j % 2 == 0:
                    p.op('dve', lambda: nc.vector.tensor_scalar(
                        out=hT[:, k, c0:c0 + rows], in0=tp[:, j * 128:j * 128 + rows], scalar1=Acol[:, k:k + 1],
                        scalar2=Bcol[:, k:k + 1], op0=ALU.mult, op1=ALU.add), reads=[tp, Acol, Bcol], writes=[hT])
                else:
                    p.op('act', lambda: nc.scalar.activation(
                        out=hT[:, k, c0:c0 + rows], in_=tp[:, j * 128:j * 128 + rows], func=AF.Identity,
                        scale=Acol[:, k:k + 1], bias=Bcol[:, k:k + 1]), reads=[tp, Acol, Bcol], writes=[hT])


def emit_Acol(p, A, g, sc):
    nc = p.nc
    p.op('dve', lambda: nc.vector.scalar_tensor_tensor(out=A[:], in0=sc[:], scalar=1.0, in1=g[:], op0=ALU.add,
                                                       op1=ALU.mult), reads=[sc, g], writes=[A])


MODW = 12288 // NCORES


def build_p1():
    nc, p = new_prog()
    wmod = p.dram("wmod", [2, D, MODW], F32, "ExternalInput")
    bmod = p.dram("bmod", [2, 3, MODW], F32, "ExternalInput")
    cT = p.dram("cT", [128, KC, 3], F32, "ExternalInput")
    mout = p.dram("m", [2, 3, MODW], F32, "ExternalOutput")
    p.setup_common()
    cs = p.sb("cs", [128, KC, 3], F32)
    sT = p.sb("sT", [128, KC, 3], BF16)
    p.dma('sp', cs[:], cT[:], reads=[cT], writes=[cs])
    p.op('act', lambda: nc.scalar.activation(out=sT[:], in_=cs[:], func=AF.Silu), reads=[cs], writes=[sT])
    for l in range(2):
        wb = p.sb(f"wb{l}", [128, KC, MODW], BF16)
        bb = p.sb(f"bb{l}", [3, MODW], F32)
        mo = p.sb(f"mo{l}", [3, MODW], F32)
        for h in range(2):
            p.dma('pool', wb[:, h * 8:(h + 1) * 8, :],
                  wmod[l, h * 1024:(h + 1) * 1024, :].rearrange("(k p) n -> p k n", p=128), reads=[wmod], writes=[wb])
        p.dma('sp', bb[:], bmod[l], reads=[bmod], writes=[bb])
        for c in range(MODW // 512):
            acc = p.acc()

            def mm():
                for k in range(KC):
                    i = nc.tensor.matmul(acc[0:3, :], lhsT=sT[:, k, :], rhs=wb[:, k, c * 512:(c + 1) * 512],
                                         start=(k == 0), stop=(k == KC - 1))
                return i
            p.op('pe', mm, reads=[sT, wb], writes=[acc])
            p.op('dve', lambda: nc.vector.tensor_tensor(out=mo[:, c * 512:(c + 1) * 512], in0=acc[0:3, :],
                                                        in1=bb[:, c * 512:(c + 1) * 512], op=ALU.add),
                 reads=[acc, bb], writes=[mo])
        p.dma('sp', mout[l], mo[:], reads=[mo], writes=[mout])
    p.wait_all('sp', [mout])
    return nc


TPC = 1024
CPC = 64


def emit_qk_post(p, ps, rows, nh, gain_b, rope_t, out_ap, out_buf, S):
    nc = p.nc
    n = nh * 128
    sq, ss4, qn = S['sq'].get(), S['ss4'].get(), S['qn'].get()
    p.op('act', lambda: nc.scalar.activation(out=sq[0:rows, 0:n], in_=ps[0:rows, 0:n], func=AF.Square),
         reads=[ps], writes=[sq])
    p.op('dve', lambda: nc.vector.tensor_reduce(out=ss4[0:rows, 0:nh],
                                                in_=sq[0:rows, 0:n].rearrange("p (h d) -> p h d", d=128),
                                                axis=AX.X, op=ALU.add), reads=[sq], writes=[ss4])
    emit_rstd(p, ss4[0:rows, 0:nh], rows, 1.0 / 128, ss4)
    p.op('dve', lambda: nc.vector.tensor_tensor(
        out=qn[0:rows, 0:n].rearrange("p (h d) -> p h d", d=128),
        in0=ps[0:rows, 0:n].rearrange("p (h d) -> p h d", d=128),
        in1=ss4[0:rows, 0:nh].unsqueeze(2).broadcast_to([rows, nh, 128]), op=ALU.mult),
        reads=[ps, ss4], writes=[qn])
    if rope_t is None:
        p.op('dve', lambda: nc.vector.tensor_tensor(
            out=out_ap.rearrange("p (h d) -> p h d", d=128),
            in0=qn[0:rows, 0:n].rearrange("p (h d) -> p h d", d=128),
            in1=gain_b[0:rows, :].unsqueeze(1).broadcast_to([rows, nh, 128]), op=ALU.mult),
            reads=[qn, gain_b], writes=[out_buf])
        return
    p.op('pool', lambda: nc.gpsimd.tensor_tensor(
        out=qn[0:rows, 0:n].rearrange("p (h d) -> p h d", d=128),
        in0=qn[0:rows, 0:n].rearrange("p (h d) -> p h d", d=128),
        in1=gain_b[0:rows, :].unsqueeze(1).broadcast_to([rows, nh, 128]), op=ALU.mult),
        reads=[qn, gain_b], writes=[qn])
    t1, t2 = S['t1'].get(), S['t2'].get()
    p.op('pool', lambda: nc.gpsimd.tensor_tensor(
        out=t1[0:rows, 0:n].rearrange("p (h d) -> p h d", d=128),
        in0=qn[0:rows, 0:n].rearrange("p (h d) -> p h d", d=128),
        in1=rope_t[0:rows, 0, :].unsqueeze(1).broadcast_to([rows, nh, 128]), op=ALU.mult),
        reads=[qn, rope_t], writes=[t1])
    for b in range(2):
        p.op('dve', lambda: nc.vector.tensor_tensor(
            out=t2[0:rows, 0:n].rearrange("p (h a b d) -> p h a b d", a=2, b=2, d=32)[:, :, :, b, :],
            in0=qn[0:rows, 0:n].rearrange("p (h a b d) -> p h a b d", a=2, b=2, d=32)[:, :, :, 1 - b, :],
            in1=rope_t[0:rows, 1, :].rearrange("p (a b d) -> p a b d", a=2, b=2, d=32)[:, :, b, :]
            .unsqueeze(1).broadcast_to([rows, nh, 2, 32]), op=ALU.mult),
            reads=[qn, rope_t], writes=[t2])
    p.op('dve', lambda: nc.vector.tensor_tensor(out=out_ap, in0=t1[0:rows, 0:n], in1=t2[0:rows, 0:n], op=ALU.add),
         reads=[t1, t2], writes=[out_buf])


def build_p2():
    nc, p = new_prog()
    x = p.dram("x", [TPC, D], F32, "ExternalInput")
    ctxr = p.dram("ctxr", [CPC, D], F32, "ExternalInput")
    cols = p.dram("cols", [128, 5, KC], F32, "ExternalInput")
    w_in = p.dram("w_in", [D, 2560], F32, "ExternalInput")
    gains = p.dram("gains", [2, 128], F32, "ExternalInput")
    rope = p.dram("rope", [TPC, 2, 128], F32, "ExternalInput")
    out = p.dram("qkvf", [TPC, 2560], BF16, "ExternalOutput")
    outc = p.dram("kvc", [CPC, 512], BF16, "ExternalOutput")
    p.setup_common()
    colsb = p.sb("colsb", [128, 5, KC], F32)
    p.dma('sp', colsb[:], cols[:], reads=[cols], writes=[colsb])
    qg = p.sb("qg", [128, 128], F32)
    kg = p.sb("kg", [128, 128], F32)
    p.dma('sp', qg[:], gains[0:1, :].partition_broadcast(128), reads=[gains], writes=[qg])
    p.dma('sp', kg[:], gains[1:2, :].partition_broadcast(128), reads=[gains], writes=[kg])
    A1 = p.sb("A1", [128, KC], F32)
    Ac = p.sb("Ac", [128, KC], F32)
    B1 = p.sb("B1", [128, KC], F32)
    Bc = p.sb("Bc", [128, KC], F32)
    p.op('dve', lambda: nc.vector.scalar_tensor_tensor(out=A1[:], in0=colsb[:, 2, :], scalar=1.0, in1=colsb[:, 0, :],
                                                       op0=ALU.add, op1=ALU.mult), reads=[colsb], writes=[A1])
    p.op('dve', lambda: nc.vector.scalar_tensor_tensor(out=Ac[:], in0=colsb[:, 4, :], scalar=1.0, in1=colsb[:, 0, :],
                                                       op0=ALU.add, op1=ALU.mult), reads=[colsb], writes=[Ac])
    p.op('dve', lambda: nc.vector.tensor_copy(out=B1[:], in_=colsb[:, 1, :]), reads=[colsb], writes=[B1])
    p.op('dve', lambda: nc.vector.tensor_copy(out=Bc[:], in_=colsb[:, 3, :]), reads=[colsb], writes=[Bc])
    wb = p.sb("wb", [128, KC, 2560], BF16)
    for h in range(4):
        p.dma('pool', wb[:, h * 4:(h + 1) * 4, :], w_in[h * 512:(h + 1) * 512, :].rearrange("(k p) n -> p k n", p=128),
              reads=[w_in], writes=[wb])
    nt = NormT(p)
    xs = Scratch(p, "xs", [128, D], F32, 2)
    hTs = Scratch(p, "hT", [128, KC, 128], BF16, 2)
    rts = Scratch(p, "rt", [128, 2, 128], F32, 2)
    obs = Scratch(p, "ob", [128, 2560], BF16, 2)
    S = {'sq': Scratch(p, "qsq", [128, 512], F32, 2), 'ss4': Scratch(p, "qss", [128, 4], F32, 2),
         'qn': Scratch(p, "qn", [128, 512], F32, 2), 't1': Scratch(p, "qt1", [128, 512], F32, 2),
         't2': Scratch(p, "qt2", [128, 512], F32, 2)}
    ntiles = TPC // 128
    for t in range(ntiles + 1):
        is_ctx = (t == ntiles)
        rows = CPC if is_ctx else 128
        xt, hT, ob = xs.get(), hTs.get(), obs.get()
        if is_ctx:
            p.dma('sp', xt[0:rows, :], ctxr[:, :], reads=[ctxr], writes=[xt])
            rt = None
        else:
            p.dma('sp', xt[:], x[t * 128:(t + 1) * 128, :], reads=[x], writes=[xt])
            rt = rts.get()
            p.dma('sp', rt[:], rope[t * 128:(t + 1) * 128, :, :], reads=[rope], writes=[rt])
        nt.emit(xt, rows, Ac if is_ctx else A1, Bc if is_ctx else B1, hT, 0)
        for c in ([2] if is_ctx else range(5)):
            acc = p.acc()

            def mm():
                for k in range(KC):
                    i = nc.tensor.matmul(acc[0:rows, :], lhsT=hT[:, k, 0:rows], rhs=wb[:, k, c * 512:(c + 1) * 512],
                                         start=(k == 0), stop=(k == KC - 1))
                return i
            p.op('pe', mm, reads=[hT, wb], writes=[acc])
            if c < 2:
                emit_qk_post(p, acc, rows, 4, qg, rt, ob[0:rows, c * 512:(c + 1) * 512], ob, S)
            elif c == 2:
                emit_qk_post(p, acc, rows, 2, kg, rt, ob[0:rows, 1024:1280], ob, S)
                p.op('act', lambda: nc.scalar.copy(out=ob[0:rows, 1280:1536], in_=acc[0:rows, 256:512]),
                     reads=[acc], writes=[ob])
            else:
                p.op('act', lambda: nc.scalar.copy(out=ob[0:rows, c * 512:(c + 1) * 512], in_=acc[0:rows, :]),
                     reads=[acc], writes=[ob])
        if is_ctx:
            p.dma('sp', outc[:, :], ob[0:rows, 1024:1536], reads=[ob], writes=[outc])
        else:
            p.dma('sp', out[t * 128:(t + 1) * 128, :], ob[:], reads=[ob], writes=[out])
    p.wait_all('sp', [out, outc])
    return nc


def p_barrier(p):
    targets = {}
    for k in p.eng:
        targets[k] = p.cnt[k]
    for k, v in p.sems.items():
        if k.startswith('d_'):
            targets[k] = p.dma_cnt.get(k, 0)
    for e in p.eng:
        for k, v in targets.items():
            if v > 0 and k != e:
                p._wait(e, (k, v))


class Phase:
    def __init__(self, p):
        from contextlib import ExitStack
        self.p = p
        self.st = ExitStack()

    def sb(self, name, shape, dt):
        t = self.st.enter_context(self.p.nc.sbuf_tensor(name, list(shape), dt))
        return Buf(t, name)

    def scratch(self, name, shape, dt, n=2):
        s = Scratch.__new__(Scratch)
        s.bufs = [self.sb(f"{name}{i}", shape, dt) for i in range(n)]
        s.i = 0
        return s

    def close(self):
        p_barrier(self.p)
        self.st.close()


class Prep:
    def __init__(self, p, ph, A2b, B2b, wr, brb, tpf):
        self.p = p
        self.A2b, self.B2b, self.wr, self.brb, self.tpf = A2b, B2b, wr, brb, tpf
        self.sq = ph.sb("pp_sq", [128, D], BF16)
        self.ss = ph.sb("pp_ss", [128, 1], F32)
        self.t32 = ph.sb("pp_t32", [128, D], F32)
        self.tb = ph.scratch("pp_tb", [128, D], BF16, 2)
        self.tT = ph.sb("pp_tT", [128, KC, 128], F32)
        self.lg = ph.scratch("pp_lg", [128, 32], F32, 2)
        self.t8 = ph.sb("pp_t8", [128, 8], F32)
        self.nmx = ph.sb("pp_nmx", [128, 1], F32)
        self.msk = ph.sb("pp_msk", [128, 32], F32)
        self.ex = ph.sb("pp_ex", [128, 32], F32)
        self.sm = ph.sb("pp_sm", [128, 1], F32)

    def emit(self, x1, t_out_ap, t_out, g_out_ap, g_out):
        p, nc = self.p, self.p.nc
        sq, ss, t32, tT = self.sq, self.ss, self.t32, self.tT
        tb, lg = self.tb.get(), self.lg.get()
        p.op('act', lambda: nc.scalar.activation(out=sq[:], in_=x1[:], func=AF.Square, accum_out=ss[:]),
             reads=[x1], writes=[sq, ss])
        emit_rstd(p, ss[:], 128, 1.0 / D, ss)
        p.op('dve', lambda: nc.vector.scalar_tensor_tensor(out=t32[:], in0=x1[:], scalar=ss[:, 0:1], in1=self.A2b[:],
                                                           op0=ALU.mult, op1=ALU.mult), reads=[x1, ss, self.A2b],
             writes=[t32])
        p.op('pool', lambda: nc.gpsimd.tensor_tensor(out=t32[:], in0=t32[:], in1=self.B2b[:], op=ALU.add),
             reads=[t32, self.B2b], writes=[t32])
        p.op('act', lambda: nc.scalar.copy(out=tb[:], in_=t32[:]), reads=[t32], writes=[tb])
        p.dma('sp', t_out_ap, tb[:], reads=[tb], writes=[t_out])
        for k4 in range(4):
            tpf = self.tpf

            def tr():
                for j in range(4):
                    k = k4 * 4 + j
                    i = nc.tensor.transpose(out=tpf[:, j * 128:(j + 1) * 128], in_=t32[:, k * 128:(k + 1) * 128],
                                            identity=p.identf[:])
                return i
            p.op('pe', tr, reads=[t32, p.identf], writes=[tpf])
            eng = 'dve' if k4 % 2 == 0 else 'act'
            if eng == 'dve':
                p.op('dve', lambda: nc.vector.tensor_copy(
                    out=tT[:, k4 * 4:(k4 + 1) * 4, :].rearrange("p k t -> p (k t)"), in_=tpf[:]),
                    reads=[tpf], writes=[tT])
            else:
                p.op('act', lambda: nc.scalar.copy(
                    out=tT[:, k4 * 4:(k4 + 1) * 4, :].rearrange("p k t -> p (k t)"), in_=tpf[:]),
                    reads=[tpf], writes=[tT])
        acc = p.acc()

        def mm():
            for k in range(KC):
                i = nc.tensor.matmul(acc[:, 0:32], lhsT=tT[:, k, :], rhs=self.wr[:, k, :], start=(k == 0),
                                     stop=(k == KC - 1))
            return i
        p.op('pe', mm, reads=[tT, self.wr], writes=[acc])
        p.op('dve', lambda: nc.vector.tensor_tensor(out=lg[:], in0=acc[:, 0:32], in1=self.brb[:], op=ALU.add),
             reads=[acc, self.brb], writes=[lg])
        emit_gates(p, lg, self.t8, self.nmx, self.msk, self.ex, self.sm)
        p.dma('sp', g_out_ap, lg[:], reads=[lg], writes=[g_out])


def emit_gates(p, lg, t8, nmx, msk, ex, sm):
    nc = p.nc
    p.op('dve', lambda: nc.vector.max(out=t8[:], in_=lg[:]), reads=[lg], writes=[t8])
    p.op('dve', lambda: nc.vector.tensor_scalar(out=msk[:], in0=lg[:], scalar1=t8[:, 3:4], scalar2=None,
                                                op0=ALU.is_ge), reads=[lg, t8], writes=[msk])
    p.op('dve', lambda: nc.vector.tensor_scalar(out=nmx[:], in0=t8[:, 0:1], scalar1=-1.0, scalar2=None,
                                                op0=ALU.mult), reads=[t8], writes=[nmx])
    p.op('act', lambda: nc.scalar.activation(out=ex[:], in_=lg[:], func=AF.Exp, bias=nmx[:, 0:1], scale=1.0),
         reads=[lg, nmx], writes=[ex])
    p.op('dve', lambda: nc.vector.tensor_tensor(out=ex[:], in0=ex[:], in1=msk[:], op=ALU.mult),
         reads=[ex, msk], writes=[ex])
    p.op('dve', lambda: nc.vector.reduce_sum(out=sm[:], in_=ex[:], axis=AX.X), reads=[ex], writes=[sm])
    p.op('dve', lambda: nc.vector.reciprocal(out=sm[:], in_=sm[:]), reads=[sm], writes=[sm])
    p.op('dve', lambda: nc.vector.tensor_scalar(out=lg[:], in0=ex[:], scalar1=sm[:, 0:1], scalar2=None,
                                                op0=ALU.mult), reads=[ex, sm], writes=[lg])


def load_rows_b(p, ph, rows_d, names, off=0):
    out = {}
    for i, nm in enumerate(names):
        b = ph.sb("rb_" + nm, [128, D], F32)
        p.dma('sp', b[:], rows_d[off + i:off + i + 1, :].partition_broadcast(128), reads=[rows_d], writes=[b])
        out[nm] = b
    return out


NKEY = 4096 + 256
NKT = NKEY // 128


def build_p3():
    nc, p = new_prog()
    x = p.dram("x", [TPC, D], F32, "ExternalInput")
    qT_d = p.dram("qT", [128, 8, TPC], BF16, "ExternalInput")
    kT_d = p.dram("kT", [128, 2, NKEY], BF16, "ExternalInput")
    v_d = p.dram("v", [NKEY, 256], BF16, "ExternalInput")
    f_d = p.dram("f", [4096, 1024], BF16, "ExternalInput")
    dc_d = p.dram("dftc", [4, 128, 32, 256], BF16, "ExternalInput")
    ds_d = p.dram("dfts", [4, 128, 32, 256], BF16, "ExternalInput")
    cdft_d = p.dram("cdft", [128, 2, 128], BF16, "ExternalInput")
    wo_d = p.dram("w_out", [D, D], F32, "ExternalInput")
    rows_d = p.dram("rows", [4, D], F32, "ExternalInput")
    wr_d = p.dram("w_router", [128, KC, 32], F32, "ExternalInput")
    br_d = p.dram("b_router", [1, 32], F32, "ExternalInput")
    x1_o = p.dram("x1", [TPC, D], F32, "ExternalOutput")
    t_o = p.dram("t", [TPC, D], BF16, "ExternalOutput")
    g_o = p.dram("gates", [TPC, 32], F32, "ExternalOutput")

    p.accs = [p.ps(f"acc{i}", [128, 512], F32) for i in range(7)]
    tpf = p.ps("tpf", [128, 512], F32)
    identf = p.sb("identf", [128, 128], F32)
    ident = p.sb("ident", [128, 128], BF16)
    ones = p.sb("ones", [128, 128], BF16)
    p.op('pool', lambda: nc.gpsimd.memset(identf[:], 0.0), writes=[identf])
    p.op('pool', lambda: nc.gpsimd.affine_select(out=identf[:], in_=identf[:], pattern=[[-1, 128]],
                                                 compare_op=ALU.not_equal, fill=1.0, base=0, channel_multiplier=1),
         reads=[identf], writes=[identf])
    p.op('dve', lambda: nc.vector.tensor_copy(out=ident[:], in_=identf[:]), reads=[identf], writes=[ident])
    p.op('pool', lambda: nc.gpsimd.memset(ones[:], 1.0), writes=[ones])
    p.ident, p.identf = ident, identf
    YT = p.sb("YT", [128, 8, TPC], BF16)
    OT = p.sb("OT", [128, 8, TPC], BF16)
    cd = p.sb("cd", [128, 2, 128], BF16)
    p.dma('sp', cd[:], cdft_d[:], reads=[cdft_d], writes=[cd])

    ph = Phase(p)
    U = ph.sb("U", [128, 32, 1024], BF16)
    for h in range(4):
        p.dma('sp', U[:, h * 8:(h + 1) * 8, :], f_d[h * 1024:(h + 1) * 1024, :].rearrange("(t p) n -> p t n", p=128),
              reads=[f_d], writes=[U])
    dcs = ph.scratch("dcs", [128, 32, 256], BF16, 2)
    dss = ph.scratch("dss", [128, 32, 256], BF16, 2)
    Zc = ph.sb("Zc", [128, 8, TPC], BF16)
    Zs = ph.sb("Zs", [128, 8, TPC], BF16)
    for lc in range(4):
        dc, ds = dcs.get(), dss.get()
        p.dma('sp', dc[:], dc_d[lc], reads=[dc_d], writes=[dc])
        p.dma('sp', ds[:], ds_d[lc], reads=[ds_d], writes=[ds])
        for g in range(8):
            for which, (dd, Z) in enumerate(((dc, Zc), (ds, Zs))):
                acc = p.acc()

                def mm():
                    for lt in range(32):
                        i = nc.tensor.matmul(acc[:, 0:256], lhsT=U[:, lt, g * 128:(g + 1) * 128], rhs=dd[:, lt, :],
                                             start=(lt == 0), stop=(lt == 31))
                    return i
                p.op('pe', mm, reads=[U, dd], writes=[acc])
                if which == 0:
                    p.op('act', lambda: nc.scalar.copy(out=Z[:, g, lc * 256:(lc + 1) * 256], in_=acc[:, 0:256]),
                         reads=[acc], writes=[Z])
                else:
                    p.op('dve', lambda: nc.vector.tensor_copy(out=Z[:, g, lc * 256:(lc + 1) * 256], in_=acc[:, 0:256]),
                         reads=[acc], writes=[Z])
    for g in range(8):
        for c in range(2):
            acc = p.acc()

            def mm():
                nc.tensor.matmul(acc[:], lhsT=cd[:, 0, :], rhs=Zc[:, g, c * 512:(c + 1) * 512], start=True, stop=False)
                return nc.tensor.matmul(acc[:], lhsT=cd[:, 1, :], rhs=Zs[:, g, c * 512:(c + 1) * 512], start=False,
                                        stop=True)
            p.op('pe', mm, reads=[cd, Zc, Zs], writes=[acc])
            p.op('act' if c == 0 else 'dve',
                 (lambda: nc.scalar.copy(out=YT[:, g, c * 512:(c + 1) * 512], in_=acc[:])) if c == 0 else
                 (lambda: nc.vector.tensor_copy(out=YT[:, g, c * 512:(c + 1) * 512], in_=acc[:])),
                 reads=[acc], writes=[YT])
    ph.close()

    wo = p.sb("wo", [128, KC, D], BF16)
    for h in range(4):
        p.dma('pool', wo[:, h * 4:(h + 1) * 4, :], wo_d[h * 512:(h + 1) * 512, :].rearrange("(k p) n -> p k n", p=128),
              reads=[wo_d], writes=[wo])

    ph = Phase(p)
    kT = ph.sb("kTs", [128, 2, NKEY], BF16)
    vv = ph.sb("vv", [128, NKT, 256], BF16)
    qT = ph.sb("qTs", [128, 8, TPC], BF16)
    p.dma('sp', kT[:], kT_d[:], reads=[kT_d], writes=[kT])
    p.dma('sp', vv[:], v_d.t.ap().rearrange("(t p) n -> p t n", p=128), reads=[v_d], writes=[vv])
    p.dma('sp', qT[:], qT_d[:], reads=[qT_d], writes=[qT])
    PTs = ph.scratch("PT", [128, 512], BF16, 3)
    rDs = ph.scratch("rD", [128, 512], F32, 2)
    scale = float(128 ** -0.5)
    it = 0
    for h in range(8):
        kvh = h // 4
        for qc in range(2):
            accO = p.accs[0 + 2 * (it % 2)]
            accD = p.accs[1 + 2 * (it % 2)]
            it += 1
            def emit_s(kt):
                accS = p.accs[4 + (kt % 3)]
                p.op('pe', lambda: nc.tensor.matmul(accS[:], lhsT=kT[:, kvh, kt * 128:(kt + 1) * 128],
                                                    rhs=qT[:, h, qc * 512:(qc + 1) * 512], start=True, stop=True),
                     reads=[kT, qT], writes=[accS])
            emit_s(0)
            for kt in range(NKT):
                accS = p.accs[4 + (kt % 3)]
                if kt + 1 < NKT:
                    emit_s(kt + 1)
                PT = PTs.get()
                p.op('act', lambda: nc.scalar.activation(out=PT[:], in_=accS[:], func=AF.Exp, scale=scale),
                     reads=[accS], writes=[PT])

                def mm2():
                    nc.tensor.matmul(accO[:], lhsT=vv[:, kt, kvh * 128:(kvh + 1) * 128], rhs=PT[:], start=(kt == 0),
                                     stop=(kt == NKT - 1))
                    return nc.tensor.matmul(accD[:], lhsT=ones[:], rhs=PT[:], start=(kt == 0), stop=(kt == NKT - 1))
                p.op('pe', mm2, reads=[vv, PT, ones], writes=[accO, accD])
            rD = rDs.get()
            p.op('dve', lambda: nc.vector.reciprocal(out=rD[:], in_=accD[:]), reads=[accD], writes=[rD])
            p.op('dve', lambda: nc.vector.tensor_tensor(out=OT[:, h, qc * 512:(qc + 1) * 512], in0=accO[:], in1=rD[:],
                                                        op=ALU.mult), reads=[accO, rD], writes=[OT])
    ph.close()

    ph = Phase(p)
    rb = load_rows_b(p, ph, rows_d, ["gate1", "A2", "B2", "sc2"])
    p.op('dve', lambda: nc.vector.scalar_tensor_tensor(out=rb["A2"][:], in0=rb["sc2"][:], scalar=1.0, in1=rb["A2"][:],
                                                       op0=ALU.add, op1=ALU.mult), reads=[rb["sc2"], rb["A2"]],
         writes=[rb["A2"]])
    wr = ph.sb("wr", [128, KC, 32], F32)
    brb = ph.sb("brb", [128, 32], F32)
    p.dma('sp', wr[:], wr_d[:], reads=[wr_d], writes=[wr])
    p.dma('sp', brb[:], br_d[0:1, :].partition_broadcast(128), reads=[br_d], writes=[brb])
    prep = Prep(p, ph, rb["A2"], rb["B2"], wr, brb, tpf)
    xs = ph.scratch("xs", [128, D], F32, 2)
    x1s = ph.scratch("x1s", [128, D], F32, 2)
    tmp = ph.scratch("optmp", [128, 512], F32, 2)
    p.acc_i = 0
    for t in range(TPC // 128):
        xt, x1 = xs.get(), x1s.get()
        p.dma('sp', xt[:], x[t * 128:(t + 1) * 128, :], reads=[x], writes=[xt])
        for c in range(4):
            acc = p.acc()

            def mm():
                for k in range(KC):
                    src = OT if k < 8 else YT
                    i = nc.tensor.matmul(acc[:], lhsT=src[:, k % 8, t * 128:(t + 1) * 128],
                                         rhs=wo[:, k, c * 512:(c + 1) * 512], start=(k == 0), stop=(k == KC - 1))
                return i
            p.op('pe', mm, reads=[OT, YT, wo], writes=[acc])
            tm = tmp.get()
            p.op('dve', lambda: nc.vector.tensor_tensor(out=tm[:], in0=acc[:], in1=rb["gate1"][:, c * 512:(c + 1) * 512],
                                                        op=ALU.mult), reads=[acc, rb["gate1"]], writes=[tm])
            p.op('pool', lambda: nc.gpsimd.tensor_tensor(out=x1[:, c * 512:(c + 1) * 512], in0=tm[:],
                                                         in1=xt[:, c * 512:(c + 1) * 512], op=ALU.add),
                 reads=[tm, xt], writes=[x1])
        p.dma('sp', x1_o[t * 128:(t + 1) * 128, :], x1[:], reads=[x1], writes=[x1_o])
        prep.emit(x1, t_o[t * 128:(t + 1) * 128, :], t_o, g_o[t * 128:(t + 1) * 128, :], g_o)
    p.wait_all('sp', [x1_o, t_o, g_o])
    ph.close()
    return nc


NTOK = 8192
EPC = 4
SCH = 1024
LIMIT = 7.0
ALPHA = 1.702


def build_p4():
    nc, p = new_prog()
    tT_d = p.dram("tT", [128, KC, NTOK], BF16, "ExternalInput")
    gts_d = p.dram("gts", [128, NTOK // 128, EPC], F32, "ExternalInput")
    w1_d = p.dram("w1", [EPC, D, D], F32, "ExternalInput")
    b1_d = p.dram("b1c", [128, EPC, 16], F32, "ExternalInput")
    w2_d = p.dram("w2", [EPC, 1024, D], F32, "ExternalInput")
    b2_d = p.dram("b2", [EPC, D], F32, "ExternalInput")
    yp_o = p.dram("yp", [NTOK, D], BF16, "ExternalOutput")
    p.accs = [p.ps(f"acc{i}", [128, 512], F32) for i in range(8)]
    ones1 = p.sb("ones1", [1, 128], BF16)
    p.op('pool', lambda: nc.gpsimd.memset(ones1[:], 1.0), writes=[ones1])
    gts = p.sb("gts_s", [128, NTOK // 128, EPC], F32)
    b1c = p.sb("b1c_s", [128, EPC, 16], F32)
    p.dma('sp', gts[:], gts_d[:], reads=[gts_d], writes=[gts])
    p.dma('sp', b1c[:], b1_d[:], reads=[b1_d], writes=[b1c])
    hT = p.sb("hT", [128, KC, SCH], BF16)
    yacc = [p.sb(f"yacc{i}", [128, D], F32) for i in range(SCH // 128)]
    w1ss = Scratch(p, "w1s", [128, KC, 512], BF16, 2)
    w2ss = Scratch(p, "w2s", [128, 2, D], BF16, 2)
    b2ss = Scratch(p, "b2s", [1, D], BF16, 2)
    actTs = Scratch(p, "actT", [128, 2, SCH], BF16, 2)
    g32s = Scratch(p, "g32", [128, 512], F32, 2)
    sgs = Scratch(p, "sg", [128, 512], F32, 2)
    l32s = Scratch(p, "l32", [128, 512], F32, 2)
    obs = Scratch(p, "ob", [128, D], BF16, 2)
    for sc in range(NTOK // SCH):
        for h in range(2):
            p.dma('sp', hT[:, h * 8:(h + 1) * 8, :], tT_d[:, h * 8:(h + 1) * 8, sc * SCH:(sc + 1) * SCH], reads=[tT_d],
                  writes=[hT])
        for e in range(EPC):
            b2s = b2ss.get()
            p.dma('pool', b2s[:], b2_d[e:e + 1, :], reads=[b2_d], writes=[b2s])
            for s in range(4):
                w1s, w2s, actT = w1ss.get(), w2ss.get(), actTs.get()
                p.dma('pool', w1s[:, :, 0:256], w1_d[e, :, s * 256:(s + 1) * 256].rearrange("(k p) n -> p k n", p=128),
                      reads=[w1_d], writes=[w1s])
                p.dma('pool', w1s[:, :, 256:512],
                      w1_d[e, :, 1024 + s * 256:1024 + (s + 1) * 256].rearrange("(k p) n -> p k n", p=128),
                      reads=[w1_d], writes=[w1s])
                p.dma('pool', w2s[:], w2_d[e, s * 256:(s + 1) * 256, :].rearrange("(k p) n -> p k n", p=128),
                      reads=[w2_d], writes=[w2s])
                for tc in range(SCH // 512):
                    for j in range(2):
                        accG, accL = p.acc(), p.acc()
                        for acc, off in ((accG, 0), (accL, 256)):
                            def mm():
                                for k in range(KC):
                                    i = nc.tensor.matmul(acc[:], lhsT=w1s[:, k, off + j * 128:off + (j + 1) * 128],
                                                         rhs=hT[:, k, tc * 512:(tc + 1) * 512], start=(k == 0),
                                                         stop=(k == KC - 1))
                                return i
                            p.op('pe', mm, reads=[w1s, hT], writes=[acc])
                        g32, sg, l32 = g32s.get(), sgs.get(), l32s.get()
                        jj = s * 2 + j
                        p.op('dve', lambda: nc.vector.tensor_scalar(out=g32[:], in0=accG[:], scalar1=b1c[:, e, jj:jj + 1],
                                                                    scalar2=LIMIT, op0=ALU.add, op1=ALU.min),
                             reads=[accG, b1c], writes=[g32])
                        p.op('act', lambda: nc.scalar.activation(out=sg[:], in_=g32[:], func=AF.Sigmoid, scale=ALPHA),
                             reads=[g32], writes=[sg])
                        p.op('dve', lambda: nc.vector.tensor_scalar(out=l32[:], in0=accL[:],
                                                                    scalar1=b1c[:, e, 8 + jj:8 + jj + 1], scalar2=LIMIT,
                                                                    op0=ALU.add, op1=ALU.min), reads=[accL, b1c],
                             writes=[l32])
                        p.op('pool', lambda: nc.gpsimd.tensor_scalar(out=l32[:], in0=l32[:], scalar1=-LIMIT, scalar2=1.0,
                                                                     op0=ALU.max, op1=ALU.add), reads=[l32], writes=[l32])
                        p.op('pool', lambda: nc.gpsimd.tensor_tensor(out=g32[:], in0=g32[:], in1=sg[:], op=ALU.mult),
                             reads=[g32, sg], writes=[g32])
                        p.op('pool', lambda: nc.gpsimd.tensor_tensor(out=actT[:, j, tc * 512:(tc + 1) * 512], in0=g32[:],
                                                                     in1=l32[:], op=ALU.mult), reads=[g32, l32],
                             writes=[actT])
                for tt in range(SCH // 128):
                    gcol = gts[:, sc * (SCH // 128) + tt, e:e + 1]
                    for c in range(4):
                        acc = p.acc()

                        def mm2():
                            if s == 0:
                                nc.tensor.matmul(acc[:], lhsT=ones1[0:1, :], rhs=b2s[0:1, c * 512:(c + 1) * 512],
                                                 start=True, stop=False)
                            for j in range(2):
                                i = nc.tensor.matmul(acc[:], lhsT=actT[:, j, tt * 128:(tt + 1) * 128],
                                                     rhs=w2s[:, j, c * 512:(c + 1) * 512], start=(j == 0 and s != 0),
                                                     stop=(j == 1))
                            return i
                        p.op('pe', mm2, reads=[actT, w2s, b2s, ones1], writes=[acc])
                        ya = yacc[tt]
                        if e == 0 and s == 0:
                            p.op('dve', lambda: nc.vector.tensor_scalar(out=ya[:, c * 512:(c + 1) * 512], in0=acc[:],
                                                                        scalar1=gcol, scalar2=None, op0=ALU.mult),
                                 reads=[acc, gts], writes=[ya])
                        else:
                            p.op('dve', lambda: nc.vector.scalar_tensor_tensor(
                                out=ya[:, c * 512:(c + 1) * 512], in0=acc[:], scalar=gcol,
                                in1=ya[:, c * 512:(c + 1) * 512], op0=ALU.mult, op1=ALU.add),
                                reads=[acc, gts, ya], writes=[ya])
        for tt in range(SCH // 128):
            ob = obs.get()
            p.op('act', lambda: nc.scalar.copy(out=ob[:], in_=yacc[tt][:]), reads=[yacc[tt]], writes=[ob])
            p.dma('sp', yp_o[sc * SCH + tt * 128:sc * SCH + (tt + 1) * 128, :], ob[:], reads=[ob], writes=[yp_o])
    p.wait_all('sp', [yp_o])
    return nc


def emit_combine(p, xt, rows, ypt, gate_b, xo, tmps):
    nc = p.nc
    for c in range(4):
        acc = p.acc()

        def mm():
            for i in range(8):
                ins = nc.tensor.matmul(acc[0:rows, :], lhsT=p.ident[0:rows, 0:rows], rhs=ypt[0:rows, i, c * 512:(c + 1) * 512],
                                       start=(i == 0), stop=(i == 7))
            return ins
        p.op('pe', mm, reads=[p.ident, ypt], writes=[acc])
        tm = tmps.get()
        p.op('dve', lambda: nc.vector.tensor_tensor(out=tm[0:rows, :], in0=acc[0:rows, :],
                                                    in1=gate_b[0:rows, c * 512:(c + 1) * 512], op=ALU.mult),
             reads=[acc, gate_b], writes=[tm])
        p.op('pool', lambda: nc.gpsimd.tensor_tensor(out=xo[0:rows, c * 512:(c + 1) * 512], in0=tm[0:rows, :],
                                                     in1=xt[0:rows, c * 512:(c + 1) * 512], op=ALU.add),
             reads=[tm, xt], writes=[xo])


def setup_ident(p):
    nc = p.nc
    identf = p.sb("identf", [128, 128], F32)
    ident = p.sb("ident", [128, 128], BF16)
    p.op('pool', lambda: nc.gpsimd.memset(identf[:], 0.0), writes=[identf])
    p.op('pool', lambda: nc.gpsimd.affine_select(out=identf[:], in_=identf[:], pattern=[[-1, 128]],
                                                 compare_op=ALU.not_equal, fill=1.0, base=0, channel_multiplier=1),
         reads=[identf], writes=[identf])
    p.op('dve', lambda: nc.vector.tensor_copy(out=ident[:], in_=identf[:]), reads=[identf], writes=[ident])
    p.ident, p.identf = ident, identf


HALO = 16
LTOK = TPC + 2 * HALO
CW = 31


def build_p6():
    nc, p = new_prog()
    x1h_d = p.dram("x1h", [LTOK, D], F32, "ExternalInput")
    yph_d = p.dram("yph", [8, LTOK, D], BF16, "ExternalInput")
    rows_d = p.dram("rows", [6, D], F32, "ExternalInput")
    cols_d = p.dram("cols", [128, 3, KC], F32, "ExternalInput")
    cvb_d = p.dram("cvb", [128, 32 + 16 + 16 + 16], F32, "ExternalInput")
    wdw_d = p.dram("wdw", [128, KC, CW], F32, "ExternalInput")
    mask_d = p.dram("mask", [128, 2], F32, "ExternalInput")
    wi_d = p.dram("cv_w_in", [KC, 128, KC, 256], F32, "ExternalInput")
    wo_d = p.dram("cv_w_out", [D, D], F32, "ExternalInput")
    wr_d = p.dram("w_router", [128, KC, 32], F32, "ExternalInput")
    br_d = p.dram("b_router", [1, 32], F32, "ExternalInput")
    x2_o = p.dram("x2", [TPC, D], F32, "ExternalOutput")
    t_o = p.dram("t", [TPC, D], BF16, "ExternalOutput")
    g_o = p.dram("gates", [TPC, 32], F32, "ExternalOutput")
    x1p = p.dram("x1p", [LTOK, D], F32)
    p.accs = [p.ps(f"acc{i}", [128, 512], F32) for i in range(6)]
    p.tps = [p.ps("tpb0", [128, 1024], BF16)]
    tpf = p.ps("tpf", [128, 512], F32)
    setup_ident(p)
    ones = p.sb("ones", [128, 128], BF16)
    ones1 = p.sb("ones1", [1, 128], BF16)
    p.op('pool', lambda: nc.gpsimd.memset(ones[:], 1.0), writes=[ones])
    p.op('pool', lambda: nc.gpsimd.memset(ones1[:], 1.0), writes=[ones1])
    colsb = p.sb("colsb", [128, 3, KC], F32)
    cvb = p.sb("cvbs", [128, 80], F32)
    wdw = p.sb("wdws", [128, KC, CW], F32)
    msk = p.sb("msks", [128, 2], F32)
    p.dma('sp', colsb[:], cols_d[:], reads=[cols_d], writes=[colsb])
    p.dma('sp', cvb[:], cvb_d[:], reads=[cvb_d], writes=[cvb])
    p.dma('sp', wdw[:], wdw_d[:], reads=[wdw_d], writes=[wdw])
    p.dma('sp', msk[:], mask_d[:], reads=[mask_d], writes=[msk])
    A1 = p.sb("A1", [128, KC], F32)
    B1 = p.sb("B1", [128, KC], F32)
    p.op('dve', lambda: nc.vector.scalar_tensor_tensor(out=A1[:], in0=colsb[:, 2, :], scalar=1.0, in1=colsb[:, 0, :],
                                                       op0=ALU.add, op1=ALU.mult), reads=[colsb], writes=[A1])
    p.op('dve', lambda: nc.vector.tensor_copy(out=B1[:], in_=colsb[:, 1, :]), reads=[colsb], writes=[B1])
    uc = p.sb("uc", [128, KC, TPC], BF16)
    mean_b = [p.sb(f"mean_b{c}", [128, 512], F32) for c in range(2)]
    rstd_b = [p.sb(f"rstd_b{c}", [128, 512], F32) for c in range(2)]
    phA = Phase(p)
    hT = phA.sb("hT", [128, KC, LTOK], BF16)

    ph = Phase(p)
    g2b = ph.sb("g2b", [128, D], F32)
    p.dma('sp', g2b[:], rows_d[0:1, :].partition_broadcast(128), reads=[rows_d], writes=[g2b])
    nt = NormT.__new__(NormT)
    nt.p = p
    nt.sq = ph.scratch("nt_sq", [128, D], BF16, 1)
    nt.ss = ph.scratch("nt_ss", [128, 1], F32, 2)
    nt.xn = ph.scratch("nt_xn", [128, D], BF16, 2)
    xs = ph.scratch("xs", [128, D], F32, 2)
    xos = ph.scratch("xo", [128, D], F32, 2)
    yps = ph.scratch("ypt", [128, 8, D], BF16, 2)
    tmps = ph.scratch("ctmp", [128, 512], F32, 2)
    ntl = (LTOK + 127) // 128
    for t in range(ntl):
        r0 = t * 128
        rows = min(128, LTOK - r0)
        xt, xo, ypt = xs.get(), xos.get(), yps.get()
        p.dma('sp', xt[0:rows, :], x1h_d[r0:r0 + rows, :], reads=[x1h_d], writes=[xt])
        p.dma('sp', ypt[0:rows, :, :], yph_d[:, r0:r0 + rows, :].rearrange("c t d -> t c d"), reads=[yph_d], writes=[ypt])
        emit_combine(p, xt, rows, ypt, g2b, xo, tmps)
        p.dma('sp', x1p[r0:r0 + rows, :], xo[0:rows, :], reads=[xo], writes=[x1p])
        nt.emit(xo, rows, A1, B1, hT, r0)
    ph.close()

    ph = Phase(p)
    wis = ph.scratch("wis", [128, KC, 256], BF16, 2)
    uTs = ph.scratch("uT", [128, LTOK], BF16, 2)
    sgs = ph.scratch("sgm", [128, 512], F32, 2)
    dgs = ph.scratch("dg", [128, CW, 128], BF16, 2)
    sqs = ph.scratch("csq", [128, 512], BF16, 2)
    accM = [p.ps(f"accM{c}", [128, 512], F32) for c in range(0)]
    accM = [p.accs[0], p.accs[1]]
    accQ = [p.accs[2], p.accs[3]]
    rot = [p.accs[4], p.accs[5], tpf]
    ri = 0
    chunks = [(0, 512), (512, 512), (1024, LTOK - 1024)]
    for ct in range(KC):
        wi, uT, dg = wis.get(), uTs.get(), dgs.get()
        p.dma('pool', wi[:], wi_d[ct], reads=[wi_d], writes=[wi])
        p.op('pool', lambda: nc.gpsimd.tensor_tensor(
            out=dg[:], in0=p.ident[:].unsqueeze(1).broadcast_to([128, CW, 128]),
            in1=wdw[:, ct, :].unsqueeze(2).broadcast_to([128, CW, 128]), op=ALU.mult), reads=[p.ident, wdw], writes=[dg])
        for (c0, cn) in chunks:
            accA, accG = rot[ri % 3], rot[(ri + 1) % 3]
            ri += 2
            for acc, off in ((accA, 0), (accG, 128)):
                def mm():
                    for k in range(KC):
                        i = nc.tensor.matmul(acc[:, 0:cn], lhsT=wi[:, k, off:off + 128], rhs=hT[:, k, c0:c0 + cn],
                                             start=(k == 0), stop=(k == KC - 1))
                    return i
                p.op('pe', mm, reads=[wi, hT], writes=[acc])
            sg = sgs.get()
            p.op('act', lambda: nc.scalar.activation(out=sg[:, 0:cn], in_=accG[:, 0:cn], func=AF.Sigmoid,
                                                     bias=cvb[:, 16 + ct:16 + ct + 1], scale=1.0), reads=[accG, cvb],
                 writes=[sg])
            p.op('dve', lambda: nc.vector.scalar_tensor_tensor(out=uT[:, c0:c0 + cn], in0=accA[:, 0:cn],
                                                               scalar=cvb[:, ct:ct + 1], in1=sg[:, 0:cn], op0=ALU.add,
                                                               op1=ALU.mult), reads=[accA, cvb, sg], writes=[uT])
        p.op('dve', lambda: nc.vector.tensor_scalar(out=uT[:, 0:HALO], in0=uT[:, 0:HALO], scalar1=msk[:, 0:1], scalar2=None,
                                                    op0=ALU.mult), reads=[uT, msk], writes=[uT])
        p.op('dve', lambda: nc.vector.tensor_scalar(out=uT[:, LTOK - HALO:LTOK], in0=uT[:, LTOK - HALO:LTOK],
                                                    scalar1=msk[:, 1:2], scalar2=None, op0=ALU.mult), reads=[uT, msk],
             writes=[uT])
        for c in range(2):
            acc = rot[ri % 3]
            ri += 1

            def mmc():
                for j in range(CW):
                    i = nc.tensor.matmul(acc[:], lhsT=dg[:, j, :], rhs=uT[:, c * 512 + j + 1:c * 512 + j + 1 + 512],
                                         start=(j == 0), stop=(j == CW - 1))
                return i
            p.op('pe', mmc, reads=[dg, uT], writes=[acc])
            p.op('dve', lambda: nc.vector.tensor_scalar(out=uc[:, ct, c * 512:(c + 1) * 512], in0=acc[:],
                                                        scalar1=cvb[:, 32 + ct:32 + ct + 1], scalar2=None, op0=ALU.add),
                 reads=[acc, cvb], writes=[uc])
            sq = sqs.get()
            p.op('act', lambda: nc.scalar.activation(out=sq[:], in_=uc[:, ct, c * 512:(c + 1) * 512], func=AF.Square),
                 reads=[uc], writes=[sq])

            def mms():
                nc.tensor.matmul(accM[c][:], lhsT=ones[:], rhs=uc[:, ct, c * 512:(c + 1) * 512], start=(ct == 0),
                                 stop=(ct == KC - 1))
                return nc.tensor.matmul(accQ[c][:], lhsT=ones[:], rhs=sq[:], start=(ct == 0), stop=(ct == KC - 1))
            p.op('pe', mms, reads=[ones, uc, sq], writes=[accM[c], accQ[c]])
    for c in range(2):
        p.op('dve', lambda: nc.vector.tensor_scalar(out=mean_b[c][:], in0=accM[c][:], scalar1=1.0 / D, scalar2=None,
                                                    op0=ALU.mult), reads=[accM[c]], writes=[mean_b[c]])
        p.op('dve', lambda: nc.vector.tensor_tensor(out=rstd_b[c][:], in0=mean_b[c][:], in1=mean_b[c][:], op=ALU.mult),
             reads=[mean_b[c]], writes=[rstd_b[c]])
        p.op('dve', lambda: nc.vector.scalar_tensor_tensor(out=rstd_b[c][:], in0=accQ[c][:], scalar=1.0 / D,
                                                           in1=rstd_b[c][:], op0=ALU.mult, op1=ALU.subtract),
             reads=[accQ[c], rstd_b[c]], writes=[rstd_b[c]])
        p.op('dve', lambda: nc.vector.tensor_scalar(out=rstd_b[c][:], in0=rstd_b[c][:], scalar1=EPS, scalar2=None,
                                                    op0=ALU.add), reads=[rstd_b[c]], writes=[rstd_b[c]])
        p.op('act', lambda: nc.scalar.sqrt(out=rstd_b[c][:], in_=rstd_b[c][:]), reads=[rstd_b[c]], writes=[rstd_b[c]])
        p.op('dve', lambda: nc.vector.reciprocal(out=rstd_b[c][:], in_=rstd_b[c][:]), reads=[rstd_b[c]],
             writes=[rstd_b[c]])
    ph.close()
    phA.close()

    ph = Phase(p)
    wo = ph.sb("wo", [128, KC, D], BF16)
    for h in range(4):
        p.dma('pool', wo[:, h * 4:(h + 1) * 4, :], wo_d[h * 512:(h + 1) * 512, :].rearrange("(k p) n -> p k n", p=128),
              reads=[wo_d], writes=[wo])
    lts = ph.scratch("lnt", [128, 512], F32, 2)
    for ct in range(KC):
        for c in range(2):
            lt = lts.get()
            p.op('dve', lambda: nc.vector.tensor_tensor(out=lt[:], in0=uc[:, ct, c * 512:(c + 1) * 512], in1=mean_b[c][:],
                                                        op=ALU.subtract), reads=[uc, mean_b[c]], writes=[lt])
            p.op('pool', lambda: nc.gpsimd.tensor_tensor(out=lt[:], in0=lt[:], in1=rstd_b[c][:], op=ALU.mult),
                 reads=[lt, rstd_b[c]], writes=[lt])
            p.op('act', lambda: nc.scalar.activation(out=uc[:, ct, c * 512:(c + 1) * 512], in_=lt[:], func=AF.Silu,
                                                     scale=cvb[:, 48 + ct:48 + ct + 1], bias=cvb[:, 64 + ct:64 + ct + 1]),
                 reads=[lt, cvb], writes=[uc])
    rb = load_rows_b(p, ph, rows_d, ["gate1", "A2", "B2", "sc2"], off=1)
    p.op('dve', lambda: nc.vector.scalar_tensor_tensor(out=rb["A2"][:], in0=rb["sc2"][:], scalar=1.0, in1=rb["A2"][:],
                                                       op0=ALU.add, op1=ALU.mult), reads=[rb["sc2"], rb["A2"]],
         writes=[rb["A2"]])
    bo = ph.sb("bo", [1, D], BF16)
    p.dma('pool', bo[:], rows_d[5:6, :], reads=[rows_d], writes=[bo])
    wr = ph.sb("wr", [128, KC, 32], F32)
    brb = ph.sb("brb", [128, 32], F32)
    p.dma('sp', wr[:], wr_d[:], reads=[wr_d], writes=[wr])
    p.dma('sp', brb[:], br_d[0:1, :].partition_broadcast(128), reads=[br_d], writes=[brb])
    prep = Prep(p, ph, rb["A2"], rb["B2"], wr, brb, tpf)
    xs = ph.scratch("xs4", [128, D], F32, 2)
    x2s = ph.scratch("x2s", [128, D], F32, 1)
    tmp = ph.scratch("optmp", [128, 512], F32, 2)
    p.acc_i = 0
    for t in range(TPC // 128):
        xt, x2 = xs.get(), x2s.get()
        p.dma('sp', xt[:], x1p[HALO + t * 128:HALO + (t + 1) * 128, :], reads=[x1p], writes=[xt])
        for c in range(4):
            acc = p.acc()

            def mm():
                nc.tensor.matmul(acc[:], lhsT=ones1[0:1, :], rhs=bo[0:1, c * 512:(c + 1) * 512], start=True, stop=False)
                for k in range(KC):
                    i = nc.tensor.matmul(acc[:], lhsT=uc[:, k, t * 128:(t + 1) * 128], rhs=wo[:, k, c * 512:(c + 1) * 512],
                                         start=False, stop=(k == KC - 1))
                return i
            p.op('pe', mm, reads=[uc, wo, bo, ones1], writes=[acc])
            tm = tmp.get()
            p.op('dve', lambda: nc.vector.tensor_tensor(out=tm[:], in0=acc[:], in1=rb["gate1"][:, c * 512:(c + 1) * 512],
                                                        op=ALU.mult), reads=[acc, rb["gate1"]], writes=[tm])
            p.op('pool', lambda: nc.gpsimd.tensor_tensor(out=x2[:, c * 512:(c + 1) * 512], in0=tm[:],
                                                         in1=xt[:, c * 512:(c + 1) * 512], op=ALU.add),
                 reads=[tm, xt], writes=[x2])
        p.dma('sp', x2_o[t * 128:(t + 1) * 128, :], x2[:], reads=[x2], writes=[x2_o])
        prep.emit(x2, t_o[t * 128:(t + 1) * 128, :], t_o, g_o[t * 128:(t + 1) * 128, :], g_o)
    p.wait_all('sp', [x2_o, t_o, g_o])
    ph.close()
    return nc


def build_p8():
    nc, p = new_prog()
    x2_d = p.dram("x2", [TPC, D], F32, "ExternalInput")
    yph_d = p.dram("yph", [8, TPC, D], BF16, "ExternalInput")
    rows_d = p.dram("rows", [2, D], F32, "ExternalInput")
    out_o = p.dram("out", [TPC, D], F32, "ExternalOutput")
    p.accs = [p.ps(f"acc{i}", [128, 512], F32) for i in range(8)]
    setup_ident(p)
    ph = Phase(p)
    rb = load_rows_b(p, ph, rows_d, ["g2", "gf"])
    xs = ph.scratch("xs", [128, D], F32, 2)
    xos = ph.scratch("xo", [128, D], F32, 2)
    yps = ph.scratch("ypt", [128, 8, D], BF16, 2)
    tmps = ph.scratch("ctmp", [128, 512], F32, 2)
    sqs = ph.scratch("sq", [128, D], BF16, 1)
    sss = ph.scratch("ss", [128, 1], F32, 2)
    oos = ph.scratch("oo", [128, D], F32, 2)
    for t in range(TPC // 128):
        r0 = t * 128
        xt, xo, ypt, sq, ss, oo = xs.get(), xos.get(), yps.get(), sqs.get(), sss.get(), oos.get()
        p.dma('sp', xt[:], x2_d[r0:r0 + 128, :], reads=[x2_d], writes=[xt])
        p.dma('sp', ypt[:], yph_d[:, r0:r0 + 128, :].rearrange("c t d -> t c d"), reads=[yph_d], writes=[ypt])
        emit_combine(p, xt, 128, ypt, rb["g2"], xo, tmps)
        p.op('act', lambda: nc.scalar.activation(out=sq[:], in_=xo[:], func=AF.Square, accum_out=ss[:]), reads=[xo],
             writes=[sq, ss])
        emit_rstd(p, ss[:], 128, 1.0 / D, ss)
        p.op('dve', lambda: nc.vector.scalar_tensor_tensor(out=oo[:], in0=xo[:], scalar=ss[:, 0:1], in1=rb["gf"][:],
                                                           op0=ALU.mult, op1=ALU.mult), reads=[xo, ss, rb["gf"]],
             writes=[oo])
        p.dma('sp', out_o[r0:r0 + 128, :], oo[:], reads=[oo], writes=[out_o])
    p.wait_all('sp', [out_o])
    ph.close()
    return nc


def _run(nc, ims):
    res = run_bass_kernel_spmd(nc, ims, core_ids=list(range(NCORES)))
    return res.results


def _cf(v, n):
    return np.ascontiguousarray(np.asarray(v).reshape(n, 128).T)


def _rope_table(pos):
    inv = (10000.0 ** (-np.arange(0, 64, 2, dtype=np.float32) / np.float32(64))).astype(np.float32)
    ar = ((pos // 64).astype(np.float32)[:, None] * inv).astype(np.float64)
    ac = ((pos % 64).astype(np.float32)[:, None] * inv).astype(np.float64)
    cr, sr, cc, sc = np.cos(ar), np.sin(ar), np.cos(ac), np.sin(ac)
    return np.ascontiguousarray(np.stack([np.concatenate([cr, cr, cc, cc], 1),
                                          np.concatenate([-sr, sr, -sc, sc], 1)], 1).astype(np.float32))


def _dft_tables(qt):
    l = np.arange(4096, dtype=np.int64)[:, None]
    lp = (qt * TPC + np.arange(TPC, dtype=np.int64))[None, :]
    ang = 2.0 * np.pi * ((l * lp) % 4096).astype(np.float64) / 4096.0
    lay = lambda a: np.ascontiguousarray(a.astype(NPBF).reshape(32, 128, 4, 256).transpose(2, 1, 0, 3))
    return lay(np.cos(ang)), lay(np.sin(ang))


def _cdft():
    c = np.arange(128, dtype=np.int64)[:, None]
    cp = np.arange(128, dtype=np.int64)[None, :]
    ang = 2.0 * np.pi * ((c * cp) % 128).astype(np.float64) / 128.0
    s = np.sqrt(4096.0 * 128.0)
    return np.ascontiguousarray(np.stack([np.cos(ang) / s, -np.sin(ang) / s], 1).astype(NPBF))


def _moe_launch(layer, t_all, gates_all, moe_w1, moe_b1, moe_w2, moe_b2):
    gi = np.ascontiguousarray(gates_all.reshape(NTOK // 128, 128, NEXP).transpose(1, 0, 2))
    info = np.asarray(_run(build_p5(), [{"g": gi}] * NCORES)[0]["info"])[0]
    rank_of = [int(v) for v in info[:NEXP]]
    expert_at = [0] * NEXP
    for e, r in enumerate(rank_of):
        expert_at[r] = e
    caps = []
    for j in range(EPC):
        caps.append(next(cb for cb in range(1, NTOK // 128 + 1) if info[NEXP + j] <= cb * 128))
    ims = []
    t_rows = np.ascontiguousarray(t_all)
    for i in range(NCORES):
        ex = [expert_at[8 * j + i] for j in range(EPC)]
        w1 = np.asarray(moe_w1[layer][ex])
        w1p = np.ascontiguousarray(np.concatenate([w1[..., 0::2], w1[..., 1::2]], -1))
        b1 = np.asarray(moe_b1[layer][ex])
        b1p = np.concatenate([b1[..., 0::2], b1[..., 1::2]], -1)
        ims.append({
            "t": t_rows,
            "gts": np.ascontiguousarray(gates_all[:, ex].reshape(NTOK // 128, 128, EPC).transpose(1, 0, 2)),
            "w1": w1p,
            "b1c": np.ascontiguousarray(b1p.reshape(EPC, 16, 128).transpose(2, 0, 1)),
            "w2": np.ascontiguousarray(moe_w2[layer][ex]),
            "b2": np.ascontiguousarray(moe_b2[layer][ex]),
        })
    res = _run(build_p4s(tuple(caps)), ims)
    overflow = any(bool((np.asarray(r["cnt"])[0] > np.array(caps) * 128).any()) for r in res)
    if overflow:
        tT = np.ascontiguousarray(t_all.reshape(NTOK, KC, 128).transpose(2, 1, 0))
        for im in ims:
            del im["t"]
            im["tT"] = tT
        res = _run(build_p4(), ims)
    return np.stack([np.asarray(r["yp"]) for r in res], 0)


def kernel(x, c, ctx, c_ctx, w_mod, b_mod, g_mix, g_ffn, ab_w_in, ab_q_gain, ab_k_gain, ab_w_out,
           cv_w_in, cv_b_in, cv_w_dw, cv_b_dw, cv_ln_g, cv_ln_b, cv_w_out, cv_b_out,
           moe_w_router, moe_b_router, moe_w1, moe_b1, moe_w2, moe_b2, g_final):
    A = lambda a: np.asarray(a, dtype=np.float32)
    x, c, ctx, c_ctx, w_mod, b_mod, g_mix, g_ffn = map(A, (x, c, ctx, c_ctx, w_mod, b_mod, g_mix, g_ffn))
    ab_w_in, ab_q_gain, ab_k_gain, ab_w_out = map(A, (ab_w_in, ab_q_gain, ab_k_gain, ab_w_out))
    cv_w_in, cv_b_in, cv_w_dw, cv_b_dw, cv_ln_g, cv_ln_b, cv_w_out, cv_b_out = map(
        A, (cv_w_in, cv_b_in, cv_w_dw, cv_b_dw, cv_ln_g, cv_ln_b, cv_w_out, cv_b_out))
    moe_w_router, moe_b_router, moe_w1, moe_b1, moe_w2, moe_b2, g_final = map(
        A, (moe_w_router, moe_b_router, moe_w1, moe_b1, moe_w2, moe_b2, g_final))
    B, L, _ = x.shape
    cores = [(i // 4, i % 4) for i in range(NCORES)]

    cv = np.stack([c[0], c[1], c_ctx])
    cT = np.ascontiguousarray(cv.T.reshape(KC, 128, 3).transpose(1, 0, 2))
    ims = []
    for i in range(NCORES):
        sl = slice(i * MODW, (i + 1) * MODW)
        ims.append({"wmod": np.ascontiguousarray(w_mod[:, :, sl]),
                    "bmod": np.ascontiguousarray(np.broadcast_to(b_mod[:, None, sl], (2, 3, MODW))), "cT": cT})
    res = _run(build_p1(), ims)
    m = np.concatenate([np.asarray(r["m"]) for r in res], -1)
    mod = m.reshape(2, 3, 6, D)

    ims = []
    for (b, qt) in cores:
        colsv = np.stack([g_mix[0], mod[0, b, 0], mod[0, b, 1], mod[0, 2, 0], mod[0, 2, 1]])
        ims.append({"x": np.ascontiguousarray(x[b, qt * TPC:(qt + 1) * TPC]),
                    "ctxr": np.ascontiguousarray(ctx[b, qt * CPC:(qt + 1) * CPC]),
                    "cols": np.ascontiguousarray(colsv.reshape(5, KC, 128).transpose(2, 0, 1)),
                    "w_in": np.ascontiguousarray(ab_w_in[0]),
                    "gains": np.ascontiguousarray(np.stack([ab_q_gain[0], ab_k_gain[0]])),
                    "rope": _rope_table(qt * TPC + np.arange(TPC))})
    res = _run(build_p2(), ims)
    qkvf = [np.asarray(r["qkvf"]) for r in res]
    kvc = [np.asarray(r["kvc"]) for r in res]
    q_b, k_b, v_b, f_b = [], [], [], []
    for b in range(B):
        lat = np.concatenate(qkvf[b * 4:(b + 1) * 4], 0)
        cx = np.concatenate(kvc[b * 4:(b + 1) * 4], 0)
        q_b.append(lat[:, 0:1024])
        k_b.append(np.concatenate([lat[:, 1024:1280], cx[:, 0:256]], 0))
        v_b.append(np.concatenate([lat[:, 1280:1536], cx[:, 256:512]], 0))
        f_b.append(lat[:, 1536:2560])

    cd = _cdft()
    dft = [_dft_tables(qt) for qt in range(4)]
    wr_l = [np.ascontiguousarray(moe_w_router[l].reshape(KC, 128, 32).transpose(1, 0, 2)) for l in range(2)]
    ims = []
    for (b, qt) in cores:
        ims.append({"x": np.ascontiguousarray(x[b, qt * TPC:(qt + 1) * TPC]),
                    "qT": np.ascontiguousarray(q_b[b][qt * TPC:(qt + 1) * TPC].reshape(TPC, 8, 128).transpose(2, 1, 0)),
                    "kT": np.ascontiguousarray(k_b[b].reshape(NKEY, 2, 128).transpose(2, 1, 0)),
                    "v": np.ascontiguousarray(v_b[b]), "f": np.ascontiguousarray(f_b[b]),
                    "dftc": dft[qt][0], "dfts": dft[qt][1], "cdft": cd,
                    "w_out": np.ascontiguousarray(ab_w_out[0]),
                    "rows": np.ascontiguousarray(np.stack([mod[0, b, 2], g_ffn[0], mod[0, b, 3], mod[0, b, 4]])),
                    "w_router": wr_l[0], "b_router": np.ascontiguousarray(moe_b_router[0][None, :])})
    res = _run(build_p3(), ims)
    x1_all = np.concatenate([np.asarray(r["x1"]) for r in res], 0)
    t_all = np.concatenate([np.asarray(r["t"]) for r in res], 0)
    g_all = np.concatenate([np.asarray(r["gates"]) for r in res], 0)
    del dft

    yp_all = _moe_launch(0, t_all, g_all, moe_w1, moe_b1, moe_w2, moe_b2)

    cvb = np.ascontiguousarray(np.concatenate([_cf(cv_b_in[0], 32), _cf(cv_b_dw[0], 16), _cf(cv_ln_g[0], 16),
                                               _cf(cv_ln_b[0], 16)], 1))
    wdw = np.ascontiguousarray(cv_w_dw[0].T.reshape(KC, 128, CW).transpose(1, 0, 2))
    wa = cv_w_in[0][:, :D].reshape(KC, 128, KC, 128)
    wg = cv_w_in[0][:, D:].reshape(KC, 128, KC, 128)
    cvwi = np.ascontiguousarray(np.concatenate([wa, wg], -1).transpose(2, 1, 0, 3))
    ims = []
    for i, (b, qt) in enumerate(cores):
        rows_g = i * TPC - HALO + np.arange(LTOK)
        valid = (rows_g >= b * L) & (rows_g < (b + 1) * L)
        x1h = np.zeros((LTOK, D), np.float32)
        x1h[valid] = x1_all[rows_g[valid]]
        yph = np.zeros((8, LTOK, D), NPBF)
        yph[:, valid] = yp_all[:, rows_g[valid]]
        colsv = np.stack([g_mix[1], mod[1, b, 0], mod[1, b, 1]])
        ims.append({"x1h": x1h, "yph": yph,
                    "rows": np.ascontiguousarray(np.stack([mod[0, b, 5], mod[1, b, 2], g_ffn[1], mod[1, b, 3],
                                                           mod[1, b, 4], cv_b_out[0]])),
                    "cols": np.ascontiguousarray(colsv.reshape(3, KC, 128).transpose(2, 0, 1)),
                    "cvb": cvb, "wdw": wdw,
                    "mask": np.ascontiguousarray(np.broadcast_to(
                        np.array([0.0 if qt == 0 else 1.0, 0.0 if qt == 3 else 1.0], np.float32), (128, 2))),
                    "cv_w_in": cvwi, "cv_w_out": np.ascontiguousarray(cv_w_out[0]),
                    "w_router": wr_l[1], "b_router": np.ascontiguousarray(moe_b_router[1][None, :])})
    res = _run(build_p6(), ims)
    x2_all = np.concatenate([np.asarray(r["x2"]) for r in res], 0)
    t2_all = np.concatenate([np.asarray(r["t"]) for r in res], 0)
    g2_all = np.concatenate([np.asarray(r["gates"]) for r in res], 0)
    del yp_all, ims

    yp2_all = _moe_launch(1, t2_all, g2_all, moe_w1, moe_b1, moe_w2, moe_b2)

    ims = []
    for i, (b, qt) in enumerate(cores):
        ims.append({"x2": np.ascontiguousarray(x2_all[i * TPC:(i + 1) * TPC]),
                    "yph": np.ascontiguousarray(yp2_all[:, i * TPC:(i + 1) * TPC]),
                    "rows": np.ascontiguousarray(np.stack([mod[1, b, 5], g_final]))})
    res = _run(build_p8(), ims)
    out = np.concatenate([np.asarray(r["out"]) for r in res], 0).reshape(B, L, D).astype(np.float32)
    return out


CAPB = 16
BIG = 1.0e6
I32 = mybir.dt.int32


def p_idma(p, out_ap, out_off, in_ap, in_off, bound, reads, dst, concurrent=True):
    nc = p.nc
    for b in reads:
        if b.w is not None:
            p._wait('pool', b.w)
    if concurrent:
        if getattr(dst, 'wbase', None) is not None:
            p._wait('pool', dst.wbase)
    else:
        if dst.w is not None:
            p._wait('pool', dst.w)
    for ev in dst.r:
        p._wait('pool', ev)
    if dst.dsem is None:
        key = 'd_' + dst.name
        p.sems[key] = nc.alloc_semaphore(key)
        dst.dsem = key
    if not hasattr(p, 'bound_regs'):
        p.bound_regs = {}
    if bound not in p.bound_regs:
        p.bound_regs[bound] = nc.gpsimd.to_reg(bound)
    inst = nc.gpsimd.indirect_dma_start(out=out_ap, out_offset=out_off, in_=in_ap, in_offset=in_off,
                                        bounds_check=p.bound_regs[bound], oob_is_err=False)
    dst.dcnt += 16
    p.dma_cnt[dst.dsem] = dst.dcnt
    inst.then_inc(p.sems[dst.dsem], 16)
    ev = (dst.dsem, dst.dcnt)
    for b in reads:
        b.r.append(ev)
    dst.w = ev
    dst.r = []
    return inst


def build_p4s(caps=(CAPB,) * 4):
    caps = tuple(int(c) for c in caps)
    CAPRS = [c * 128 for c in caps]
    NT = NTOK // 128
    nc, p = new_prog()
    t_d = p.dram("t", [NTOK, D], BF16, "ExternalInput")
    gts_d = p.dram("gts", [128, NT, EPC], F32, "ExternalInput")
    w1_d = p.dram("w1", [EPC, D, D], F32, "ExternalInput")
    b1_d = p.dram("b1c", [128, EPC, 16], F32, "ExternalInput")
    w2_d = p.dram("w2", [EPC, 1024, D], F32, "ExternalInput")
    b2_d = p.dram("b2", [EPC, D], F32, "ExternalInput")
    yp_o = p.dram("yp", [NTOK, D], BF16, "ExternalOutput")
    cnt_o = p.dram("cnt", [1, EPC], F32, "ExternalOutput")
    Xg = [p.dram(f"Xg{e}", [CAPRS[e], D], BF16) for e in range(EPC)]
    Yg = [p.dram(f"Yg{e}", [CAPRS[e], D], BF16) for e in range(EPC)]
    p.accs = [p.ps(f"acc{i}", [128, 512], F32) for i in range(6)]
    p.tps = [p.ps(f"tpb{i}", [128, 1024], BF16) for i in range(2)]
    setup_ident(p)
    ones = p.sb("ones", [128, 128], BF16)
    ones1 = p.sb("ones1", [1, 128], BF16)
    tri = p.sb("tri", [128, 128], BF16)
    trif = p.sb("trif", [128, 128], F32)
    p.op('pool', lambda: nc.gpsimd.memset(ones[:], 1.0), writes=[ones])
    p.op('pool', lambda: nc.gpsimd.memset(ones1[:], 1.0), writes=[ones1])
    p.op('pool', lambda: nc.gpsimd.memset(trif[:], 1.0), writes=[trif])
    p.op('pool', lambda: nc.gpsimd.affine_select(out=trif[:], in_=trif[:], pattern=[[1, 128]], compare_op=ALU.is_gt,
                                                 fill=0.0, base=0, channel_multiplier=-1), reads=[trif], writes=[trif])
    p.op('dve', lambda: nc.vector.tensor_copy(out=tri[:], in_=trif[:]), reads=[trif], writes=[tri])
    gts = p.sb("gts_s", [128, NT, EPC], F32)
    b1c = p.sb("b1c_s", [128, EPC, 16], F32)
    p.dma('sp', gts[:], gts_d[:], reads=[gts_d], writes=[gts])
    p.dma('sp', b1c[:], b1_d[:], reads=[b1_d], writes=[b1c])
    b2b = p.sb("b2b", [1, EPC, D], BF16)
    rank_si = p.sb("rank_si", [128, NT * EPC], I32)
    rank_gi = p.sb("rank_gi", [128, NT * EPC], I32)
    phW = Phase(p)
    w1b = phW.sb("w1b", [128, KC, D], BF16)
    w2b = phW.sb("w2b", [128, 8, D], BF16)
    p.dma('pool', b2b[:], b2_d.t.ap().unsqueeze(0) if hasattr(b2_d.t.ap(), "unsqueeze") else b2_d[:, :], reads=[b2_d],
          writes=[b2b])

    def load_w(e):
        for h in range(4):
            p.dma('pool', w1b[:, h * 4:(h + 1) * 4, :], w1_d[e, h * 512:(h + 1) * 512, :].rearrange("(k p) n -> p k n", p=128),
                  reads=[w1_d], writes=[w1b])
        for h in range(2):
            p.dma('pool', w2b[:, h * 4:(h + 1) * 4, :], w2_d[e, h * 512:(h + 1) * 512, :].rearrange("(k p) n -> p k n", p=128),
                  reads=[w2_d], writes=[w2b])
    load_w(0)

    NF = NT * EPC
    phR = Phase(p)
    maskf = phR.sb("maskf", [128, NF], F32)
    maskb = phR.sb("maskb", [128, NF], BF16)
    gflat = gts[:].rearrange("p t e -> p (t e)")
    p.op('dve', lambda: nc.vector.tensor_scalar(out=maskf[:], in0=gflat, scalar1=0.0, scalar2=None, op0=ALU.is_gt),
         reads=[gts], writes=[maskf])
    p.op('dve', lambda: nc.vector.tensor_copy(out=maskb[:], in_=maskf[:]), reads=[maskf], writes=[maskb])
    accW, accT = p.accs[0], p.accs[1]
    p.op('pe', lambda: nc.tensor.matmul(accW[:, 0:NF], lhsT=tri[:], rhs=maskb[:], start=True, stop=True),
         reads=[tri, maskb], writes=[accW])
    p.op('pe', lambda: nc.tensor.matmul(accT[:, 0:NF], lhsT=ones[:], rhs=maskb[:], start=True, stop=True),
         reads=[ones, maskb], writes=[accT])
    sa = phR.sb("scan_a", [128, NF], F32)
    sbb = phR.sb("scan_b", [128, NF], F32)
    tot = phR.sb("tot", [128, NF], F32)
    rank = phR.sb("rank", [128, NF], F32)
    p.op('dve', lambda: nc.vector.tensor_copy(out=tot[:], in_=accT[:, 0:NF]), reads=[accT], writes=[tot])
    p.op('act', lambda: nc.scalar.copy(out=sa[:], in_=accT[:, 0:NF]), reads=[accT], writes=[sa])
    cur, nxt = sa, sbb
    s = 1
    while s < NT:
        w = s * EPC
        p.op('dve', lambda: nc.vector.tensor_copy(out=nxt[:, 0:w], in_=cur[:, 0:w]), reads=[cur], writes=[nxt])
        p.op('dve', lambda: nc.vector.tensor_tensor(out=nxt[:, w:NF], in0=cur[:, w:NF], in1=cur[:, 0:NF - w], op=ALU.add),
             reads=[cur], writes=[nxt])
        cur, nxt = nxt, cur
        s *= 2
    incl = cur
    p.op('dve', lambda: nc.vector.tensor_tensor(out=rank[:], in0=accW[:, 0:NF], in1=incl[:], op=ALU.add),
         reads=[accW, incl], writes=[rank])
    p.op('dve', lambda: nc.vector.tensor_tensor(out=rank[:], in0=rank[:], in1=tot[:], op=ALU.subtract),
         reads=[rank, tot], writes=[rank])
    p.dma('sp', cnt_o[:, :], incl[0:1, NF - EPC:NF], reads=[incl], writes=[cnt_o])
    p.op('dve', lambda: nc.vector.tensor_scalar(out=maskf[:], in0=maskf[:], scalar1=-BIG, scalar2=BIG, op0=ALU.mult,
                                                op1=ALU.add), reads=[maskf], writes=[maskf])
    p.op('dve', lambda: nc.vector.tensor_tensor(out=rank[:], in0=rank[:], in1=maskf[:], op=ALU.add),
         reads=[rank, maskf], writes=[rank])
    p.op('dve', lambda: nc.vector.tensor_copy(out=rank_si[:], in_=rank[:]), reads=[rank], writes=[rank_si])
    for e in range(EPC):
        rv = rank[:].rearrange("p (t e) -> p t e", e=EPC)[:, :, e]
        p.op('dve', lambda: nc.vector.tensor_scalar(out=rv, in0=rv, scalar1=float(CAPRS[e] - 1), scalar2=None,
                                                    op0=ALU.min), reads=[rank], writes=[rank])
    p.op('dve', lambda: nc.vector.tensor_copy(out=rank_gi[:], in_=rank[:]), reads=[rank], writes=[rank_gi])

    p.wait_all('sp', [cnt_o])
    phR.close()
    phD = Phase(p)
    zt = phD.sb("zt", [128, D], BF16)
    p.op('dve', lambda: nc.vector.memset(zt[:], 0.0), writes=[zt])
    for e in range(EPC):
        CAPR = CAPRS[e]
        for r in range(0, CAPR, 128 * 8):
            n8 = min(8, (CAPR - r) // 128)
            p.dma('sp', Xg[e][r:r + n8 * 128, :].rearrange("(a p) d -> p a d", p=128),
                  zt[:].unsqueeze(1).broadcast_to([128, n8, D]), reads=[zt], writes=[Xg[e]])
        Xg[e].wbase = Xg[e].w
    tts = phD.scratch("tt", [128, D], BF16, 4)
    for t in range(NT):
        tt = tts.get()
        p.dma('sp', tt[:], t_d[t * 128:(t + 1) * 128, :], reads=[t_d], writes=[tt])
        for e in range(EPC):
            col = t * EPC + e
            p_idma(p, Xg[e][:, :], bass.IndirectOffsetOnAxis(ap=rank_si[:, col:col + 1], axis=0), tt[:, :], None,
                   CAPRS[e] - 1, [tt, rank_si], Xg[e])

    phD.close()
    phE = Phase(p)
    xgs = phE.scratch("xgt", [128, D], BF16, 3)
    XTs = phE.scratch("XT", [128, KC, 512], BF16, 2)
    actTs = phE.scratch("actT", [128, 8, 512], BF16, 2)
    g32s = phE.scratch("g32", [128, 512], F32, 2)
    sgs = phE.scratch("sg", [128, 512], F32, 2)
    l32s = phE.scratch("l32", [128, 512], F32, 2)
    yos = phE.scratch("yo", [128, D], BF16, 2)
    for e in range(EPC):
        if e > 0:
            load_w(e)
        for ch in range((caps[e] + 3) // 4):
            XT, actT = XTs.get(), actTs.get()
            nb = min(4, caps[e] - 4 * ch)
            N = nb * 128
            for ti in range(nb):
                r0 = (ch * 4 + ti) * 128
                xg = xgs.get()
                p.dma('sp', xg[:], Xg[e][r0:r0 + 128, :], reads=[Xg[e]], writes=[xg])
                for k8 in range(2):
                    tp = p.tp()

                    def tr():
                        for j in range(8):
                            k = k8 * 8 + j
                            i = nc.tensor.transpose(out=tp[:, j * 128:(j + 1) * 128], in_=xg[:, k * 128:(k + 1) * 128],
                                                    identity=p.ident[:])
                        return i
                    p.op('pe', tr, reads=[xg, p.ident], writes=[tp])
                    dst = XT[:, k8 * 8:(k8 + 1) * 8, ti * 128:(ti + 1) * 128]
                    src = tp[:].rearrange("p (k t) -> p k t", t=128)
                    if k8 == 0:
                        p.op('act', lambda: nc.scalar.copy(out=dst, in_=src), reads=[tp], writes=[XT])
                    else:
                        p.op('dve', lambda: nc.vector.tensor_copy(out=dst, in_=src), reads=[tp], writes=[XT])
            for jj in range(8):
                accG, accL = p.acc(), p.acc()
                for acc, off in ((accG, 0), (accL, 1024)):
                    def mm():
                        for k in range(KC):
                            i = nc.tensor.matmul(acc[:, 0:N], lhsT=w1b[:, k, off + jj * 128:off + (jj + 1) * 128],
                                                 rhs=XT[:, k, 0:N], start=(k == 0), stop=(k == KC - 1))
                        return i
                    p.op('pe', mm, reads=[w1b, XT], writes=[acc])
                g32, sg, l32 = g32s.get(), sgs.get(), l32s.get()
                p.op('dve', lambda: nc.vector.tensor_scalar(out=g32[:, 0:N], in0=accG[:, 0:N], scalar1=b1c[:, e, jj:jj + 1],
                                                            scalar2=LIMIT, op0=ALU.add, op1=ALU.min),
                     reads=[accG, b1c], writes=[g32])
                p.op('act', lambda: nc.scalar.activation(out=sg[:, 0:N], in_=g32[:, 0:N], func=AF.Sigmoid, scale=ALPHA),
                     reads=[g32], writes=[sg])
                p.op('dve', lambda: nc.vector.tensor_scalar(out=l32[:, 0:N], in0=accL[:, 0:N], scalar1=b1c[:, e, 8 + jj:8 + jj + 1],
                                                            scalar2=LIMIT, op0=ALU.add, op1=ALU.min),
                     reads=[accL, b1c], writes=[l32])
                p.op('dve', lambda: nc.vector.tensor_scalar(out=l32[:, 0:N], in0=l32[:, 0:N], scalar1=-LIMIT, scalar2=1.0,
                                                            op0=ALU.max, op1=ALU.add), reads=[l32], writes=[l32])
                p.op('dve', lambda: nc.vector.tensor_tensor(out=g32[:, 0:N], in0=g32[:, 0:N], in1=sg[:, 0:N], op=ALU.mult),
                     reads=[g32, sg], writes=[g32])
                p.op('dve', lambda: nc.vector.tensor_tensor(out=actT[:, jj, 0:N], in0=g32[:, 0:N], in1=l32[:, 0:N], op=ALU.mult),
                     reads=[g32, l32], writes=[actT])
            for ti in range(nb):
                r0 = (ch * 4 + ti) * 128
                yo = yos.get()
                for c in range(4):
                    acc = p.acc()

                    def mm2():
                        nc.tensor.matmul(acc[:], lhsT=ones1[0:1, :], rhs=b2b[0:1, e, c * 512:(c + 1) * 512], start=True,
                                         stop=False)
                        for j in range(8):
                            i = nc.tensor.matmul(acc[:], lhsT=actT[:, j, ti * 128:(ti + 1) * 128],
                                                 rhs=w2b[:, j, c * 512:(c + 1) * 512], start=False, stop=(j == 7))
                        return i
                    p.op('pe', mm2, reads=[actT, w2b, b2b, ones1], writes=[acc])
                    p.op('act', lambda: nc.scalar.copy(out=yo[:, c * 512:(c + 1) * 512], in_=acc[:]), reads=[acc],
                         writes=[yo])
                p.dma('sp', Yg[e][r0:r0 + 128, :], yo[:], reads=[yo], writes=[Yg[e]])

    phE.close()
    phW.close()
    phC = Phase(p)
    ygs = phC.scratch("yg", [128, D], BF16, 8)
    dgs = phC.scratch("dgc", [128, EPC, 128], BF16, 2)
    obs = phC.scratch("ob", [128, D], BF16, 2)
    for yb in ygs.bufs:
        p.op('dve', lambda: nc.vector.memset(yb[:], 0.0), writes=[yb])
    p.acc_i = 0
    for t in range(NT):
        ob, dg = obs.get(), dgs.get()
        yl = []
        for e in range(EPC):
            col = t * EPC + e
            yg = ygs.get()
            p_idma(p, yg[:, :], None, Yg[e][:, :], bass.IndirectOffsetOnAxis(ap=rank_si[:, col:col + 1], axis=0),
                   CAPRS[e] - 1, [Yg[e], rank_si], yg, concurrent=False)
            yl.append(yg)
            p.op('dve', lambda: nc.vector.tensor_scalar(out=dg[:, e, :], in0=p.ident[:], scalar1=gts[:, t, e:e + 1],
                                                        scalar2=None, op0=ALU.mult), reads=[p.ident, gts], writes=[dg])
        for c in range(4):
            acc = p.acc()

            def mmc():
                for e in range(EPC):
                    i = nc.tensor.matmul(acc[:], lhsT=dg[:, e, :], rhs=yl[e][:, c * 512:(c + 1) * 512], start=(e == 0),
                                         stop=(e == EPC - 1))
                return i
            p.op('pe', mmc, reads=[dg] + yl, writes=[acc])
            if c % 2 == 0:
                p.op('act', lambda: nc.scalar.copy(out=ob[:, c * 512:(c + 1) * 512], in_=acc[:]), reads=[acc], writes=[ob])
            else:
                p.op('dve', lambda: nc.vector.tensor_copy(out=ob[:, c * 512:(c + 1) * 512], in_=acc[:]), reads=[acc],
                     writes=[ob])
        p.dma('sp', yp_o[t * 128:(t + 1) * 128, :], ob[:], reads=[ob], writes=[yp_o])
    p.wait_all('sp', [yp_o, cnt_o])
    phC.close()
    return nc


NEXP = 32


def build_p5():
    NT = NTOK // 128
    nc, p = new_prog()
    g_d = p.dram("g", [128, NT, NEXP], F32, "ExternalInput")
    out_o = p.dram("info", [1, NEXP + EPC], F32, "ExternalOutput")
    p.accs = [p.ps(f"acc{i}", [128, 512], F32) for i in range(4)]
    ones = p.sb("ones", [128, 128], BF16)
    p.op('pool', lambda: nc.gpsimd.memset(ones[:], 1.0), writes=[ones])
    g = p.sb("gs", [128, NT, NEXP], F32)
    mk = p.sb("mk", [128, NT * NEXP], BF16)
    tot = p.sb("tot", [128, NT * NEXP], F32)
    p.dma('sp', g[:], g_d[:], reads=[g_d], writes=[g])
    p.op('dve', lambda: nc.vector.tensor_scalar(out=mk[:], in0=g[:].rearrange("p t e -> p (t e)"), scalar1=0.0,
                                                scalar2=None, op0=ALU.is_gt), reads=[g], writes=[mk])
    for c in range(NT * NEXP // 512):
        acc = p.acc()
        p.op('pe', lambda: nc.tensor.matmul(acc[:], lhsT=ones[:], rhs=mk[:, c * 512:(c + 1) * 512], start=True, stop=True),
             reads=[ones, mk], writes=[acc])
        p.op('act', lambda: nc.scalar.copy(out=tot[:, c * 512:(c + 1) * 512], in_=acc[:]), reads=[acc], writes=[tot])
    cnt = p.sb("cnt", [128, NEXP], F32)
    p.op('dve', lambda: nc.vector.tensor_reduce(out=cnt[:], in_=tot[:].rearrange("p (t e) -> p e t", e=NEXP), axis=AX.X,
                                                op=ALU.add), reads=[tot], writes=[cnt])
    io = p.sb("io", [128, NEXP], F32)
    p.op('pool', lambda: nc.gpsimd.iota(io[:], pattern=[[1, NEXP]], base=0, channel_multiplier=0,
                                        allow_small_or_imprecise_dtypes=True), writes=[io])
    cnt2 = p.sb("cnt2", [128, NEXP], F32)
    p.op('dve', lambda: nc.vector.scalar_tensor_tensor(out=cnt2[:], in0=io[:], scalar=1.0 / 64, in1=cnt[:], op0=ALU.mult,
                                                       op1=ALU.add), reads=[io, cnt], writes=[cnt2])
    cmp = p.sb("cmp", [128, NEXP, NEXP], F32)
    p.op('dve', lambda: nc.vector.tensor_tensor(out=cmp[:], in0=cnt2[:].unsqueeze(1).broadcast_to([128, NEXP, NEXP]),
                                                in1=cnt2[:].unsqueeze(2).broadcast_to([128, NEXP, NEXP]), op=ALU.is_gt),
         reads=[cnt2], writes=[cmp])
    res = p.sb("res", [128, NEXP + EPC], F32)
    p.op('dve', lambda: nc.vector.tensor_reduce(out=res[:, 0:NEXP], in_=cmp[:], axis=AX.X, op=ALU.add), reads=[cmp],
         writes=[res])
    sel = p.sb("sel", [128, NEXP], F32)
    sel2 = p.sb("sel2", [128, NEXP], F32)
    for j in range(EPC):
        p.op('dve', lambda: nc.vector.tensor_scalar(out=sel[:], in0=res[:, 0:NEXP], scalar1=float(8 * j) - 0.5,
                                                    scalar2=None, op0=ALU.is_gt), reads=[res], writes=[sel])
        p.op('dve', lambda: nc.vector.tensor_scalar(out=sel2[:], in0=res[:, 0:NEXP], scalar1=float(8 * j + 8) - 0.5,
                                                    scalar2=None, op0=ALU.is_lt), reads=[res], writes=[sel2])
        p.op('dve', lambda: nc.vector.tensor_tensor(out=sel[:], in0=sel[:], in1=sel2[:], op=ALU.mult), reads=[sel, sel2],
             writes=[sel])
        p.op('dve', lambda: nc.vector.tensor_tensor(out=sel[:], in0=sel[:], in1=cnt[:], op=ALU.mult), reads=[sel, cnt],
             writes=[sel])
        p.op('dve', lambda: nc.vector.reduce_max(out=res[:, NEXP + j:NEXP + j + 1], in_=sel[:], axis=AX.X), reads=[sel],
             writes=[res])
    p.dma('sp', out_o[:, :], res[0:1, :], reads=[res], writes=[out_o])
    p.wait_all('sp', [out_o])
    return nc
```
